# Optimizing a Trainium2 kernel written in Bass

```python
import math
import jax
import jax.numpy as jnp
from jax import lax
import numpy as np

D_MODEL = 1024
BATCH = 8
SEQ = 8192
DEPTH = 2

CTX_LEN = 256
GRID_W = 64
EPS = 1e-6
F32 = jnp.float32

HY_W = 512
HY_SHORT = 3
HY_EMB = 33
HY_BANDS = (HY_EMB - 1) // 2
HY_FILT_HID = 64
HY_MOD_SHIFT = 0.05
HY_DECAY_TARGET = 0.01
HY_FAST_PCT = 0.3
HY_SLOW_PCT = 1.5

GDN_HEADS = 4
GDN_DK = 128
GDN_DV = 128
GDN_SHORT = 3
GDN_CHUNK = 64

MLA_HEADS = 8
MLA_NOPE = 64
MLA_ROPE = 32
MLA_V = 64
MLA_Q_LORA = 768
MLA_KV_LORA = 256
ROPE_THETA = 10000.0
Q_BLOCK = 128

N_EXPERTS = 16
N_GROUPS = 4
EXPERTS_PER_GROUP = N_EXPERTS // N_GROUPS
TOP_K = 2
GROUP_SCORE_TOPK = 2
EXPERT_FF = 512

N_BRANCH = 3
IN_SIZES = (3 * HY_W, GDN_HEADS * (2 * GDN_DK + GDN_DV), GDN_HEADS * GDN_DV, 2 * GDN_HEADS, 2 * GDN_HEADS, MLA_Q_LORA, MLA_KV_LORA, MLA_ROPE, N_BRANCH * D_MODEL)

kernel_name = 'hybrid_hyena_gdn_mla_moe_diffusion_trunk'


def rms_norm(x, g):
    xf = x.astype(F32)
    y = xf * lax.rsqrt(jnp.mean(xf * xf, axis=-1, keepdims=True) + EPS)
    return (y * g.astype(F32)).astype(x.dtype)


def l2_normalize(x):
    xf = x.astype(F32)
    return xf * lax.rsqrt(jnp.sum(xf * xf, axis=-1, keepdims=True) + EPS)


def centred_conv(x, w):
    k_w = w.shape[0]
    r = k_w // 2
    L = x.shape[1]
    xp = jnp.pad(x, ((0, 0), (r, r), (0, 0)))
    y = xp[:, 0:L] * w[0]
    for j in range(1, k_w):
        y = y + xp[:, j:j + L] * w[j]
    return y


def split_in(p):
    parts = []
    off = 0
    for n in IN_SIZES:
        parts.append(p[..., off:off + n])
        off += n
    return parts


def hyena_filters(L, lp):
    t = jnp.linspace(0.0, 1.0, L, dtype=F32)[:, None]
    w = (2.0 * math.pi / L) * jnp.arange(L, dtype=F32)[:, None]
    f = jnp.linspace(1e-4, HY_BANDS - 1, HY_BANDS, dtype=F32)[None, :]
    z = jnp.concatenate([t, jnp.cos(f * w), -jnp.sin(f * w)], axis=-1)
    freq = lp['hy_f_freq'].astype(F32)
    hdn = jnp.sin(freq * (z @ lp['hy_f_w1'].astype(F32) + lp['hy_f_b1'].astype(F32)))
    hdn = jnp.sin(freq * (hdn @ lp['hy_f_w2'].astype(F32) + lp['hy_f_b2'].astype(F32)))
    h = hdn @ lp['hy_f_w3'].astype(F32)
    window = jnp.exp(-t * jnp.abs(lp['hy_decay'].astype(F32))) + HY_MOD_SHIFT
    return h * window


def bidir_long_conv(u, filt, bias):
    B, L, C = u.shape
    kbuf = jnp.concatenate([filt[:, :C], jnp.zeros((1, C), F32), filt[:0:-1, C:]], axis=0)
    uf = jnp.fft.rfft(u.astype(F32), n=2 * L, axis=1)
    kf = jnp.fft.rfft(kbuf, axis=0)
    y = jnp.fft.irfft(uf * kf[None], n=2 * L, axis=1)[:, :L]
    return (y + u.astype(F32) * bias.astype(F32)).astype(u.dtype)


def hyena_mixer(p, lp):
    filt = hyena_filters(p.shape[1], lp)
    u = centred_conv(p, lp['hy_conv_w']) + lp['hy_conv_b']
    x0, x1, v = jnp.split(u, 3, axis=-1)
    return x0 * bidir_long_conv(x1 * v, filt, lp['hy_bias'])


def gdn_features(qkv, a, b, lp):
    B, L, _ = qkv.shape
    qkv = jax.nn.silu(centred_conv(qkv, lp['gdn_conv_w']))
    nk = GDN_HEADS * GDN_DK
    q = l2_normalize(qkv[..., :nk].reshape(B, L, GDN_HEADS, GDN_DK)) * (GDN_DK ** -0.5)
    k = l2_normalize(qkv[..., nk:2 * nk].reshape(B, L, GDN_HEADS, GDN_DK))
    v = qkv[..., 2 * nk:].reshape(B, L, GDN_HEADS, GDN_DV).astype(F32)
    a = a.astype(F32).reshape(B, L, 2, GDN_HEADS)
    g = -jnp.exp(lp['gdn_a_log'].astype(F32)) * jax.nn.softplus(a + lp['gdn_dt_bias'].astype(F32))
    beta = jax.nn.sigmoid(b.astype(F32).reshape(B, L, 2, GDN_HEADS))
    return q, k, v, g, beta


def gated_delta_chunked(q, k, v, g, beta, s0):
    B, L, H, DK = q.shape
    DV = v.shape[-1]
    C = GDN_CHUNK
    N = L // C

    def chunks5(a):
        return a.astype(F32).reshape(B, N, C, H, a.shape[-1]).transpose(1, 0, 3, 2, 4)

    def chunks4(a):
        return a.astype(F32).reshape(B, N, C, H).transpose(1, 0, 3, 2)

    qc, kc, vc = chunks5(q), chunks5(k), chunks5(v)
    gc = jnp.cumsum(chunks4(g), axis=-1)
    bc = chunks4(beta)
    causal = jnp.tril(jnp.ones((C, C), dtype=bool))
    strict = jnp.tril(jnp.ones((C, C), dtype=bool), -1)
    gamma = jnp.exp(jnp.where(causal, gc[..., :, None] - gc[..., None, :], -jnp.inf))
    kb = kc * bc[..., None]
    m = jnp.where(strict, jnp.einsum('nbhid,nbhjd->nbhij', kb, kc) * gamma, 0.0)
    eye = jnp.eye(C, dtype=F32)
    t_inv = lax.linalg.triangular_solve(eye + m, jnp.broadcast_to(eye, m.shape), left_side=True, lower=True, unit_diagonal=True)
    u = t_inv @ (vc * bc[..., None])
    w = t_inv @ (kb * jnp.exp(gc)[..., None])
    a_intra = jnp.einsum('nbhid,nbhjd->nbhij', qc, kc) * gamma
    q_dec = qc * jnp.exp(gc)[..., None]
    k_dec = kc * jnp.exp(gc[..., -1:] - gc)[..., None]
    g_end = jnp.exp(gc[..., -1])

    def step(s, xs):
        u_n, w_n, a_n, qd_n, kd_n, ge_n = xs
        v_new = u_n - w_n @ s
        o_n = qd_n @ s + a_n @ v_new
        s = s * ge_n[..., None, None] + jnp.swapaxes(kd_n, -1, -2) @ v_new
        return s, o_n

    s_fin, o = lax.scan(step, s0.astype(F32), (u, w, a_intra, q_dec, k_dec, g_end))
    o = o.transpose(1, 0, 3, 2, 4).reshape(B, L, H, DV)
    return o, s_fin


def gdn_bidir(feats, s0_fwd, s0_bwd):
    q, k, v, g, beta = feats
    o_f, s_f = gated_delta_chunked(q, k, v, g[:, :, 0], beta[:, :, 0], s0_fwd)
    rev = lambda a: jnp.flip(a, axis=1)
    o_b, s_b = gated_delta_chunked(rev(q), rev(k), rev(v), rev(g[:, :, 1]), rev(beta[:, :, 1]), s0_bwd)
    return o_f + rev(o_b), s_f, s_b


def gdn_output(o, z, lp):
    B, L, H, DV = o.shape
    on = o * lax.rsqrt(jnp.mean(o * o, axis=-1, keepdims=True) + EPS) * lp['gdn_norm_g'].astype(F32)
    y = on * jax.nn.silu(z.astype(F32)).reshape(B, L, H, DV)
    return y.reshape(B, L, H * DV).astype(z.dtype) @ lp['gdn_out']


def rope_2d(x, row, col):
    nf = MLA_ROPE // 4
    half = MLA_ROPE // 2
    inv_freq = ROPE_THETA ** (-jnp.arange(nf, dtype=F32) / nf)
    xf = x.astype(F32)

    def rotate(xa, pos):
        ang = pos.astype(F32)[:, None] * inv_freq[None, :]
        cos = jnp.cos(ang)[None, :, None, :]
        sin = jnp.sin(ang)[None, :, None, :]
        x1, x2 = xa[..., :nf], xa[..., nf:]
        return jnp.concatenate([x1 * cos - x2 * sin, x1 * sin + x2 * cos], axis=-1)

    return jnp.concatenate([rotate(xf[..., :half], row), rotate(xf[..., half:], col)], axis=-1).astype(x.dtype)


def mla_queries(cq, lp):
    B, L, _ = cq.shape
    q = (rms_norm(cq, lp['mla_q_norm_g']) @ lp['mla_w_uq']).reshape(B, L, MLA_HEADS, MLA_NOPE + MLA_ROPE)
    return q[..., :MLA_NOPE], q[..., MLA_NOPE:]


def mla_keys_values(ckv, lp):
    B, L, _ = ckv.shape
    kv = (rms_norm(ckv, lp['mla_kv_norm_g']) @ lp['mla_w_ukv']).reshape(B, L, MLA_HEADS, MLA_NOPE + MLA_V)
    return kv[..., :MLA_NOPE], kv[..., MLA_NOPE:]


def mla_attend(qn, qr, kn, kr, v):
    B, S, H, _ = qn.shape
    nb = S // Q_BLOCK
    scale = (MLA_NOPE + MLA_ROPE) ** -0.5

    def to_blocks(a):
        return jnp.swapaxes(a.reshape((B, nb, Q_BLOCK) + a.shape[2:]), 0, 1)

    def one_block(blk):
        qn_b, qr_b = blk
        s = jnp.einsum('bqhd,bkhd->bhqk', qn_b, kn, preferred_element_type=F32)
        s = s + jnp.einsum('bqhr,bkr->bhqk', qr_b, kr, preferred_element_type=F32)
        p = jax.nn.softmax(s * scale, axis=-1)
        return jnp.einsum('bhqk,bkhd->bqhd', p.astype(v.dtype), v)

    o = lax.map(one_block, (to_blocks(qn), to_blocks(qr)))
    return jnp.swapaxes(o, 0, 1).reshape(B, S, H * MLA_V)


def merge_branches(gate_raw, y_hy, y_gdn, y_mla, w_out):
    g_hy, g_gdn, g_mla = jnp.split(jax.nn.sigmoid(gate_raw.astype(F32)), N_BRANCH, axis=-1)
    merged = g_hy * y_hy + g_gdn * y_gdn + g_mla * y_mla
    return merged.astype(w_out.dtype) @ w_out


def moe_ffn(h, router_w, router_b, lp):
    T, D = h.shape
    scores = jax.nn.sigmoid(jnp.dot(h, router_w, preferred_element_type=F32))
    sel = scores + router_b.astype(F32)
    grouped = sel.reshape(T, N_GROUPS, EXPERTS_PER_GROUP)
    group_score = jnp.sum(lax.top_k(grouped, GROUP_SCORE_TOPK)[0], axis=-1)
    grp = jnp.argmax(group_score, axis=-1)
    group_mask = jnp.arange(N_GROUPS)[None, :] == grp[:, None]
    masked = jnp.where(group_mask[:, :, None], grouped, -jnp.inf).reshape(T, N_EXPERTS)
    _, idx = lax.top_k(masked, TOP_K)
    wts = jnp.take_along_axis(scores, idx, axis=-1)
    wts = wts / jnp.sum(wts, axis=-1, keepdims=True)
    gate = jnp.sum(jax.nn.one_hot(idx, N_EXPERTS, dtype=F32) * wts[..., None], axis=1)
    out = jnp.zeros((T, D), F32)
    for e in range(N_EXPERTS):
        he = jax.nn.silu(h @ lp['moe_w1'][e]) * (h @ lp['moe_w3'][e])
        out = out + gate[:, e:e + 1] * (he @ lp['moe_w2'][e])
    return out.astype(h.dtype)


def trunk_layer(x, cx, mod, mod_c, row, col, lp, router_w, router_b, update_ctx):
    B, S, D = x.shape
    sh1, sc1, gt1, sh2, sc2, gt2 = jnp.split(mod[:, None, :], 6, axis=-1)
    csh1, csc1, cgt1, csh2, csc2, cgt2 = jnp.split(mod_c, 6, axis=-1)
    h = rms_norm(x, lp['norm1_g']) * (1.0 + sc1) + sh1
    hc = rms_norm(cx, lp['norm1_g']) * (1.0 + csc1) + csh1
    hy_l, qkv_l, z_l, a_l, b_l, cq_l, ckv_l, kr_l, gate_l = split_in(h @ lp['w_in'])
    hy_c, qkv_c, z_c, a_c, b_c, cq_c, ckv_c, kr_c, gate_c = split_in(hc @ lp['w_in'])

    s_zero = jnp.zeros((B, GDN_HEADS, GDN_DK, GDN_DV), F32)
    o_c, s_f, s_b = gdn_bidir(gdn_features(qkv_c, a_c, b_c, lp), s_zero, s_zero)
    o_l, _, _ = gdn_bidir(gdn_features(qkv_l, a_l, b_l, lp), s_f, s_b)
    y_gdn = gdn_output(o_l, z_l, lp)

    kn_c, v_c = mla_keys_values(ckv_c, lp)
    kn_l, v_l = mla_keys_values(ckv_l, lp)
    kr_lat = rope_2d(kr_l[:, :, None, :], row, col)[:, :, 0, :]
    qn_l, qr_l = mla_queries(cq_l, lp)
    qr_l = rope_2d(qr_l, row, col)
    attn_l = mla_attend(qn_l, qr_l, jnp.concatenate([kn_l, kn_c], axis=1), jnp.concatenate([kr_lat, kr_c], axis=1), jnp.concatenate([v_l, v_c], axis=1))
    y_mla = attn_l @ lp['mla_out']

    y_hy = hyena_mixer(hy_l, lp) @ lp['hy_out']

    mix = merge_branches(gate_l, y_hy, y_gdn, y_mla, lp['w_out'])
    x = x + (gt1 * mix).astype(x.dtype)
    h2 = rms_norm(x, lp['norm2_g']) * (1.0 + sc2) + sh2
    x = x + (gt2 * moe_ffn(h2.reshape(B * S, D), router_w, router_b, lp).reshape(B, S, D)).astype(x.dtype)

    if update_ctx:
        Bc, Lc, _ = cx.shape
        y_hy_c = hyena_mixer(hy_c, lp) @ lp['hy_out']
        y_gdn_c = gdn_output(o_c, z_c, lp)
        qn_c, qr_c = mla_queries(cq_c, lp)
        y_mla_c = mla_attend(qn_c, qr_c, kn_c, kr_c, v_c) @ lp['mla_out']
        mix_c = merge_branches(gate_c, y_hy_c, y_gdn_c, y_mla_c, lp['w_out'])
        cx = cx + (cgt1 * mix_c).astype(cx.dtype)
        hc2 = rms_norm(cx, lp['norm2_g']) * (1.0 + csc2) + csh2
        cx = cx + (cgt2 * moe_ffn(hc2.reshape(Bc * Lc, D), router_w, router_b, lp).reshape(Bc, Lc, D)).astype(cx.dtype)
    return x, cx


def setup_inputs(seed: int = 0) -> dict:
    key = jax.random.key(seed)
    ks = iter(jax.random.split(key, 48))
    D = D_MODEL
    n_in = sum(IN_SIZES)

    def nrm(shape, scale):
        return scale * jax.random.normal(next(ks), shape, F32)

    def gain(shape):
        return 1.0 + 0.05 * jax.random.normal(next(ks), shape, F32)

    dt = jnp.exp(jax.random.uniform(next(ks), (DEPTH, 2, GDN_HEADS), F32, math.log(1e-3), math.log(1e-1)))
    decay_lo = -math.log(HY_DECAY_TARGET) / HY_SLOW_PCT
    decay_hi = -math.log(HY_DECAY_TARGET) / HY_FAST_PCT
    return {
        'x': nrm((BATCH, SEQ, D), 1.0),
        'c': nrm((BATCH, D), 1.0),
        'ctx': nrm((BATCH, CTX_LEN, D), 1.0),
        'c_ctx': nrm((D,), 1.0),
        'w_ada': nrm((DEPTH, D, 6 * D), 0.5 * D ** -0.5),
        'b_ada': nrm((DEPTH, 6 * D), 0.02),
        'norm1_g': gain((DEPTH, D)),
        'norm2_g': gain((DEPTH, D)),
        'w_in': nrm((DEPTH, D, n_in), D ** -0.5),
        'hy_conv_w': nrm((DEPTH, HY_SHORT, 3 * HY_W), HY_SHORT ** -0.5),
        'hy_conv_b': nrm((DEPTH, 3 * HY_W), 0.02),
        'hy_f_w1': nrm((DEPTH, HY_EMB, HY_FILT_HID), HY_EMB ** -0.5),
        'hy_f_b1': nrm((DEPTH, HY_FILT_HID), 0.1),
        'hy_f_w2': nrm((DEPTH, HY_FILT_HID, HY_FILT_HID), HY_FILT_HID ** -0.5),
        'hy_f_b2': nrm((DEPTH, HY_FILT_HID), 0.1),
        'hy_f_w3': nrm((DEPTH, HY_FILT_HID, 2 * HY_W), 0.1 * HY_FILT_HID ** -0.5),
        'hy_f_freq': gain((DEPTH, HY_FILT_HID)),
        'hy_decay': jax.random.uniform(next(ks), (DEPTH, 2 * HY_W), F32, decay_lo, decay_hi),
        'hy_bias': nrm((DEPTH, HY_W), 0.5),
        'hy_out': nrm((DEPTH, HY_W, D), HY_W ** -0.5),
        'gdn_conv_w': nrm((DEPTH, GDN_SHORT, GDN_HEADS * (2 * GDN_DK + GDN_DV)), GDN_SHORT ** -0.5),
        'gdn_a_log': jnp.log(jax.random.uniform(next(ks), (DEPTH, 2, GDN_HEADS), F32, 1.0, 16.0)),
        'gdn_dt_bias': dt + jnp.log(-jnp.expm1(-dt)),
        'gdn_norm_g': gain((DEPTH, GDN_DV)),
        'gdn_out': nrm((DEPTH, GDN_HEADS * GDN_DV, D), (GDN_HEADS * GDN_DV) ** -0.5),
        'mla_q_norm_g': gain((DEPTH, MLA_Q_LORA)),
        'mla_w_uq': nrm((DEPTH, MLA_Q_LORA, MLA_HEADS * (MLA_NOPE + MLA_ROPE)), MLA_Q_LORA ** -0.5),
        'mla_kv_norm_g': gain((DEPTH, MLA_KV_LORA)),
        'mla_w_ukv': nrm((DEPTH, MLA_KV_LORA, MLA_HEADS * (MLA_NOPE + MLA_V)), MLA_KV_LORA ** -0.5),
        'mla_out': nrm((DEPTH, MLA_HEADS * MLA_V, D), (MLA_HEADS * MLA_V) ** -0.5),
        'w_out': nrm((DEPTH, D, D), D ** -0.5),
        'moe_w1': nrm((DEPTH, N_EXPERTS, D, EXPERT_FF), D ** -0.5),
        'moe_w3': nrm((DEPTH, N_EXPERTS, D, EXPERT_FF), D ** -0.5),
        'moe_w2': nrm((DEPTH, N_EXPERTS, EXPERT_FF, D), EXPERT_FF ** -0.5),
        'router_w': nrm((D, N_EXPERTS), D ** -0.5),
        'router_b': nrm((N_EXPERTS,), 0.01),
        'final_norm_g': gain((D,)),
    }


def reference(x, c, ctx, c_ctx, w_ada, b_ada, norm1_g, norm2_g, w_in, hy_conv_w, hy_conv_b, hy_f_w1, hy_f_b1, hy_f_w2, hy_f_b2, hy_f_w3, hy_f_freq, hy_decay, hy_bias, hy_out, gdn_conv_w, gdn_a_log, gdn_dt_bias, gdn_norm_g, gdn_out, mla_q_norm_g, mla_w_uq, mla_kv_norm_g, mla_w_ukv, mla_out, w_out, moe_w1, moe_w3, moe_w2, router_w, router_b, final_norm_g):
    S = x.shape[1]
    rows = S // GRID_W
    row = jnp.repeat(jnp.arange(rows, dtype=jnp.int32), GRID_W)
    col = jnp.tile(jnp.arange(GRID_W, dtype=jnp.int32), rows)
    cx = ctx
    for l in range(DEPTH):
        lp = {
            'norm1_g': norm1_g[l], 'norm2_g': norm2_g[l], 'w_in': w_in[l],
            'hy_conv_w': hy_conv_w[l], 'hy_conv_b': hy_conv_b[l],
            'hy_f_w1': hy_f_w1[l], 'hy_f_b1': hy_f_b1[l], 'hy_f_w2': hy_f_w2[l], 'hy_f_b2': hy_f_b2[l],
            'hy_f_w3': hy_f_w3[l], 'hy_f_freq': hy_f_freq[l], 'hy_decay': hy_decay[l], 'hy_bias': hy_bias[l],
            'hy_out': hy_out[l],
            'gdn_conv_w': gdn_conv_w[l], 'gdn_a_log': gdn_a_log[l], 'gdn_dt_bias': gdn_dt_bias[l],
            'gdn_norm_g': gdn_norm_g[l], 'gdn_out': gdn_out[l],
            'mla_q_norm_g': mla_q_norm_g[l], 'mla_w_uq': mla_w_uq[l], 'mla_kv_norm_g': mla_kv_norm_g[l],
            'mla_w_ukv': mla_w_ukv[l], 'mla_out': mla_out[l],
            'w_out': w_out[l], 'moe_w1': moe_w1[l], 'moe_w3': moe_w3[l], 'moe_w2': moe_w2[l],
        }
        mod = jax.nn.silu(c) @ w_ada[l] + b_ada[l]
        mod_c = jax.nn.silu(c_ctx) @ w_ada[l] + b_ada[l]
        x, cx = trunk_layer(x, cx, mod, mod_c, row, col, lp, router_w, router_b, l < DEPTH - 1)
    return rms_norm(x, final_norm_g)
```

```python
import numpy as np
from contextlib import ExitStack
import concourse.bass as bass
import concourse.mybir as mybir
from concourse.bass_utils import run_bass_kernel_spmd

F32 = mybir.dt.float32
BF16 = mybir.dt.bfloat16
AF = mybir.ActivationFunctionType
ALU = mybir.AluOpType
AX = mybir.AxisListType


class Buf:
    __slots__ = ("t", "w", "r", "pr", "excl")

    def __init__(self, t=None, excl=False):
        self.t = t
        self.w = []
        self.r = []
        self.pr = []
        self.excl = excl

    def __getitem__(self, k):
        return self.t[k]


class KB:
    SEM_EPOCH = 20000
    NDMA = 10

    def __init__(self):
        self.nc = bass.Bass("TRN2", target_bir_lowering=False)
        nc = self.nc
        self.es = ExitStack()
        self.eng = {"pe": nc.tensor, "act": nc.scalar, "dve": nc.vector, "pool": nc.gpsimd, "sp": nc.sync}
        self.csem = {}
        self.seen = {e: {} for e in self.eng}
        self.dpool = {}
        self.nsem = 0
        self.last_tok = {}
        self.out_toks = []
        self.ninst = 0

    def _newsem(self, name):
        self.nsem += 1
        return self.es.enter_context(self.nc.semaphore(f"{name}_{self.nsem}"))

    def sb(self, name, shape, dt, stack=None):
        self.nsem += 1
        name = f"{name}_u{self.nsem}"
        t = (stack or self.es).enter_context(self.nc.sbuf_tensor(name, list(shape), dt))
        return Buf(t)

    def ps(self, name, shape=(128, 512), dt=F32, stack=None):
        t = (stack or self.es).enter_context(self.nc.psum_tensor(name, list(shape), dt))
        return Buf(t, excl=True)

    def dram(self, name, shape, dt, kind="Internal"):
        return self.nc.dram_tensor(name, list(shape), dt, kind=kind).ap()

    def _wait(self, e, toks):
        en = self.eng[e]
        best = {}
        for (s, v) in toks:
            if best.get(s, (None, 0))[1] < v:
                best[s] = (s, v)
        for s, v in best.values():
            if self.seen[e].get(s.num, 0) < v:
                en.wait_ge(s, v)
                self.seen[e][s.num] = v
                self.ninst += 1

    def _deps(self, e, reads, writes, waw):
        toks = []
        for b in reads:
            toks.extend(b.w)
            if getattr(b, "excl", False):
                toks.extend(b.r)
        for b in writes:
            toks.extend(b.r)
            toks.extend(b.pr)
            if waw or b.r:
                toks.extend(b.w)
        return toks

    def _commit(self, tok, reads, writes):
        for b in writes:
            if b.r:
                b.pr = list(b.r) + list(b.w)
                b.w = [tok]
                b.r = []
            else:
                b.w = [t for t in b.w if t[0] is not tok[0]] + [tok]
        for b in reads:
            if b not in writes:
                b.r = [t for t in b.r if t[0] is not tok[0]] + [tok]

    def op(self, e, fn, reads=(), writes=(), waw=False):
        toks = self._deps(e, reads, writes, waw)
        if e == "pe":
            mysem = self.csem.get("pe")
            if mysem is not None:
                toks = [t for t in toks if t[0] is not mysem[0]]
        self._wait(e, toks)
        s = self.csem.get(e)
        if s is None or s[1] >= self.SEM_EPOCH:
            s = [self._newsem("c" + e), 0]
            self.csem[e] = s
        ins = fn()
        s[1] += 1
        ins.then_inc(s[0], 1)
        tok = (s[0], s[1])
        self.last_tok[e] = tok
        self._commit(tok, reads, writes)
        self.ninst += 1
        return tok

    def dma(self, q, out, in_, reads=(), writes=(), waw=False, **kw):
        toks = self._deps(q, reads, writes, waw)
        pool = self.dpool.setdefault(q, {"sems": [], "uses": [], "i": 0})
        if len(pool["sems"]) < self.NDMA:
            pool["sems"].append(self._newsem("d" + q))
            pool["uses"].append(0)
            k = len(pool["sems"]) - 1
        else:
            k = pool["i"] % self.NDMA
        pool["i"] += 1
        s = pool["sems"][k]
        u = pool["uses"][k]
        if u > 0:
            toks.append((s, 16 * u))
        self._wait(q, toks)
        ins = self.eng[q].dma_start(out=out, in_=in_, **kw)
        ins.then_inc(s, 16)
        pool["uses"][k] = u + 1
        tok = (s, 16 * (u + 1))
        self._commit(tok, reads, writes)
        self.ninst += 1
        return tok

    def all_tokens(self):
        toks = list(self.last_tok.values())
        for q, pool in self.dpool.items():
            for s, u in zip(pool["sems"], pool["uses"]):
                if u > 0:
                    toks.append((s, 16 * u))
        return toks

    def barrier(self):
        toks = self.all_tokens()
        for e in self.eng:
            self._wait(e, toks)

    def finish(self):
        self.barrier()


D = 1024
NIN = 7728
IN_SEGS = [("hy", 0, 1536), ("qkv", 1536, 1536), ("z", 3072, 512), ("ab", 3584, 16), ("cq", 3600, 768),
           ("ckv", 4368, 256), ("kr", 4624, 32), ("gate", 4656, 3072)]


def mchunks():
    out = []
    for name, c0, w in IN_SEGS:
        o = 0
        while o < w:
            m = min(128, w - o)
            out.append((name, c0 + o, m))
            o += m
    return out


class Ctx:
    pass


def setup_consts(kb, C):
    C.ones_bf = kb.sb("ones_bf", [128, 128], BF16)
    kb.op("dve", lambda: kb.nc.vector.memset(C.ones_bf[:], 1.0), writes=[C.ones_bf])
    C.eps_t = kb.sb("eps_t", [128, 1], F32)
    kb.op("dve", lambda: kb.nc.vector.memset(C.eps_t[:], 1e-6), writes=[C.eps_t])
    C.one_t = kb.sb("one_t", [128, 1], F32)
    kb.op("dve", lambda: kb.nc.vector.memset(C.one_t[:], 1.0), writes=[C.one_t])
    C.psum = [kb.ps(f"ps{i}") for i in range(8)]
    C.ident_f = kb.sb("ident_f", [128, 128], F32)
    C.ident_d = kb.dram("ident_f_d", [128, 128], F32, "ExternalInput")
    kb.dma("sp", C.ident_f[:], C.ident_d, writes=[C.ident_f])
    C.psi = 0


def next_ps(C):
    p = C.psum[C.psi % 8]
    C.psi += 1
    return p


def load_w_bf16(kb, dst, dst_ap, src_ap, q="pool"):
    return kb.dma(q, dst_ap, src_ap, writes=[dst])


def stage_in(kb, C, st, tiles, AB, w_sb, pinT):
    nc = kb.nc
    NB = 2
    xt = [kb.sb(f"in_x{i}", [128, 8, 512], F32, st) for i in range(1)]
    sq = [kb.sb(f"in_sq{i}", [128, 8, 512], BF16, st) for i in range(1)]
    rs = [kb.sb(f"in_rs{i}", [128, 512], F32, st) for i in range(NB)]
    tmp = [kb.sb(f"in_tmp{i}", [128, 512], F32, st) for i in range(4)]
    hT = [kb.sb(f"in_h{i}", [128, 8, 512], BF16, st) for i in range(NB)]
    ob = [kb.sb(f"in_o{i}", [128, 512], BF16, st) for i in range(6)]
    mcs = mchunks()
    oi = 0
    for ti, (src, t0, n, d0, which) in enumerate(tiles):
        b = ti % NB
        X, SQ, RS, H = xt[0], sq[0], rs[b], hT[b]
        kb.dma("sp", X[:, :, :n], src.rearrange("(k p) t -> p k t", p=128)[:, :, t0:t0 + n], writes=[X])
        rmsnorm_tile(kb, C, X, n, PV(lambda k: AB[:, 0, which, k:k + 1], [AB]), PV(lambda k: AB[:, 1, which, k:k + 1], [AB]), which, H, SQ, RS, tmp)
        for mi, (name, c0, m) in enumerate(mcs):
            ps = next_ps(C)
            for k in range(8):
                kb.op("pe", lambda: nc.tensor.matmul(ps[:m, :n], lhsT=w_sb[:, k, c0:c0 + m], rhs=H[:, k, :n], start=(k == 0), stop=(k == 7)),
                      reads=[H, w_sb], writes=[ps])
            O = ob[oi % 6]
            oi += 1
            if name == "gate":
                kb.op("act", lambda: nc.scalar.activation(out=O[:m, :n], in_=ps[:m, :n], func=AF.Sigmoid), reads=[ps], writes=[O])
            elif mi % 2 == 0:
                kb.op("dve", lambda: nc.vector.tensor_copy(out=O[:m, :n], in_=ps[:m, :n]), reads=[ps], writes=[O])
            else:
                kb.op("act", lambda: nc.scalar.copy(out=O[:m, :n], in_=ps[:m, :n]), reads=[ps], writes=[O])
            kb.dma("pool", pinT[c0:c0 + m, d0:d0 + n], O[:m, :n], reads=[O])


def stage_mod(kb, C, st, c2, wada, bada, g1, g2, modv, AB):
    nc = kb.nc
    sc = kb.sb("mod_sc", [128, 8, 2], F32, st)
    ba = kb.sb("mod_ba", [128, 48], F32, st)
    gg = kb.sb("mod_g", [128, 2, 8], F32, st)
    wa = [kb.sb(f"mod_wa{i}", [128, 8, 1536], F32, st) for i in range(2)]
    kb.dma("sp", sc[:], c2, writes=[sc])
    kb.dma("sp", ba[:], bada, writes=[ba])
    kb.dma("sp", gg[:, 0, :], g1, writes=[gg])
    kb.dma("sp", gg[:, 1, :], g2, writes=[gg])
    kb.op("act", lambda: nc.scalar.activation(out=sc[:], in_=sc[:], func=AF.Silu), reads=[sc], writes=[sc])
    for cg in range(4):
        W = wa[cg % 2]
        kb.dma("sp", W[:], wada.rearrange("(k p) n -> p k n", p=128)[:, :, cg * 1536:(cg + 1) * 1536], writes=[W])
        for m in range(12):
            j = cg * 12 + m
            ps = next_ps(C)
            for k in range(8):
                kb.op("pe", lambda: nc.tensor.matmul(ps[:, 0:2], lhsT=W[:, k, m * 128:(m + 1) * 128], rhs=sc[:, k, :], start=(k == 0), stop=(k == 7)),
                      reads=[W, sc], writes=[ps])
            kb.op("dve", lambda: nc.vector.tensor_scalar(out=modv[:, j, :], in0=ps[:, 0:2], scalar1=ba[:, j:j + 1], scalar2=None, op0=ALU.add),
                  reads=[ps, ba], writes=[modv])
    for which in range(2):
        for half, gi in ((0, 0), (1, 1)):
            o = half * 24
            kb.op("dve", lambda: nc.vector.scalar_tensor_tensor(out=AB[:, half * 3 + 0, which, :], in0=modv[:, o + 8:o + 16, which], scalar=1.0, in1=gg[:, gi, :],
                                                               op0=ALU.add, op1=ALU.mult), reads=[modv, gg], writes=[AB])
            kb.op("dve", lambda: nc.vector.tensor_copy(out=AB[:, half * 3 + 1, which, :], in_=modv[:, o:o + 8, which]), reads=[modv], writes=[AB])
            kb.op("dve", lambda: nc.vector.tensor_copy(out=AB[:, half * 3 + 2, which, :], in_=modv[:, o + 16:o + 24, which]), reads=[modv], writes=[AB])


def rmsnorm_tile(kb, C, X, n, Avec, Bvec, which, H, SQ, RS, tmp, Hf=None, nk=8, dim=D):
    nc = kb.nc
    kb.op("act", lambda: nc.scalar.activation(out=SQ[:, :nk, :n], in_=X[:, :nk, :n], func=AF.Square), reads=[X], writes=[SQ])
    pss = next_ps(C)
    for k in range(nk):
        kb.op("pe", lambda: nc.tensor.matmul(pss[:, :n], lhsT=C.ones_bf[:], rhs=SQ[:, k, :n], start=(k == 0), stop=(k == nk - 1)),
              reads=[SQ, C.ones_bf], writes=[pss])
    kb.op("act", lambda: nc.scalar.activation(out=RS[:, :n], in_=pss[:, :n], func=AF.Sqrt, scale=1.0 / dim, bias=C.eps_t[:, 0:1]), reads=[pss, C.eps_t], writes=[RS])
    kb.op("dve", lambda: nc.vector.reciprocal(out=RS[:, :n], in_=RS[:, :n]), reads=[RS], writes=[RS])
    for k in range(nk):
        T = tmp[k % len(tmp)]
        kb.op("dve", lambda: nc.vector.scalar_tensor_tensor(out=T[:, :n], in0=X[:, k, :n], scalar=Avec(k), in1=RS[:, :n],
                                                           op0=ALU.mult, op1=ALU.mult), reads=[X, RS] + Avec.bufs, writes=[T])
        if Bvec is not None:
            kb.op("act", lambda: nc.scalar.activation(out=H[:, k, :n], in_=T[:, :n], func=AF.Identity, bias=Bvec(k)),
                  reads=[T] + Bvec.bufs, writes=[H])
            if Hf is not None:
                kb.op("act", lambda: nc.scalar.activation(out=Hf[:, k, :n], in_=T[:, :n], func=AF.Identity, bias=Bvec(k)),
                      reads=[T] + Bvec.bufs, writes=[Hf])
        else:
            kb.op("act", lambda: nc.scalar.copy(out=H[:, k, :n], in_=T[:, :n]), reads=[T], writes=[H])


class PV:
    def __init__(self, fn, bufs):
        self.fn = fn
        self.bufs = bufs

    def __call__(self, k):
        return self.fn(k)


def stage_merge(kb, C, st, tiles, AB, yT, pinT, w3, wout, xoutT):
    nc = kb.nc
    NB = 2
    xt = [kb.sb(f"mg_x{i}", [128, 8, 512], F32, st) for i in range(NB)]
    yy = [kb.sb(f"mg_y{i}", [128, 3, 4, 512], BF16, st) for i in range(NB)]
    gt = [kb.sb(f"mg_g{i}", [128, 24, 512], BF16, st) for i in range(1)]
    mg = [kb.sb(f"mg_m{i}", [128, 8, 512], BF16, st) for i in range(NB)]
    tt = [kb.sb(f"mg_t{i}", [128, 3, 512], F32, st) for i in range(2)]
    xo = [kb.sb(f"mg_xo{i}", [128, 8, 512], F32, st) for i in range(1)]
    for ti, (src, t0, n, p0, which, dst) in enumerate(tiles):
        b = ti % NB
        X, Y, G, M, XO = xt[b], yy[b], gt[0], mg[b], xo[0]
        kb.dma("sp", X[:, :, :n], src.rearrange("(k p) t -> p k t", p=128)[:, :, t0:t0 + n], writes=[X])
        for j in range(3):
            kb.dma("sp", Y[:, j, :, :n], yT[j].rearrange("(k p) t -> p k t", p=128)[:, :, p0:p0 + n], writes=[Y])
        kb.dma("sp", G[:, :, :n], pinT[4656:7728, :].rearrange("(k p) t -> p k t", p=128)[:, :, p0:p0 + n], writes=[G])
        for m in range(8):
            TT = tt[m % 2]
            pss = []
            for j in range(3):
                ps = next_ps(C)
                pss.append(ps)
                for k in range(4):
                    kb.op("pe", lambda: nc.tensor.matmul(ps[:, :n], lhsT=w3[:, j, k, m * 128:(m + 1) * 128], rhs=Y[:, j, k, :n], start=(k == 0), stop=(k == 3)),
                          reads=[Y, w3], writes=[ps])
            for j in range(3):
                kb.op("dve", lambda: nc.vector.tensor_tensor(out=TT[:, j, :n], in0=pss[j][:, :n], in1=G[:, j * 8 + m, :n], op=ALU.mult),
                      reads=[pss[j], G], writes=[TT])
            kb.op("pool", lambda: nc.gpsimd.tensor_tensor(out=TT[:, 0, :n], in0=TT[:, 0, :n], in1=TT[:, 1, :n], op=ALU.add), reads=[TT], writes=[TT])
            kb.op("pool", lambda: nc.gpsimd.tensor_tensor(out=M[:, m, :n], in0=TT[:, 0, :n], in1=TT[:, 2, :n], op=ALU.add), reads=[TT], writes=[M])
        for m in range(8):
            ps = next_ps(C)
            for k in range(8):
                kb.op("pe", lambda: nc.tensor.matmul(ps[:, :n], lhsT=wout[:, k, m * 128:(m + 1) * 128], rhs=M[:, k, :n], start=(k == 0), stop=(k == 7)),
                      reads=[M, wout], writes=[ps])
            kb.op("dve", lambda: nc.vector.scalar_tensor_tensor(out=XO[:, m, :n], in0=ps[:, :n], scalar=AB[:, 2, which, m:m + 1], in1=X[:, m, :n],
                                                               op0=ALU.mult, op1=ALU.add), reads=[ps, AB, X], writes=[XO])
        kb.dma("pool", dst.rearrange("(k p) t -> p k t", p=128)[:, :, t0:t0 + n], XO[:, :, :n], reads=[XO])


def stage_moe(kb, C, st, supers, AB, rw_sb, rb_bc, w1d, w3d, w2d):
    nc = kb.nc
    X = kb.sb("moe_x", [128, 8, 512], F32, st)
    SQ = kb.sb("moe_sq", [128, 8, 512], BF16, st)
    RS = kb.sb("moe_rs", [128, 512], F32, st)
    tmp = [kb.sb(f"moe_tmp{i}", [128, 512], F32, st) for i in range(2)]
    Hf = kb.sb("moe_hf", [128, 8, 512], F32, st)
    Hb = kb.sb("moe_hb", [128, 8, 1024], BF16, st)
    acc = kb.sb("moe_acc", [128, 8, 1024], F32, st)
    gate = kb.sb("moe_gate", [128, 8, 16], F32, st)
    rt = [kb.sb(f"moe_rt{i}", [128, 64], F32, st) for i in range(2)]
    w1 = [kb.sb(f"moe_w1_{i}", [128, 8, 512], BF16, st) for i in range(2)]
    w3 = [kb.sb(f"moe_w3_{i}", [128, 8, 512], BF16, st) for i in range(2)]
    w2 = [kb.sb(f"moe_w2_{i}", [128, 4, 1024], BF16, st) for i in range(2)]
    he = [kb.sb(f"moe_he{i}", [128, 4, 512], BF16, st) for i in range(2)]
    sl = [kb.sb(f"moe_sl{i}", [128, 512], F32, st) for i in range(2)]
    xo = [kb.sb(f"moe_xo{i}", [128, 8, 512], F32, st) for i in range(1)]
    wi = 0
    for (src, t0, n, which, dst) in supers:
        tl = [(o, min(512, n - o)) for o in range(0, n, 512)]
        srcv = src.rearrange("(k p) t -> p k t", p=128)
        dstv = dst.rearrange("(k p) t -> p k t", p=128)
        for (o, tn) in tl:
            kb.dma("sp", X[:, :, :tn], srcv[:, :, t0 + o:t0 + o + tn], writes=[X])
            Hview = Buf(None)
            rmsnorm_tile(kb, C, X, tn, PV(lambda k: AB[:, 3, which, k:k + 1], [AB]), PV(lambda k: AB[:, 4, which, k:k + 1], [AB]), which,
                         _Off(Hb, o), SQ, RS, tmp, Hf=Hf)
            for s in range(tn // 128):
                sg = (o // 128) + s
                R = rt[sg % 2]
                ps = next_ps(C)
                for k in range(8):
                    kb.op("pe", lambda: nc.tensor.matmul(ps[:, 0:16], lhsT=Hf[:, k, s * 128:(s + 1) * 128], rhs=rw_sb[:, k, :], start=(k == 0), stop=(k == 7)),
                          reads=[Hf, rw_sb], writes=[ps])
                sc = R[:, 0:16]
                sel = R[:, 16:32]
                kb.op("act", lambda: nc.scalar.activation(out=sc, in_=ps[:, 0:16], func=AF.Sigmoid), reads=[ps], writes=[R])
                kb.op("dve", lambda: nc.vector.tensor_tensor(out=sel, in0=sc, in1=rb_bc[:, :], op=ALU.add), reads=[R, rb_bc], writes=[R])
                sel3 = R[:, 16:32].rearrange("p (g j) -> p g j", j=4)
                P3 = R[:, 32:56].rearrange("p (g j) -> p g j", j=6)
                pi = 0
                for a in range(4):
                    for b2 in range(a + 1, 4):
                        kb.op("dve", lambda: nc.vector.tensor_tensor(out=P3[:, :, pi], in0=sel3[:, :, a], in1=sel3[:, :, b2], op=ALU.add), reads=[R], writes=[R])
                        pi += 1
                kb.op("dve", lambda: nc.vector.tensor_reduce(out=R[:, 56:60], in_=P3, axis=AX.X, op=ALU.max), reads=[R], writes=[R])
                kb.op("dve", lambda: nc.vector.tensor_reduce(out=R[:, 60:61], in_=R[:, 56:60], axis=AX.X, op=ALU.max), reads=[R], writes=[R])
                kb.op("dve", lambda: nc.vector.tensor_scalar(out=R[:, 56:60], in0=R[:, 56:60], scalar1=R[:, 60:61], scalar2=None, op0=ALU.is_ge), reads=[R], writes=[R])
                kb.op("dve", lambda: nc.vector.scalar_tensor_tensor(out=sel3, in0=sel3, scalar=2.0, in1=R[:, 56:60].unsqueeze(2).to_broadcast([128, 4, 4]),
                                                                   op0=ALU.add, op1=ALU.mult), reads=[R], writes=[R])
                M1 = R[:, 32:48]
                M2 = R[:, 48:64]
                kb.op("dve", lambda: nc.vector.tensor_reduce(out=R[:, 61:62], in_=sel, axis=AX.X, op=ALU.max), reads=[R], writes=[R])
                G = gate[:, sg, :]
                kb.op("dve", lambda: nc.vector.tensor_scalar(out=G, in0=sel, scalar1=R[:, 61:62], scalar2=None, op0=ALU.is_ge), reads=[R], writes=[gate])
                kb.op("dve", lambda: nc.vector.scalar_tensor_tensor(out=M1, in0=G, scalar=-10.0, in1=sel, op0=ALU.mult, op1=ALU.add), reads=[R, gate], writes=[R])
                kb.op("dve", lambda: nc.vector.tensor_reduce(out=R[:, 61:62], in_=M1, axis=AX.X, op=ALU.max), reads=[R], writes=[R])
                kb.op("dve", lambda: nc.vector.scalar_tensor_tensor(out=G, in0=M1, scalar=R[:, 61:62], in1=G, op0=ALU.is_ge, op1=ALU.add), reads=[R, gate], writes=[gate])
                kb.op("dve", lambda: nc.vector.tensor_tensor(out=G, in0=G, in1=sc, op=ALU.mult), reads=[R, gate], writes=[gate])
                kb.op("dve", lambda: nc.vector.tensor_reduce(out=R[:, 62:63], in_=G, axis=AX.X, op=ALU.add), reads=[gate], writes=[R])
                kb.op("dve", lambda: nc.vector.reciprocal(out=R[:, 62:63], in_=R[:, 62:63]), reads=[R], writes=[R])
                kb.op("dve", lambda: nc.vector.tensor_scalar(out=G, in0=G, scalar1=R[:, 62:63], scalar2=None, op0=ALU.mult), reads=[R, gate], writes=[gate])
        for e in range(16):
            W1, W3, W2 = w1[wi % 2], w3[wi % 2], w2[wi % 2]
            wi += 1
            kb.dma("pool", W1[:], w1d[e].rearrange("(k p) f -> p k f", p=128), writes=[W1])
            kb.dma("pool", W3[:], w3d[e].rearrange("(k p) f -> p k f", p=128), writes=[W3])
            kb.dma("pool", W2[:], w2d[e].rearrange("(k p) f -> p k f", p=128), writes=[W2])
            for ti, (o, tn) in enumerate(tl):
                HE = he[ti % 2]
                for m in range(4):
                    p1 = next_ps(C)
                    p3 = next_ps(C)
                    for k in range(8):
                        kb.op("pe", lambda: nc.tensor.matmul(p1[:, :tn], lhsT=W1[:, k, m * 128:(m + 1) * 128], rhs=Hb[:, k, o:o + tn], start=(k == 0), stop=(k == 7)),
                              reads=[Hb, W1], writes=[p1])
                    for k in range(8):
                        kb.op("pe", lambda: nc.tensor.matmul(p3[:, :tn], lhsT=W3[:, k, m * 128:(m + 1) * 128], rhs=Hb[:, k, o:o + tn], start=(k == 0), stop=(k == 7)),
                              reads=[Hb, W3], writes=[p3])
                    S = sl[m % 2]
                    kb.op("act", lambda: nc.scalar.activation(out=S[:, :tn], in_=p1[:, :tn], func=AF.Silu), reads=[p1], writes=[S])
                    kb.op("dve", lambda: nc.vector.tensor_tensor(out=HE[:, m, :tn], in0=p3[:, :tn], in1=S[:, :tn], op=ALU.mult), reads=[p3, S], writes=[HE])
                for s in range(tn // 128):
                    sg = (o // 128) + s
                    for hf in range(2):
                        po = next_ps(C)
                        for m in range(4):
                            kb.op("pe", lambda: nc.tensor.matmul(po[:, :], lhsT=HE[:, m, s * 128:(s + 1) * 128], rhs=W2[:, m, hf * 512:(hf + 1) * 512], start=(m == 0), stop=(m == 3)),
                                  reads=[HE, W2], writes=[po])
                        A = acc[:, sg, hf * 512:(hf + 1) * 512]
                        if e == 0:
                            kb.op("dve", lambda: nc.vector.tensor_scalar(out=A, in0=po[:, :], scalar1=gate[:, sg, e:e + 1], scalar2=None, op0=ALU.mult),
                                  reads=[po, gate], writes=[acc])
                        else:
                            kb.op("dve", lambda: nc.vector.scalar_tensor_tensor(out=A, in0=po[:, :], scalar=gate[:, sg, e:e + 1], in1=A, op0=ALU.mult, op1=ALU.add),
                                  reads=[po, gate, acc], writes=[acc])
        XO = xo[0]
        for (o, tn) in tl:
            kb.dma("sp", X[:, :, :tn], srcv[:, :, t0 + o:t0 + o + tn], writes=[X])
            for m in range(8):
                pt = next_ps(C)
                for s in range(tn // 128):
                    sg = (o // 128) + s
                    kb.op("pe", lambda: nc.tensor.transpose(pt[:, s * 128:(s + 1) * 128], acc[:, sg, m * 128:(m + 1) * 128], C.ident_f[:]),
                          reads=[acc, C.ident_f], writes=[pt])
                kb.op("dve", lambda: nc.vector.scalar_tensor_tensor(out=XO[:, m, :tn], in0=pt[:, :tn], scalar=AB[:, 5, which, m:m + 1], in1=X[:, m, :tn],
                                                                   op0=ALU.mult, op1=ALU.add), reads=[pt, AB, X], writes=[XO])
            kb.dma("pool", dstv[:, :, t0 + o:t0 + o + tn], XO[:, :, :tn], reads=[XO])


class _Off:
    def __init__(self, b, off):
        self.b = b
        self.off = off

    def __getitem__(self, key):
        p, k, sl_ = key
        return self.b.t[p, k, self.off + (sl_.start or 0):self.off + sl_.stop]

    @property
    def w(self):
        return self.b.w

    @w.setter
    def w(self, v):
        self.b.w = v

    @property
    def pr(self):
        return self.b.pr

    @pr.setter
    def pr(self, v):
        self.b.pr = v

    @property
    def r(self):
        return self.b.r

    @r.setter
    def r(self, v):
        self.b.r = v


def stage_mla_prep(kb, C, st, tiles, pinT, qg, kvg, wuq, wukv, r96, r32, cs96, cs32, qTd, kTd, vd):
    nc = kb.nc
    cq = [kb.sb(f"mp_cq{i}", [128, 6, 512], BF16, st) for i in range(2)]
    ckv = [kb.sb(f"mp_ckv{i}", [128, 2, 512], BF16, st) for i in range(2)]
    kr = [kb.sb(f"mp_kr{i}", [32, 512], BF16, st) for i in range(2)]
    SQ = kb.sb("mp_sq", [128, 6, 512], BF16, st)
    RS = kb.sb("mp_rs", [128, 512], F32, st)
    tmp = [kb.sb(f"mp_tmp{i}", [128, 512], F32, st) for i in range(2)]
    cqn = kb.sb("mp_cqn", [128, 6, 512], BF16, st)
    ckvn = kb.sb("mp_ckvn", [128, 2, 512], BF16, st)
    t96 = [kb.sb(f"mp_t96{i}", [96, 2, 512], F32, st) for i in range(2)]
    t32 = [kb.sb(f"mp_t32{i}", [32, 2, 512], F32, st) for i in range(2)]
    qb = [kb.sb(f"mp_qb{i}", [96, 512], BF16, st) for i in range(2)]
    qf = [kb.sb(f"mp_qf{i}", [96, 512], F32, st) for i in range(2)]
    qo = [kb.sb(f"mp_qo{i}", [96, 512], BF16, st) for i in range(3)]
    ko = [kb.sb(f"mp_ko{i}", [64, 512], BF16, st) for i in range(6)]
    qi2 = [0]
    qr_ = [kb.sb(f"mp_qr{i}", [32, 512], BF16, st) for i in range(3)]
    krf2 = [kb.sb(f"mp_krf2{i}", [32, 512], F32, st) for i in range(2)]
    krg2 = [kb.sb(f"mp_krg2{i}", [32, 512], F32, st) for i in range(2)]
    kro2 = [kb.sb(f"mp_kro2{i}", [32, 512], BF16, st) for i in range(3)]
    kro = [kb.sb(f"mp_kro{i}", [32, 512], BF16, st) for i in range(2)]
    krf = [kb.sb(f"mp_krf{i}", [32, 512], F32, st) for i in range(2)]
    vo = [kb.sb(f"mp_vo{i}", [128, 512], BF16, st) for i in range(3)]
    qi = 0
    for ti, (p0, n, rope, tl0) in enumerate(tiles):
        b = ti % 2
        CQ, CKV, KR, T96, T32 = cq[b], ckv[b], kr[b], t96[b], t32[b]
        kb.dma("sp", CQ[:, :, :n], pinT[3600:4368, :].rearrange("(k p) t -> p k t", p=128)[:, :, p0:p0 + n], writes=[CQ])
        kb.dma("sp", CKV[:, :, :n], pinT[4368:4624, :].rearrange("(k p) t -> p k t", p=128)[:, :, p0:p0 + n], writes=[CKV])
        kb.dma("sp", KR[:, :n], pinT[4624:4656, p0:p0 + n], writes=[KR])
        if rope:
            kb.dma("sp", T32[:, :, :n], cs32.rearrange("c d t -> d c t")[:, :, tl0:tl0 + n], writes=[T32])
        rmsnorm_tile(kb, C, CQ, n, PV(lambda k: qg[:, k:k + 1], [qg]), None, 0, cqn, SQ, RS, tmp, nk=6, dim=768)
        rmsnorm_tile(kb, C, CKV, n, PV(lambda k: kvg[:, k:k + 1], [kvg]), None, 0, ckvn, SQ, RS, tmp, nk=2, dim=256)
        KRO = kro[b]
        if rope:
            KRF = krf[b]
            ps = next_ps(C)
            kb.op("pe", lambda: nc.tensor.matmul(ps[:32, :n], lhsT=r32[:, :], rhs=KR[:, :n], start=True, stop=True), reads=[KR, r32], writes=[ps])
            kb.op("dve", lambda: nc.vector.tensor_tensor(out=KRF[:, :n], in0=ps[:32, :n], in1=T32[:, 1, :n], op=ALU.mult), reads=[ps, T32], writes=[KRF])
            KRG = krg2[b]
            kb.op("pool", lambda: nc.gpsimd.tensor_tensor(out=KRG[:, :n], in0=T32[:, 0, :n], in1=KR[:, :n], op=ALU.mult), reads=[T32, KR], writes=[KRG])
            kb.op("pool", lambda: nc.gpsimd.tensor_tensor(out=KRO[:, :n], in0=KRG[:, :n], in1=KRF[:, :n], op=ALU.add), reads=[KRG, KRF], writes=[KRO])
        else:
            kb.op("pool", lambda: nc.gpsimd.tensor_copy(out=KRO[:, :n], in_=KR[:, :n]), reads=[KR], writes=[KRO])
        for h in range(8):
            kb.dma("pool", kTd[h, 64:96, p0:p0 + n], KRO[:, :n], reads=[KRO])
        for h in range(8):
            ps = next_ps(C)
            for k in range(6):
                kb.op("pe", lambda: nc.tensor.matmul(ps[:64, :n], lhsT=wuq[:, k, h * 96:h * 96 + 64], rhs=cqn[:, k, :n], start=(k == 0), stop=(k == 5)),
                      reads=[cqn, wuq], writes=[ps])
            QO = ko[qi2[0] % 6]
            qi2[0] += 1
            kb.op("act", lambda: nc.scalar.copy(out=QO[:, :n], in_=ps[:64, :n]), reads=[ps], writes=[QO])
            kb.dma("pool", qTd[h, 0:64, p0:p0 + n], QO[:, :n], reads=[QO])
            ps = next_ps(C)
            for k in range(6):
                kb.op("pe", lambda: nc.tensor.matmul(ps[:32, :n], lhsT=wuq[:, k, h * 96 + 64:h * 96 + 96], rhs=cqn[:, k, :n], start=(k == 0), stop=(k == 5)),
                      reads=[cqn, wuq], writes=[ps])
            QR = qr_[qi % 3]
            kb.op("act", lambda: nc.scalar.copy(out=QR[:, :n], in_=ps[:32, :n]), reads=[ps], writes=[QR])
            if rope:
                QF, QG, QO2 = krf2[qi % 2], krg2[qi % 2], kro2[qi % 3]
                ps2 = next_ps(C)
                kb.op("pe", lambda: nc.tensor.matmul(ps2[:32, :n], lhsT=r32[:, :], rhs=QR[:, :n], start=True, stop=True), reads=[QR, r32], writes=[ps2])
                kb.op("dve", lambda: nc.vector.tensor_tensor(out=QF[:, :n], in0=ps2[:32, :n], in1=T32[:, 1, :n], op=ALU.mult), reads=[ps2, T32], writes=[QF])
                kb.op("pool", lambda: nc.gpsimd.tensor_tensor(out=QG[:, :n], in0=T32[:, 0, :n], in1=QR[:, :n], op=ALU.mult), reads=[T32, QR], writes=[QG])
                kb.op("pool", lambda: nc.gpsimd.tensor_tensor(out=QO2[:, :n], in0=QG[:, :n], in1=QF[:, :n], op=ALU.add), reads=[QG, QF], writes=[QO2])
                kb.dma("pool", qTd[h, 64:96, p0:p0 + n], QO2[:, :n], reads=[QO2])
            else:
                kb.dma("pool", qTd[h, 64:96, p0:p0 + n], QR[:, :n], reads=[QR])
            ps = next_ps(C)
            for k in range(2):
                kb.op("pe", lambda: nc.tensor.matmul(ps[:64, :n], lhsT=wukv[:, k, h * 128:h * 128 + 64], rhs=ckvn[:, k, :n], start=(k == 0), stop=(k == 1)),
                      reads=[ckvn, wukv], writes=[ps])
            KO = ko[qi2[0] % 6]
            qi2[0] += 1
            kb.op("act", lambda: nc.scalar.copy(out=KO[:, :n], in_=ps[:64, :n]), reads=[ps], writes=[KO])
            kb.dma("pool", kTd[h, 0:64, p0:p0 + n], KO[:, :n], reads=[KO])
            qi += 1
        for s in range(n // 128):
            ps = next_ps(C)
            for k in range(2):
                kb.op("pe", lambda: nc.tensor.matmul(ps[:, :].rearrange("p (h c) -> p h c", c=64), lhsT=ckvn[:, k, s * 128:(s + 1) * 128],
                                                     rhs=wukv[:, k, :].rearrange("p (h c) -> p h c", c=128)[:, :, 64:128], start=(k == 0), stop=(k == 1)),
                      reads=[ckvn, wukv], writes=[ps])
            VO = vo[s % 3]
            kb.op("dve", lambda: nc.vector.tensor_copy(out=VO[:, :], in_=ps[:, :]), reads=[ps], writes=[VO])
            kb.dma("pool", vd.rearrange("h t c -> t h c")[p0 + s * 128:p0 + (s + 1) * 128, :, :], VO[:, :].rearrange("p (h c) -> p h c", c=64), reads=[VO])


def stage_mla_attn(kb, C, st, jobs, qTd, kTd, vd, attnT, T):
    nc = kb.nc
    scale = 96.0 ** -0.5
    NKT = T // 128
    kT = [kb.sb(f"at_k{i}", [96, T], BF16, st) for i in range(2)]
    V = [kb.sb(f"at_v{i}", [128, NKT, 64], BF16, st) for i in range(2)]
    Q = [kb.sb(f"at_q{i}", [96, 512], BF16, st) for i in range(2)]
    P = [kb.sb(f"at_p{i}", [128, 512], BF16, st) for i in range(4)]
    rec = [kb.sb(f"at_r{i}", [64, 512], F32, st) for i in range(2)]
    O = [kb.sb(f"at_o{i}", [64, 512], BF16, st) for i in range(2)]
    sps = C.psum[0:4]
    aps = C.psum[4:8]
    si = 0
    qi = 0
    for h in range(8):
        K_, V_ = kT[h % 2], V[h % 2]
        kb.dma("sp", K_[:, :], kTd[h], writes=[K_])
        kb.dma("sp", V_[:, :, :], vd[h].rearrange("(j p) c -> p j c", p=128), writes=[V_])
        for (q0, nq, ktiles) in jobs:
            for o in range(0, nq, 512):
                n = min(512, nq - o)
                Qt = Q[qi % 2]
                po, pd = aps[(qi % 2) * 2], aps[(qi % 2) * 2 + 1]
                kb.dma("sp", Qt[:, :n], qTd[h, :, q0 + o:q0 + o + n], writes=[Qt])
                for ji, j in enumerate(ktiles):
                    ps = sps[si % 4]
                    Pt = P[si % 4]
                    si += 1
                    kb.op("pe", lambda: nc.tensor.matmul(ps[:, :n], lhsT=K_[:, j * 128:(j + 1) * 128], rhs=Qt[:, :n], start=True, stop=True),
                          reads=[K_, Qt], writes=[ps])
                    kb.op("act", lambda: nc.scalar.activation(out=Pt[:, :n], in_=ps[:, :n], func=AF.Exp, scale=scale), reads=[ps], writes=[Pt])
                    first, last = ji == 0, ji == len(ktiles) - 1
                    kb.op("pe", lambda: nc.tensor.matmul(po[:64, :n], lhsT=V_[:, j, :], rhs=Pt[:, :n], start=first, stop=last), reads=[V_, Pt], writes=[po])
                    kb.op("pe", lambda: nc.tensor.matmul(pd[:64, :n], lhsT=C.ones_bf[:, 0:64], rhs=Pt[:, :n], start=first, stop=last), reads=[C.ones_bf, Pt], writes=[pd])
                R_, O_ = rec[qi % 2], O[qi % 2]
                kb.op("dve", lambda: nc.vector.reciprocal(out=R_[:, :n], in_=pd[:64, :n]), reads=[pd], writes=[R_])
                kb.op("dve", lambda: nc.vector.tensor_tensor(out=O_[:, :n], in0=po[:64, :n], in1=R_[:, :n], op=ALU.mult), reads=[po, R_], writes=[O_])
                kb.dma("pool", attnT[h * 64:(h + 1) * 64, q0 + o:q0 + o + n], O_[:, :n], reads=[O_])
                qi += 1


def hy_sin(kb, nc, out, ps, n, fr, tmpa, tmpb):
    kb.op("act", lambda: nc.scalar.activation(out=tmpa[:64, :n], in_=ps[:64, :n], func=AF.Sin, scale=fr[:, 0:1], bias=fr[:, 1:2]), reads=[ps, fr], writes=[tmpa])
    kb.op("act", lambda: nc.scalar.activation(out=tmpb[:64, :n], in_=ps[:64, :n], func=AF.Sin, scale=fr[:, 2:3], bias=fr[:, 3:4]), reads=[ps, fr], writes=[tmpb])
    kb.op("dve", lambda: nc.vector.tensor_tensor(out=tmpb[:64, :n], in0=tmpb[:64, :n], in1=tmpb[:64, :n], op=ALU.mult), reads=[tmpb], writes=[tmpb])
    kb.op("dve", lambda: nc.vector.tensor_scalar(out=tmpb[:64, :n], in0=tmpb[:64, :n], scalar1=-2.0, scalar2=1.0, op0=ALU.mult, op1=ALU.add), reads=[tmpb], writes=[tmpb])
    kb.op("dve", lambda: nc.vector.scalar_tensor_tensor(out=out[:64, :n], in0=tmpa[:64, :n], scalar=2.0, in1=tmpb[:64, :n], op0=ALU.mult, op1=ALU.mult), reads=[tmpa, tmpb], writes=[out])


def stage_hy_filter(kb, C, st, L, zT, tn, w1d, b1d, w2d, b2d, w3d, frd, decd, hTd):
    nc = kb.nc
    w1 = kb.sb("hf_w1", [33, 64], F32, st)
    w2 = kb.sb("hf_w2", [64, 64], F32, st)
    w3 = kb.sb("hf_w3", [64, 1024], F32, st)
    v = kb.sb("hf_v", [64, 3], F32, st)
    fr1 = kb.sb("hf_fr1", [64, 4], F32, st)
    fr2 = kb.sb("hf_fr2", [64, 4], F32, st)
    dec = kb.sb("hf_dec", [128, 8], F32, st)
    kb.dma("sp", w1[:], w1d, writes=[w1])
    kb.dma("sp", w2[:], w2d, writes=[w2])
    kb.dma("sp", w3[:], w3d, writes=[w3])
    kb.dma("sp", v[:, 0:1], b1d.rearrange("(p o) -> p o", o=1), writes=[v])
    kb.dma("sp", v[:, 1:2], b2d.rearrange("(p o) -> p o", o=1), writes=[v])
    kb.dma("sp", v[:, 2:3], frd.rearrange("(p o) -> p o", o=1), writes=[v])
    kb.dma("sp", dec[:], decd, writes=[dec])
    for fr, bi in ((fr1, 0), (fr2, 1)):
        kb.op("dve", lambda: nc.vector.tensor_scalar(out=fr[:, 0:1], in0=v[:, 2:3], scalar1=0.5, scalar2=None, op0=ALU.mult), reads=[v], writes=[fr])
        kb.op("dve", lambda: nc.vector.scalar_tensor_tensor(out=fr[:, 1:2], in0=v[:, 2:3], scalar=0.5, in1=v[:, bi:bi + 1], op0=ALU.mult, op1=ALU.mult), reads=[v], writes=[fr])
        kb.op("dve", lambda: nc.vector.tensor_scalar(out=fr[:, 2:4], in0=fr[:, 0:2], scalar1=0.5, scalar2=None, op0=ALU.mult), reads=[fr], writes=[fr])
    kb.op("act", lambda: nc.scalar.activation(out=dec[:], in_=dec[:], func=AF.Abs), reads=[dec], writes=[dec])
    kb.op("dve", lambda: nc.vector.tensor_scalar(out=dec[:], in0=dec[:], scalar1=-1.0, scalar2=None, op0=ALU.mult), reads=[dec], writes=[dec])
    z = [kb.sb(f"hf_z{i}", [33, 512], F32, st) for i in range(2)]
    tb = [kb.sb(f"hf_tb{i}", [128, 512], F32, st) for i in range(2)]
    ta = kb.sb("hf_ta", [64, 512], F32, st)
    tc_ = kb.sb("hf_tc", [64, 512], F32, st)
    h1 = kb.sb("hf_h1", [64, 512], F32, st)
    h2 = kb.sb("hf_h2", [64, 512], F32, st)
    win = [kb.sb(f"hf_win{i}", [128, 512], F32, st) for i in range(2)]
    ho = [kb.sb(f"hf_ho{i}", [128, 512], BF16, st) for i in range(3)]
    oi = 0
    for ti, o in enumerate(range(0, L, 512)):
        n = min(512, L - o)
        Z, TB = z[ti % 2], tb[ti % 2]
        kb.dma("sp", Z[:, :n], zT[:, o:o + n], writes=[Z])
        kb.dma("sp", TB[:, :n], tn[:, o:o + n], writes=[TB])
        ps = next_ps(C)
        kb.op("pe", lambda: nc.tensor.matmul(ps[:64, :n], lhsT=w1[:, :], rhs=Z[:, :n], start=True, stop=True), reads=[w1, Z], writes=[ps])
        hy_sin(kb, nc, h1, ps, n, fr1, ta, tc_)
        ps = next_ps(C)
        kb.op("pe", lambda: nc.tensor.matmul(ps[:64, :n], lhsT=w2[:, :], rhs=h1[:, :n], start=True, stop=True), reads=[w2, h1], writes=[ps])
        hy_sin(kb, nc, h2, ps, n, fr2, ta, tc_)
        for cch in range(8):
            ps = next_ps(C)
            kb.op("pe", lambda: nc.tensor.matmul(ps[:, :n], lhsT=w3[:, cch * 128:(cch + 1) * 128], rhs=h2[:, :n], start=True, stop=True), reads=[w3, h2], writes=[ps])
            W = win[cch % 2]
            kb.op("act", lambda: nc.scalar.activation(out=W[:, :n], in_=TB[:, :n], func=AF.Exp, scale=dec[:, cch:cch + 1]), reads=[TB, dec], writes=[W])
            HO = ho[oi % 3]
            oi += 1
            kb.op("dve", lambda: nc.vector.scalar_tensor_tensor(out=HO[:, :n], in0=W[:, :n], scalar=HY_SHIFT, in1=ps[:, :n], op0=ALU.add, op1=ALU.mult), reads=[W, ps], writes=[HO])
            if cch >= 4 and o == 0:
                kb.op("dve", lambda: nc.vector.memset(HO[:, 0:1], 0.0), reads=[HO], writes=[HO], waw=True)
            kb.dma("pool", hTd[cch * 128:(cch + 1) * 128, o:o + n], HO[:, :n], reads=[HO])


HY_SHIFT = 0.05


def stage_hy_conv3(kb, C, st, segs, pinT, cwd, cbd, uvT, x0T):
    nc = kb.nc
    cw = kb.sb("hc_w", [128, 3, 12], F32, st)
    cb = kb.sb("hc_b", [128, 12], F32, st)
    kb.dma("sp", cw[:], cwd, writes=[cw])
    kb.dma("sp", cb[:], cbd, writes=[cb])
    P = [kb.sb(f"hc_p{i}", [128, 12, 514], BF16, st) for i in range(2)]
    U = [kb.sb(f"hc_u{i}", [128, 512], F32, st) for i in range(6)]
    O = [kb.sb(f"hc_o{i}", [128, 512], BF16, st) for i in range(4)]
    ti = 0
    ui = 0
    oi = 0
    for (p0, Ls, d0) in segs:
        for o in range(0, Ls, 512):
            n = min(512, Ls - o)
            Pt = P[ti % 2]
            ti += 1
            lo = 1 if o == 0 else 0
            hi = 1 if o + n == Ls else 0
            if lo:
                kb.op("pool", lambda: nc.gpsimd.memset(Pt[:, :, 0:1], 0.0), writes=[Pt])
            if hi:
                kb.op("pool", lambda: nc.gpsimd.memset(Pt[:, :, n + 1:n + 2], 0.0), writes=[Pt])
            kb.dma("sp", Pt[:, :, lo:n + 2 - hi], pinT[0:1536, :].rearrange("(k p) t -> p k t", p=128)[:, :, p0 + o - 1 + lo:p0 + o + n + 1 - hi], writes=[Pt])
            us = []
            for k in range(12):
                Ut = U[ui % 6]
                ui += 1
                kb.op("act", lambda: nc.scalar.activation(out=Ut[:, :n], in_=Pt[:, k, 1:n + 1], func=AF.Identity, scale=cw[:, 1, k:k + 1], bias=cb[:, k:k + 1]), reads=[Pt, cw, cb], writes=[Ut])
                kb.op("dve", lambda: nc.vector.scalar_tensor_tensor(out=Ut[:, :n], in0=Pt[:, k, 0:n], scalar=cw[:, 0, k:k + 1], in1=Ut[:, :n], op0=ALU.mult, op1=ALU.add), reads=[Pt, cw, Ut], writes=[Ut])
                eng = "dve" if k < 4 else "pool"
                e_ = nc.vector if k < 4 else nc.gpsimd
                if k < 4:
                    Ot = O[oi % 4]
                    oi += 1
                    kb.op("dve", lambda: nc.vector.scalar_tensor_tensor(out=Ot[:, :n], in0=Pt[:, k, 2:n + 2], scalar=cw[:, 2, k:k + 1], in1=Ut[:, :n], op0=ALU.mult, op1=ALU.add), reads=[Pt, cw, Ut], writes=[Ot])
                    kb.dma("pool", x0T[k * 128:(k + 1) * 128, d0 + o:d0 + o + n], Ot[:, :n], reads=[Ot])
                else:
                    kb.op("dve", lambda: nc.vector.scalar_tensor_tensor(out=Ut[:, :n], in0=Pt[:, k, 2:n + 2], scalar=cw[:, 2, k:k + 1], in1=Ut[:, :n], op0=ALU.mult, op1=ALU.add), reads=[Pt, cw, Ut], writes=[Ut])
                    us.append(Ut)
                if k >= 8:
                    Ot = O[oi % 4]
                    oi += 1
                    X1 = us[k - 8]
                    kb.op("pool", lambda: nc.gpsimd.tensor_tensor(out=Ot[:, :n], in0=X1[:, :n], in1=Ut[:, :n], op=ALU.mult), reads=[X1, Ut], writes=[Ot])
                    kb.dma("pool", uvT[(k - 8) * 128:(k - 7) * 128, d0 + o:d0 + o + n], Ot[:, :n], reads=[Ot])


def load_fft_consts(kb, C, fad, fbd, twd):
    C.FA = kb.sb("FA", [128, 3, 256], BF16)
    C.FB = kb.sb("FB", [128, 4, 128], BF16)
    C.TW = kb.sb("TW", [128, 3, 128], F32)
    kb.dma("pool", C.FA[:], fad.rearrange("j p n -> p j n"), writes=[C.FA])
    kb.dma("pool", C.FB[:], fbd.rearrange("j p n -> p j n"), writes=[C.FB])
    kb.dma("sp", C.TW[:], twd.rearrange("j p n -> p j n"), writes=[C.TW])


def _twiddle(kb, C, nc, ps, Yr, Yi, c, ti_idx, tmps):
    psv = ps[:, :].rearrange("p (c r k) -> p c r k", c=2, r=2)
    Tr = C.TW[:, 0, :].unsqueeze(1).to_broadcast([128, 2, 128])
    Ti = C.TW[:, ti_idx, :].unsqueeze(1).to_broadcast([128, 2, 128])
    t1, t2, t3, t4 = tmps
    kb.op("dve", lambda: nc.vector.tensor_tensor(out=t1[:, :, :], in0=psv[:, :, 0, :], in1=Tr, op=ALU.mult), reads=[ps, C.TW], writes=[t1])
    kb.op("dve", lambda: nc.vector.tensor_tensor(out=t2[:, :, :], in0=psv[:, :, 1, :], in1=Ti, op=ALU.mult), reads=[ps, C.TW], writes=[t2])
    kb.op("pool", lambda: nc.gpsimd.tensor_tensor(out=Yr[:, c:c + 2, :], in0=t1[:, :, :], in1=t2[:, :, :], op=ALU.subtract), reads=[t1, t2], writes=[Yr])
    kb.op("dve", lambda: nc.vector.tensor_tensor(out=t3[:, :, :], in0=psv[:, :, 0, :], in1=Ti, op=ALU.mult), reads=[ps, C.TW], writes=[t3])
    kb.op("dve", lambda: nc.vector.tensor_tensor(out=t4[:, :, :], in0=psv[:, :, 1, :], in1=Tr, op=ALU.mult), reads=[ps, C.TW], writes=[t4])
    kb.op("pool", lambda: nc.gpsimd.tensor_tensor(out=Yi[:, c:c + 2, :], in0=t3[:, :, :], in1=t4[:, :, :], op=ALU.add), reads=[t3, t4], writes=[Yi])


def stage_hy_fft(kb, C, st, uv, hf, hb, yout, nb):
    nc = kb.nc
    GC = 32
    xin = [kb.sb(f"ff_x{i}", [64, GC, 128], BF16, st) for i in range(3)]
    Yr = [kb.sb(f"ff_yr{i}", [128, GC, 128], BF16, st) for i in range(3)]
    Yi = [kb.sb(f"ff_yi{i}", [128, GC, 128], BF16, st) for i in range(3)]
    Kr = kb.sb("ff_kr", [128, GC, 128], F32, st)
    Ki = kb.sb("ff_ki", [128, GC, 128], F32, st)
    Zr = kb.sb("ff_zr", [128, GC, 128], BF16, st)
    Zi = kb.sb("ff_zi", [128, GC, 128], BF16, st)
    tmpsA = [[kb.sb(f"ff_t{j}_{i}", [128, 2, 128], F32, st) for i in range(4)] for j in range(2)]
    tq = [kb.sb(f"ff_q{i}", [128, 512], F32, st) for i in range(4)]
    yo = [kb.sb(f"ff_yo{i}", [64, GC, 128], BF16, st) for i in range(2)]
    Cm, Sm, Sn, Cn = (C.FB[:, j, :] for j in range(4))
    pi_ = 0
    for g in range(512 // GC):
        c0 = g * GC
        for s, src in enumerate((uv, hf, hb)):
            kb.dma("sp", xin[s][:nb, :, :], src[c0:c0 + GC, :].rearrange("c (b p) -> b c p", p=128), writes=[xin[s]])
        for s in range(3):
            for c in range(0, GC, 2):
                ps = next_ps(C)
                for cc in range(2):
                    kb.op("pe", lambda: nc.tensor.matmul(ps[:, cc * 256:(cc + 1) * 256], lhsT=xin[s][:nb, c + cc, :], rhs=C.FA[:nb, 0, :], start=True, stop=True),
                          reads=[xin[s], C.FA], writes=[ps])
                _twiddle(kb, C, nc, ps, Yr[s], Yi[s], c, 1, tmpsA[pi_ % 2])
                pi_ += 1

        def q(Y, c):
            return Y[:, c:c + 4, :].rearrange("p c k -> p (c k)")
        for c in range(0, GC, 4):
            pr = next_ps(C)
            terms = [(Cm, Yr[1]), (Sm, Yi[1]), (Cm, Yr[2]), (Sm, Yi[2])]
            for i, (F_, Y_) in enumerate(terms):
                kb.op("pe", lambda: nc.tensor.matmul(pr[:, :], lhsT=F_, rhs=q(Y_, c), start=(i == 0), stop=(i == 3)), reads=[C.FB, Y_], writes=[pr])
            kb.op("act", lambda: nc.scalar.copy(out=q(Kr, c), in_=pr[:, :]), reads=[pr], writes=[Kr])
            pim = next_ps(C)
            terms = [(Cm, Yi[1]), (Sn, Yr[1]), (Cn, Yi[2]), (Sm, Yr[2])]
            for i, (F_, Y_) in enumerate(terms):
                kb.op("pe", lambda: nc.tensor.matmul(pim[:, :], lhsT=F_, rhs=q(Y_, c), start=(i == 0), stop=(i == 3)), reads=[C.FB, Y_], writes=[pim])
            kb.op("act", lambda: nc.scalar.copy(out=q(Ki, c), in_=pim[:, :]), reads=[pim], writes=[Ki])
        for c in range(0, GC, 4):
            pr = next_ps(C)
            for i, (F_, Y_) in enumerate([(Cm, Yr[0]), (Sm, Yi[0])]):
                kb.op("pe", lambda: nc.tensor.matmul(pr[:, :], lhsT=F_, rhs=q(Y_, c), start=(i == 0), stop=(i == 1)), reads=[C.FB, Y_], writes=[pr])
            pim = next_ps(C)
            for i, (F_, Y_) in enumerate([(Cm, Yi[0]), (Sn, Yr[0])]):
                kb.op("pe", lambda: nc.tensor.matmul(pim[:, :], lhsT=F_, rhs=q(Y_, c), start=(i == 0), stop=(i == 1)), reads=[C.FB, Y_], writes=[pim])
            kb.op("dve", lambda: nc.vector.tensor_tensor(out=tq[0][:, :], in0=pr[:, :], in1=q(Kr, c), op=ALU.mult), reads=[pr, Kr], writes=[tq[0]])
            kb.op("dve", lambda: nc.vector.tensor_tensor(out=tq[1][:, :], in0=pim[:, :], in1=q(Ki, c), op=ALU.mult), reads=[pim, Ki], writes=[tq[1]])
            kb.op("pool", lambda: nc.gpsimd.tensor_tensor(out=q(Zr, c), in0=tq[0][:, :], in1=tq[1][:, :], op=ALU.subtract), reads=[tq[0], tq[1]], writes=[Zr])
            kb.op("dve", lambda: nc.vector.tensor_tensor(out=tq[2][:, :], in0=pr[:, :], in1=q(Ki, c), op=ALU.mult), reads=[pr, Ki], writes=[tq[2]])
            kb.op("dve", lambda: nc.vector.tensor_tensor(out=tq[3][:, :], in0=pim[:, :], in1=q(Kr, c), op=ALU.mult), reads=[pim, Kr], writes=[tq[3]])
            kb.op("pool", lambda: nc.gpsimd.tensor_tensor(out=q(Zi, c), in0=tq[2][:, :], in1=tq[3][:, :], op=ALU.add), reads=[tq[2], tq[3]], writes=[Zi])
        for c in range(0, GC, 2):
            ps = next_ps(C)
            for cc in range(2):
                kb.op("pe", lambda: nc.tensor.matmul(ps[:, cc * 256:(cc + 1) * 256], lhsT=Zr[:, c + cc, :], rhs=C.FA[:, 1, :], start=True, stop=False), reads=[Zr, C.FA], writes=[ps])
                kb.op("pe", lambda: nc.tensor.matmul(ps[:, cc * 256:(cc + 1) * 256], lhsT=Zi[:, c + cc, :], rhs=C.FA[:, 2, :], start=False, stop=True), reads=[Zi, C.FA], writes=[ps])
            _twiddle(kb, C, nc, ps, Yr[0], Yi[0], c, 2, tmpsA[pi_ % 2])
            pi_ += 1
        YO = yo[g % 2]
        for c in range(0, GC, 4):
            ps = next_ps(C)
            kb.op("pe", lambda: nc.tensor.matmul(ps[:nb, :], lhsT=C.FB[:, 0, 0:nb], rhs=q(Yr[0], c), start=True, stop=False), reads=[C.FB, Yr[0]], writes=[ps])
            kb.op("pe", lambda: nc.tensor.matmul(ps[:nb, :], lhsT=C.FB[:, 2, 0:nb], rhs=q(Yi[0], c), start=False, stop=True), reads=[C.FB, Yi[0]], writes=[ps])
            kb.op("act", lambda: nc.scalar.activation(out=YO[:nb, c:c + 4, :].rearrange("p c k -> p (c k)"), in_=ps[:nb, :], func=AF.Copy, scale=1.0 / 16384.0), reads=[ps], writes=[YO])
        kb.dma("pool", yout[c0:c0 + GC, :].rearrange("c (b p) -> b c p", p=128), YO[:nb, :, :], reads=[YO])


def stage_hy_gate(kb, C, st, tiles, yconv, uvT, x0T, hbd, yT0):
    nc = kb.nc
    hb = kb.sb("hg_b", [128, 4], F32, st)
    kb.dma("sp", hb[:], hbd, writes=[hb])
    A = [kb.sb(f"hg_a{i}", [128, 3, 4, 512], BF16, st) for i in range(2)]
    T_ = [kb.sb(f"hg_t{i}", [128, 512], F32, st) for i in range(2)]
    O = [kb.sb(f"hg_o{i}", [128, 4, 512], BF16, st) for i in range(2)]
    for ti, (d0, n) in enumerate(tiles):
        At, Ot = A[ti % 2], O[ti % 2]
        for j, src in enumerate((yconv, uvT, x0T)):
            kb.dma("sp", At[:, j, :, :n], src.rearrange("(k p) t -> p k t", p=128)[:, :, d0:d0 + n], writes=[At])
        for k in range(4):
            Tt = T_[k % 2]
            kb.op("dve", lambda: nc.vector.scalar_tensor_tensor(out=Tt[:, :n], in0=At[:, 1, k, :n], scalar=hb[:, k:k + 1], in1=At[:, 0, k, :n], op0=ALU.mult, op1=ALU.add),
                  reads=[At, hb], writes=[Tt])
            kb.op("pool", lambda: nc.gpsimd.tensor_tensor(out=Ot[:, k, :n], in0=Tt[:, :n], in1=At[:, 2, k, :n], op=ALU.mult), reads=[Tt, At], writes=[Ot])
        kb.dma("pool", yT0.rearrange("(k p) t -> p k t", p=128)[:, :, d0:d0 + n], Ot[:, :, :n], reads=[Ot])


def stage_gdn_prep(kb, C, st, segs, pinT, cwd, alogd, dtbd, qkvT, gbT):
    nc = kb.nc
    cw = kb.sb("gp_w", [128, 3, 12], F32, st)
    kb.dma("sp", cw[:], cwd, writes=[cw])
    av = kb.sb("gp_av", [8, 2], F32, st)
    kb.dma("sp", av[:, 0:1], alogd.rearrange("(p o) -> p o", o=1), writes=[av])
    kb.dma("sp", av[:, 1:2], dtbd.rearrange("(p o) -> p o", o=1), writes=[av])
    kb.op("act", lambda: nc.scalar.activation(out=av[:, 0:1], in_=av[:, 0:1], func=AF.Exp), reads=[av], writes=[av])
    kb.op("dve", lambda: nc.vector.tensor_scalar(out=av[:, 0:1], in0=av[:, 0:1], scalar1=-1.0, scalar2=None, op0=ALU.mult), reads=[av], writes=[av])
    P = [kb.sb(f"gp_p{i}", [128, 12, 514], BF16, st) for i in range(2)]
    U = [kb.sb(f"gp_u{i}", [128, 512], F32, st) for i in range(4)]
    SQ = [kb.sb(f"gp_sq{i}", [128, 512], BF16, st) for i in range(2)]
    RS = [kb.sb(f"gp_rs{i}", [128, 512], F32, st) for i in range(2)]
    O = [kb.sb(f"gp_o{i}", [128, 512], F32, st) for i in range(4)]
    A8 = [kb.sb(f"gp_a8{i}", [8, 512], BF16, st) for i in range(2)]
    B8 = [kb.sb(f"gp_b8{i}", [8, 512], BF16, st) for i in range(2)]
    G8 = [kb.sb(f"gp_g8{i}", [8, 512], F32, st) for i in range(2)]
    E8 = [kb.sb(f"gp_e8{i}", [8, 512], F32, st) for i in range(2)]
    GT = [kb.sb(f"gp_gt{i}", [128, 16], F32, st) for i in range(3)]
    ti = ui = oi = gi = 0
    for (p0, Ls) in segs:
        for o in range(0, Ls, 512):
            n = min(512, Ls - o)
            Pt = P[ti % 2]
            lo = 1 if o == 0 else 0
            hi = 1 if o + n == Ls else 0
            if lo:
                kb.op("pool", lambda: nc.gpsimd.memset(Pt[:, :, 0:1], 0.0), writes=[Pt])
            if hi:
                kb.op("pool", lambda: nc.gpsimd.memset(Pt[:, :, n + 1:n + 2], 0.0), writes=[Pt])
            kb.dma("sp", Pt[:, :, lo:n + 2 - hi], pinT[1536:3072, :].rearrange("(k p) t -> p k t", p=128)[:, :, p0 + o - 1 + lo:p0 + o + n + 1 - hi], writes=[Pt])
            for k in range(12):
                Ut = U[ui % 4]
                ui += 1
                Ot = O[oi % 4]
                oi += 1
                kb.op("act", lambda: nc.scalar.activation(out=Ut[:, :n], in_=Pt[:, k, 1:n + 1], func=AF.Identity, scale=cw[:, 1, k:k + 1]), reads=[Pt, cw], writes=[Ut])
                kb.op("dve", lambda: nc.vector.scalar_tensor_tensor(out=Ut[:, :n], in0=Pt[:, k, 0:n], scalar=cw[:, 0, k:k + 1], in1=Ut[:, :n], op0=ALU.mult, op1=ALU.add), reads=[Pt, cw, Ut], writes=[Ut])
                kb.op("dve", lambda: nc.vector.scalar_tensor_tensor(out=Ut[:, :n], in0=Pt[:, k, 2:n + 2], scalar=cw[:, 2, k:k + 1], in1=Ut[:, :n], op0=ALU.mult, op1=ALU.add), reads=[Pt, cw, Ut], writes=[Ut])
                if k >= 8:
                    kb.op("act", lambda: nc.scalar.activation(out=Ot[:, :n], in_=Ut[:, :n], func=AF.Silu), reads=[Ut], writes=[Ot])
                else:
                    S_, R_ = SQ[k % 2], RS[k % 2]
                    kb.op("act", lambda: nc.scalar.activation(out=Ut[:, :n], in_=Ut[:, :n], func=AF.Silu), reads=[Ut], writes=[Ut])
                    kb.op("pool", lambda: nc.gpsimd.tensor_tensor(out=S_[:, :n], in0=Ut[:, :n], in1=Ut[:, :n], op=ALU.mult), reads=[Ut], writes=[S_])
                    ps = next_ps(C)
                    kb.op("pe", lambda: nc.tensor.matmul(ps[:, :n], lhsT=C.ones_bf[:], rhs=S_[:, :n], start=True, stop=True), reads=[S_, C.ones_bf], writes=[ps])
                    kb.op("act", lambda: nc.scalar.activation(out=R_[:, :n], in_=ps[:, :n], func=AF.Sqrt, bias=C.eps_t[:, 0:1]), reads=[ps, C.eps_t], writes=[R_])
                    kb.op("dve", lambda: nc.vector.reciprocal(out=R_[:, :n], in_=R_[:, :n]), reads=[R_], writes=[R_])
                    sc_ = (128.0 ** -0.5) if k < 4 else 1.0
                    kb.op("dve", lambda: nc.vector.scalar_tensor_tensor(out=Ot[:, :n], in0=Ut[:, :n], scalar=sc_, in1=R_[:, :n], op0=ALU.mult, op1=ALU.mult), reads=[Ut, R_], writes=[Ot])
                kb.dma("pool", qkvT[k * 128:(k + 1) * 128, p0 + o:p0 + o + n], Ot[:, :n], reads=[Ot])
            a8, b8, g8, e8 = A8[ti % 2], B8[ti % 2], G8[ti % 2], E8[ti % 2]
            kb.dma("sp", a8[:, :n], pinT[3584:3592, p0 + o:p0 + o + n], writes=[a8])
            kb.dma("sp", b8[:, :n], pinT[3592:3600, p0 + o:p0 + o + n], writes=[b8])
            kb.op("act", lambda: nc.scalar.activation(out=g8[:, :n], in_=a8[:, :n], func=AF.Exp, bias=av[:, 1:2]), reads=[a8, av], writes=[g8])
            kb.op("act", lambda: nc.scalar.activation(out=g8[:, :n], in_=g8[:, :n], func=AF.Ln, bias=C.one_t[:8, 0:1]), reads=[g8, C.one_t], writes=[g8])
            kb.op("dve", lambda: nc.vector.tensor_scalar(out=g8[:, :n], in0=g8[:, :n], scalar1=av[:, 0:1], scalar2=None, op0=ALU.mult), reads=[g8, av], writes=[g8])
            kb.op("act", lambda: nc.scalar.activation(out=e8[:, :n], in_=b8[:, :n], func=AF.Sigmoid), reads=[b8], writes=[e8])
            for s in range(n // 128):
                ps = next_ps(C)
                kb.op("pe", lambda: nc.tensor.transpose(ps[:, 0:8], g8[:, s * 128:(s + 1) * 128], C.ident_f[:8, :8]), reads=[g8, C.ident_f], writes=[ps])
                kb.op("pe", lambda: nc.tensor.transpose(ps[:, 8:16], e8[:, s * 128:(s + 1) * 128], C.ident_f[:8, :8]), reads=[e8, C.ident_f], writes=[ps])
                G_ = GT[gi % 3]
                gi += 1
                kb.op("dve", lambda: nc.vector.tensor_copy(out=G_[:, :], in_=ps[:, 0:16]), reads=[ps], writes=[G_])
                kb.dma("pool", gbT[p0 + o + s * 128:p0 + o + (s + 1) * 128, :], G_[:, :], reads=[G_])
            ti += 1


def load_gdn_consts(kb, C, trifd, tribd, ms2d, mi1d):
    C.triF = kb.sb("triF", [64, 64], F32)
    C.triB = kb.sb("triB", [64, 64], F32)
    C.mS2 = kb.sb("mS2", [64, 8, 64], F32)
    C.mI1 = kb.sb("mI1", [64, 8, 64], F32)
    C.ones_f = kb.sb("ones_f", [64, 128], F32)
    C.identI = kb.sb("identI", [64, 8, 64], F32)
    kb.dma("sp", C.triF[:], trifd, writes=[C.triF])
    kb.dma("sp", C.triB[:], tribd, writes=[C.triB])
    kb.dma("sp", C.mS2[:], ms2d, writes=[C.mS2])
    kb.dma("sp", C.mI1[:], mi1d, writes=[C.mI1])
    kb.op("dve", lambda: kb.nc.vector.memset(C.ones_f[:], 1.0), writes=[C.ones_f])
    for u in range(8):
        kb.op("dve", lambda: kb.nc.vector.tensor_copy(out=C.identI[:, u, :], in_=C.ident_f[:64, :64]), reads=[C.ident_f], writes=[C.identI])


def stage_gdn_scan(kb, C, st, fo, bo, qkvT, gbT, ofd, obd):
    nc = kb.nc
    V_ = nc.vector
    X = [[kb.sb(f"gs_x{d}_{i}", [128, 12, 64], F32, st) for i in range(2)] for d in range(2)]
    GB = [kb.sb(f"gs_gb{i}", [64, 2, 8], F32, st) for i in range(2)]
    QT = kb.sb("gs_qt", [128, 8, 64], BF16, st)
    KT = kb.sb("gs_kt", [128, 8, 64], BF16, st)
    Gbc = kb.sb("gs_gbc", [64, 8, 128], F32, st)
    gcs = kb.sb("gs_gc", [64, 8], F32, st)
    sm = kb.sb("gs_sm", [128, 6, 8], F32, st)
    Dm = kb.sb("gs_dm", [64, 8, 64], F32, st)
    D2 = kb.sb("gs_d2", [64, 8, 64], F32, st)
    E1 = kb.sb("gs_e1", [64, 8, 64], F32, st)
    E2 = kb.sb("gs_e2", [64, 8, 64], F32, st)
    EG = kb.sb("gs_eg", [128, 8, 64], F32, st)
    Mm = kb.sb("gs_m", [64, 8, 64], F32, st)
    Nn = kb.sb("gs_n", [64, 8, 64], F32, st)
    AT = kb.sb("gs_at", [64, 8, 64], BF16, st)
    PN = kb.sb("gs_pn", [64, 8, 64], F32, st)
    PM = kb.sb("gs_pm", [64, 8, 64], F32, st)
    XN = [kb.sb(f"gs_xn{i}", [64, 8, 64], F32, st) for i in range(2)]
    XM = [kb.sb(f"gs_xm{i}", [64, 8, 64], F32, st) for i in range(2)]
    TT = kb.sb("gs_tt", [64, 8, 64], BF16, st)
    Kbg = kb.sb("gs_kbg", [64, 8, 128], BF16, st)
    Kd = kb.sb("gs_kd", [64, 8, 128], BF16, st)
    Vb = kb.sb("gs_vb", [64, 8, 128], BF16, st)
    Uu = kb.sb("gs_u", [64, 8, 128], F32, st)
    WT = kb.sb("gs_wt", [128, 8, 64], BF16, st)
    QD = kb.sb("gs_qd", [128, 8, 64], BF16, st)
    Vn = kb.sb("gs_vn", [64, 8, 128], BF16, st)
    Ot = [kb.sb(f"gs_o{i}", [64, 8, 128], F32, st) for i in range(2)]
    S = kb.sb("gs_s", [128, 8, 128], F32, st)
    Sb = kb.sb("gs_sb", [128, 8, 128], BF16, st)
    kb.op("dve", lambda: V_.memset(S[:], 0.0), writes=[S])
    kb.op("pool", lambda: nc.gpsimd.memset(Sb[:], 0.0), writes=[Sb])

    def bc(ap2, shape):
        return ap2.unsqueeze(2).to_broadcast(shape)

    def v3(ps, p=64, w=64):
        return ps[:p, :].rearrange("p (u k) -> p u k", k=w)
    nsteps = len(fo)
    for s in range(nsteps):
        cf, cb = fo[s], bo[s]
        Xd = [X[0][s % 2], X[1][s % 2]]
        G = GB[s % 2]
        for d, c in ((0, cf), (1, cb)):
            kb.dma("sp", Xd[d][:], qkvT.rearrange("(k p) t -> p k t", p=128)[:, :, c * 64:(c + 1) * 64], writes=[Xd[d]])
            kb.dma("sp", G[:, :, d * 4:(d + 1) * 4], gbT[c * 64:(c + 1) * 64, :].rearrange("t (a d h) -> t a d h", a=2, d=2)[:, :, d, :], writes=[G])
        for d in range(2):
            kb.op("act", lambda: nc.scalar.copy(out=QT[:, d * 4:(d + 1) * 4, :], in_=Xd[d][:, 0:4, :]), reads=[Xd[d]], writes=[QT])
            kb.op("pool", lambda: nc.gpsimd.tensor_copy(out=KT[:, d * 4:(d + 1) * 4, :], in_=Xd[d][:, 4:8, :]), reads=[Xd[d]], writes=[KT])
        psK = [next_ps(C), next_ps(C)]
        psV = [next_ps(C), next_ps(C)]
        for u in range(8):
            d, h = divmod(u, 4)
            kb.op("pe", lambda: nc.tensor.transpose(psK[d][:64, h * 128:(h + 1) * 128], Xd[d][:, 4 + h, :], C.ident_f[:]), reads=[Xd[d], C.ident_f], writes=[psK[d]])
            kb.op("pe", lambda: nc.tensor.transpose(psV[d][:64, h * 128:(h + 1) * 128], Xd[d][:, 8 + h, :], C.ident_f[:]), reads=[Xd[d], C.ident_f], writes=[psV[d]])
        kb.op("dve", lambda: V_.tensor_copy(out=Gbc[:], in_=bc(G[:, 0, :], [64, 8, 128])), reads=[G], writes=[Gbc])
        psg = next_ps(C)
        kb.op("pe", lambda: nc.tensor.matmul(psg[:64, 0:4], lhsT=C.triF[:], rhs=G[:, 0, 0:4], start=True, stop=True), reads=[C.triF, G], writes=[psg])
        kb.op("pe", lambda: nc.tensor.matmul(psg[:64, 4:8], lhsT=C.triB[:], rhs=G[:, 0, 4:8], start=True, stop=True), reads=[C.triB, G], writes=[psg])
        kb.op("pe", lambda: nc.tensor.matmul(psg[:, 8:16], lhsT=C.ones_f[:], rhs=G[:, 0, :], start=True, stop=True), reads=[C.ones_f, G], writes=[psg])
        psr = next_ps(C)
        for u in range(8):
            tri = C.triF if u < 4 else C.triB
            kb.op("pe", lambda: nc.tensor.matmul(psr[:, u * 64:(u + 1) * 64], lhsT=Gbc[:, u, :], rhs=tri[:], start=True, stop=True), reads=[Gbc, tri], writes=[psr])
        kb.op("dve", lambda: V_.tensor_copy(out=gcs[:], in_=psg[:64, 0:8]), reads=[psg], writes=[gcs])
        kb.op("dve", lambda: V_.tensor_tensor(out=Dm[:], in0=v3(psr), in1=bc(gcs[:, :], [64, 8, 64]), op=ALU.subtract), reads=[psr, gcs], writes=[Dm])
        kb.op("pool", lambda: nc.gpsimd.tensor_scalar(out=D2[:], in0=Dm[:], scalar1=-1.0, scalar2=0.0, op0=ALU.mult, op1=ALU.min), reads=[Dm], writes=[D2])
        kb.op("dve", lambda: V_.tensor_scalar(out=Dm[:], in0=Dm[:], scalar1=0.0, scalar2=None, op0=ALU.min), reads=[Dm], writes=[Dm])
        kb.op("act", lambda: nc.scalar.activation(out=E1[:], in_=Dm[:], func=AF.Exp), reads=[Dm], writes=[E1])
        kb.op("act", lambda: nc.scalar.activation(out=E2[:], in_=D2[:], func=AF.Exp), reads=[D2], writes=[E2])
        kb.op("act", lambda: nc.scalar.activation(out=EG[:].rearrange("p u k -> p (u k)"), in_=psr[:, :], func=AF.Exp), reads=[psr], writes=[EG])
        kb.op("act", lambda: nc.scalar.activation(out=sm[:64, 0, :], in_=gcs[:, :], func=AF.Exp), reads=[gcs], writes=[sm])
        kb.op("dve", lambda: V_.tensor_tensor(out=sm[:64, 4, :], in0=psg[:64, 8:16], in1=gcs[:, :], op=ALU.subtract), reads=[psg, gcs], writes=[sm])
        kb.op("act", lambda: nc.scalar.activation(out=sm[:64, 1, :], in_=sm[:64, 4, :], func=AF.Exp), reads=[sm], writes=[sm])
        kb.op("act", lambda: nc.scalar.activation(out=sm[:, 2, :], in_=psg[:, 8:16], func=AF.Exp), reads=[psg], writes=[sm])
        kb.op("dve", lambda: V_.tensor_tensor(out=sm[:64, 3, :], in0=sm[:64, 0, :], in1=G[:, 1, :], op=ALU.mult), reads=[sm, G], writes=[sm])
        kb.op("pool", lambda: nc.gpsimd.tensor_tensor(out=E1[:], in0=E1[:], in1=C.mI1[:], op=ALU.mult), reads=[E1, C.mI1], writes=[E1])
        kb.op("pool", lambda: nc.gpsimd.tensor_tensor(out=E2[:], in0=E2[:], in1=C.mS2[:], op=ALU.mult), reads=[E2, C.mS2], writes=[E2])
        for d in range(2):
            pk4 = psK[d][:64, :].rearrange("p (u k) -> p u k", k=128)
            pv4 = psV[d][:64, :].rearrange("p (u k) -> p u k", k=128)
            us = slice(d * 4, (d + 1) * 4)
            kb.op("dve", lambda: V_.tensor_tensor(out=Kbg[:, us, :], in0=pk4, in1=bc(sm[:64, 3, us], [64, 4, 128]), op=ALU.mult), reads=[psK[d], sm], writes=[Kbg])
            kb.op("dve", lambda: V_.tensor_tensor(out=Kd[:, us, :], in0=pk4, in1=bc(sm[:64, 1, us], [64, 4, 128]), op=ALU.mult), reads=[psK[d], sm], writes=[Kd])
            kb.op("dve", lambda: V_.tensor_tensor(out=Vb[:, us, :], in0=pv4, in1=bc(G[:, 1, us], [64, 4, 128]), op=ALU.mult), reads=[psV[d], G], writes=[Vb])
        kb.op("pool", lambda: nc.gpsimd.tensor_tensor(out=QD[:], in0=QT[:], in1=EG[:], op=ALU.mult), reads=[QT, EG], writes=[QD])
        pkk = next_ps(C)
        pqk = next_ps(C)
        for u in range(8):
            kb.op("pe", lambda: nc.tensor.matmul(pkk[:64, u * 64:(u + 1) * 64], lhsT=KT[:, u, :], rhs=KT[:, u, :], start=True, stop=True), reads=[KT], writes=[pkk])
            kb.op("pe", lambda: nc.tensor.matmul(pqk[:64, u * 64:(u + 1) * 64], lhsT=KT[:, u, :], rhs=QT[:, u, :], start=True, stop=True), reads=[KT, QT], writes=[pqk])
        kb.op("dve", lambda: V_.tensor_tensor(out=Mm[:], in0=v3(pkk), in1=E2[:], op=ALU.mult), reads=[pkk, E2], writes=[Mm])
        kb.op("dve", lambda: V_.tensor_tensor(out=Mm[:], in0=Mm[:], in1=bc(G[:, 1, :], [64, 8, 64]), op=ALU.mult), reads=[Mm, G], writes=[Mm])
        kb.op("dve", lambda: V_.tensor_tensor(out=AT[:], in0=v3(pqk), in1=E1[:], op=ALU.mult), reads=[pqk, E1], writes=[AT])
        pn = next_ps(C)
        for u in range(8):
            kb.op("pe", lambda: nc.tensor.transpose(pn[:64, u * 64:(u + 1) * 64], Mm[:, u, :], C.ident_f[:64, :64]), reads=[Mm, C.ident_f], writes=[pn])
        kb.op("act", lambda: nc.scalar.copy(out=Nn[:], in_=v3(pn)), reads=[pn], writes=[Nn])
        SK = ''
        if 'a' not in SK:
            kb.op("dve", lambda: V_.scalar_tensor_tensor(out=PN[:], in0=v3(pn), scalar=-1.0, in1=C.identI[:], op0=ALU.mult, op1=ALU.add), reads=[C.identI, pn], writes=[PN])
        if 'b' not in SK:
            kb.op("pool", lambda: nc.gpsimd.tensor_tensor(out=PM[:], in0=C.identI[:], in1=Mm[:], op=ALU.subtract), reads=[C.identI, Mm], writes=[PM])
        pa, pb = next_ps(C), next_ps(C)
        for u in range(8):
            if 'c' not in SK:
                kb.op("pe", lambda: nc.tensor.matmul(pa[:64, u * 64:(u + 1) * 64], lhsT=Mm[:, u, :], rhs=Nn[:, u, :], start=True, stop=True), reads=[Mm, Nn], writes=[pa])
            if 'd' not in SK:
                kb.op("pe", lambda: nc.tensor.matmul(pb[:64, u * 64:(u + 1) * 64], lhsT=Nn[:, u, :], rhs=Mm[:, u, :], start=True, stop=True), reads=[Mm, Nn], writes=[pb])
        xn, xm = XN[0], XM[0]
        if 'e' not in SK:
            kb.op("act", lambda: nc.scalar.copy(out=xn[:], in_=v3(pa)), reads=[pa], writes=[xn])
        if 'f' not in SK:
            kb.op("dve", lambda: V_.tensor_copy(out=xm[:], in_=v3(pb)), reads=[pb], writes=[xm])
        for lv in range(5):
            last = lv == 4
            pa = next_ps(C)
            for u in range(8):
                kb.op("pe", lambda: nc.tensor.matmul(pa[:64, u * 64:(u + 1) * 64], lhsT=PM[:, u, :], rhs=xn[:, u, :], start=True, stop=True), reads=[PM, xn], writes=[pa])
            if not last:
                pb = next_ps(C)
                for u in range(8):
                    kb.op("pe", lambda: nc.tensor.matmul(pb[:64, u * 64:(u + 1) * 64], lhsT=xn[:, u, :], rhs=PM[:, u, :], start=True, stop=True), reads=[PM, xn], writes=[pb])
                pc, pd = next_ps(C), next_ps(C)
                for u in range(8):
                    kb.op("pe", lambda: nc.tensor.matmul(pc[:64, u * 64:(u + 1) * 64], lhsT=xm[:, u, :], rhs=xn[:, u, :], start=True, stop=True), reads=[xm, xn], writes=[pc])
                    kb.op("pe", lambda: nc.tensor.matmul(pd[:64, u * 64:(u + 1) * 64], lhsT=xn[:, u, :], rhs=xm[:, u, :], start=True, stop=True), reads=[xm, xn], writes=[pd])
                xn2, xm2 = XN[(lv + 1) % 2], XM[(lv + 1) % 2]
                kb.op("act", lambda: nc.scalar.copy(out=xn2[:], in_=v3(pc)), reads=[pc], writes=[xn2])
                kb.op("act", lambda: nc.scalar.copy(out=xm2[:], in_=v3(pd)), reads=[pd], writes=[xm2])
                kb.op("dve", lambda: V_.tensor_tensor(out=PM[:], in0=v3(pb), in1=PM[:], op=ALU.add), reads=[PM, pb, pa], writes=[PM])
                kb.op("dve", lambda: V_.tensor_tensor(out=PN[:], in0=v3(pa), in1=PN[:], op=ALU.add), reads=[PN, pa], writes=[PN])
                xn, xm = xn2, xm2
            else:
                kb.op("dve", lambda: V_.tensor_tensor(out=TT[:], in0=v3(pa), in1=PN[:], op=ALU.add), reads=[PN, pa], writes=[TT])
        pu = [next_ps(C), next_ps(C)]
        pw = next_ps(C)
        for u in range(8):
            d, h = divmod(u, 4)
            kb.op("pe", lambda: nc.tensor.matmul(pu[d][:64, h * 128:(h + 1) * 128], lhsT=TT[:, u, :], rhs=Vb[:, u, :], start=True, stop=True), reads=[TT, Vb], writes=[pu[d]])
            kb.op("pe", lambda: nc.tensor.matmul(pw[:, u * 64:(u + 1) * 64], lhsT=Kbg[:, u, :], rhs=TT[:, u, :], start=True, stop=True), reads=[TT, Kbg], writes=[pw])
        for d in range(2):
            kb.op("act", lambda: nc.scalar.copy(out=Uu[:, d * 4:(d + 1) * 4, :].rearrange("p u k -> p (u k)"), in_=pu[d][:64, :]), reads=[pu[d]], writes=[Uu])
        kb.op("act", lambda: nc.scalar.copy(out=WT[:].rearrange("p u k -> p (u k)"), in_=pw[:, :]), reads=[pw], writes=[WT])
        pws = [next_ps(C), next_ps(C)]
        for u in range(8):
            d, h = divmod(u, 4)
            kb.op("pe", lambda: nc.tensor.matmul(pws[d][:64, h * 128:(h + 1) * 128], lhsT=WT[:, u, :], rhs=Sb[:, u, :], start=True, stop=True), reads=[WT, Sb], writes=[pws[d]])
        for d in range(2):
            kb.op("dve", lambda: V_.scalar_tensor_tensor(out=Vn[:, d * 4:(d + 1) * 4, :].rearrange("p u k -> p (u k)"), in0=pws[d][:64, :], scalar=-1.0,
                                                 in1=Uu[:, d * 4:(d + 1) * 4, :].rearrange("p u k -> p (u k)"), op0=ALU.mult, op1=ALU.add), reads=[Uu, pws[d]], writes=[Vn])
        po = [next_ps(C), next_ps(C)]
        for u in range(8):
            d, h = divmod(u, 4)
            kb.op("pe", lambda: nc.tensor.matmul(po[d][:64, h * 128:(h + 1) * 128], lhsT=QD[:, u, :], rhs=Sb[:, u, :], start=True, stop=False), reads=[QD, Sb], writes=[po[d]])
            kb.op("pe", lambda: nc.tensor.matmul(po[d][:64, h * 128:(h + 1) * 128], lhsT=AT[:, u, :], rhs=Vn[:, u, :], start=False, stop=True), reads=[AT, Vn], writes=[po[d]])
        O_ = Ot[s % 2]
        for d, (c, dst) in enumerate(((cf, ofd), (cb, obd))):
            kb.op("act", lambda: nc.scalar.copy(out=O_[:, d * 4:(d + 1) * 4, :].rearrange("p u k -> p (u k)"), in_=po[d][:64, :]), reads=[po[d]], writes=[O_])
            kb.dma("pool", dst[c * 64:(c + 1) * 64, :], O_[:, d * 4:(d + 1) * 4, :].rearrange("p u k -> p (u k)"), reads=[O_])
        pss = [next_ps(C), next_ps(C)]
        for u in range(8):
            d, h = divmod(u, 4)
            kb.op("pe", lambda: nc.tensor.matmul(pss[d][:, h * 128:(h + 1) * 128], lhsT=Kd[:, u, :], rhs=Vn[:, u, :], start=True, stop=True), reads=[Kd, Vn], writes=[pss[d]])
        kb.op("dve", lambda: V_.tensor_tensor(out=S[:], in0=S[:], in1=bc(sm[:, 2, :], [128, 8, 128]), op=ALU.mult), reads=[S, sm], writes=[S])
        for d in range(2):
            Sv = S[:, d * 4:(d + 1) * 4, :].rearrange("p u k -> p (u k)")
            kb.op("dve", lambda: V_.tensor_tensor(out=Sv, in0=pss[d][:, :], in1=Sv, op=ALU.add), reads=[S, pss[d]], writes=[S])
        kb.op("act", lambda: nc.scalar.copy(out=Sb[:], in_=S[:]), reads=[S], writes=[Sb])


def stage_gdn_out(kb, C, st, tiles, ofd, obd, pinT, gnd, yT1):
    nc = kb.nc
    gbc = kb.sb("go_g", [128, 128], F32, st)
    kb.dma("sp", gbc[:], gnd, writes=[gbc])
    Z = [kb.sb(f"go_z{i}", [128, 4, 512], BF16, st) for i in range(2)]
    ZS = [kb.sb(f"go_zs{i}", [128, 4, 512], F32, st) for i in range(2)]
    OF = [kb.sb(f"go_of{i}", [128, 512], F32, st) for i in range(2)]
    OB = [kb.sb(f"go_ob{i}", [128, 512], F32, st) for i in range(2)]
    junk = kb.sb("go_junk", [128, 4, 128], F32, st)
    ss = [kb.sb(f"go_ss{i}", [128, 4], F32, st) for i in range(2)]
    ON = [kb.sb(f"go_on{i}", [128, 512], F32, st) for i in range(2)]
    Y = [kb.sb(f"go_y{i}", [128, 4, 512], BF16, st) for i in range(2)]
    si = 0
    for ti, (p0, n) in enumerate(tiles):
        Zt, ZSt, Yt = Z[ti % 2], ZS[ti % 2], Y[ti % 2]
        kb.dma("sp", Zt[:, :, :n], pinT[3072:3584, :].rearrange("(k p) t -> p k t", p=128)[:, :, p0:p0 + n], writes=[Zt])
        kb.op("act", lambda: nc.scalar.activation(out=ZSt[:, :, :n], in_=Zt[:, :, :n], func=AF.Silu), reads=[Zt], writes=[ZSt])
        pts = [next_ps(C) for _ in range(4)]
        for s in range(n // 128):
            of_, ob_, ss_, on_ = OF[si % 2], OB[si % 2], ss[si % 2], ON[si % 2]
            si += 1
            r0 = p0 + s * 128
            kb.dma("sp", of_[:], ofd[r0:r0 + 128, :], writes=[of_])
            kb.dma("sp", ob_[:], obd[r0:r0 + 128, :], writes=[ob_])
            kb.op("dve", lambda: nc.vector.tensor_tensor(out=of_[:], in0=of_[:], in1=ob_[:], op=ALU.add), reads=[of_, ob_], writes=[of_])
            for h in range(4):
                kb.op("act", lambda: nc.scalar.activation(out=junk[:, h, :], in_=of_[:, h * 128:(h + 1) * 128], func=AF.Square, accum_out=ss_[:, h:h + 1]), reads=[of_], writes=[junk, ss_], waw=(h == 0))
            kb.op("act", lambda: nc.scalar.activation(out=ss_[:], in_=ss_[:], func=AF.Sqrt, scale=1.0 / 128.0, bias=C.eps_t[:, 0:1]), reads=[ss_, C.eps_t], writes=[ss_])
            kb.op("dve", lambda: nc.vector.reciprocal(out=ss_[:], in_=ss_[:]), reads=[ss_], writes=[ss_])
            for h in range(4):
                kb.op("dve", lambda: nc.vector.scalar_tensor_tensor(out=on_[:, h * 128:(h + 1) * 128], in0=of_[:, h * 128:(h + 1) * 128], scalar=ss_[:, h:h + 1], in1=gbc[:],
                                                                   op0=ALU.mult, op1=ALU.mult), reads=[of_, ss_, gbc], writes=[on_])
            for h in range(4):
                kb.op("pe", lambda: nc.tensor.transpose(pts[h][:, s * 128:(s + 1) * 128], on_[:, h * 128:(h + 1) * 128], C.ident_f[:]), reads=[on_, C.ident_f], writes=[pts[h]])
        for h in range(4):
            kb.op("dve", lambda: nc.vector.tensor_tensor(out=Yt[:, h, :n], in0=pts[h][:, :n], in1=ZSt[:, h, :n], op=ALU.mult), reads=[pts[h], ZSt], writes=[Yt])
        kb.dma("pool", yT1.rearrange("(k p) t -> p k t", p=128)[:, :, p0:p0 + n], Yt[:, :, :n], reads=[Yt])

def rope_consts(S=8192, GW=64):
    nf=8
    inv=(10000.0**(-np.arange(nf,dtype=np.float32)/nf)).astype(np.float32)
    t=np.arange(S); row=(t//GW).astype(np.float32); col=(t%GW).astype(np.float32)
    c32=np.zeros((32,S),np.float32); s32=np.zeros((32,S),np.float32)
    for d in range(32):
        pos=row if d<16 else col
        ang=(pos*inv[d%8]).astype(np.float32)
        c32[d]=np.cos(ang); s32[d]=np.sin(ang)
    Rm=np.zeros((32,32),np.float32)
    for m in range(32):
        if m%16<8: Rm[m,m+8]=-1.0
        else: Rm[m,m-8]=1.0
    r32T=np.ascontiguousarray(Rm.T)
    c96=np.ones((96,S),np.float32); s96=np.zeros((96,S),np.float32)
    c96[64:]=c32; s96[64:]=s32
    R96=np.zeros((96,96),np.float32); R96[64:,64:]=Rm
    return np.stack([c96,s96]),np.stack([c32,s32]),np.ascontiguousarray(R96.T),r32T

def fft_consts():
    import ml_dtypes
    p=np.arange(128)
    ang=2*np.pi*np.outer(p,p)/128.0
    Cm=np.cos(ang); Sm=np.sin(ang)
    FA=np.stack([np.concatenate([Cm,-Sm],1),np.concatenate([Cm,Sm],1),np.concatenate([-Sm,Cm],1)]).astype(np.float32)
    FB=np.stack([Cm,Sm,-Sm,-Cm]).astype(np.float32)
    a2=2*np.pi*np.outer(p,p)/16384.0
    TW=np.stack([np.cos(a2),-np.sin(a2),np.sin(a2)]).astype(np.float32)
    return FA,FB,TW

def hyena_pos(L):
    t=np.linspace(0.0,1.0,L,dtype=np.float32)[:,None]
    w=((2.0*np.pi/L)*np.arange(L,dtype=np.float32))[:,None].astype(np.float32)
    f=np.linspace(1e-4,15,16,dtype=np.float32)[None,:]
    z=np.concatenate([t,np.cos(f*w),-np.sin(f*w)],-1).astype(np.float32)
    return np.ascontiguousarray(z.T), np.ascontiguousarray(t[:,0])

def gdn_consts():
    i=np.arange(64)
    triF=(i[:,None]<=i[None,:]).astype(np.float32)
    triB=(i[:,None]>=i[None,:]).astype(np.float32)
    mS2=np.zeros((64,8,64),np.float32); mI1=np.zeros((64,8,64),np.float32)
    for u in range(8):
        if u<4:
            mS2[:,u,:]=(i[None,:]<i[:,None]); mI1[:,u,:]=(i[None,:]>=i[:,None])
        else:
            mS2[:,u,:]=(i[None,:]>i[:,None]); mI1[:,u,:]=(i[None,:]<=i[:,None])
    return triF,triB,mS2,mI1


CTXL = 256


def build_program(S=8192, final=True, debug=False, nlayers=2):
    T = CTXL + S
    kb = KB()
    C = Ctx()
    nc = kb.nc
    setup_consts(kb, C)
    EI = "ExternalInput"
    d = {}

    def inp(name, shape, dt=F32):
        d[name] = kb.dram(name, shape, dt, EI)
        return d[name]
    xT = inp("xT", [1024, S])
    cxT = inp("cxT", [1024, CTXL])
    c2 = inp("c2", [128, 8, 2])
    inp("w_ada", [2, 1024, 6144]); inp("b_ada_l", [2, 128, 48]); inp("g1_l", [2, 128, 8]); inp("g2_l", [2, 128, 8])
    inp("w_in", [2, 1024, NIN])
    inp("hy_conv_w", [2, 128, 3, 12]); inp("hy_conv_b", [2, 128, 12]); inp("hy_f_w1", [2, 33, 64]); inp("hy_f_b1", [2, 64]); inp("hy_f_w2", [2, 64, 64])
    inp("hy_f_b2", [2, 64]); inp("hy_f_w3", [2, 64, 1024]); inp("hy_f_freq", [2, 64]); inp("hy_decay", [2, 128, 8]); inp("hy_bias", [2, 128, 4])
    inp("hy_out", [2, 512, 1024]); inp("gdn_conv_w", [2, 128, 3, 12]); inp("gdn_a_log", [2, 8]); inp("gdn_dt_bias", [2, 8]); inp("gdn_norm_g", [2, 128, 128])
    inp("gdn_out", [2, 512, 1024]); inp("qg_l", [2, 128, 6]); inp("mla_w_uq", [2, 768, 768]); inp("kvg_l", [2, 128, 2]); inp("mla_w_ukv", [2, 256, 1024])
    inp("mla_out", [2, 512, 1024]); inp("w_out", [2, 1024, 1024]); inp("moe_w1", [2, 16, 1024, 512]); inp("moe_w3", [2, 16, 1024, 512]); inp("moe_w2", [2, 16, 512, 1024])
    inp("router_w", [1024, 16]); inp("router_b", [128, 16]); inp("fg_l", [128, 8])
    inp("r32", [32, 32]); inp("cs32", [2, 32, S]); inp("fad", [3, 128, 256]); inp("fbd", [4, 128, 128]); inp("twd", [3, 128, 128])
    inp("zT_lat", [33, S]); inp("tn_lat", [128, S]); inp("zT_ctx", [33, CTXL]); inp("tn_ctx", [128, CTXL])
    inp("trif", [64, 64]); inp("trib", [64, 64]); inp("ms2", [64, 8, 64]); inp("mi1", [64, 8, 64])
    outT = kb.dram("outT", [1024, S], F32, "ExternalOutput")
    dk = "ExternalOutput" if debug else "Internal"
    pinT = kb.dram("pinT", [NIN, T], BF16, dk)
    XA = kb.dram("XA", [1024, T], F32, dk)
    XB = kb.dram("XB", [1024, T], F32, dk)
    hT_lat = kb.dram("hT_lat", [1024, S], BF16)
    hT_ctx = kb.dram("hT_ctx", [1024, CTXL], BF16)
    uvT = kb.dram("uvT", [512, T], BF16)
    x0T = kb.dram("x0T", [512, T], BF16)
    yconv = kb.dram("yconv", [512, T], BF16)
    yT = kb.dram("yT", [3, 512, T], BF16, dk)
    qkvT = kb.dram("qkvT", [1536, T], F32)
    gbT = kb.dram("gbT", [T, 16], F32)
    ofd = kb.dram("ofd", [T, 512], F32)
    obd = kb.dram("obd", [T, 512], F32)
    qTd = kb.dram("qTd", [8, 96, T], BF16)
    kTd = kb.dram("kTd", [8, 96, T], BF16)
    vd = kb.dram("vd", [8, T, 64], BF16)
    load_fft_consts(kb, C, d["fad"], d["fbd"], d["twd"])
    load_gdn_consts(kb, C, d["trif"], d["trib"], d["ms2"], d["mi1"])
    modv = kb.sb("modv", [128, 48, 2], F32)
    AB = kb.sb("AB", [128, 6, 2, 8], F32)
    rw_sb = kb.sb("rw_sb", [128, 8, 16], F32)
    rb_bc = kb.sb("rb_bc", [128, 16], F32)
    r32 = kb.sb("r32_s", [32, 32], BF16)
    qg = kb.sb("qg_s", [128, 6], F32)
    kvg = kb.sb("kvg_s", [128, 2], F32)
    fg = kb.sb("fg_s", [128, 8], F32)
    kb.dma("sp", rw_sb[:], d["router_w"].rearrange("(k p) e -> p k e", p=128), writes=[rw_sb])
    kb.dma("sp", rb_bc[:], d["router_b"], writes=[rb_bc])
    kb.dma("pool", r32[:], d["r32"], writes=[r32])
    kb.dma("sp", fg[:], d["fg_l"], writes=[fg])
    lat_tiles = [(o, min(512, S - o)) for o in range(0, S, 512)]
    NC_ = T // 64
    fo = list(range(NC_))
    bo = [3, 2, 1, 0] + list(range(NC_ - 1, 3, -1))

    def stage():
        kb.barrier()
        return ExitStack()
    for l in range(nlayers):
        first = l == 0
        upd = first
        xsrc, cxsrc = (xT, cxT) if first else (XB[:, CTXL:], XB[:, 0:CTXL])
        st = stage()
        stage_mod(kb, C, st, c2, d["w_ada"][l], d["b_ada_l"][l], d["g1_l"][l], d["g2_l"][l], modv, AB)
        kb.dma("sp", qg[:], d["qg_l"][l], writes=[qg])
        kb.dma("sp", kvg[:], d["kvg_l"][l], writes=[kvg])
        st.close()
        st = stage()
        w_sb = kb.sb("w_sb", [128, 8, NIN], BF16, st)
        for k in range(8):
            kb.dma("pool", w_sb[:, k, :], d["w_in"][l][k * 128:(k + 1) * 128, :], writes=[w_sb])
        tiles = [(cxsrc, 0, CTXL, 0, 1)] + [(xsrc, o, n, CTXL + o, 0) for (o, n) in lat_tiles]
        stage_in(kb, C, st, tiles, AB, w_sb, pinT)
        st.close()
        st = stage()
        stage_hy_filter(kb, C, st, S, d["zT_lat"], d["tn_lat"], d["hy_f_w1"][l], d["hy_f_b1"][l], d["hy_f_w2"][l], d["hy_f_b2"][l], d["hy_f_w3"][l],
                        d["hy_f_freq"][l], d["hy_decay"][l], hT_lat)
        st.close()
        if upd:
            st = stage()
            stage_hy_filter(kb, C, st, CTXL, d["zT_ctx"], d["tn_ctx"], d["hy_f_w1"][l], d["hy_f_b1"][l], d["hy_f_w2"][l], d["hy_f_b2"][l], d["hy_f_w3"][l],
                            d["hy_f_freq"][l], d["hy_decay"][l], hT_ctx)
            st.close()
        st = stage()
        segs = ([(0, CTXL, 0)] if upd else []) + [(CTXL, S, CTXL)]
        stage_hy_conv3(kb, C, st, segs, pinT, d["hy_conv_w"][l], d["hy_conv_b"][l], uvT, x0T)
        st.close()
        st = stage()
        stage_hy_fft(kb, C, st, uvT[:, CTXL:], hT_lat[0:512, :], hT_lat[512:1024, :], yconv[:, CTXL:], S // 128)
        st.close()
        if upd:
            st = stage()
            stage_hy_fft(kb, C, st, uvT[:, 0:CTXL], hT_ctx[0:512, :], hT_ctx[512:1024, :], yconv[:, 0:CTXL], CTXL // 128)
            st.close()
        st = stage()
        gt = ([(0, CTXL)] if upd else []) + [(CTXL + o, n) for (o, n) in lat_tiles]
        stage_hy_gate(kb, C, st, gt, yconv, uvT, x0T, d["hy_bias"][l], yT[0])
        st.close()
        st = stage()
        stage_gdn_prep(kb, C, st, [(0, CTXL), (CTXL, S)], pinT, d["gdn_conv_w"][l], d["gdn_a_log"][l], d["gdn_dt_bias"][l], qkvT, gbT)
        st.close()
        st = stage()
        stage_gdn_scan(kb, C, st, fo, bo, qkvT, gbT, ofd, obd)
        st.close()
        st = stage()
        stage_gdn_out(kb, C, st, gt, ofd, obd, pinT, d["gdn_norm_g"][l], yT[1])
        st.close()
        st = stage()
        wuq = kb.sb("wuq_s", [128, 6, 768], BF16, st)
        wukv = kb.sb("wukv_s", [128, 2, 1024], BF16, st)
        kb.dma("pool", wuq[:], d["mla_w_uq"][l].rearrange("(k p) n -> p k n", p=128), writes=[wuq])
        kb.dma("pool", wukv[:], d["mla_w_ukv"][l].rearrange("(k p) n -> p k n", p=128), writes=[wukv])
        mt = [(0, CTXL, False, 0)] + [(CTXL + o, n, True, o) for (o, n) in lat_tiles]
        stage_mla_prep(kb, C, st, mt, pinT, qg, kvg, wuq, wukv, None, r32, None, d["cs32"], qTd, kTd, vd)
        st.close()
        st = stage()
        jobs = [(CTXL, S, list(range(T // 128)))] + ([(0, CTXL, [0, 1])] if upd else [])
        stage_mla_attn(kb, C, st, jobs, qTd, kTd, vd, yT[2], T)
        st.close()
        st = stage()
        w3s = kb.sb("w3s", [128, 3, 4, 1024], BF16, st)
        wos = kb.sb("wos", [128, 8, 1024], BF16, st)
        for j, nm in enumerate(("hy_out", "gdn_out", "mla_out")):
            kb.dma("pool", w3s[:, j], d[nm][l].rearrange("(k p) n -> p k n", p=128), writes=[w3s])
        kb.dma("pool", wos[:], d["w_out"][l].rearrange("(k p) n -> p k n", p=128), writes=[wos])
        mtiles = ([(cxsrc, 0, CTXL, 0, 1, XA[:, 0:CTXL])] if upd else []) + [(xsrc, o, n, CTXL + o, 0, XA[:, CTXL:]) for (o, n) in lat_tiles]
        stage_merge(kb, C, st, mtiles, AB, yT, pinT, w3s, wos, None)
        st.close()
        st = stage()
        supers = ([(XA[:, 0:CTXL], 0, CTXL, 1, XB[:, 0:CTXL])] if upd else []) + [(XA[:, CTXL:], o, min(1024, S - o), 0, XB[:, CTXL:]) for o in range(0, S, 1024)]
        stage_moe(kb, C, st, supers, AB, rw_sb, rb_bc, d["moe_w1"][l], d["moe_w3"][l], d["moe_w2"][l])
        st.close()
    st = stage()
    X = [kb.sb(f"fn_x{i}", [128, 8, 512], F32, st) for i in range(2)]
    SQ = kb.sb("fn_sq", [128, 8, 512], BF16, st)
    RS = kb.sb("fn_rs", [128, 512], F32, st)
    tmp = [kb.sb(f"fn_t{i}", [128, 512], F32, st) for i in range(2)]
    H = [kb.sb(f"fn_h{i}", [128, 8, 512], F32, st) for i in range(2)]
    src = XB[:, CTXL:].rearrange("(k p) t -> p k t", p=128)
    dst = outT.rearrange("(k p) t -> p k t", p=128)
    for ti, (o, n) in enumerate(lat_tiles):
        Xt, Ht = X[ti % 2], H[ti % 2]
        kb.dma("sp", Xt[:, :, :n], src[:, :, o:o + n], writes=[Xt])
        rmsnorm_tile(kb, C, Xt, n, PV(lambda k: fg[:, k:k + 1], [fg]), None, 0, Ht, SQ, RS, tmp)
        kb.dma("pool", dst[:, :, o:o + n], Ht[:, :, :n], reads=[Ht])
    kb.finish()
    st.close()
    return kb


def _lay(v, k):
    return np.ascontiguousarray(np.asarray(v, np.float32).reshape(k, 128).T)


def make_inputs(inputs, b, S=8192):
    f = lambda a: np.ascontiguousarray(np.asarray(a, np.float32))
    x = np.asarray(inputs["x"][b], np.float32)
    im = {}
    im["xT"] = np.ascontiguousarray(x.T)
    im["cxT"] = np.ascontiguousarray(np.asarray(inputs["ctx"][b], np.float32).T)
    im["c2"] = np.ascontiguousarray(np.stack([_lay(inputs["c"][b], 8), _lay(inputs["c_ctx"], 8)], -1))
    return im


def shared_inputs(inputs, S=8192):
    f = lambda a: np.ascontiguousarray(np.asarray(a, np.float32))
    sh = {}
    for nm in ("w_ada", "w_in", "hy_f_w1", "hy_f_b1", "hy_f_w2", "hy_f_b2", "hy_f_w3", "hy_f_freq", "hy_out",
               "gdn_out", "mla_w_uq", "mla_w_ukv", "mla_out", "w_out", "moe_w1", "moe_w3", "moe_w2", "router_w"):
        sh[nm] = f(inputs[nm])
    cwl = lambda w: np.ascontiguousarray(f(w).reshape(2, 3, 12, 128).transpose(0, 3, 1, 2))
    vl = lambda v, k: np.ascontiguousarray(f(v).reshape(2, k, 128).transpose(0, 2, 1))
    sh["hy_conv_w"] = cwl(inputs["hy_conv_w"]); sh["gdn_conv_w"] = cwl(inputs["gdn_conv_w"])
    sh["hy_conv_b"] = vl(inputs["hy_conv_b"], 12); sh["hy_decay"] = vl(inputs["hy_decay"], 8); sh["hy_bias"] = vl(inputs["hy_bias"], 4)
    sh["gdn_norm_g"] = np.ascontiguousarray(np.broadcast_to(f(inputs["gdn_norm_g"])[:, None, :], (2, 128, 128)))
    sh["router_b"] = np.ascontiguousarray(np.broadcast_to(f(inputs["router_b"])[None, :], (128, 16)))
    sh["b_ada_l"] = np.stack([np.ascontiguousarray(f(inputs["b_ada"])[l].reshape(48, 128).T) for l in range(2)])
    sh["g1_l"] = np.stack([_lay(inputs["norm1_g"][l], 8) for l in range(2)])
    sh["g2_l"] = np.stack([_lay(inputs["norm2_g"][l], 8) for l in range(2)])
    sh["qg_l"] = np.stack([_lay(inputs["mla_q_norm_g"][l], 6) for l in range(2)])
    sh["kvg_l"] = np.stack([_lay(inputs["mla_kv_norm_g"][l], 2) for l in range(2)])
    sh["fg_l"] = _lay(inputs["final_norm_g"], 8)
    sh["gdn_a_log"] = f(inputs["gdn_a_log"]).reshape(2, 8)
    sh["gdn_dt_bias"] = f(inputs["gdn_dt_bias"]).reshape(2, 8)
    cs96, cs32, r96T, r32T = rope_consts(S)
    sh["r32"] = r32T
    sh["cs32"] = cs32
    FA, FB, TW = fft_consts()
    sh["fad"], sh["fbd"], sh["twd"] = FA, FB, TW
    sh["zT_lat"], tl_ = hyena_pos(S)
    sh["zT_ctx"], tc_ = hyena_pos(CTXL)
    sh["tn_lat"] = np.ascontiguousarray(np.broadcast_to(tl_[None, :], (128, S)))
    sh["tn_ctx"] = np.ascontiguousarray(np.broadcast_to(tc_[None, :], (128, CTXL)))
    sh["trif"], sh["trib"], sh["ms2"], sh["mi1"] = gdn_consts()
    sh["ident_f_d"] = np.eye(128, dtype=np.float32)
    return sh


def kernel(**inputs):
    x = np.asarray(inputs["x"])
    B, S, D_ = x.shape
    kb = build_program(S)
    sh = shared_inputs(inputs, S)
    in_maps = []
    for b in range(B):
        im = dict(sh)
        im.update(make_inputs(inputs, b, S))
        in_maps.append(im)
    res = run_bass_kernel_spmd(kb.nc, in_maps, core_ids=list(range(B)))
    out = np.stack([np.ascontiguousarray(np.asarray(r["outT"], np.float32).T) for r in res.results], 0)
    return out.astype(np.float32)
```

```python
import numpy as np
from contextlib import ExitStack
import concourse.bass as bass
import concourse.mybir as mybir
from concourse.bass_utils import run_bass_kernel_spmd

F32 = mybir.dt.float32
BF16 = mybir.dt.bfloat16
AF = mybir.ActivationFunctionType
ALU = mybir.AluOpType
AX = mybir.AxisListType


class Buf:
    __slots__ = ("t", "w", "r", "pr", "excl")

    def __init__(self, t=None, excl=False):
        self.t = t
        self.w = []
        self.r = []
        self.pr = []
        self.excl = excl

    def __getitem__(self, k):
        return self.t[k]


class KB:
    SEM_EPOCH = 20000
    NDMA = 10

    def __init__(self):
        self.nc = bass.Bass("TRN2", target_bir_lowering=False)
        nc = self.nc
        self.es = ExitStack()
        self.eng = {"pe": nc.tensor, "act": nc.scalar, "dve": nc.vector, "pool": nc.gpsimd, "sp": nc.sync}
        self.csem = {}
        self.seen = {e: {} for e in self.eng}
        self.dpool = {}
        self.nsem = 0
        self.last_tok = {}
        self.out_toks = []
        self.ninst = 0

    def _newsem(self, name):
        self.nsem += 1
        return self.es.enter_context(self.nc.semaphore(f"{name}_{self.nsem}"))

    def sb(self, name, shape, dt, stack=None):
        self.nsem += 1
        name = f"{name}_u{self.nsem}"
        t = (stack or self.es).enter_context(self.nc.sbuf_tensor(name, list(shape), dt))
        return Buf(t)

    def ps(self, name, shape=(128, 512), dt=F32, stack=None):
        t = (stack or self.es).enter_context(self.nc.psum_tensor(name, list(shape), dt))
        return Buf(t, excl=True)

    def dram(self, name, shape, dt, kind="Internal"):
        return self.nc.dram_tensor(name, list(shape), dt, kind=kind).ap()

    def _wait(self, e, toks):
        en = self.eng[e]
        best = {}
        for (s, v) in toks:
            if best.get(s, (None, 0))[1] < v:
                best[s] = (s, v)
        for s, v in best.values():
            if self.seen[e].get(s.num, 0) < v:
                en.wait_ge(s, v)
                self.seen[e][s.num] = v
                self.ninst += 1

    def _deps(self, e, reads, writes, waw):
        toks = []
        for b in reads:
            toks.extend(b.w)
            if getattr(b, "excl", False):
                toks.extend(b.r)
        for b in writes:
            toks.extend(b.r)
            toks.extend(b.pr)
            if waw or b.r:
                toks.extend(b.w)
        return toks

    def _commit(self, tok, reads, writes):
        for b in writes:
            if b.r:
                b.pr = list(b.r) + list(b.w)
                b.w = [tok]
                b.r = []
            else:
                b.w = [t for t in b.w if t[0] is not tok[0]] + [tok]
        for b in reads:
            if b not in writes:
                b.r = [t for t in b.r if t[0] is not tok[0]] + [tok]

    def op(self, e, fn, reads=(), writes=(), waw=False):
        toks = self._deps(e, reads, writes, waw)
        if e == "pe":
            mysem = self.csem.get("pe")
            if mysem is not None:
                toks = [t for t in toks if t[0] is not mysem[0]]
        self._wait(e, toks)
        s = self.csem.get(e)
        if s is None or s[1] >= self.SEM_EPOCH:
            s = [self._newsem("c" + e), 0]
            self.csem[e] = s
        ins = fn()
        s[1] += 1
        ins.then_inc(s[0], 1)
        tok = (s[0], s[1])
        self.last_tok[e] = tok
        self._commit(tok, reads, writes)
        self.ninst += 1
        return tok

    def dma(self, q, out, in_, reads=(), writes=(), waw=False, **kw):
        toks = self._deps(q, reads, writes, waw)
        pool = self.dpool.setdefault(q, {"sems": [], "uses": [], "i": 0})
        if len(pool["sems"]) < self.NDMA:
            pool["sems"].append(self._newsem("d" + q))
            pool["uses"].append(0)
            k = len(pool["sems"]) - 1
        else:
            k = pool["i"] % self.NDMA
        pool["i"] += 1
        s = pool["sems"][k]
        u = pool["uses"][k]
        if u > 0:
            toks.append((s, 16 * u))
        self._wait(q, toks)
        ins = self.eng[q].dma_start(out=out, in_=in_, **kw)
        ins.then_inc(s, 16)
        pool["uses"][k] = u + 1
        tok = (s, 16 * (u + 1))
        self._commit(tok, reads, writes)
        self.ninst += 1
        return tok

    def all_tokens(self):
        toks = list(self.last_tok.values())
        for q, pool in self.dpool.items():
            for s, u in zip(pool["sems"], pool["uses"]):
                if u > 0:
                    toks.append((s, 16 * u))
        return toks

    def barrier(self):
        toks = self.all_tokens()
        for e in self.eng:
            self._wait(e, toks)

    def finish(self):
        self.barrier()


D = 1024
NIN = 7728
IN_SEGS = [("hy", 0, 1536), ("qkv", 1536, 1536), ("z", 3072, 512), ("ab", 3584, 16), ("cq", 3600, 768),
           ("ckv", 4368, 256), ("kr", 4624, 32), ("gate", 4656, 3072)]


def mchunks():
    out = []
    for name, c0, w in IN_SEGS:
        o = 0
        while o < w:
            m = min(128, w - o)
            out.append((name, c0 + o, m))
            o += m
    return out


class Ctx:
    pass


def setup_consts(kb, C):
    C.ones_bf = kb.sb("ones_bf", [128, 128], BF16)
    kb.op("dve", lambda: kb.nc.vector.memset(C.ones_bf[:], 1.0), writes=[C.ones_bf])
    C.eps_t = kb.sb("eps_t", [128, 1], F32)
    kb.op("dve", lambda: kb.nc.vector.memset(C.eps_t[:], 1e-6), writes=[C.eps_t])
    C.one_t = kb.sb("one_t", [128, 1], F32)
    kb.op("dve", lambda: kb.nc.vector.memset(C.one_t[:], 1.0), writes=[C.one_t])
    C.psum = [kb.ps(f"ps{i}") for i in range(8)]
    C.ident_f = kb.sb("ident_f", [128, 128], F32)
    C.ident_d = kb.dram("ident_f_d", [128, 128], F32, "ExternalInput")
    kb.dma("sp", C.ident_f[:], C.ident_d, writes=[C.ident_f])
    C.psi = 0


def next_ps(C):
    p = C.psum[C.psi % 8]
    C.psi += 1
    return p


def load_w_bf16(kb, dst, dst_ap, src_ap, q="pool"):
    return kb.dma(q, dst_ap, src_ap, writes=[dst])


def stage_in(kb, C, st, tiles, AB, w_sb, pinT):
    nc = kb.nc
    NB = 2
    xt = [kb.sb(f"in_x{i}", [128, 8, 512], F32, st) for i in range(1)]
    sq = [kb.sb(f"in_sq{i}", [128, 8, 512], BF16, st) for i in range(1)]
    rs = [kb.sb(f"in_rs{i}", [128, 512], F32, st) for i in range(NB)]
    tmp = [kb.sb(f"in_tmp{i}", [128, 512], F32, st) for i in range(4)]
    hT = [kb.sb(f"in_h{i}", [128, 8, 512], BF16, st) for i in range(NB)]
    ob = [kb.sb(f"in_o{i}", [128, 512], BF16, st) for i in range(6)]
    mcs = mchunks()
    oi = 0
    for ti, (src, t0, n, d0, which) in enumerate(tiles):
        b = ti % NB
        X, SQ, RS, H = xt[0], sq[0], rs[b], hT[b]
        kb.dma("sp", X[:, :, :n], src.rearrange("(k p) t -> p k t", p=128)[:, :, t0:t0 + n], writes=[X])
        rmsnorm_tile(kb, C, X, n, PV(lambda k: AB[:, 0, which, k:k + 1], [AB]), PV(lambda k: AB[:, 1, which, k:k + 1], [AB]), which, H, SQ, RS, tmp)
        for mi, (name, c0, m) in enumerate(mcs):
            ps = next_ps(C)
            for k in range(8):
                kb.op("pe", lambda: nc.tensor.matmul(ps[:m, :n], lhsT=w_sb[:, k, c0:c0 + m], rhs=H[:, k, :n], start=(k == 0), stop=(k == 7)),
                      reads=[H, w_sb], writes=[ps])
            O = ob[oi % 6]
            oi += 1
            if name == "gate":
                kb.op("act", lambda: nc.scalar.activation(out=O[:m, :n], in_=ps[:m, :n], func=AF.Sigmoid), reads=[ps], writes=[O])
            elif mi % 2 == 0:
                kb.op("dve", lambda: nc.vector.tensor_copy(out=O[:m, :n], in_=ps[:m, :n]), reads=[ps], writes=[O])
            else:
                kb.op("act", lambda: nc.scalar.copy(out=O[:m, :n], in_=ps[:m, :n]), reads=[ps], writes=[O])
            kb.dma("pool", pinT[c0:c0 + m, d0:d0 + n], O[:m, :n], reads=[O])


def stage_mod(kb, C, st, c2, wada, bada, g1, g2, modv, AB):
    nc = kb.nc
    sc = kb.sb("mod_sc", [128, 8, 2], F32, st)
    ba = kb.sb("mod_ba", [128, 48], F32, st)
    gg = kb.sb("mod_g", [128, 2, 8], F32, st)
    wa = [kb.sb(f"mod_wa{i}", [128, 8, 1536], F32, st) for i in range(2)]
    kb.dma("sp", sc[:], c2, writes=[sc])
    kb.dma("sp", ba[:], bada, writes=[ba])
    kb.dma("sp", gg[:, 0, :], g1, writes=[gg])
    kb.dma("sp", gg[:, 1, :], g2, writes=[gg])
    kb.op("act", lambda: nc.scalar.activation(out=sc[:], in_=sc[:], func=AF.Silu), reads=[sc], writes=[sc])
    for cg in range(4):
        W = wa[cg % 2]
        kb.dma("sp", W[:], wada.rearrange("(k p) n -> p k n", p=128)[:, :, cg * 1536:(cg + 1) * 1536], writes=[W])
        for m in range(12):
            j = cg * 12 + m
            ps = next_ps(C)
            for k in range(8):
                kb.op("pe", lambda: nc.tensor.matmul(ps[:, 0:2], lhsT=W[:, k, m * 128:(m + 1) * 128], rhs=sc[:, k, :], start=(k == 0), stop=(k == 7)),
                      reads=[W, sc], writes=[ps])
            kb.op("dve", lambda: nc.vector.tensor_scalar(out=modv[:, j, :], in0=ps[:, 0:2], scalar1=ba[:, j:j + 1], scalar2=None, op0=ALU.add),
                  reads=[ps, ba], writes=[modv])
    for which in range(2):
        for half, gi in ((0, 0), (1, 1)):
            o = half * 24
            kb.op("dve", lambda: nc.vector.scalar_tensor_tensor(out=AB[:, half * 3 + 0, which, :], in0=modv[:, o + 8:o + 16, which], scalar=1.0, in1=gg[:, gi, :],
                                                               op0=ALU.add, op1=ALU.mult), reads=[modv, gg], writes=[AB])
            kb.op("dve", lambda: nc.vector.tensor_copy(out=AB[:, half * 3 + 1, which, :], in_=modv[:, o:o + 8, which]), reads=[modv], writes=[AB])
            kb.op("dve", lambda: nc.vector.tensor_copy(out=AB[:, half * 3 + 2, which, :], in_=modv[:, o + 16:o + 24, which]), reads=[modv], writes=[AB])


def rmsnorm_tile(kb, C, X, n, Avec, Bvec, which, H, SQ, RS, tmp, Hf=None, nk=8, dim=D):
    nc = kb.nc
    kb.op("act", lambda: nc.scalar.activation(out=SQ[:, :nk, :n], in_=X[:, :nk, :n], func=AF.Square), reads=[X], writes=[SQ])
    pss = next_ps(C)
    for k in range(nk):
        kb.op("pe", lambda: nc.tensor.matmul(pss[:, :n], lhsT=C.ones_bf[:], rhs=SQ[:, k, :n], start=(k == 0), stop=(k == nk - 1)),
              reads=[SQ, C.ones_bf], writes=[pss])
    kb.op("act", lambda: nc.scalar.activation(out=RS[:, :n], in_=pss[:, :n], func=AF.Sqrt, scale=1.0 / dim, bias=C.eps_t[:, 0:1]), reads=[pss, C.eps_t], writes=[RS])
    kb.op("dve", lambda: nc.vector.reciprocal(out=RS[:, :n], in_=RS[:, :n]), reads=[RS], writes=[RS])
    for k in range(nk):
        T = tmp[k % len(tmp)]
        kb.op("dve", lambda: nc.vector.scalar_tensor_tensor(out=T[:, :n], in0=X[:, k, :n], scalar=Avec(k), in1=RS[:, :n],
                                                           op0=ALU.mult, op1=ALU.mult), reads=[X, RS] + Avec.bufs, writes=[T])
        if Bvec is not None:
            kb.op("act", lambda: nc.scalar.activation(out=H[:, k, :n], in_=T[:, :n], func=AF.Identity, bias=Bvec(k)),
                  reads=[T] + Bvec.bufs, writes=[H])
            if Hf is not None:
                kb.op("act", lambda: nc.scalar.activation(out=Hf[:, k, :n], in_=T[:, :n], func=AF.Identity, bias=Bvec(k)),
                      reads=[T] + Bvec.bufs, writes=[Hf])
        else:
            kb.op("act", lambda: nc.scalar.copy(out=H[:, k, :n], in_=T[:, :n]), reads=[T], writes=[H])


class PV:
    def __init__(self, fn, bufs):
        self.fn = fn
        self.bufs = bufs

    def __call__(self, k):
        return self.fn(k)


def stage_merge(kb, C, st, tiles, AB, yT, pinT, w3, wout, xoutT):
    nc = kb.nc
    NB = 2
    xt = [kb.sb(f"mg_x{i}", [128, 8, 512], F32, st) for i in range(NB)]
    yy = [kb.sb(f"mg_y{i}", [128, 3, 4, 512], BF16, st) for i in range(NB)]
    gt = [kb.sb(f"mg_g{i}", [128, 24, 512], BF16, st) for i in range(1)]
    mg = [kb.sb(f"mg_m{i}", [128, 8, 512], BF16, st) for i in range(NB)]
    tt = [kb.sb(f"mg_t{i}", [128, 3, 512], F32, st) for i in range(2)]
    xo = [kb.sb(f"mg_xo{i}", [128, 8, 512], F32, st) for i in range(1)]
    for ti, (src, t0, n, p0, which, dst) in enumerate(tiles):
        b = ti % NB
        X, Y, G, M, XO = xt[b], yy[b], gt[0], mg[b], xo[0]
        kb.dma("sp", X[:, :, :n], src.rearrange("(k p) t -> p k t", p=128)[:, :, t0:t0 + n], writes=[X])
        for j in range(3):
            kb.dma("sp", Y[:, j, :, :n], yT[j].rearrange("(k p) t -> p k t", p=128)[:, :, p0:p0 + n], writes=[Y])
        kb.dma("sp", G[:, :, :n], pinT[4656:7728, :].rearrange("(k p) t -> p k t", p=128)[:, :, p0:p0 + n], writes=[G])
        for m in range(8):
            TT = tt[m % 2]
            pss = []
            for j in range(3):
                ps = next_ps(C)
                pss.append(ps)
                for k in range(4):
                    kb.op("pe", lambda: nc.tensor.matmul(ps[:, :n], lhsT=w3[:, j, k, m * 128:(m + 1) * 128], rhs=Y[:, j, k, :n], start=(k == 0), stop=(k == 3)),
                          reads=[Y, w3], writes=[ps])
            for j in range(3):
                kb.op("dve", lambda: nc.vector.tensor_tensor(out=TT[:, j, :n], in0=pss[j][:, :n], in1=G[:, j * 8 + m, :n], op=ALU.mult),
                      reads=[pss[j], G], writes=[TT])
            kb.op("pool", lambda: nc.gpsimd.tensor_tensor(out=TT[:, 0, :n], in0=TT[:, 0, :n], in1=TT[:, 1, :n], op=ALU.add), reads=[TT], writes=[TT])
            kb.op("pool", lambda: nc.gpsimd.tensor_tensor(out=M[:, m, :n], in0=TT[:, 0, :n], in1=TT[:, 2, :n], op=ALU.add), reads=[TT], writes=[M])
        for m in range(8):
            ps = next_ps(C)
            for k in range(8):
                kb.op("pe", lambda: nc.tensor.matmul(ps[:, :n], lhsT=wout[:, k, m * 128:(m + 1) * 128], rhs=M[:, k, :n], start=(k == 0), stop=(k == 7)),
                      reads=[M, wout], writes=[ps])
            kb.op("dve", lambda: nc.vector.scalar_tensor_tensor(out=XO[:, m, :n], in0=ps[:, :n], scalar=AB[:, 2, which, m:m + 1], in1=X[:, m, :n],
                                                               op0=ALU.mult, op1=ALU.add), reads=[ps, AB, X], writes=[XO])
        kb.dma("pool", dst.rearrange("(k p) t -> p k t", p=128)[:, :, t0:t0 + n], XO[:, :, :n], reads=[XO])


def stage_moe(kb, C, st, supers, AB, rw_sb, rb_bc, w1d, w3d, w2d):
    nc = kb.nc
    X = kb.sb("moe_x", [128, 8, 512], F32, st)
    SQ = kb.sb("moe_sq", [128, 8, 512], BF16, st)
    RS = kb.sb("moe_rs", [128, 512], F32, st)
    tmp = [kb.sb(f"moe_tmp{i}", [128, 512], F32, st) for i in range(2)]
    Hf = kb.sb("moe_hf", [128, 8, 512], F32, st)
    Hb = kb.sb("moe_hb", [128, 8, 1024], BF16, st)
    acc = kb.sb("moe_acc", [128, 8, 1024], F32, st)
    gate = kb.sb("moe_gate", [128, 8, 16], F32, st)
    rt = [kb.sb(f"moe_rt{i}", [128, 64], F32, st) for i in range(2)]
    w1 = [kb.sb(f"moe_w1_{i}", [128, 8, 512], BF16, st) for i in range(2)]
    w3 = [kb.sb(f"moe_w3_{i}", [128, 8, 512], BF16, st) for i in range(2)]
    w2 = [kb.sb(f"moe_w2_{i}", [128, 4, 1024], BF16, st) for i in range(2)]
    he = [kb.sb(f"moe_he{i}", [128, 4, 512], BF16, st) for i in range(2)]
    sl = [kb.sb(f"moe_sl{i}", [128, 512], F32, st) for i in range(2)]
    xo = [kb.sb(f"moe_xo{i}", [128, 8, 512], F32, st) for i in range(1)]
    wi = 0
    for (src, t0, n, which, dst) in supers:
        tl = [(o, min(512, n - o)) for o in range(0, n, 512)]
        srcv = src.rearrange("(k p) t -> p k t", p=128)
        dstv = dst.rearrange("(k p) t -> p k t", p=128)
        for (o, tn) in tl:
            kb.dma("sp", X[:, :, :tn], srcv[:, :, t0 + o:t0 + o + tn], writes=[X])
            Hview = Buf(None)
            rmsnorm_tile(kb, C, X, tn, PV(lambda k: AB[:, 3, which, k:k + 1], [AB]), PV(lambda k: AB[:, 4, which, k:k + 1], [AB]), which,
                         _Off(Hb, o), SQ, RS, tmp, Hf=Hf)
            for s in range(tn // 128):
                sg = (o // 128) + s
                R = rt[sg % 2]
                ps = next_ps(C)
                for k in range(8):
                    kb.op("pe", lambda: nc.tensor.matmul(ps[:, 0:16], lhsT=Hf[:, k, s * 128:(s + 1) * 128], rhs=rw_sb[:, k, :], start=(k == 0), stop=(k == 7)),
                          reads=[Hf, rw_sb], writes=[ps])
                sc = R[:, 0:16]
                sel = R[:, 16:32]
                kb.op("act", lambda: nc.scalar.activation(out=sc, in_=ps[:, 0:16], func=AF.Sigmoid), reads=[ps], writes=[R])
                kb.op("dve", lambda: nc.vector.tensor_tensor(out=sel, in0=sc, in1=rb_bc[:, :], op=ALU.add), reads=[R, rb_bc], writes=[R])
                sel3 = R[:, 16:32].rearrange("p (g j) -> p g j", j=4)
                P3 = R[:, 32:56].rearrange("p (g j) -> p g j", j=6)
                pi = 0
                for a in range(4):
                    for b2 in range(a + 1, 4):
                        kb.op("dve", lambda: nc.vector.tensor_tensor(out=P3[:, :, pi], in0=sel3[:, :, a], in1=sel3[:, :, b2], op=ALU.add), reads=[R], writes=[R])
                        pi += 1
                kb.op("dve", lambda: nc.vector.tensor_reduce(out=R[:, 56:60], in_=P3, axis=AX.X, op=ALU.max), reads=[R], writes=[R])
                kb.op("dve", lambda: nc.vector.tensor_reduce(out=R[:, 60:61], in_=R[:, 56:60], axis=AX.X, op=ALU.max), reads=[R], writes=[R])
                kb.op("dve", lambda: nc.vector.tensor_scalar(out=R[:, 56:60], in0=R[:, 56:60], scalar1=R[:, 60:61], scalar2=None, op0=ALU.is_ge), reads=[R], writes=[R])
                kb.op("dve", lambda: nc.vector.scalar_tensor_tensor(out=sel3, in0=sel3, scalar=2.0, in1=R[:, 56:60].unsqueeze(2).to_broadcast([128, 4, 4]),
                                                                   op0=ALU.add, op1=ALU.mult), reads=[R], writes=[R])
                M1 = R[:, 32:48]
                M2 = R[:, 48:64]
                kb.op("dve", lambda: nc.vector.tensor_reduce(out=R[:, 61:62], in_=sel, axis=AX.X, op=ALU.max), reads=[R], writes=[R])
                G = gate[:, sg, :]
                kb.op("dve", lambda: nc.vector.tensor_scalar(out=G, in0=sel, scalar1=R[:, 61:62], scalar2=None, op0=ALU.is_ge), reads=[R], writes=[gate])
                kb.op("dve", lambda: nc.vector.scalar_tensor_tensor(out=M1, in0=G, scalar=-10.0, in1=sel, op0=ALU.mult, op1=ALU.add), reads=[R, gate], writes=[R])
                kb.op("dve", lambda: nc.vector.tensor_reduce(out=R[:, 61:62], in_=M1, axis=AX.X, op=ALU.max), reads=[R], writes=[R])
                kb.op("dve", lambda: nc.vector.scalar_tensor_tensor(out=G, in0=M1, scalar=R[:, 61:62], in1=G, op0=ALU.is_ge, op1=ALU.add), reads=[R, gate], writes=[gate])
                kb.op("dve", lambda: nc.vector.tensor_tensor(out=G, in0=G, in1=sc, op=ALU.mult), reads=[R, gate], writes=[gate])
                kb.op("dve", lambda: nc.vector.tensor_reduce(out=R[:, 62:63], in_=G, axis=AX.X, op=ALU.add), reads=[gate], writes=[R])
                kb.op("dve", lambda: nc.vector.reciprocal(out=R[:, 62:63], in_=R[:, 62:63]), reads=[R], writes=[R])
                kb.op("dve", lambda: nc.vector.tensor_scalar(out=G, in0=G, scalar1=R[:, 62:63], scalar2=None, op0=ALU.mult), reads=[R, gate], writes=[gate])
        for e in range(16):
            W1, W3, W2 = w1[wi % 2], w3[wi % 2], w2[wi % 2]
            wi += 1
            kb.dma("pool", W1[:], w1d[e].rearrange("(k p) f -> p k f", p=128), writes=[W1])
            kb.dma("pool", W3[:], w3d[e].rearrange("(k p) f -> p k f", p=128), writes=[W3])
            kb.dma("pool", W2[:], w2d[e].rearrange("(k p) f -> p k f", p=128), writes=[W2])
            for ti, (o, tn) in enumerate(tl):
                HE = he[ti % 2]
                for m in range(4):
                    p1 = next_ps(C)
                    p3 = next_ps(C)
                    for k in range(8):
                        kb.op("pe", lambda: nc.tensor.matmul(p1[:, :tn], lhsT=W1[:, k, m * 128:(m + 1) * 128], rhs=Hb[:, k, o:o + tn], start=(k == 0), stop=(k == 7)),
                              reads=[Hb, W1], writes=[p1])
                    for k in range(8):
                        kb.op("pe", lambda: nc.tensor.matmul(p3[:, :tn], lhsT=W3[:, k, m * 128:(m + 1) * 128], rhs=Hb[:, k, o:o + tn], start=(k == 0), stop=(k == 7)),
                              reads=[Hb, W3], writes=[p3])
                    S = sl[m % 2]
                    kb.op("act", lambda: nc.scalar.activation(out=S[:, :tn], in_=p1[:, :tn], func=AF.Silu), reads=[p1], writes=[S])
                    kb.op("dve", lambda: nc.vector.tensor_tensor(out=HE[:, m, :tn], in0=p3[:, :tn], in1=S[:, :tn], op=ALU.mult), reads=[p3, S], writes=[HE])
                for s in range(tn // 128):
                    sg = (o // 128) + s
                    for hf in range(2):
                        po = next_ps(C)
                        for m in range(4):
                            kb.op("pe", lambda: nc.tensor.matmul(po[:, :], lhsT=HE[:, m, s * 128:(s + 1) * 128], rhs=W2[:, m, hf * 512:(hf + 1) * 512], start=(m == 0), stop=(m == 3)),
                                  reads=[HE, W2], writes=[po])
                        A = acc[:, sg, hf * 512:(hf + 1) * 512]
                        if e == 0:
                            kb.op("dve", lambda: nc.vector.tensor_scalar(out=A, in0=po[:, :], scalar1=gate[:, sg, e:e + 1], scalar2=None, op0=ALU.mult),
                                  reads=[po, gate], writes=[acc])
                        else:
                            kb.op("dve", lambda: nc.vector.scalar_tensor_tensor(out=A, in0=po[:, :], scalar=gate[:, sg, e:e + 1], in1=A, op0=ALU.mult, op1=ALU.add),
                                  reads=[po, gate, acc], writes=[acc])
        XO = xo[0]
        for (o, tn) in tl:
            kb.dma("sp", X[:, :, :tn], srcv[:, :, t0 + o:t0 + o + tn], writes=[X])
            for m in range(8):
                pt = next_ps(C)
                for s in range(tn // 128):
                    sg = (o // 128) + s
                    kb.op("pe", lambda: nc.tensor.transpose(pt[:, s * 128:(s + 1) * 128], acc[:, sg, m * 128:(m + 1) * 128], C.ident_f[:]),
                          reads=[acc, C.ident_f], writes=[pt])
                kb.op("dve", lambda: nc.vector.scalar_tensor_tensor(out=XO[:, m, :tn], in0=pt[:, :tn], scalar=AB[:, 5, which, m:m + 1], in1=X[:, m, :tn],
                                                                   op0=ALU.mult, op1=ALU.add), reads=[pt, AB, X], writes=[XO])
            kb.dma("pool", dstv[:, :, t0 + o:t0 + o + tn], XO[:, :, :tn], reads=[XO])


class _Off:
    def __init__(self, b, off):
        self.b = b
        self.off = off

    def __getitem__(self, key):
        p, k, sl_ = key
        return self.b.t[p, k, self.off + (sl_.start or 0):self.off + sl_.stop]

    @property
    def w(self):
        return self.b.w

    @w.setter
    def w(self, v):
        self.b.w = v

    @property
    def pr(self):
        return self.b.pr

    @pr.setter
    def pr(self, v):
        self.b.pr = v

    @property
    def r(self):
        return self.b.r

    @r.setter
    def r(self, v):
        self.b.r = v


def stage_mla_prep(kb, C, st, tiles, pinT, qg, kvg, wuq, wukv, r96, r32, cs96, cs32, qTd, kTd, vd):
    nc = kb.nc
    cq = [kb.sb(f"mp_cq{i}", [128, 6, 512], BF16, st) for i in range(2)]
    ckv = [kb.sb(f"mp_ckv{i}", [128, 2, 512], BF16, st) for i in range(2)]
    kr = [kb.sb(f"mp_kr{i}", [32, 512], BF16, st) for i in range(2)]
    SQ = kb.sb("mp_sq", [128, 6, 512], BF16, st)
    RS = kb.sb("mp_rs", [128, 512], F32, st)
    tmp = [kb.sb(f"mp_tmp{i}", [128, 512], F32, st) for i in range(2)]
    cqn = kb.sb("mp_cqn", [128, 6, 512], BF16, st)
    ckvn = kb.sb("mp_ckvn", [128, 2, 512], BF16, st)
    t96 = [kb.sb(f"mp_t96{i}", [96, 2, 512], F32, st) for i in range(2)]
    t32 = [kb.sb(f"mp_t32{i}", [32, 2, 512], F32, st) for i in range(2)]
    qb = [kb.sb(f"mp_qb{i}", [96, 512], BF16, st) for i in range(2)]
    qf = [kb.sb(f"mp_qf{i}", [96, 512], F32, st) for i in range(2)]
    qo = [kb.sb(f"mp_qo{i}", [96, 512], BF16, st) for i in range(3)]
    ko = [kb.sb(f"mp_ko{i}", [64, 512], BF16, st) for i in range(6)]
    qi2 = [0]
    qr_ = [kb.sb(f"mp_qr{i}", [32, 512], BF16, st) for i in range(3)]
    krf2 = [kb.sb(f"mp_krf2{i}", [32, 512], F32, st) for i in range(2)]
    krg2 = [kb.sb(f"mp_krg2{i}", [32, 512], F32, st) for i in range(2)]
    kro2 = [kb.sb(f"mp_kro2{i}", [32, 512], BF16, st) for i in range(3)]
    kro = [kb.sb(f"mp_kro{i}", [32, 512], BF16, st) for i in range(2)]
    krf = [kb.sb(f"mp_krf{i}", [32, 512], F32, st) for i in range(2)]
    vo = [kb.sb(f"mp_vo{i}", [128, 512], BF16, st) for i in range(3)]
    qi = 0
    for ti, (p0, n, rope, tl0) in enumerate(tiles):
        b = ti % 2
        CQ, CKV, KR, T96, T32 = cq[b], ckv[b], kr[b], t96[b], t32[b]
        kb.dma("sp", CQ[:, :, :n], pinT[3600:4368, :].rearrange("(k p) t -> p k t", p=128)[:, :, p0:p0 + n], writes=[CQ])
        kb.dma("sp", CKV[:, :, :n], pinT[4368:4624, :].rearrange("(k p) t -> p k t", p=128)[:, :, p0:p0 + n], writes=[CKV])
        kb.dma("sp", KR[:, :n], pinT[4624:4656, p0:p0 + n], writes=[KR])
        if rope:
            kb.dma("sp", T32[:, :, :n], cs32.rearrange("c d t -> d c t")[:, :, tl0:tl0 + n], writes=[T32])
        rmsnorm_tile(kb, C, CQ, n, PV(lambda k: qg[:, k:k + 1], [qg]), None, 0, cqn, SQ, RS, tmp, nk=6, dim=768)
        rmsnorm_tile(kb, C, CKV, n, PV(lambda k: kvg[:, k:k + 1], [kvg]), None, 0, ckvn, SQ, RS, tmp, nk=2, dim=256)
        KRO = kro[b]
        if rope:
            KRF = krf[b]
            ps = next_ps(C)
            kb.op("pe", lambda: nc.tensor.matmul(ps[:32, :n], lhsT=r32[:, :], rhs=KR[:, :n], start=True, stop=True), reads=[KR, r32], writes=[ps])
            kb.op("dve", lambda: nc.vector.tensor_tensor(out=KRF[:, :n], in0=ps[:32, :n], in1=T32[:, 1, :n], op=ALU.mult), reads=[ps, T32], writes=[KRF])
            KRG = krg2[b]
            kb.op("pool", lambda: nc.gpsimd.tensor_tensor(out=KRG[:, :n], in0=T32[:, 0, :n], in1=KR[:, :n], op=ALU.mult), reads=[T32, KR], writes=[KRG])
            kb.op("pool", lambda: nc.gpsimd.tensor_tensor(out=KRO[:, :n], in0=KRG[:, :n], in1=KRF[:, :n], op=ALU.add), reads=[KRG, KRF], writes=[KRO])
        else:
            kb.op("pool", lambda: nc.gpsimd.tensor_copy(out=KRO[:, :n], in_=KR[:, :n]), reads=[KR], writes=[KRO])
        for h in range(8):
            kb.dma("pool", kTd[h, 64:96, p0:p0 + n], KRO[:, :n], reads=[KRO])
        for h in range(8):
            ps = next_ps(C)
            for k in range(6):
                kb.op("pe", lambda: nc.tensor.matmul(ps[:64, :n], lhsT=wuq[:, k, h * 96:h * 96 + 64], rhs=cqn[:, k, :n], start=(k == 0), stop=(k == 5)),
                      reads=[cqn, wuq], writes=[ps])
            QO = ko[qi2[0] % 6]
            qi2[0] += 1
            kb.op("act", lambda: nc.scalar.copy(out=QO[:, :n], in_=ps[:64, :n]), reads=[ps], writes=[QO])
            kb.dma("pool", qTd[h, 0:64, p0:p0 + n], QO[:, :n], reads=[QO])
            ps = next_ps(C)
            for k in range(6):
                kb.op("pe", lambda: nc.tensor.matmul(ps[:32, :n], lhsT=wuq[:, k, h * 96 + 64:h * 96 + 96], rhs=cqn[:, k, :n], start=(k == 0), stop=(k == 5)),
                      reads=[cqn, wuq], writes=[ps])
            QR = qr_[qi % 3]
            kb.op("act", lambda: nc.scalar.copy(out=QR[:, :n], in_=ps[:32, :n]), reads=[ps], writes=[QR])
            if rope:
                QF, QG, QO2 = krf2[qi % 2], krg2[qi % 2], kro2[qi % 3]
                ps2 = next_ps(C)
                kb.op("pe", lambda: nc.tensor.matmul(ps2[:32, :n], lhsT=r32[:, :], rhs=QR[:, :n], start=True, stop=True), reads=[QR, r32], writes=[ps2])
                kb.op("dve", lambda: nc.vector.tensor_tensor(out=QF[:, :n], in0=ps2[:32, :n], in1=T32[:, 1, :n], op=ALU.mult), reads=[ps2, T32], writes=[QF])
                kb.op("pool", lambda: nc.gpsimd.tensor_tensor(out=QG[:, :n], in0=T32[:, 0, :n], in1=QR[:, :n], op=ALU.mult), reads=[T32, QR], writes=[QG])
                kb.op("pool", lambda: nc.gpsimd.tensor_tensor(out=QO2[:, :n], in0=QG[:, :n], in1=QF[:, :n], op=ALU.add), reads=[QG, QF], writes=[QO2])
                kb.dma("pool", qTd[h, 64:96, p0:p0 + n], QO2[:, :n], reads=[QO2])
            else:
                kb.dma("pool", qTd[h, 64:96, p0:p0 + n], QR[:, :n], reads=[QR])
            ps = next_ps(C)
            for k in range(2):
                kb.op("pe", lambda: nc.tensor.matmul(ps[:64, :n], lhsT=wukv[:, k, h * 128:h * 128 + 64], rhs=ckvn[:, k, :n], start=(k == 0), stop=(k == 1)),
                      reads=[ckvn, wukv], writes=[ps])
            KO = ko[qi2[0] % 6]
            qi2[0] += 1
            kb.op("act", lambda: nc.scalar.copy(out=KO[:, :n], in_=ps[:64, :n]), reads=[ps], writes=[KO])
            kb.dma("pool", kTd[h, 0:64, p0:p0 + n], KO[:, :n], reads=[KO])
            qi += 1
        for s in range(n // 128):
            ps = next_ps(C)
            for k in range(2):
                kb.op("pe", lambda: nc.tensor.matmul(ps[:, :].rearrange("p (h c) -> p h c", c=64), lhsT=ckvn[:, k, s * 128:(s + 1) * 128],
                                                     rhs=wukv[:, k, :].rearrange("p (h c) -> p h c", c=128)[:, :, 64:128], start=(k == 0), stop=(k == 1)),
                      reads=[ckvn, wukv], writes=[ps])
            VO = vo[s % 3]
            kb.op("dve", lambda: nc.vector.tensor_copy(out=VO[:, :], in_=ps[:, :]), reads=[ps], writes=[VO])
            kb.dma("pool", vd.rearrange("h t c -> t h c")[p0 + s * 128:p0 + (s + 1) * 128, :, :], VO[:, :].rearrange("p (h c) -> p h c", c=64), reads=[VO])


def stage_mla_attn(kb, C, st, jobs, qTd, kTd, vd, attnT, T):
    nc = kb.nc
    scale = 96.0 ** -0.5
    NKT = T // 128
    LOOK = 3
    kT = [kb.sb(f"at_k{i}", [96, T], BF16, st) for i in range(2)]
    V = [kb.sb(f"at_v{i}", [128, NKT, 65], BF16, st) for i in range(2)]
    Q = [kb.sb(f"at_q{i}", [96, 512], BF16, st) for i in range(2)]
    P = [kb.sb(f"at_p{i}", [128, 512], BF16, st) for i in range(6)]
    rrow = [kb.sb(f"at_rr{i}", [65, 512], F32, st) for i in range(2)]
    rbc = [kb.sb(f"at_rb{i}", [64, 512], F32, st) for i in range(2)]
    O = [kb.sb(f"at_o{i}", [64, 512], BF16, st) for i in range(2)]
    ones65 = kb.sb("at_ones", [65, 64], F32, st)
    kb.op("dve", lambda: nc.vector.memset(ones65[:], 1.0), writes=[ones65])
    for i in range(2):
        kb.op("pool", lambda: nc.gpsimd.memset(V[i][:, :, 64:65], 1.0), writes=[V[i]])
    sps = C.psum[0:4]
    aps = C.psum[4:8]
    si = 0
    pi_ = 0
    qi = 0
    for h in range(8):
        K_, V_ = kT[h % 2], V[h % 2]
        kb.dma("sp", K_[:, :], kTd[h], writes=[K_])
        kb.dma("sp", V_[:, :, 0:64], vd[h].rearrange("(j p) c -> p j c", p=128), writes=[V_])
        for (q0, nq, ktiles) in jobs:
            for o in range(0, nq, 512):
                n = min(512, nq - o)
                Qt = Q[qi % 2]
                po, pb = aps[(qi % 2) * 2], aps[(qi % 2) * 2 + 1]
                kb.dma("sp", Qt[:, :n], qTd[h, :, q0 + o:q0 + o + n], writes=[Qt])
                nk = len(ktiles)
                pend = []
                for ji in range(nk + LOOK):
                    if ji < nk:
                        j = ktiles[ji]
                        ps = sps[si % 4]
                        si += 1
                        Pt = P[pi_ % 6]
                        pi_ += 1
                        kb.op("pe", lambda: nc.tensor.matmul(ps[:, :n], lhsT=K_[:, j * 128:(j + 1) * 128], rhs=Qt[:, :n], start=True, stop=True),
                              reads=[K_, Qt], writes=[ps])
                        kb.op("act", lambda: nc.scalar.activation(out=Pt[:, :n], in_=ps[:, :n], func=AF.Exp, scale=scale), reads=[ps], writes=[Pt])
                        pend.append((j, Pt))
                    if ji >= LOOK:
                        jj = ji - LOOK
                        j, Pt = pend[jj]
                        kb.op("pe", lambda: nc.tensor.matmul(po[:65, :n], lhsT=V_[:, j, :], rhs=Pt[:, :n], start=(jj == 0), stop=(jj == nk - 1)),
                              reads=[V_, Pt], writes=[po])
                RR, RB, O_ = rrow[qi % 2], rbc[qi % 2], O[qi % 2]
                kb.op("dve", lambda: nc.vector.reciprocal(out=RR[64:65, :n], in_=po[64:65, :n]), reads=[po], writes=[RR])
                kb.op("pe", lambda: nc.tensor.matmul(pb[:64, :n], lhsT=ones65[64:65, :], rhs=RR[64:65, :n], start=True, stop=True), reads=[ones65, RR], writes=[pb])
                kb.op("act", lambda: nc.scalar.copy(out=RB[:, :n], in_=pb[:64, :n]), reads=[pb], writes=[RB])
                kb.op("dve", lambda: nc.vector.tensor_tensor(out=O_[:, :n], in0=po[:64, :n], in1=RB[:, :n], op=ALU.mult), reads=[po, RB], writes=[O_])
                kb.dma("pool", attnT[h * 64:(h + 1) * 64, q0 + o:q0 + o + n], O_[:, :n], reads=[O_])
                qi += 1


def hy_sin(kb, nc, out, ps, n, fr, tmpa, tmpb):
    kb.op("act", lambda: nc.scalar.activation(out=tmpa[:64, :n], in_=ps[:64, :n], func=AF.Sin, scale=fr[:, 0:1], bias=fr[:, 1:2]), reads=[ps, fr], writes=[tmpa])
    kb.op("act", lambda: nc.scalar.activation(out=tmpb[:64, :n], in_=ps[:64, :n], func=AF.Sin, scale=fr[:, 2:3], bias=fr[:, 3:4]), reads=[ps, fr], writes=[tmpb])
    kb.op("dve", lambda: nc.vector.tensor_tensor(out=tmpb[:64, :n], in0=tmpb[:64, :n], in1=tmpb[:64, :n], op=ALU.mult), reads=[tmpb], writes=[tmpb])
    kb.op("dve", lambda: nc.vector.tensor_scalar(out=tmpb[:64, :n], in0=tmpb[:64, :n], scalar1=-2.0, scalar2=1.0, op0=ALU.mult, op1=ALU.add), reads=[tmpb], writes=[tmpb])
    kb.op("dve", lambda: nc.vector.scalar_tensor_tensor(out=out[:64, :n], in0=tmpa[:64, :n], scalar=2.0, in1=tmpb[:64, :n], op0=ALU.mult, op1=ALU.mult), reads=[tmpa, tmpb], writes=[out])


def stage_hy_filter(kb, C, st, L, zT, tn, w1d, b1d, w2d, b2d, w3d, frd, decd, hTd):
    nc = kb.nc
    w1 = kb.sb("hf_w1", [33, 64], F32, st)
    w2 = kb.sb("hf_w2", [64, 64], F32, st)
    w3 = kb.sb("hf_w3", [64, 1024], F32, st)
    v = kb.sb("hf_v", [64, 3], F32, st)
    fr1 = kb.sb("hf_fr1", [64, 4], F32, st)
    fr2 = kb.sb("hf_fr2", [64, 4], F32, st)
    dec = kb.sb("hf_dec", [128, 8], F32, st)
    kb.dma("sp", w1[:], w1d, writes=[w1])
    kb.dma("sp", w2[:], w2d, writes=[w2])
    kb.dma("sp", w3[:], w3d, writes=[w3])
    kb.dma("sp", v[:, 0:1], b1d.rearrange("(p o) -> p o", o=1), writes=[v])
    kb.dma("sp", v[:, 1:2], b2d.rearrange("(p o) -> p o", o=1), writes=[v])
    kb.dma("sp", v[:, 2:3], frd.rearrange("(p o) -> p o", o=1), writes=[v])
    kb.dma("sp", dec[:], decd, writes=[dec])
    for fr, bi in ((fr1, 0), (fr2, 1)):
        kb.op("dve", lambda: nc.vector.tensor_scalar(out=fr[:, 0:1], in0=v[:, 2:3], scalar1=0.5, scalar2=None, op0=ALU.mult), reads=[v], writes=[fr])
        kb.op("dve", lambda: nc.vector.scalar_tensor_tensor(out=fr[:, 1:2], in0=v[:, 2:3], scalar=0.5, in1=v[:, bi:bi + 1], op0=ALU.mult, op1=ALU.mult), reads=[v], writes=[fr])
        kb.op("dve", lambda: nc.vector.tensor_scalar(out=fr[:, 2:4], in0=fr[:, 0:2], scalar1=0.5, scalar2=None, op0=ALU.mult), reads=[fr], writes=[fr])
    kb.op("act", lambda: nc.scalar.activation(out=dec[:], in_=dec[:], func=AF.Abs), reads=[dec], writes=[dec])
    kb.op("dve", lambda: nc.vector.tensor_scalar(out=dec[:], in0=dec[:], scalar1=-1.0, scalar2=None, op0=ALU.mult), reads=[dec], writes=[dec])
    z = [kb.sb(f"hf_z{i}", [33, 512], F32, st) for i in range(2)]
    tb = [kb.sb(f"hf_tb{i}", [128, 512], F32, st) for i in range(2)]
    ta = kb.sb("hf_ta", [64, 512], F32, st)
    tc_ = kb.sb("hf_tc", [64, 512], F32, st)
    h1 = kb.sb("hf_h1", [64, 512], F32, st)
    h2 = kb.sb("hf_h2", [64, 512], F32, st)
    win = [kb.sb(f"hf_win{i}", [128, 512], F32, st) for i in range(2)]
    ho = [kb.sb(f"hf_ho{i}", [128, 512], BF16, st) for i in range(3)]
    oi = 0
    for ti, o in enumerate(range(0, L, 512)):
        n = min(512, L - o)
        Z, TB = z[ti % 2], tb[ti % 2]
        kb.dma("sp", Z[:, :n], zT[:, o:o + n], writes=[Z])
        kb.dma("sp", TB[:, :n], tn[:, o:o + n], writes=[TB])
        ps = next_ps(C)
        kb.op("pe", lambda: nc.tensor.matmul(ps[:64, :n], lhsT=w1[:, :], rhs=Z[:, :n], start=True, stop=True), reads=[w1, Z], writes=[ps])
        hy_sin(kb, nc, h1, ps, n, fr1, ta, tc_)
        ps = next_ps(C)
        kb.op("pe", lambda: nc.tensor.matmul(ps[:64, :n], lhsT=w2[:, :], rhs=h1[:, :n], start=True, stop=True), reads=[w2, h1], writes=[ps])
        hy_sin(kb, nc, h2, ps, n, fr2, ta, tc_)
        for cch in range(8):
            ps = next_ps(C)
            kb.op("pe", lambda: nc.tensor.matmul(ps[:, :n], lhsT=w3[:, cch * 128:(cch + 1) * 128], rhs=h2[:, :n], start=True, stop=True), reads=[w3, h2], writes=[ps])
            W = win[cch % 2]
            kb.op("act", lambda: nc.scalar.activation(out=W[:, :n], in_=TB[:, :n], func=AF.Exp, scale=dec[:, cch:cch + 1]), reads=[TB, dec], writes=[W])
            HO = ho[oi % 3]
            oi += 1
            kb.op("dve", lambda: nc.vector.scalar_tensor_tensor(out=HO[:, :n], in0=W[:, :n], scalar=HY_SHIFT, in1=ps[:, :n], op0=ALU.add, op1=ALU.mult), reads=[W, ps], writes=[HO])
            if cch >= 4 and o == 0:
                kb.op("dve", lambda: nc.vector.memset(HO[:, 0:1], 0.0), reads=[HO], writes=[HO], waw=True)
            kb.dma("pool", hTd[cch * 128:(cch + 1) * 128, o:o + n], HO[:, :n], reads=[HO])


HY_SHIFT = 0.05


def stage_hy_conv3(kb, C, st, segs, pinT, cwd, cbd, uvT, x0T):
    nc = kb.nc
    cw = kb.sb("hc_w", [128, 3, 12], F32, st)
    cb = kb.sb("hc_b", [128, 12], F32, st)
    kb.dma("sp", cw[:], cwd, writes=[cw])
    kb.dma("sp", cb[:], cbd, writes=[cb])
    P = [kb.sb(f"hc_p{i}", [128, 12, 514], BF16, st) for i in range(2)]
    U = [kb.sb(f"hc_u{i}", [128, 512], F32, st) for i in range(6)]
    O = [kb.sb(f"hc_o{i}", [128, 512], BF16, st) for i in range(4)]
    ti = 0
    ui = 0
    oi = 0
    for (p0, Ls, d0) in segs:
        for o in range(0, Ls, 512):
            n = min(512, Ls - o)
            Pt = P[ti % 2]
            ti += 1
            lo = 1 if o == 0 else 0
            hi = 1 if o + n == Ls else 0
            if lo:
                kb.op("pool", lambda: nc.gpsimd.memset(Pt[:, :, 0:1], 0.0), writes=[Pt])
            if hi:
                kb.op("pool", lambda: nc.gpsimd.memset(Pt[:, :, n + 1:n + 2], 0.0), writes=[Pt])
            kb.dma("sp", Pt[:, :, lo:n + 2 - hi], pinT[0:1536, :].rearrange("(k p) t -> p k t", p=128)[:, :, p0 + o - 1 + lo:p0 + o + n + 1 - hi], writes=[Pt])
            us = []
            for k in range(12):
                Ut = U[ui % 6]
                ui += 1
                kb.op("act", lambda: nc.scalar.activation(out=Ut[:, :n], in_=Pt[:, k, 1:n + 1], func=AF.Identity, scale=cw[:, 1, k:k + 1], bias=cb[:, k:k + 1]), reads=[Pt, cw, cb], writes=[Ut])
                kb.op("dve", lambda: nc.vector.scalar_tensor_tensor(out=Ut[:, :n], in0=Pt[:, k, 0:n], scalar=cw[:, 0, k:k + 1], in1=Ut[:, :n], op0=ALU.mult, op1=ALU.add), reads=[Pt, cw, Ut], writes=[Ut])
                eng = "dve" if k < 4 else "pool"
                e_ = nc.vector if k < 4 else nc.gpsimd
                if k < 4:
                    Ot = O[oi % 4]
                    oi += 1
                    kb.op("dve", lambda: nc.vector.scalar_tensor_tensor(out=Ot[:, :n], in0=Pt[:, k, 2:n + 2], scalar=cw[:, 2, k:k + 1], in1=Ut[:, :n], op0=ALU.mult, op1=ALU.add), reads=[Pt, cw, Ut], writes=[Ot])
                    kb.dma("pool", x0T[k * 128:(k + 1) * 128, d0 + o:d0 + o + n], Ot[:, :n], reads=[Ot])
                else:
                    kb.op("dve", lambda: nc.vector.scalar_tensor_tensor(out=Ut[:, :n], in0=Pt[:, k, 2:n + 2], scalar=cw[:, 2, k:k + 1], in1=Ut[:, :n], op0=ALU.mult, op1=ALU.add), reads=[Pt, cw, Ut], writes=[Ut])
                    us.append(Ut)
                if k >= 8:
                    Ot = O[oi % 4]
                    oi += 1
                    X1 = us[k - 8]
                    kb.op("pool", lambda: nc.gpsimd.tensor_tensor(out=Ot[:, :n], in0=X1[:, :n], in1=Ut[:, :n], op=ALU.mult), reads=[X1, Ut], writes=[Ot])
                    kb.dma("pool", uvT[(k - 8) * 128:(k - 7) * 128, d0 + o:d0 + o + n], Ot[:, :n], reads=[Ot])


def load_fft_consts(kb, C, fad, fbd, twd):
    C.FA = kb.sb("FA", [128, 3, 256], BF16)
    C.FB = kb.sb("FB", [128, 4, 128], BF16)
    C.TW = kb.sb("TW", [128, 3, 128], F32)
    kb.dma("pool", C.FA[:], fad.rearrange("j p n -> p j n"), writes=[C.FA])
    kb.dma("pool", C.FB[:], fbd.rearrange("j p n -> p j n"), writes=[C.FB])
    kb.dma("sp", C.TW[:], twd.rearrange("j p n -> p j n"), writes=[C.TW])


def _twiddle(kb, C, nc, ps, Yr, Yi, c, ti_idx, tmps):
    psv = ps[:, :].rearrange("p (c r k) -> p c r k", c=2, r=2)
    Tr = C.TW[:, 0, :].unsqueeze(1).to_broadcast([128, 2, 128])
    Ti = C.TW[:, ti_idx, :].unsqueeze(1).to_broadcast([128, 2, 128])
    t1, t2, t3, t4 = tmps
    kb.op("dve", lambda: nc.vector.tensor_tensor(out=t1[:, :, :], in0=psv[:, :, 0, :], in1=Tr, op=ALU.mult), reads=[ps, C.TW], writes=[t1])
    kb.op("dve", lambda: nc.vector.tensor_tensor(out=t2[:, :, :], in0=psv[:, :, 1, :], in1=Ti, op=ALU.mult), reads=[ps, C.TW], writes=[t2])
    kb.op("pool", lambda: nc.gpsimd.tensor_tensor(out=Yr[:, c:c + 2, :], in0=t1[:, :, :], in1=t2[:, :, :], op=ALU.subtract), reads=[t1, t2], writes=[Yr])
    kb.op("dve", lambda: nc.vector.tensor_tensor(out=t3[:, :, :], in0=psv[:, :, 0, :], in1=Ti, op=ALU.mult), reads=[ps, C.TW], writes=[t3])
    kb.op("dve", lambda: nc.vector.tensor_tensor(out=t4[:, :, :], in0=psv[:, :, 1, :], in1=Tr, op=ALU.mult), reads=[ps, C.TW], writes=[t4])
    kb.op("pool", lambda: nc.gpsimd.tensor_tensor(out=Yi[:, c:c + 2, :], in0=t3[:, :, :], in1=t4[:, :, :], op=ALU.add), reads=[t3, t4], writes=[Yi])


def stage_hy_fft(kb, C, st, uv, hf, hb, yout, nb):
    nc = kb.nc
    GC = 32
    xin = [kb.sb(f"ff_x{i}", [64, GC, 128], BF16, st) for i in range(3)]
    Yr = [kb.sb(f"ff_yr{i}", [128, GC, 128], BF16, st) for i in range(3)]
    Yi = [kb.sb(f"ff_yi{i}", [128, GC, 128], BF16, st) for i in range(3)]
    Kr = kb.sb("ff_kr", [128, GC, 128], F32, st)
    Ki = kb.sb("ff_ki", [128, GC, 128], F32, st)
    Zr = kb.sb("ff_zr", [128, GC, 128], BF16, st)
    Zi = kb.sb("ff_zi", [128, GC, 128], BF16, st)
    tmpsA = [[kb.sb(f"ff_t{j}_{i}", [128, 2, 128], F32, st) for i in range(4)] for j in range(2)]
    tq = [kb.sb(f"ff_q{i}", [128, 512], F32, st) for i in range(4)]
    yo = [kb.sb(f"ff_yo{i}", [64, GC, 128], BF16, st) for i in range(2)]
    Cm, Sm, Sn, Cn = (C.FB[:, j, :] for j in range(4))
    pi_ = 0
    for g in range(512 // GC):
        c0 = g * GC
        for s, src in enumerate((uv, hf, hb)):
            kb.dma("sp", xin[s][:nb, :, :], src[c0:c0 + GC, :].rearrange("c (b p) -> b c p", p=128), writes=[xin[s]])
        for s in range(3):
            for c in range(0, GC, 2):
                ps = next_ps(C)
                for cc in range(2):
                    kb.op("pe", lambda: nc.tensor.matmul(ps[:, cc * 256:(cc + 1) * 256], lhsT=xin[s][:nb, c + cc, :], rhs=C.FA[:nb, 0, :], start=True, stop=True),
                          reads=[xin[s], C.FA], writes=[ps])
                _twiddle(kb, C, nc, ps, Yr[s], Yi[s], c, 1, tmpsA[pi_ % 2])
                pi_ += 1

        def q(Y, c):
            return Y[:, c:c + 4, :].rearrange("p c k -> p (c k)")
        for c in range(0, GC, 4):
            pr = next_ps(C)
            terms = [(Cm, Yr[1]), (Sm, Yi[1]), (Cm, Yr[2]), (Sm, Yi[2])]
            for i, (F_, Y_) in enumerate(terms):
                kb.op("pe", lambda: nc.tensor.matmul(pr[:, :], lhsT=F_, rhs=q(Y_, c), start=(i == 0), stop=(i == 3)), reads=[C.FB, Y_], writes=[pr])
            kb.op("act", lambda: nc.scalar.copy(out=q(Kr, c), in_=pr[:, :]), reads=[pr], writes=[Kr])
            pim = next_ps(C)
            terms = [(Cm, Yi[1]), (Sn, Yr[1]), (Cn, Yi[2]), (Sm, Yr[2])]
            for i, (F_, Y_) in enumerate(terms):
                kb.op("pe", lambda: nc.tensor.matmul(pim[:, :], lhsT=F_, rhs=q(Y_, c), start=(i == 0), stop=(i == 3)), reads=[C.FB, Y_], writes=[pim])
            kb.op("act", lambda: nc.scalar.copy(out=q(Ki, c), in_=pim[:, :]), reads=[pim], writes=[Ki])
        for c in range(0, GC, 4):
            pr = next_ps(C)
            for i, (F_, Y_) in enumerate([(Cm, Yr[0]), (Sm, Yi[0])]):
                kb.op("pe", lambda: nc.tensor.matmul(pr[:, :], lhsT=F_, rhs=q(Y_, c), start=(i == 0), stop=(i == 1)), reads=[C.FB, Y_], writes=[pr])
            pim = next_ps(C)
            for i, (F_, Y_) in enumerate([(Cm, Yi[0]), (Sn, Yr[0])]):
                kb.op("pe", lambda: nc.tensor.matmul(pim[:, :], lhsT=F_, rhs=q(Y_, c), start=(i == 0), stop=(i == 1)), reads=[C.FB, Y_], writes=[pim])
            kb.op("dve", lambda: nc.vector.tensor_tensor(out=tq[0][:, :], in0=pr[:, :], in1=q(Kr, c), op=ALU.mult), reads=[pr, Kr], writes=[tq[0]])
            kb.op("dve", lambda: nc.vector.tensor_tensor(out=tq[1][:, :], in0=pim[:, :], in1=q(Ki, c), op=ALU.mult), reads=[pim, Ki], writes=[tq[1]])
            kb.op("pool", lambda: nc.gpsimd.tensor_tensor(out=q(Zr, c), in0=tq[0][:, :], in1=tq[1][:, :], op=ALU.subtract), reads=[tq[0], tq[1]], writes=[Zr])
            kb.op("dve", lambda: nc.vector.tensor_tensor(out=tq[2][:, :], in0=pr[:, :], in1=q(Ki, c), op=ALU.mult), reads=[pr, Ki], writes=[tq[2]])
            kb.op("dve", lambda: nc.vector.tensor_tensor(out=tq[3][:, :], in0=pim[:, :], in1=q(Kr, c), op=ALU.mult), reads=[pim, Kr], writes=[tq[3]])
            kb.op("pool", lambda: nc.gpsimd.tensor_tensor(out=q(Zi, c), in0=tq[2][:, :], in1=tq[3][:, :], op=ALU.add), reads=[tq[2], tq[3]], writes=[Zi])
        for c in range(0, GC, 2):
            ps = next_ps(C)
            for cc in range(2):
                kb.op("pe", lambda: nc.tensor.matmul(ps[:, cc * 256:(cc + 1) * 256], lhsT=Zr[:, c + cc, :], rhs=C.FA[:, 1, :], start=True, stop=False), reads=[Zr, C.FA], writes=[ps])
                kb.op("pe", lambda: nc.tensor.matmul(ps[:, cc * 256:(cc + 1) * 256], lhsT=Zi[:, c + cc, :], rhs=C.FA[:, 2, :], start=False, stop=True), reads=[Zi, C.FA], writes=[ps])
            _twiddle(kb, C, nc, ps, Yr[0], Yi[0], c, 2, tmpsA[pi_ % 2])
            pi_ += 1
        YO = yo[g % 2]
        for c in range(0, GC, 4):
            ps = next_ps(C)
            kb.op("pe", lambda: nc.tensor.matmul(ps[:nb, :], lhsT=C.FB[:, 0, 0:nb], rhs=q(Yr[0], c), start=True, stop=False), reads=[C.FB, Yr[0]], writes=[ps])
            kb.op("pe", lambda: nc.tensor.matmul(ps[:nb, :], lhsT=C.FB[:, 2, 0:nb], rhs=q(Yi[0], c), start=False, stop=True), reads=[C.FB, Yi[0]], writes=[ps])
            kb.op("act", lambda: nc.scalar.activation(out=YO[:nb, c:c + 4, :].rearrange("p c k -> p (c k)"), in_=ps[:nb, :], func=AF.Copy, scale=1.0 / 16384.0), reads=[ps], writes=[YO])
        kb.dma("pool", yout[c0:c0 + GC, :].rearrange("c (b p) -> b c p", p=128), YO[:nb, :, :], reads=[YO])


def stage_hy_gate(kb, C, st, tiles, yconv, uvT, x0T, hbd, yT0):
    nc = kb.nc
    hb = kb.sb("hg_b", [128, 4], F32, st)
    kb.dma("sp", hb[:], hbd, writes=[hb])
    A = [kb.sb(f"hg_a{i}", [128, 3, 4, 512], BF16, st) for i in range(2)]
    T_ = [kb.sb(f"hg_t{i}", [128, 512], F32, st) for i in range(2)]
    O = [kb.sb(f"hg_o{i}", [128, 4, 512], BF16, st) for i in range(2)]
    for ti, (d0, n) in enumerate(tiles):
        At, Ot = A[ti % 2], O[ti % 2]
        for j, src in enumerate((yconv, uvT, x0T)):
            kb.dma("sp", At[:, j, :, :n], src.rearrange("(k p) t -> p k t", p=128)[:, :, d0:d0 + n], writes=[At])
        for k in range(4):
            Tt = T_[k % 2]
            kb.op("dve", lambda: nc.vector.scalar_tensor_tensor(out=Tt[:, :n], in0=At[:, 1, k, :n], scalar=hb[:, k:k + 1], in1=At[:, 0, k, :n], op0=ALU.mult, op1=ALU.add),
                  reads=[At, hb], writes=[Tt])
            kb.op("pool", lambda: nc.gpsimd.tensor_tensor(out=Ot[:, k, :n], in0=Tt[:, :n], in1=At[:, 2, k, :n], op=ALU.mult), reads=[Tt, At], writes=[Ot])
        kb.dma("pool", yT0.rearrange("(k p) t -> p k t", p=128)[:, :, d0:d0 + n], Ot[:, :, :n], reads=[Ot])


def stage_gdn_prep(kb, C, st, segs, pinT, cwd, alogd, dtbd, qkvT, gbT):
    nc = kb.nc
    cw = kb.sb("gp_w", [128, 3, 12], F32, st)
    kb.dma("sp", cw[:], cwd, writes=[cw])
    av = kb.sb("gp_av", [8, 2], F32, st)
    kb.dma("sp", av[:, 0:1], alogd.rearrange("(p o) -> p o", o=1), writes=[av])
    kb.dma("sp", av[:, 1:2], dtbd.rearrange("(p o) -> p o", o=1), writes=[av])
    kb.op("act", lambda: nc.scalar.activation(out=av[:, 0:1], in_=av[:, 0:1], func=AF.Exp), reads=[av], writes=[av])
    kb.op("dve", lambda: nc.vector.tensor_scalar(out=av[:, 0:1], in0=av[:, 0:1], scalar1=-1.0, scalar2=None, op0=ALU.mult), reads=[av], writes=[av])
    P = [kb.sb(f"gp_p{i}", [128, 12, 514], BF16, st) for i in range(2)]
    U = [kb.sb(f"gp_u{i}", [128, 512], F32, st) for i in range(4)]
    SQ = [kb.sb(f"gp_sq{i}", [128, 512], BF16, st) for i in range(2)]
    RS = [kb.sb(f"gp_rs{i}", [128, 512], F32, st) for i in range(2)]
    O = [kb.sb(f"gp_o{i}", [128, 512], F32, st) for i in range(4)]
    A8 = [kb.sb(f"gp_a8{i}", [8, 512], BF16, st) for i in range(2)]
    B8 = [kb.sb(f"gp_b8{i}", [8, 512], BF16, st) for i in range(2)]
    G8 = [kb.sb(f"gp_g8{i}", [8, 512], F32, st) for i in range(2)]
    E8 = [kb.sb(f"gp_e8{i}", [8, 512], F32, st) for i in range(2)]
    GT = [kb.sb(f"gp_gt{i}", [128, 16], F32, st) for i in range(3)]
    ti = ui = oi = gi = 0
    for (p0, Ls) in segs:
        for o in range(0, Ls, 512):
            n = min(512, Ls - o)
            Pt = P[ti % 2]
            lo = 1 if o == 0 else 0
            hi = 1 if o + n == Ls else 0
            if lo:
                kb.op("pool", lambda: nc.gpsimd.memset(Pt[:, :, 0:1], 0.0), writes=[Pt])
            if hi:
                kb.op("pool", lambda: nc.gpsimd.memset(Pt[:, :, n + 1:n + 2], 0.0), writes=[Pt])
            kb.dma("sp", Pt[:, :, lo:n + 2 - hi], pinT[1536:3072, :].rearrange("(k p) t -> p k t", p=128)[:, :, p0 + o - 1 + lo:p0 + o + n + 1 - hi], writes=[Pt])
            for k in range(12):
                Ut = U[ui % 4]
                ui += 1
                Ot = O[oi % 4]
                oi += 1
                kb.op("act", lambda: nc.scalar.activation(out=Ut[:, :n], in_=Pt[:, k, 1:n + 1], func=AF.Identity, scale=cw[:, 1, k:k + 1]), reads=[Pt, cw], writes=[Ut])
                kb.op("dve", lambda: nc.vector.scalar_tensor_tensor(out=Ut[:, :n], in0=Pt[:, k, 0:n], scalar=cw[:, 0, k:k + 1], in1=Ut[:, :n], op0=ALU.mult, op1=ALU.add), reads=[Pt, cw, Ut], writes=[Ut])
                kb.op("dve", lambda: nc.vector.scalar_tensor_tensor(out=Ut[:, :n], in0=Pt[:, k, 2:n + 2], scalar=cw[:, 2, k:k + 1], in1=Ut[:, :n], op0=ALU.mult, op1=ALU.add), reads=[Pt, cw, Ut], writes=[Ut])
                if k >= 8:
                    kb.op("act", lambda: nc.scalar.activation(out=Ot[:, :n], in_=Ut[:, :n], func=AF.Silu), reads=[Ut], writes=[Ot])
                else:
                    S_, R_ = SQ[k % 2], RS[k % 2]
                    kb.op("act", lambda: nc.scalar.activation(out=Ut[:, :n], in_=Ut[:, :n], func=AF.Silu), reads=[Ut], writes=[Ut])
                    kb.op("pool", lambda: nc.gpsimd.tensor_tensor(out=S_[:, :n], in0=Ut[:, :n], in1=Ut[:, :n], op=ALU.mult), reads=[Ut], writes=[S_])
                    ps = next_ps(C)
                    kb.op("pe", lambda: nc.tensor.matmul(ps[:, :n], lhsT=C.ones_bf[:], rhs=S_[:, :n], start=True, stop=True), reads=[S_, C.ones_bf], writes=[ps])
                    kb.op("act", lambda: nc.scalar.activation(out=R_[:, :n], in_=ps[:, :n], func=AF.Sqrt, bias=C.eps_t[:, 0:1]), reads=[ps, C.eps_t], writes=[R_])
                    kb.op("dve", lambda: nc.vector.reciprocal(out=R_[:, :n], in_=R_[:, :n]), reads=[R_], writes=[R_])
                    sc_ = (128.0 ** -0.5) if k < 4 else 1.0
                    kb.op("dve", lambda: nc.vector.scalar_tensor_tensor(out=Ot[:, :n], in0=Ut[:, :n], scalar=sc_, in1=R_[:, :n], op0=ALU.mult, op1=ALU.mult), reads=[Ut, R_], writes=[Ot])
                kb.dma("pool", qkvT[k * 128:(k + 1) * 128, p0 + o:p0 + o + n], Ot[:, :n], reads=[Ot])
            a8, b8, g8, e8 = A8[ti % 2], B8[ti % 2], G8[ti % 2], E8[ti % 2]
            kb.dma("sp", a8[:, :n], pinT[3584:3592, p0 + o:p0 + o + n], writes=[a8])
            kb.dma("sp", b8[:, :n], pinT[3592:3600, p0 + o:p0 + o + n], writes=[b8])
            kb.op("act", lambda: nc.scalar.activation(out=g8[:, :n], in_=a8[:, :n], func=AF.Exp, bias=av[:, 1:2]), reads=[a8, av], writes=[g8])
            kb.op("act", lambda: nc.scalar.activation(out=g8[:, :n], in_=g8[:, :n], func=AF.Ln, bias=C.one_t[:8, 0:1]), reads=[g8, C.one_t], writes=[g8])
            kb.op("dve", lambda: nc.vector.tensor_scalar(out=g8[:, :n], in0=g8[:, :n], scalar1=av[:, 0:1], scalar2=None, op0=ALU.mult), reads=[g8, av], writes=[g8])
            kb.op("act", lambda: nc.scalar.activation(out=e8[:, :n], in_=b8[:, :n], func=AF.Sigmoid), reads=[b8], writes=[e8])
            for s in range(n // 128):
                ps = next_ps(C)
                kb.op("pe", lambda: nc.tensor.transpose(ps[:, 0:8], g8[:, s * 128:(s + 1) * 128], C.ident_f[:8, :8]), reads=[g8, C.ident_f], writes=[ps])
                kb.op("pe", lambda: nc.tensor.transpose(ps[:, 8:16], e8[:, s * 128:(s + 1) * 128], C.ident_f[:8, :8]), reads=[e8, C.ident_f], writes=[ps])
                G_ = GT[gi % 3]
                gi += 1
                kb.op("dve", lambda: nc.vector.tensor_copy(out=G_[:, :], in_=ps[:, 0:16]), reads=[ps], writes=[G_])
                kb.dma("pool", gbT[p0 + o + s * 128:p0 + o + (s + 1) * 128, :], G_[:, :], reads=[G_])
            ti += 1


def load_gdn_consts(kb, C, trifd, tribd, ms2d, mi1d):
    C.triF = kb.sb("triF", [64, 64], F32)
    C.triB = kb.sb("triB", [64, 64], F32)
    C.mS2 = kb.sb("mS2", [64, 8, 64], F32)
    C.mI1 = kb.sb("mI1", [64, 8, 64], F32)
    C.ones_f = kb.sb("ones_f", [64, 128], F32)
    C.identI = kb.sb("identI", [64, 8, 64], F32)
    kb.dma("sp", C.triF[:], trifd, writes=[C.triF])
    kb.dma("sp", C.triB[:], tribd, writes=[C.triB])
    kb.dma("sp", C.mS2[:], ms2d, writes=[C.mS2])
    kb.dma("sp", C.mI1[:], mi1d, writes=[C.mI1])
    kb.op("dve", lambda: kb.nc.vector.memset(C.ones_f[:], 1.0), writes=[C.ones_f])
    for u in range(8):
        kb.op("dve", lambda: kb.nc.vector.tensor_copy(out=C.identI[:, u, :], in_=C.ident_f[:64, :64]), reads=[C.ident_f], writes=[C.identI])


def stage_gdn_scan(kb, C, st, fo, bo, qkvT, gbT, ofd, obd):
    nc = kb.nc
    V_ = nc.vector
    X = [[kb.sb(f"gs_x{d}_{i}", [128, 12, 64], F32, st) for i in range(2)] for d in range(2)]
    GB = [kb.sb(f"gs_gb{i}", [64, 2, 8], F32, st) for i in range(2)]
    QT = kb.sb("gs_qt", [128, 8, 64], BF16, st)
    KT = kb.sb("gs_kt", [128, 8, 64], BF16, st)
    Gbc = kb.sb("gs_gbc", [64, 8, 128], F32, st)
    gcs = kb.sb("gs_gc", [64, 8], F32, st)
    sm = kb.sb("gs_sm", [128, 6, 8], F32, st)
    Dm = kb.sb("gs_dm", [64, 8, 64], F32, st)
    D2 = kb.sb("gs_d2", [64, 8, 64], F32, st)
    E1 = kb.sb("gs_e1", [64, 8, 64], F32, st)
    E2 = kb.sb("gs_e2", [64, 8, 64], F32, st)
    EG = kb.sb("gs_eg", [128, 8, 64], F32, st)
    Mm = kb.sb("gs_m", [64, 8, 64], F32, st)
    Nn = kb.sb("gs_n", [64, 8, 64], F32, st)
    AT = kb.sb("gs_at", [64, 8, 64], BF16, st)
    PN = kb.sb("gs_pn", [64, 8, 64], F32, st)
    PM = kb.sb("gs_pm", [64, 8, 64], F32, st)
    XN = [kb.sb(f"gs_xn{i}", [64, 8, 64], F32, st) for i in range(2)]
    XM = [kb.sb(f"gs_xm{i}", [64, 8, 64], F32, st) for i in range(2)]
    TT = kb.sb("gs_tt", [64, 8, 64], BF16, st)
    Kbg = kb.sb("gs_kbg", [64, 8, 128], BF16, st)
    Kd = kb.sb("gs_kd", [64, 8, 128], BF16, st)
    Vb = kb.sb("gs_vb", [64, 8, 128], BF16, st)
    Uu = kb.sb("gs_u", [64, 8, 128], F32, st)
    WT = kb.sb("gs_wt", [128, 8, 64], BF16, st)
    QD = kb.sb("gs_qd", [128, 8, 64], BF16, st)
    Vn = kb.sb("gs_vn", [64, 8, 128], BF16, st)
    Ot = [kb.sb(f"gs_o{i}", [64, 8, 128], F32, st) for i in range(2)]
    S = kb.sb("gs_s", [128, 8, 128], F32, st)
    Sb = kb.sb("gs_sb", [128, 8, 128], BF16, st)
    kb.op("dve", lambda: V_.memset(S[:], 0.0), writes=[S])
    kb.op("pool", lambda: nc.gpsimd.memset(Sb[:], 0.0), writes=[Sb])

    def bc(ap2, shape):
        return ap2.unsqueeze(2).to_broadcast(shape)

    def v3(ps, p=64, w=64):
        return ps[:p, :].rearrange("p (u k) -> p u k", k=w)
    nsteps = len(fo)
    for s in range(nsteps):
        cf, cb = fo[s], bo[s]
        Xd = [X[0][s % 2], X[1][s % 2]]
        G = GB[s % 2]
        for d, c in ((0, cf), (1, cb)):
            kb.dma("sp", Xd[d][:], qkvT.rearrange("(k p) t -> p k t", p=128)[:, :, c * 64:(c + 1) * 64], writes=[Xd[d]])
            kb.dma("sp", G[:, :, d * 4:(d + 1) * 4], gbT[c * 64:(c + 1) * 64, :].rearrange("t (a d h) -> t a d h", a=2, d=2)[:, :, d, :], writes=[G])
        for d in range(2):
            kb.op("act", lambda: nc.scalar.copy(out=QT[:, d * 4:(d + 1) * 4, :], in_=Xd[d][:, 0:4, :]), reads=[Xd[d]], writes=[QT])
            kb.op("pool", lambda: nc.gpsimd.tensor_copy(out=KT[:, d * 4:(d + 1) * 4, :], in_=Xd[d][:, 4:8, :]), reads=[Xd[d]], writes=[KT])
        psK = [next_ps(C), next_ps(C)]
        psV = [next_ps(C), next_ps(C)]
        for u in range(8):
            d, h = divmod(u, 4)
            kb.op("pe", lambda: nc.tensor.transpose(psK[d][:64, h * 128:(h + 1) * 128], Xd[d][:, 4 + h, :], C.ident_f[:]), reads=[Xd[d], C.ident_f], writes=[psK[d]])
            kb.op("pe", lambda: nc.tensor.transpose(psV[d][:64, h * 128:(h + 1) * 128], Xd[d][:, 8 + h, :], C.ident_f[:]), reads=[Xd[d], C.ident_f], writes=[psV[d]])
        kb.op("dve", lambda: V_.tensor_copy(out=Gbc[:], in_=bc(G[:, 0, :], [64, 8, 128])), reads=[G], writes=[Gbc])
        psg = next_ps(C)
        kb.op("pe", lambda: nc.tensor.matmul(psg[:64, 0:4], lhsT=C.triF[:], rhs=G[:, 0, 0:4], start=True, stop=True), reads=[C.triF, G], writes=[psg])
        kb.op("pe", lambda: nc.tensor.matmul(psg[:64, 4:8], lhsT=C.triB[:], rhs=G[:, 0, 4:8], start=True, stop=True), reads=[C.triB, G], writes=[psg])
        kb.op("pe", lambda: nc.tensor.matmul(psg[:, 8:16], lhsT=C.ones_f[:], rhs=G[:, 0, :], start=True, stop=True), reads=[C.ones_f, G], writes=[psg])
        psr = next_ps(C)
        for u in range(8):
            tri = C.triF if u < 4 else C.triB
            kb.op("pe", lambda: nc.tensor.matmul(psr[:, u * 64:(u + 1) * 64], lhsT=Gbc[:, u, :], rhs=tri[:], start=True, stop=True), reads=[Gbc, tri], writes=[psr])
        kb.op("dve", lambda: V_.tensor_copy(out=gcs[:], in_=psg[:64, 0:8]), reads=[psg], writes=[gcs])
        kb.op("dve", lambda: V_.tensor_tensor(out=Dm[:], in0=v3(psr), in1=bc(gcs[:, :], [64, 8, 64]), op=ALU.subtract), reads=[psr, gcs], writes=[Dm])
        kb.op("pool", lambda: nc.gpsimd.tensor_scalar(out=D2[:], in0=Dm[:], scalar1=-1.0, scalar2=0.0, op0=ALU.mult, op1=ALU.min), reads=[Dm], writes=[D2])
        kb.op("dve", lambda: V_.tensor_scalar(out=Dm[:], in0=Dm[:], scalar1=0.0, scalar2=None, op0=ALU.min), reads=[Dm], writes=[Dm])
        kb.op("act", lambda: nc.scalar.activation(out=E1[:], in_=Dm[:], func=AF.Exp), reads=[Dm], writes=[E1])
        kb.op("act", lambda: nc.scalar.activation(out=E2[:], in_=D2[:], func=AF.Exp), reads=[D2], writes=[E2])
        kb.op("act", lambda: nc.scalar.activation(out=EG[:].rearrange("p u k -> p (u k)"), in_=psr[:, :], func=AF.Exp), reads=[psr], writes=[EG])
        kb.op("act", lambda: nc.scalar.activation(out=sm[:64, 0, :], in_=gcs[:, :], func=AF.Exp), reads=[gcs], writes=[sm])
        kb.op("dve", lambda: V_.tensor_tensor(out=sm[:64, 4, :], in0=psg[:64, 8:16], in1=gcs[:, :], op=ALU.subtract), reads=[psg, gcs], writes=[sm])
        kb.op("act", lambda: nc.scalar.activation(out=sm[:64, 1, :], in_=sm[:64, 4, :], func=AF.Exp), reads=[sm], writes=[sm])
        kb.op("act", lambda: nc.scalar.activation(out=sm[:, 2, :], in_=psg[:, 8:16], func=AF.Exp), reads=[psg], writes=[sm])
        kb.op("dve", lambda: V_.tensor_tensor(out=sm[:64, 3, :], in0=sm[:64, 0, :], in1=G[:, 1, :], op=ALU.mult), reads=[sm, G], writes=[sm])
        kb.op("pool", lambda: nc.gpsimd.tensor_tensor(out=E1[:], in0=E1[:], in1=C.mI1[:], op=ALU.mult), reads=[E1, C.mI1], writes=[E1])
        kb.op("pool", lambda: nc.gpsimd.tensor_tensor(out=E2[:], in0=E2[:], in1=C.mS2[:], op=ALU.mult), reads=[E2, C.mS2], writes=[E2])
        for d in range(2):
            pk4 = psK[d][:64, :].rearrange("p (u k) -> p u k", k=128)
            pv4 = psV[d][:64, :].rearrange("p (u k) -> p u k", k=128)
            us = slice(d * 4, (d + 1) * 4)
            kb.op("dve", lambda: V_.tensor_tensor(out=Kbg[:, us, :], in0=pk4, in1=bc(sm[:64, 3, us], [64, 4, 128]), op=ALU.mult), reads=[psK[d], sm], writes=[Kbg])
            kb.op("dve", lambda: V_.tensor_tensor(out=Kd[:, us, :], in0=pk4, in1=bc(sm[:64, 1, us], [64, 4, 128]), op=ALU.mult), reads=[psK[d], sm], writes=[Kd])
            kb.op("dve", lambda: V_.tensor_tensor(out=Vb[:, us, :], in0=pv4, in1=bc(G[:, 1, us], [64, 4, 128]), op=ALU.mult), reads=[psV[d], G], writes=[Vb])
        kb.op("pool", lambda: nc.gpsimd.tensor_tensor(out=QD[:], in0=QT[:], in1=EG[:], op=ALU.mult), reads=[QT, EG], writes=[QD])
        pkk = next_ps(C)
        pqk = next_ps(C)
        for u in range(8):
            kb.op("pe", lambda: nc.tensor.matmul(pkk[:64, u * 64:(u + 1) * 64], lhsT=KT[:, u, :], rhs=KT[:, u, :], start=True, stop=True), reads=[KT], writes=[pkk])
            kb.op("pe", lambda: nc.tensor.matmul(pqk[:64, u * 64:(u + 1) * 64], lhsT=KT[:, u, :], rhs=QT[:, u, :], start=True, stop=True), reads=[KT, QT], writes=[pqk])
        kb.op("dve", lambda: V_.tensor_tensor(out=Mm[:], in0=v3(pkk), in1=E2[:], op=ALU.mult), reads=[pkk, E2], writes=[Mm])
        kb.op("dve", lambda: V_.tensor_tensor(out=Mm[:], in0=Mm[:], in1=bc(G[:, 1, :], [64, 8, 64]), op=ALU.mult), reads=[Mm, G], writes=[Mm])
        kb.op("dve", lambda: V_.tensor_tensor(out=AT[:], in0=v3(pqk), in1=E1[:], op=ALU.mult), reads=[pqk, E1], writes=[AT])
        pn = next_ps(C)
        for u in range(8):
            kb.op("pe", lambda: nc.tensor.transpose(pn[:64, u * 64:(u + 1) * 64], Mm[:, u, :], C.ident_f[:64, :64]), reads=[Mm, C.ident_f], writes=[pn])
        kb.op("act", lambda: nc.scalar.copy(out=Nn[:], in_=v3(pn)), reads=[pn], writes=[Nn])
        SK = ''
        if 'a' not in SK:
            kb.op("dve", lambda: V_.scalar_tensor_tensor(out=PN[:], in0=v3(pn), scalar=-1.0, in1=C.identI[:], op0=ALU.mult, op1=ALU.add), reads=[C.identI, pn], writes=[PN])
        if 'b' not in SK:
            kb.op("pool", lambda: nc.gpsimd.tensor_tensor(out=PM[:], in0=C.identI[:], in1=Mm[:], op=ALU.subtract), reads=[C.identI, Mm], writes=[PM])
        pa, pb = next_ps(C), next_ps(C)
        for u in range(8):
            if 'c' not in SK:
                kb.op("pe", lambda: nc.tensor.matmul(pa[:64, u * 64:(u + 1) * 64], lhsT=Mm[:, u, :], rhs=Nn[:, u, :], start=True, stop=True), reads=[Mm, Nn], writes=[pa])
            if 'd' not in SK:
                kb.op("pe", lambda: nc.tensor.matmul(pb[:64, u * 64:(u + 1) * 64], lhsT=Nn[:, u, :], rhs=Mm[:, u, :], start=True, stop=True), reads=[Mm, Nn], writes=[pb])
        xn, xm = XN[0], XM[0]
        if 'e' not in SK:
            kb.op("act", lambda: nc.scalar.copy(out=xn[:], in_=v3(pa)), reads=[pa], writes=[xn])
        if 'f' not in SK:
            kb.op("dve", lambda: V_.tensor_copy(out=xm[:], in_=v3(pb)), reads=[pb], writes=[xm])
        for lv in range(5):
            last = lv == 4
            pa = next_ps(C)
            for u in range(8):
                kb.op("pe", lambda: nc.tensor.matmul(pa[:64, u * 64:(u + 1) * 64], lhsT=PM[:, u, :], rhs=xn[:, u, :], start=True, stop=True), reads=[PM, xn], writes=[pa])
            if not last:
                pb = next_ps(C)
                for u in range(8):
                    kb.op("pe", lambda: nc.tensor.matmul(pb[:64, u * 64:(u + 1) * 64], lhsT=xn[:, u, :], rhs=PM[:, u, :], start=True, stop=True), reads=[PM, xn], writes=[pb])
                pc, pd = next_ps(C), next_ps(C)
                for u in range(8):
                    kb.op("pe", lambda: nc.tensor.matmul(pc[:64, u * 64:(u + 1) * 64], lhsT=xm[:, u, :], rhs=xn[:, u, :], start=True, stop=True), reads=[xm, xn], writes=[pc])
                    kb.op("pe", lambda: nc.tensor.matmul(pd[:64, u * 64:(u + 1) * 64], lhsT=xn[:, u, :], rhs=xm[:, u, :], start=True, stop=True), reads=[xm, xn], writes=[pd])
                xn2, xm2 = XN[(lv + 1) % 2], XM[(lv + 1) % 2]
                kb.op("act", lambda: nc.scalar.copy(out=xn2[:], in_=v3(pc)), reads=[pc], writes=[xn2])
                kb.op("act", lambda: nc.scalar.copy(out=xm2[:], in_=v3(pd)), reads=[pd], writes=[xm2])
                kb.op("dve", lambda: V_.tensor_tensor(out=PM[:], in0=v3(pb), in1=PM[:], op=ALU.add), reads=[PM, pb, pa], writes=[PM])
                kb.op("dve", lambda: V_.tensor_tensor(out=PN[:], in0=v3(pa), in1=PN[:], op=ALU.add), reads=[PN, pa], writes=[PN])
                xn, xm = xn2, xm2
            else:
                kb.op("dve", lambda: V_.tensor_tensor(out=TT[:], in0=v3(pa), in1=PN[:], op=ALU.add), reads=[PN, pa], writes=[TT])
        pu = [next_ps(C), next_ps(C)]
        pw = next_ps(C)
        for u in range(8):
            d, h = divmod(u, 4)
            kb.op("pe", lambda: nc.tensor.matmul(pu[d][:64, h * 128:(h + 1) * 128], lhsT=TT[:, u, :], rhs=Vb[:, u, :], start=True, stop=True), reads=[TT, Vb], writes=[pu[d]])
            kb.op("pe", lambda: nc.tensor.matmul(pw[:, u * 64:(u + 1) * 64], lhsT=Kbg[:, u, :], rhs=TT[:, u, :], start=True, stop=True), reads=[TT, Kbg], writes=[pw])
        for d in range(2):
            kb.op("act", lambda: nc.scalar.copy(out=Uu[:, d * 4:(d + 1) * 4, :].rearrange("p u k -> p (u k)"), in_=pu[d][:64, :]), reads=[pu[d]], writes=[Uu])
        kb.op("act", lambda: nc.scalar.copy(out=WT[:].rearrange("p u k -> p (u k)"), in_=pw[:, :]), reads=[pw], writes=[WT])
        pws = [next_ps(C), next_ps(C)]
        for u in range(8):
            d, h = divmod(u, 4)
            kb.op("pe", lambda: nc.tensor.matmul(pws[d][:64, h * 128:(h + 1) * 128], lhsT=WT[:, u, :], rhs=Sb[:, u, :], start=True, stop=True), reads=[WT, Sb], writes=[pws[d]])
        for d in range(2):
            kb.op("dve", lambda: V_.scalar_tensor_tensor(out=Vn[:, d * 4:(d + 1) * 4, :].rearrange("p u k -> p (u k)"), in0=pws[d][:64, :], scalar=-1.0,
                                                 in1=Uu[:, d * 4:(d + 1) * 4, :].rearrange("p u k -> p (u k)"), op0=ALU.mult, op1=ALU.add), reads=[Uu, pws[d]], writes=[Vn])
        po = [next_ps(C), next_ps(C)]
        for u in range(8):
            d, h = divmod(u, 4)
            kb.op("pe", lambda: nc.tensor.matmul(po[d][:64, h * 128:(h + 1) * 128], lhsT=QD[:, u, :], rhs=Sb[:, u, :], start=True, stop=False), reads=[QD, Sb], writes=[po[d]])
            kb.op("pe", lambda: nc.tensor.matmul(po[d][:64, h * 128:(h + 1) * 128], lhsT=AT[:, u, :], rhs=Vn[:, u, :], start=False, stop=True), reads=[AT, Vn], writes=[po[d]])
        O_ = Ot[s % 2]
        for d, (c, dst) in enumerate(((cf, ofd), (cb, obd))):
            kb.op("act", lambda: nc.scalar.copy(out=O_[:, d * 4:(d + 1) * 4, :].rearrange("p u k -> p (u k)"), in_=po[d][:64, :]), reads=[po[d]], writes=[O_])
            kb.dma("pool", dst[c * 64:(c + 1) * 64, :], O_[:, d * 4:(d + 1) * 4, :].rearrange("p u k -> p (u k)"), reads=[O_])
        pss = [next_ps(C), next_ps(C)]
        for u in range(8):
            d, h = divmod(u, 4)
            kb.op("pe", lambda: nc.tensor.matmul(pss[d][:, h * 128:(h + 1) * 128], lhsT=Kd[:, u, :], rhs=Vn[:, u, :], start=True, stop=True), reads=[Kd, Vn], writes=[pss[d]])
        kb.op("dve", lambda: V_.tensor_tensor(out=S[:], in0=S[:], in1=bc(sm[:, 2, :], [128, 8, 128]), op=ALU.mult), reads=[S, sm], writes=[S])
        for d in range(2):
            Sv = S[:, d * 4:(d + 1) * 4, :].rearrange("p u k -> p (u k)")
            kb.op("dve", lambda: V_.tensor_tensor(out=Sv, in0=pss[d][:, :], in1=Sv, op=ALU.add), reads=[S, pss[d]], writes=[S])
        kb.op("act", lambda: nc.scalar.copy(out=Sb[:], in_=S[:]), reads=[S], writes=[Sb])


def stage_gdn_out(kb, C, st, tiles, ofd, obd, pinT, gnd, yT1):
    nc = kb.nc
    gbc = kb.sb("go_g", [128, 128], F32, st)
    kb.dma("sp", gbc[:], gnd, writes=[gbc])
    Z = [kb.sb(f"go_z{i}", [128, 4, 512], BF16, st) for i in range(2)]
    ZS = [kb.sb(f"go_zs{i}", [128, 4, 512], F32, st) for i in range(2)]
    OF = [kb.sb(f"go_of{i}", [128, 512], F32, st) for i in range(2)]
    OB = [kb.sb(f"go_ob{i}", [128, 512], F32, st) for i in range(2)]
    junk = kb.sb("go_junk", [128, 4, 128], F32, st)
    ss = [kb.sb(f"go_ss{i}", [128, 4], F32, st) for i in range(2)]
    ON = [kb.sb(f"go_on{i}", [128, 512], F32, st) for i in range(2)]
    Y = [kb.sb(f"go_y{i}", [128, 4, 512], BF16, st) for i in range(2)]
    si = 0
    for ti, (p0, n) in enumerate(tiles):
        Zt, ZSt, Yt = Z[ti % 2], ZS[ti % 2], Y[ti % 2]
        kb.dma("sp", Zt[:, :, :n], pinT[3072:3584, :].rearrange("(k p) t -> p k t", p=128)[:, :, p0:p0 + n], writes=[Zt])
        kb.op("act", lambda: nc.scalar.activation(out=ZSt[:, :, :n], in_=Zt[:, :, :n], func=AF.Silu), reads=[Zt], writes=[ZSt])
        pts = [next_ps(C) for _ in range(4)]
        for s in range(n // 128):
            of_, ob_, ss_, on_ = OF[si % 2], OB[si % 2], ss[si % 2], ON[si % 2]
            si += 1
            r0 = p0 + s * 128
            kb.dma("sp", of_[:], ofd[r0:r0 + 128, :], writes=[of_])
            kb.dma("sp", ob_[:], obd[r0:r0 + 128, :], writes=[ob_])
            kb.op("dve", lambda: nc.vector.tensor_tensor(out=of_[:], in0=of_[:], in1=ob_[:], op=ALU.add), reads=[of_, ob_], writes=[of_])
            for h in range(4):
                kb.op("act", lambda: nc.scalar.activation(out=junk[:, h, :], in_=of_[:, h * 128:(h + 1) * 128], func=AF.Square, accum_out=ss_[:, h:h + 1]), reads=[of_], writes=[junk, ss_], waw=(h == 0))
            kb.op("act", lambda: nc.scalar.activation(out=ss_[:], in_=ss_[:], func=AF.Sqrt, scale=1.0 / 128.0, bias=C.eps_t[:, 0:1]), reads=[ss_, C.eps_t], writes=[ss_])
            kb.op("dve", lambda: nc.vector.reciprocal(out=ss_[:], in_=ss_[:]), reads=[ss_], writes=[ss_])
            for h in range(4):
                kb.op("dve", lambda: nc.vector.scalar_tensor_tensor(out=on_[:, h * 128:(h + 1) * 128], in0=of_[:, h * 128:(h + 1) * 128], scalar=ss_[:, h:h + 1], in1=gbc[:],
                                                                   op0=ALU.mult, op1=ALU.mult), reads=[of_, ss_, gbc], writes=[on_])
            for h in range(4):
                kb.op("pe", lambda: nc.tensor.transpose(pts[h][:, s * 128:(s + 1) * 128], on_[:, h * 128:(h + 1) * 128], C.ident_f[:]), reads=[on_, C.ident_f], writes=[pts[h]])
        for h in range(4):
            kb.op("dve", lambda: nc.vector.tensor_tensor(out=Yt[:, h, :n], in0=pts[h][:, :n], in1=ZSt[:, h, :n], op=ALU.mult), reads=[pts[h], ZSt], writes=[Yt])
        kb.dma("pool", yT1.rearrange("(k p) t -> p k t", p=128)[:, :, p0:p0 + n], Yt[:, :, :n], reads=[Yt])

def rope_consts(S=8192, GW=64):
    nf=8
    inv=(10000.0**(-np.arange(nf,dtype=np.float32)/nf)).astype(np.float32)
    t=np.arange(S); row=(t//GW).astype(np.float32); col=(t%GW).astype(np.float32)
    c32=np.zeros((32,S),np.float32); s32=np.zeros((32,S),np.float32)
    for d in range(32):
        pos=row if d<16 else col
        ang=(pos*inv[d%8]).astype(np.float32)
        c32[d]=np.cos(ang); s32[d]=np.sin(ang)
    Rm=np.zeros((32,32),np.float32)
    for m in range(32):
        if m%16<8: Rm[m,m+8]=-1.0
        else: Rm[m,m-8]=1.0
    r32T=np.ascontiguousarray(Rm.T)
    c96=np.ones((96,S),np.float32); s96=np.zeros((96,S),np.float32)
    c96[64:]=c32; s96[64:]=s32
    R96=np.zeros((96,96),np.float32); R96[64:,64:]=Rm
    return np.stack([c96,s96]),np.stack([c32,s32]),np.ascontiguousarray(R96.T),r32T

def fft_consts():
    import ml_dtypes
    p=np.arange(128)
    ang=2*np.pi*np.outer(p,p)/128.0
    Cm=np.cos(ang); Sm=np.sin(ang)
    FA=np.stack([np.concatenate([Cm,-Sm],1),np.concatenate([Cm,Sm],1),np.concatenate([-Sm,Cm],1)]).astype(np.float32)
    FB=np.stack([Cm,Sm,-Sm,-Cm]).astype(np.float32)
    a2=2*np.pi*np.outer(p,p)/16384.0
    TW=np.stack([np.cos(a2),-np.sin(a2),np.sin(a2)]).astype(np.float32)
    return FA,FB,TW

def hyena_pos(L):
    t=np.linspace(0.0,1.0,L,dtype=np.float32)[:,None]
    w=((2.0*np.pi/L)*np.arange(L,dtype=np.float32))[:,None].astype(np.float32)
    f=np.linspace(1e-4,15,16,dtype=np.float32)[None,:]
    z=np.concatenate([t,np.cos(f*w),-np.sin(f*w)],-1).astype(np.float32)
    return np.ascontiguousarray(z.T), np.ascontiguousarray(t[:,0])

def gdn_consts():
    i=np.arange(64)
    triF=(i[:,None]<=i[None,:]).astype(np.float32)
    triB=(i[:,None]>=i[None,:]).astype(np.float32)
    mS2=np.zeros((64,8,64),np.float32); mI1=np.zeros((64,8,64),np.float32)
    for u in range(8):
        if u<4:
            mS2[:,u,:]=(i[None,:]<i[:,None]); mI1[:,u,:]=(i[None,:]>=i[:,None])
        else:
            mS2[:,u,:]=(i[None,:]>i[:,None]); mI1[:,u,:]=(i[None,:]<=i[:,None])
    return triF,triB,mS2,mI1


CTXL = 256


def build_program(S=8192, final=True, debug=False, nlayers=2):
    T = CTXL + S
    kb = KB()
    C = Ctx()
    nc = kb.nc
    setup_consts(kb, C)
    EI = "ExternalInput"
    d = {}

    def inp(name, shape, dt=F32):
        d[name] = kb.dram(name, shape, dt, EI)
        return d[name]
    xT = inp("xT", [1024, S])
    cxT = inp("cxT", [1024, CTXL])
    c2 = inp("c2", [128, 8, 2])
    inp("w_ada", [2, 1024, 6144]); inp("b_ada_l", [2, 128, 48]); inp("g1_l", [2, 128, 8]); inp("g2_l", [2, 128, 8])
    inp("w_in", [2, 1024, NIN])
    inp("hy_conv_w", [2, 128, 3, 12]); inp("hy_conv_b", [2, 128, 12]); inp("hy_f_w1", [2, 33, 64]); inp("hy_f_b1", [2, 64]); inp("hy_f_w2", [2, 64, 64])
    inp("hy_f_b2", [2, 64]); inp("hy_f_w3", [2, 64, 1024]); inp("hy_f_freq", [2, 64]); inp("hy_decay", [2, 128, 8]); inp("hy_bias", [2, 128, 4])
    inp("hy_out", [2, 512, 1024]); inp("gdn_conv_w", [2, 128, 3, 12]); inp("gdn_a_log", [2, 8]); inp("gdn_dt_bias", [2, 8]); inp("gdn_norm_g", [2, 128, 128])
    inp("gdn_out", [2, 512, 1024]); inp("qg_l", [2, 128, 6]); inp("mla_w_uq", [2, 768, 768]); inp("kvg_l", [2, 128, 2]); inp("mla_w_ukv", [2, 256, 1024])
    inp("mla_out", [2, 512, 1024]); inp("w_out", [2, 1024, 1024]); inp("moe_w1", [2, 16, 1024, 512]); inp("moe_w3", [2, 16, 1024, 512]); inp("moe_w2", [2, 16, 512, 1024])
    inp("router_w", [1024, 16]); inp("router_b", [128, 16]); inp("fg_l", [128, 8])
    inp("r32", [32, 32]); inp("cs32", [2, 32, S]); inp("fad", [3, 128, 256]); inp("fbd", [4, 128, 128]); inp("twd", [3, 128, 128])
    inp("zT_lat", [33, S]); inp("tn_lat", [128, S]); inp("zT_ctx", [33, CTXL]); inp("tn_ctx", [128, CTXL])
    inp("trif", [64, 64]); inp("trib", [64, 64]); inp("ms2", [64, 8, 64]); inp("mi1", [64, 8, 64])
    outT = kb.dram("outT", [1024, S], F32, "ExternalOutput")
    dk = "ExternalOutput" if debug else "Internal"
    pinT = kb.dram("pinT", [NIN, T], BF16, dk)
    XA = kb.dram("XA", [1024, T], F32, dk)
    XB = kb.dram("XB", [1024, T], F32, dk)
    hT_lat = kb.dram("hT_lat", [1024, S], BF16)
    hT_ctx = kb.dram("hT_ctx", [1024, CTXL], BF16)
    uvT = kb.dram("uvT", [512, T], BF16)
    x0T = kb.dram("x0T", [512, T], BF16)
    yconv = kb.dram("yconv", [512, T], BF16)
    yT = kb.dram("yT", [3, 512, T], BF16, dk)
    qkvT = kb.dram("qkvT", [1536, T], F32)
    gbT = kb.dram("gbT", [T, 16], F32)
    ofd = kb.dram("ofd", [T, 512], F32)
    obd = kb.dram("obd", [T, 512], F32)
    qTd = kb.dram("qTd", [8, 96, T], BF16)
    kTd = kb.dram("kTd", [8, 96, T], BF16)
    vd = kb.dram("vd", [8, T, 64], BF16)
    load_fft_consts(kb, C, d["fad"], d["fbd"], d["twd"])
    load_gdn_consts(kb, C, d["trif"], d["trib"], d["ms2"], d["mi1"])
    modv = kb.sb("modv", [128, 48, 2], F32)
    AB = kb.sb("AB", [128, 6, 2, 8], F32)
    rw_sb = kb.sb("rw_sb", [128, 8, 16], F32)
    rb_bc = kb.sb("rb_bc", [128, 16], F32)
    r32 = kb.sb("r32_s", [32, 32], BF16)
    qg = kb.sb("qg_s", [128, 6], F32)
    kvg = kb.sb("kvg_s", [128, 2], F32)
    fg = kb.sb("fg_s", [128, 8], F32)
    kb.dma("sp", rw_sb[:], d["router_w"].rearrange("(k p) e -> p k e", p=128), writes=[rw_sb])
    kb.dma("sp", rb_bc[:], d["router_b"], writes=[rb_bc])
    kb.dma("pool", r32[:], d["r32"], writes=[r32])
    kb.dma("sp", fg[:], d["fg_l"], writes=[fg])
    lat_tiles = [(o, min(512, S - o)) for o in range(0, S, 512)]
    NC_ = T // 64
    fo = list(range(NC_))
    bo = [3, 2, 1, 0] + list(range(NC_ - 1, 3, -1))

    def stage():
        kb.barrier()
        return ExitStack()
    for l in range(nlayers):
        first = l == 0
        upd = first
        xsrc, cxsrc = (xT, cxT) if first else (XB[:, CTXL:], XB[:, 0:CTXL])
        st = stage()
        stage_mod(kb, C, st, c2, d["w_ada"][l], d["b_ada_l"][l], d["g1_l"][l], d["g2_l"][l], modv, AB)
        kb.dma("sp", qg[:], d["qg_l"][l], writes=[qg])
        kb.dma("sp", kvg[:], d["kvg_l"][l], writes=[kvg])
        st.close()
        st = stage()
        w_sb = kb.sb("w_sb", [128, 8, NIN], BF16, st)
        for k in range(8):
            kb.dma("pool", w_sb[:, k, :], d["w_in"][l][k * 128:(k + 1) * 128, :], writes=[w_sb])
        tiles = [(cxsrc, 0, CTXL, 0, 1)] + [(xsrc, o, n, CTXL + o, 0) for (o, n) in lat_tiles]
        stage_in(kb, C, st, tiles, AB, w_sb, pinT)
        st.close()
        st = stage()
        stage_hy_filter(kb, C, st, S, d["zT_lat"], d["tn_lat"], d["hy_f_w1"][l], d["hy_f_b1"][l], d["hy_f_w2"][l], d["hy_f_b2"][l], d["hy_f_w3"][l],
                        d["hy_f_freq"][l], d["hy_decay"][l], hT_lat)
        st.close()
        if upd:
            st = stage()
            stage_hy_filter(kb, C, st, CTXL, d["zT_ctx"], d["tn_ctx"], d["hy_f_w1"][l], d["hy_f_b1"][l], d["hy_f_w2"][l], d["hy_f_b2"][l], d["hy_f_w3"][l],
                            d["hy_f_freq"][l], d["hy_decay"][l], hT_ctx)
            st.close()
        st = stage()
        segs = ([(0, CTXL, 0)] if upd else []) + [(CTXL, S, CTXL)]
        stage_hy_conv3(kb, C, st, segs, pinT, d["hy_conv_w"][l], d["hy_conv_b"][l], uvT, x0T)
        st.close()
        st = stage()
        stage_hy_fft(kb, C, st, uvT[:, CTXL:], hT_lat[0:512, :], hT_lat[512:1024, :], yconv[:, CTXL:], S // 128)
        st.close()
        if upd:
            st = stage()
            stage_hy_fft(kb, C, st, uvT[:, 0:CTXL], hT_ctx[0:512, :], hT_ctx[512:1024, :], yconv[:, 0:CTXL], CTXL // 128)
            st.close()
        st = stage()
        gt = ([(0, CTXL)] if upd else []) + [(CTXL + o, n) for (o, n) in lat_tiles]
        stage_hy_gate(kb, C, st, gt, yconv, uvT, x0T, d["hy_bias"][l], yT[0])
        st.close()
        st = stage()
        stage_gdn_prep(kb, C, st, [(0, CTXL), (CTXL, S)], pinT, d["gdn_conv_w"][l], d["gdn_a_log"][l], d["gdn_dt_bias"][l], qkvT, gbT)
        st.close()
        st = stage()
        stage_gdn_scan(kb, C, st, fo, bo, qkvT, gbT, ofd, obd)
        st.close()
        st = stage()
        stage_gdn_out(kb, C, st, gt, ofd, obd, pinT, d["gdn_norm_g"][l], yT[1])
        st.close()
        st = stage()
        wuq = kb.sb("wuq_s", [128, 6, 768], BF16, st)
        wukv = kb.sb("wukv_s", [128, 2, 1024], BF16, st)
        kb.dma("pool", wuq[:], d["mla_w_uq"][l].rearrange("(k p) n -> p k n", p=128), writes=[wuq])
        kb.dma("pool", wukv[:], d["mla_w_ukv"][l].rearrange("(k p) n -> p k n", p=128), writes=[wukv])
        mt = [(0, CTXL, False, 0)] + [(CTXL + o, n, True, o) for (o, n) in lat_tiles]
        stage_mla_prep(kb, C, st, mt, pinT, qg, kvg, wuq, wukv, None, r32, None, d["cs32"], qTd, kTd, vd)
        st.close()
        st = stage()
        jobs = [(CTXL, S, list(range(T // 128)))] + ([(0, CTXL, [0, 1])] if upd else [])
        stage_mla_attn(kb, C, st, jobs, qTd, kTd, vd, yT[2], T)
        st.close()
        st = stage()
        w3s = kb.sb("w3s", [128, 3, 4, 1024], BF16, st)
        wos = kb.sb("wos", [128, 8, 1024], BF16, st)
        for j, nm in enumerate(("hy_out", "gdn_out", "mla_out")):
            kb.dma("pool", w3s[:, j], d[nm][l].rearrange("(k p) n -> p k n", p=128), writes=[w3s])
        kb.dma("pool", wos[:], d["w_out"][l].rearrange("(k p) n -> p k n", p=128), writes=[wos])
        mtiles = ([(cxsrc, 0, CTXL, 0, 1, XA[:, 0:CTXL])] if upd else []) + [(xsrc, o, n, CTXL + o, 0, XA[:, CTXL:]) for (o, n) in lat_tiles]
        stage_merge(kb, C, st, mtiles, AB, yT, pinT, w3s, wos, None)
        st.close()
        st = stage()
        supers = ([(XA[:, 0:CTXL], 0, CTXL, 1, XB[:, 0:CTXL])] if upd else []) + [(XA[:, CTXL:], o, min(1024, S - o), 0, XB[:, CTXL:]) for o in range(0, S, 1024)]
        stage_moe(kb, C, st, supers, AB, rw_sb, rb_bc, d["moe_w1"][l], d["moe_w3"][l], d["moe_w2"][l])
        st.close()
    st = stage()
    X = [kb.sb(f"fn_x{i}", [128, 8, 512], F32, st) for i in range(2)]
    SQ = kb.sb("fn_sq", [128, 8, 512], BF16, st)
    RS = kb.sb("fn_rs", [128, 512], F32, st)
    tmp = [kb.sb(f"fn_t{i}", [128, 512], F32, st) for i in range(2)]
    H = [kb.sb(f"fn_h{i}", [128, 8, 512], F32, st) for i in range(2)]
    src = XB[:, CTXL:].rearrange("(k p) t -> p k t", p=128)
    dst = outT.rearrange("(k p) t -> p k t", p=128)
    for ti, (o, n) in enumerate(lat_tiles):
        Xt, Ht = X[ti % 2], H[ti % 2]
        kb.dma("sp", Xt[:, :, :n], src[:, :, o:o + n], writes=[Xt])
        rmsnorm_tile(kb, C, Xt, n, PV(lambda k: fg[:, k:k + 1], [fg]), None, 0, Ht, SQ, RS, tmp)
        kb.dma("pool", dst[:, :, o:o + n], Ht[:, :, :n], reads=[Ht])
    kb.finish()
    st.close()
    return kb


def _lay(v, k):
    return np.ascontiguousarray(np.asarray(v, np.float32).reshape(k, 128).T)


def make_inputs(inputs, b, S=8192):
    f = lambda a: np.ascontiguousarray(np.asarray(a, np.float32))
    x = np.asarray(inputs["x"][b], np.float32)
    im = {}
    im["xT"] = np.ascontiguousarray(x.T)
    im["cxT"] = np.ascontiguousarray(np.asarray(inputs["ctx"][b], np.float32).T)
    im["c2"] = np.ascontiguousarray(np.stack([_lay(inputs["c"][b], 8), _lay(inputs["c_ctx"], 8)], -1))
    return im


def shared_inputs(inputs, S=8192):
    f = lambda a: np.ascontiguousarray(np.asarray(a, np.float32))
    sh = {}
    for nm in ("w_ada", "w_in", "hy_f_w1", "hy_f_b1", "hy_f_w2", "hy_f_b2", "hy_f_w3", "hy_f_freq", "hy_out",
               "gdn_out", "mla_w_uq", "mla_w_ukv", "mla_out", "w_out", "moe_w1", "moe_w3", "moe_w2", "router_w"):
        sh[nm] = f(inputs[nm])
    cwl = lambda w: np.ascontiguousarray(f(w).reshape(2, 3, 12, 128).transpose(0, 3, 1, 2))
    vl = lambda v, k: np.ascontiguousarray(f(v).reshape(2, k, 128).transpose(0, 2, 1))
    sh["hy_conv_w"] = cwl(inputs["hy_conv_w"]); sh["gdn_conv_w"] = cwl(inputs["gdn_conv_w"])
    sh["hy_conv_b"] = vl(inputs["hy_conv_b"], 12); sh["hy_decay"] = vl(inputs["hy_decay"], 8); sh["hy_bias"] = vl(inputs["hy_bias"], 4)
    sh["gdn_norm_g"] = np.ascontiguousarray(np.broadcast_to(f(inputs["gdn_norm_g"])[:, None, :], (2, 128, 128)))
    sh["router_b"] = np.ascontiguousarray(np.broadcast_to(f(inputs["router_b"])[None, :], (128, 16)))
    sh["b_ada_l"] = np.stack([np.ascontiguousarray(f(inputs["b_ada"])[l].reshape(48, 128).T) for l in range(2)])
    sh["g1_l"] = np.stack([_lay(inputs["norm1_g"][l], 8) for l in range(2)])
    sh["g2_l"] = np.stack([_lay(inputs["norm2_g"][l], 8) for l in range(2)])
    sh["qg_l"] = np.stack([_lay(inputs["mla_q_norm_g"][l], 6) for l in range(2)])
    sh["kvg_l"] = np.stack([_lay(inputs["mla_kv_norm_g"][l], 2) for l in range(2)])
    sh["fg_l"] = _lay(inputs["final_norm_g"], 8)
    sh["gdn_a_log"] = f(inputs["gdn_a_log"]).reshape(2, 8)
    sh["gdn_dt_bias"] = f(inputs["gdn_dt_bias"]).reshape(2, 8)
    cs96, cs32, r96T, r32T = rope_consts(S)
    sh["r32"] = r32T
    sh["cs32"] = cs32
    FA, FB, TW = fft_consts()
    sh["fad"], sh["fbd"], sh["twd"] = FA, FB, TW
    sh["zT_lat"], tl_ = hyena_pos(S)
    sh["zT_ctx"], tc_ = hyena_pos(CTXL)
    sh["tn_lat"] = np.ascontiguousarray(np.broadcast_to(tl_[None, :], (128, S)))
    sh["tn_ctx"] = np.ascontiguousarray(np.broadcast_to(tc_[None, :], (128, CTXL)))
    sh["trif"], sh["trib"], sh["ms2"], sh["mi1"] = gdn_consts()
    sh["ident_f_d"] = np.eye(128, dtype=np.float32)
    return sh


def kernel(**inputs):
    x = np.asarray(inputs["x"])
    B, S, D_ = x.shape
    kb = build_program(S)
    sh = shared_inputs(inputs, S)
    in_maps = []
    for b in range(B):
        im = dict(sh)
        im.update(make_inputs(inputs, b, S))
        in_maps.append(im)
    res = run_bass_kernel_spmd(kb.nc, in_maps, core_ids=list(range(B)))
    out = np.stack([np.ascontiguousarray(np.asarray(r["outT"], np.float32).T) for r in res.results], 0)
    return out.astype(np.float32)
```

```python
import numpy as np
from contextlib import ExitStack
import concourse.bass as bass
import concourse.mybir as mybir
from concourse.bass_utils import run_bass_kernel_spmd

F32 = mybir.dt.float32
BF16 = mybir.dt.bfloat16
AF = mybir.ActivationFunctionType
ALU = mybir.AluOpType
AX = mybir.AxisListType


class Buf:
    __slots__ = ("t", "w", "r", "pr", "excl")

    def __init__(self, t=None, excl=False):
        self.t = t
        self.w = []
        self.r = []
        self.pr = []
        self.excl = excl

    def __getitem__(self, k):
        return self.t[k]


class KB:
    SEM_EPOCH = 20000
    NDMA = 10

    def __init__(self):
        self.nc = bass.Bass("TRN2", target_bir_lowering=False)
        nc = self.nc
        self.es = ExitStack()
        self.eng = {"pe": nc.tensor, "act": nc.scalar, "dve": nc.vector, "pool": nc.gpsimd, "sp": nc.sync}
        self.csem = {}
        self.seen = {e: {} for e in self.eng}
        self.dpool = {}
        self.nsem = 0
        self.last_tok = {}
        self.out_toks = []
        self.ninst = 0

    def _newsem(self, name):
        self.nsem += 1
        return self.es.enter_context(self.nc.semaphore(f"{name}_{self.nsem}"))

    def sb(self, name, shape, dt, stack=None):
        self.nsem += 1
        name = f"{name}_u{self.nsem}"
        t = (stack or self.es).enter_context(self.nc.sbuf_tensor(name, list(shape), dt))
        return Buf(t)

    def ps(self, name, shape=(128, 512), dt=F32, stack=None):
        t = (stack or self.es).enter_context(self.nc.psum_tensor(name, list(shape), dt))
        return Buf(t, excl=True)

    def dram(self, name, shape, dt, kind="Internal"):
        return self.nc.dram_tensor(name, list(shape), dt, kind=kind).ap()

    def _wait(self, e, toks):
        en = self.eng[e]
        best = {}
        for (s, v) in toks:
            if best.get(s, (None, 0))[1] < v:
                best[s] = (s, v)
        for s, v in best.values():
            if self.seen[e].get(s.num, 0) < v:
                en.wait_ge(s, v)
                self.seen[e][s.num] = v
                self.ninst += 1

    def _deps(self, e, reads, writes, waw):
        toks = []
        for b in reads:
            toks.extend(b.w)
            if getattr(b, "excl", False):
                toks.extend(b.r)
        for b in writes:
            toks.extend(b.r)
            toks.extend(b.pr)
            if waw or b.r:
                toks.extend(b.w)
        return toks

    def _commit(self, tok, reads, writes):
        for b in writes:
            if b.r:
                b.pr = list(b.r) + list(b.w)
                b.w = [tok]
                b.r = []
            else:
                b.w = [t for t in b.w if t[0] is not tok[0]] + [tok]
        for b in reads:
            if b not in writes:
                b.r = [t for t in b.r if t[0] is not tok[0]] + [tok]

    def op(self, e, fn, reads=(), writes=(), waw=False):
        toks = self._deps(e, reads, writes, waw)
        if e == "pe":
            mysem = self.csem.get("pe")
            if mysem is not None:
                toks = [t for t in toks if t[0] is not mysem[0]]
        self._wait(e, toks)
        s = self.csem.get(e)
        if s is None or s[1] >= self.SEM_EPOCH:
            s = [self._newsem("c" + e), 0]
            self.csem[e] = s
        ins = fn()
        s[1] += 1
        ins.then_inc(s[0], 1)
        tok = (s[0], s[1])
        self.last_tok[e] = tok
        self._commit(tok, reads, writes)
        self.ninst += 1
        return tok

    def dma(self, q, out, in_, reads=(), writes=(), waw=False, **kw):
        toks = self._deps(q, reads, writes, waw)
        pool = self.dpool.setdefault(q, {"sems": [], "uses": [], "i": 0})
        if len(pool["sems"]) < self.NDMA:
            pool["sems"].append(self._newsem("d" + q))
            pool["uses"].append(0)
            k = len(pool["sems"]) - 1
        else:
            k = pool["i"] % self.NDMA
        pool["i"] += 1
        s = pool["sems"][k]
        u = pool["uses"][k]
        if u > 0:
            toks.append((s, 16 * u))
        self._wait(q, toks)
        ins = self.eng[q].dma_start(out=out, in_=in_, **kw)
        ins.then_inc(s, 16)
        pool["uses"][k] = u + 1
        tok = (s, 16 * (u + 1))
        self._commit(tok, reads, writes)
        self.ninst += 1
        return tok

    def all_tokens(self):
        toks = list(self.last_tok.values())
        for q, pool in self.dpool.items():
            for s, u in zip(pool["sems"], pool["uses"]):
                if u > 0:
                    toks.append((s, 16 * u))
        return toks

    def barrier(self):
        toks = self.all_tokens()
        for e in self.eng:
            self._wait(e, toks)

    def finish(self):
        self.barrier()


D = 1024
NIN = 7728
IN_SEGS = [("hy", 0, 1536), ("qkv", 1536, 1536), ("z", 3072, 512), ("ab", 3584, 16), ("cq", 3600, 768),
           ("ckv", 4368, 256), ("kr", 4624, 32), ("gate", 4656, 3072)]


def mchunks():
    out = []
    for name, c0, w in IN_SEGS:
        o = 0
        while o < w:
            m = min(128, w - o)
            out.append((name, c0 + o, m))
            o += m
    return out


class Ctx:
    pass


def setup_consts(kb, C):
    C.ones_bf = kb.sb("ones_bf", [128, 128], BF16)
    kb.op("dve", lambda: kb.nc.vector.memset(C.ones_bf[:], 1.0), writes=[C.ones_bf])
    C.eps_t = kb.sb("eps_t", [128, 1], F32)
    kb.op("dve", lambda: kb.nc.vector.memset(C.eps_t[:], 1e-6), writes=[C.eps_t])
    C.one_t = kb.sb("one_t", [128, 1], F32)
    kb.op("dve", lambda: kb.nc.vector.memset(C.one_t[:], 1.0), writes=[C.one_t])
    C.psum = [kb.ps(f"ps{i}") for i in range(8)]
    C.ident_f = kb.sb("ident_f", [128, 128], F32)
    C.ident_d = kb.dram("ident_f_d", [128, 128], F32, "ExternalInput")
    kb.dma("sp", C.ident_f[:], C.ident_d, writes=[C.ident_f])
    C.psi = 0


def next_ps(C):
    p = C.psum[C.psi % 8]
    C.psi += 1
    return p


def load_w_bf16(kb, dst, dst_ap, src_ap, q="pool"):
    return kb.dma(q, dst_ap, src_ap, writes=[dst])


def stage_in(kb, C, st, tiles, AB, w_sb, pinT):
    nc = kb.nc
    NB = 2
    xt = [kb.sb(f"in_x{i}", [128, 8, 512], F32, st) for i in range(1)]
    sq = [kb.sb(f"in_sq{i}", [128, 8, 512], BF16, st) for i in range(1)]
    rs = [kb.sb(f"in_rs{i}", [128, 512], F32, st) for i in range(NB)]
    tmp = [kb.sb(f"in_tmp{i}", [128, 512], F32, st) for i in range(4)]
    hT = [kb.sb(f"in_h{i}", [128, 8, 512], BF16, st) for i in range(NB)]
    ob = [kb.sb(f"in_o{i}", [128, 512], BF16, st) for i in range(6)]
    mcs = mchunks()
    oi = 0
    for ti, (src, t0, n, d0, which) in enumerate(tiles):
        b = ti % NB
        X, SQ, RS, H = xt[0], sq[0], rs[b], hT[b]
        kb.dma("sp", X[:, :, :n], src.rearrange("(k p) t -> p k t", p=128)[:, :, t0:t0 + n], writes=[X])
        rmsnorm_tile(kb, C, X, n, PV(lambda k: AB[:, 0, which, k:k + 1], [AB]), PV(lambda k: AB[:, 1, which, k:k + 1], [AB]), which, H, SQ, RS, tmp)
        for mi, (name, c0, m) in enumerate(mcs):
            ps = next_ps(C)
            for k in range(8):
                kb.op("pe", lambda: nc.tensor.matmul(ps[:m, :n], lhsT=w_sb[:, k, c0:c0 + m], rhs=H[:, k, :n], start=(k == 0), stop=(k == 7)),
                      reads=[H, w_sb], writes=[ps])
            O = ob[oi % 6]
            oi += 1
            if name == "gate":
                kb.op("act", lambda: nc.scalar.activation(out=O[:m, :n], in_=ps[:m, :n], func=AF.Sigmoid), reads=[ps], writes=[O])
            elif mi % 2 == 0:
                kb.op("dve", lambda: nc.vector.tensor_copy(out=O[:m, :n], in_=ps[:m, :n]), reads=[ps], writes=[O])
            else:
                kb.op("act", lambda: nc.scalar.copy(out=O[:m, :n], in_=ps[:m, :n]), reads=[ps], writes=[O])
            kb.dma("pool", pinT[c0:c0 + m, d0:d0 + n], O[:m, :n], reads=[O])


def stage_mod(kb, C, st, c2, wada, bada, g1, g2, modv, AB):
    nc = kb.nc
    sc = kb.sb("mod_sc", [128, 8, 2], F32, st)
    ba = kb.sb("mod_ba", [128, 48], F32, st)
    gg = kb.sb("mod_g", [128, 2, 8], F32, st)
    wa = [kb.sb(f"mod_wa{i}", [128, 8, 1536], F32, st) for i in range(2)]
    kb.dma("sp", sc[:], c2, writes=[sc])
    kb.dma("sp", ba[:], bada, writes=[ba])
    kb.dma("sp", gg[:, 0, :], g1, writes=[gg])
    kb.dma("sp", gg[:, 1, :], g2, writes=[gg])
    kb.op("act", lambda: nc.scalar.activation(out=sc[:], in_=sc[:], func=AF.Silu), reads=[sc], writes=[sc])
    for cg in range(4):
        W = wa[cg % 2]
        kb.dma("sp", W[:], wada.rearrange("(k p) n -> p k n", p=128)[:, :, cg * 1536:(cg + 1) * 1536], writes=[W])
        for m in range(12):
            j = cg * 12 + m
            ps = next_ps(C)
            for k in range(8):
                kb.op("pe", lambda: nc.tensor.matmul(ps[:, 0:2], lhsT=W[:, k, m * 128:(m + 1) * 128], rhs=sc[:, k, :], start=(k == 0), stop=(k == 7)),
                      reads=[W, sc], writes=[ps])
            kb.op("dve", lambda: nc.vector.tensor_scalar(out=modv[:, j, :], in0=ps[:, 0:2], scalar1=ba[:, j:j + 1], scalar2=None, op0=ALU.add),
                  reads=[ps, ba], writes=[modv])
    for which in range(2):
        for half, gi in ((0, 0), (1, 1)):
            o = half * 24
            kb.op("dve", lambda: nc.vector.scalar_tensor_tensor(out=AB[:, half * 3 + 0, which, :], in0=modv[:, o + 8:o + 16, which], scalar=1.0, in1=gg[:, gi, :],
                                                               op0=ALU.add, op1=ALU.mult), reads=[modv, gg], writes=[AB])
            kb.op("dve", lambda: nc.vector.tensor_copy(out=AB[:, half * 3 + 1, which, :], in_=modv[:, o:o + 8, which]), reads=[modv], writes=[AB])
            kb.op("dve", lambda: nc.vector.tensor_copy(out=AB[:, half * 3 + 2, which, :], in_=modv[:, o + 16:o + 24, which]), reads=[modv], writes=[AB])


def rmsnorm_tile(kb, C, X, n, Avec, Bvec, which, H, SQ, RS, tmp, Hf=None, nk=8, dim=D):
    nc = kb.nc
    kb.op("act", lambda: nc.scalar.activation(out=SQ[:, :nk, :n], in_=X[:, :nk, :n], func=AF.Square), reads=[X], writes=[SQ])
    pss = next_ps(C)
    for k in range(nk):
        kb.op("pe", lambda: nc.tensor.matmul(pss[:, :n], lhsT=C.ones_bf[:], rhs=SQ[:, k, :n], start=(k == 0), stop=(k == nk - 1)),
              reads=[SQ, C.ones_bf], writes=[pss])
    kb.op("act", lambda: nc.scalar.activation(out=RS[:, :n], in_=pss[:, :n], func=AF.Sqrt, scale=1.0 / dim, bias=C.eps_t[:, 0:1]), reads=[pss, C.eps_t], writes=[RS])
    kb.op("dve", lambda: nc.vector.reciprocal(out=RS[:, :n], in_=RS[:, :n]), reads=[RS], writes=[RS])
    for k in range(nk):
        T = tmp[k % len(tmp)]
        kb.op("dve", lambda: nc.vector.scalar_tensor_tensor(out=T[:, :n], in0=X[:, k, :n], scalar=Avec(k), in1=RS[:, :n],
                                                           op0=ALU.mult, op1=ALU.mult), reads=[X, RS] + Avec.bufs, writes=[T])
        if Bvec is not None:
            kb.op("act", lambda: nc.scalar.activation(out=H[:, k, :n], in_=T[:, :n], func=AF.Identity, bias=Bvec(k)),
                  reads=[T] + Bvec.bufs, writes=[H])
            if Hf is not None:
                kb.op("act", lambda: nc.scalar.activation(out=Hf[:, k, :n], in_=T[:, :n], func=AF.Identity, bias=Bvec(k)),
                      reads=[T] + Bvec.bufs, writes=[Hf])
        else:
            kb.op("act", lambda: nc.scalar.copy(out=H[:, k, :n], in_=T[:, :n]), reads=[T], writes=[H])


class PV:
    def __init__(self, fn, bufs):
        self.fn = fn
        self.bufs = bufs

    def __call__(self, k):
        return self.fn(k)


def stage_merge(kb, C, st, tiles, AB, yT, pinT, w3, wout, xoutT):
    nc = kb.nc
    NB = 2
    xt = [kb.sb(f"mg_x{i}", [128, 8, 512], F32, st) for i in range(NB)]
    yy = [kb.sb(f"mg_y{i}", [128, 3, 4, 512], BF16, st) for i in range(NB)]
    gt = [kb.sb(f"mg_g{i}", [128, 24, 512], BF16, st) for i in range(1)]
    mg = [kb.sb(f"mg_m{i}", [128, 8, 512], BF16, st) for i in range(NB)]
    tt = [kb.sb(f"mg_t{i}", [128, 3, 512], F32, st) for i in range(2)]
    xo = [kb.sb(f"mg_xo{i}", [128, 8, 512], F32, st) for i in range(1)]
    for ti, (src, t0, n, p0, which, dst) in enumerate(tiles):
        b = ti % NB
        X, Y, G, M, XO = xt[b], yy[b], gt[0], mg[b], xo[0]
        kb.dma("sp", X[:, :, :n], src.rearrange("(k p) t -> p k t", p=128)[:, :, t0:t0 + n], writes=[X])
        for j in range(3):
            kb.dma("sp", Y[:, j, :, :n], yT[j].rearrange("(k p) t -> p k t", p=128)[:, :, p0:p0 + n], writes=[Y])
        kb.dma("sp", G[:, :, :n], pinT[4656:7728, :].rearrange("(k p) t -> p k t", p=128)[:, :, p0:p0 + n], writes=[G])
        for m in range(8):
            TT = tt[m % 2]
            pss = []
            for j in range(3):
                ps = next_ps(C)
                pss.append(ps)
                for k in range(4):
                    kb.op("pe", lambda: nc.tensor.matmul(ps[:, :n], lhsT=w3[:, j, k, m * 128:(m + 1) * 128], rhs=Y[:, j, k, :n], start=(k == 0), stop=(k == 3)),
                          reads=[Y, w3], writes=[ps])
            for j in range(3):
                kb.op("dve", lambda: nc.vector.tensor_tensor(out=TT[:, j, :n], in0=pss[j][:, :n], in1=G[:, j * 8 + m, :n], op=ALU.mult),
                      reads=[pss[j], G], writes=[TT])
            kb.op("pool", lambda: nc.gpsimd.tensor_tensor(out=TT[:, 0, :n], in0=TT[:, 0, :n], in1=TT[:, 1, :n], op=ALU.add), reads=[TT], writes=[TT])
            kb.op("pool", lambda: nc.gpsimd.tensor_tensor(out=M[:, m, :n], in0=TT[:, 0, :n], in1=TT[:, 2, :n], op=ALU.add), reads=[TT], writes=[M])
        for m in range(8):
            ps = next_ps(C)
            for k in range(8):
                kb.op("pe", lambda: nc.tensor.matmul(ps[:, :n], lhsT=wout[:, k, m * 128:(m + 1) * 128], rhs=M[:, k, :n], start=(k == 0), stop=(k == 7)),
                      reads=[M, wout], writes=[ps])
            kb.op("dve", lambda: nc.vector.scalar_tensor_tensor(out=XO[:, m, :n], in0=ps[:, :n], scalar=AB[:, 2, which, m:m + 1], in1=X[:, m, :n],
                                                               op0=ALU.mult, op1=ALU.add), reads=[ps, AB, X], writes=[XO])
        kb.dma("pool", dst.rearrange("(k p) t -> p k t", p=128)[:, :, t0:t0 + n], XO[:, :, :n], reads=[XO])


def stage_moe(kb, C, st, supers, AB, rw_sb, rb_bc, w1d, w3d, w2d):
    nc = kb.nc
    X = kb.sb("moe_x", [128, 8, 512], F32, st)
    SQ = kb.sb("moe_sq", [128, 8, 512], BF16, st)
    RS = kb.sb("moe_rs", [128, 512], F32, st)
    tmp = [kb.sb(f"moe_tmp{i}", [128, 512], F32, st) for i in range(2)]
    Hf = kb.sb("moe_hf", [128, 8, 512], F32, st)
    Hb = kb.sb("moe_hb", [128, 8, 1024], BF16, st)
    acc = kb.sb("moe_acc", [128, 8, 1024], F32, st)
    gate = kb.sb("moe_gate", [128, 8, 16], F32, st)
    rt = [kb.sb(f"moe_rt{i}", [128, 64], F32, st) for i in range(2)]
    w1 = [kb.sb(f"moe_w1_{i}", [128, 8, 512], BF16, st) for i in range(2)]
    w3 = [kb.sb(f"moe_w3_{i}", [128, 8, 512], BF16, st) for i in range(2)]
    w2 = [kb.sb(f"moe_w2_{i}", [128, 4, 1024], BF16, st) for i in range(2)]
    he = [kb.sb(f"moe_he{i}", [128, 4, 512], BF16, st) for i in range(2)]
    sl = [kb.sb(f"moe_sl{i}", [128, 512], F32, st) for i in range(2)]
    xo = [kb.sb(f"moe_xo{i}", [128, 8, 512], F32, st) for i in range(1)]
    wi = 0
    for (src, t0, n, which, dst) in supers:
        tl = [(o, min(512, n - o)) for o in range(0, n, 512)]
        srcv = src.rearrange("(k p) t -> p k t", p=128)
        dstv = dst.rearrange("(k p) t -> p k t", p=128)
        for (o, tn) in tl:
            kb.dma("sp", X[:, :, :tn], srcv[:, :, t0 + o:t0 + o + tn], writes=[X])
            Hview = Buf(None)
            rmsnorm_tile(kb, C, X, tn, PV(lambda k: AB[:, 3, which, k:k + 1], [AB]), PV(lambda k: AB[:, 4, which, k:k + 1], [AB]), which,
                         _Off(Hb, o), SQ, RS, tmp, Hf=Hf)
            for s in range(tn // 128):
                sg = (o // 128) + s
                R = rt[sg % 2]
                ps = next_ps(C)
                for k in range(8):
                    kb.op("pe", lambda: nc.tensor.matmul(ps[:, 0:16], lhsT=Hf[:, k, s * 128:(s + 1) * 128], rhs=rw_sb[:, k, :], start=(k == 0), stop=(k == 7)),
                          reads=[Hf, rw_sb], writes=[ps])
                sc = R[:, 0:16]
                sel = R[:, 16:32]
                kb.op("act", lambda: nc.scalar.activation(out=sc, in_=ps[:, 0:16], func=AF.Sigmoid), reads=[ps], writes=[R])
                kb.op("dve", lambda: nc.vector.tensor_tensor(out=sel, in0=sc, in1=rb_bc[:, :], op=ALU.add), reads=[R, rb_bc], writes=[R])
                sel3 = R[:, 16:32].rearrange("p (g j) -> p g j", j=4)
                P3 = R[:, 32:56].rearrange("p (g j) -> p g j", j=6)
                pi = 0
                for a in range(4):
                    for b2 in range(a + 1, 4):
                        kb.op("dve", lambda: nc.vector.tensor_tensor(out=P3[:, :, pi], in0=sel3[:, :, a], in1=sel3[:, :, b2], op=ALU.add), reads=[R], writes=[R])
                        pi += 1
                kb.op("dve", lambda: nc.vector.tensor_reduce(out=R[:, 56:60], in_=P3, axis=AX.X, op=ALU.max), reads=[R], writes=[R])
                kb.op("dve", lambda: nc.vector.tensor_reduce(out=R[:, 60:61], in_=R[:, 56:60], axis=AX.X, op=ALU.max), reads=[R], writes=[R])
                kb.op("dve", lambda: nc.vector.tensor_scalar(out=R[:, 56:60], in0=R[:, 56:60], scalar1=R[:, 60:61], scalar2=None, op0=ALU.is_ge), reads=[R], writes=[R])
                kb.op("dve", lambda: nc.vector.scalar_tensor_tensor(out=sel3, in0=sel3, scalar=2.0, in1=R[:, 56:60].unsqueeze(2).to_broadcast([128, 4, 4]),
                                                                   op0=ALU.add, op1=ALU.mult), reads=[R], writes=[R])
                M1 = R[:, 32:48]
                M2 = R[:, 48:64]
                kb.op("dve", lambda: nc.vector.tensor_reduce(out=R[:, 61:62], in_=sel, axis=AX.X, op=ALU.max), reads=[R], writes=[R])
                G = gate[:, sg, :]
                kb.op("dve", lambda: nc.vector.tensor_scalar(out=G, in0=sel, scalar1=R[:, 61:62], scalar2=None, op0=ALU.is_ge), reads=[R], writes=[gate])
                kb.op("dve", lambda: nc.vector.scalar_tensor_tensor(out=M1, in0=G, scalar=-10.0, in1=sel, op0=ALU.mult, op1=ALU.add), reads=[R, gate], writes=[R])
                kb.op("dve", lambda: nc.vector.tensor_reduce(out=R[:, 61:62], in_=M1, axis=AX.X, op=ALU.max), reads=[R], writes=[R])
                kb.op("dve", lambda: nc.vector.scalar_tensor_tensor(out=G, in0=M1, scalar=R[:, 61:62], in1=G, op0=ALU.is_ge, op1=ALU.add), reads=[R, gate], writes=[gate])
                kb.op("dve", lambda: nc.vector.tensor_tensor(out=G, in0=G, in1=sc, op=ALU.mult), reads=[R, gate], writes=[gate])
                kb.op("dve", lambda: nc.vector.tensor_reduce(out=R[:, 62:63], in_=G, axis=AX.X, op=ALU.add), reads=[gate], writes=[R])
                kb.op("dve", lambda: nc.vector.reciprocal(out=R[:, 62:63], in_=R[:, 62:63]), reads=[R], writes=[R])
                kb.op("dve", lambda: nc.vector.tensor_scalar(out=G, in0=G, scalar1=R[:, 62:63], scalar2=None, op0=ALU.mult), reads=[R, gate], writes=[gate])
        for e in range(16):
            W1, W3, W2 = w1[wi % 2], w3[wi % 2], w2[wi % 2]
            wi += 1
            kb.dma("pool", W1[:], w1d[e].rearrange("(k p) f -> p k f", p=128), writes=[W1])
            kb.dma("pool", W3[:], w3d[e].rearrange("(k p) f -> p k f", p=128), writes=[W3])
            kb.dma("pool", W2[:], w2d[e].rearrange("(k p) f -> p k f", p=128), writes=[W2])
            for ti, (o, tn) in enumerate(tl):
                HE = he[ti % 2]
                for m in range(4):
                    p1 = next_ps(C)
                    p3 = next_ps(C)
                    for k in range(8):
                        kb.op("pe", lambda: nc.tensor.matmul(p1[:, :tn], lhsT=W1[:, k, m * 128:(m + 1) * 128], rhs=Hb[:, k, o:o + tn], start=(k == 0), stop=(k == 7)),
                              reads=[Hb, W1], writes=[p1])
                    for k in range(8):
                        kb.op("pe", lambda: nc.tensor.matmul(p3[:, :tn], lhsT=W3[:, k, m * 128:(m + 1) * 128], rhs=Hb[:, k, o:o + tn], start=(k == 0), stop=(k == 7)),
                              reads=[Hb, W3], writes=[p3])
                    S = sl[m % 2]
                    kb.op("act", lambda: nc.scalar.activation(out=S[:, :tn], in_=p1[:, :tn], func=AF.Silu), reads=[p1], writes=[S])
                    kb.op("dve", lambda: nc.vector.tensor_tensor(out=HE[:, m, :tn], in0=p3[:, :tn], in1=S[:, :tn], op=ALU.mult), reads=[p3, S], writes=[HE])
                for s in range(tn // 128):
                    sg = (o // 128) + s
                    for hf in range(2):
                        po = next_ps(C)
                        for m in range(4):
                            kb.op("pe", lambda: nc.tensor.matmul(po[:, :], lhsT=HE[:, m, s * 128:(s + 1) * 128], rhs=W2[:, m, hf * 512:(hf + 1) * 512], start=(m == 0), stop=(m == 3)),
                                  reads=[HE, W2], writes=[po])
                        A = acc[:, sg, hf * 512:(hf + 1) * 512]
                        if e == 0:
                            kb.op("dve", lambda: nc.vector.tensor_scalar(out=A, in0=po[:, :], scalar1=gate[:, sg, e:e + 1], scalar2=None, op0=ALU.mult),
                                  reads=[po, gate], writes=[acc])
                        else:
                            kb.op("dve", lambda: nc.vector.scalar_tensor_tensor(out=A, in0=po[:, :], scalar=gate[:, sg, e:e + 1], in1=A, op0=ALU.mult, op1=ALU.add),
                                  reads=[po, gate, acc], writes=[acc])
        XO = xo[0]
        for (o, tn) in tl:
            kb.dma("sp", X[:, :, :tn], srcv[:, :, t0 + o:t0 + o + tn], writes=[X])
            for m in range(8):
                pt = next_ps(C)
                for s in range(tn // 128):
                    sg = (o // 128) + s
                    kb.op("pe", lambda: nc.tensor.transpose(pt[:, s * 128:(s + 1) * 128], acc[:, sg, m * 128:(m + 1) * 128], C.ident_f[:]),
                          reads=[acc, C.ident_f], writes=[pt])
                kb.op("dve", lambda: nc.vector.scalar_tensor_tensor(out=XO[:, m, :tn], in0=pt[:, :tn], scalar=AB[:, 5, which, m:m + 1], in1=X[:, m, :tn],
                                                                   op0=ALU.mult, op1=ALU.add), reads=[pt, AB, X], writes=[XO])
            kb.dma("pool", dstv[:, :, t0 + o:t0 + o + tn], XO[:, :, :tn], reads=[XO])


class _Off:
    def __init__(self, b, off):
        self.b = b
        self.off = off

    def __getitem__(self, key):
        p, k, sl_ = key
        return self.b.t[p, k, self.off + (sl_.start or 0):self.off + sl_.stop]

    @property
    def w(self):
        return self.b.w

    @w.setter
    def w(self, v):
        self.b.w = v

    @property
    def pr(self):
        return self.b.pr

    @pr.setter
    def pr(self, v):
        self.b.pr = v

    @property
    def r(self):
        return self.b.r

    @r.setter
    def r(self, v):
        self.b.r = v


def stage_mla_prep(kb, C, st, tiles, pinT, qg, kvg, wuq, wukv, r96, r32, cs96, cs32, qTd, kTd, vd):
    nc = kb.nc
    cq = [kb.sb(f"mp_cq{i}", [128, 6, 512], BF16, st) for i in range(2)]
    ckv = [kb.sb(f"mp_ckv{i}", [128, 2, 512], BF16, st) for i in range(2)]
    kr = [kb.sb(f"mp_kr{i}", [32, 512], BF16, st) for i in range(2)]
    SQ = kb.sb("mp_sq", [128, 6, 512], BF16, st)
    RS = kb.sb("mp_rs", [128, 512], F32, st)
    tmp = [kb.sb(f"mp_tmp{i}", [128, 512], F32, st) for i in range(2)]
    cqn = kb.sb("mp_cqn", [128, 6, 512], BF16, st)
    ckvn = kb.sb("mp_ckvn", [128, 2, 512], BF16, st)
    t96 = [kb.sb(f"mp_t96{i}", [96, 2, 512], F32, st) for i in range(2)]
    t32 = [kb.sb(f"mp_t32{i}", [32, 2, 512], F32, st) for i in range(2)]
    qb = [kb.sb(f"mp_qb{i}", [96, 512], BF16, st) for i in range(2)]
    qf = [kb.sb(f"mp_qf{i}", [96, 512], F32, st) for i in range(2)]
    qo = [kb.sb(f"mp_qo{i}", [96, 512], BF16, st) for i in range(3)]
    ko = [kb.sb(f"mp_ko{i}", [64, 512], BF16, st) for i in range(6)]
    qi2 = [0]
    qr_ = [kb.sb(f"mp_qr{i}", [32, 512], BF16, st) for i in range(3)]
    krf2 = [kb.sb(f"mp_krf2{i}", [32, 512], F32, st) for i in range(2)]
    krg2 = [kb.sb(f"mp_krg2{i}", [32, 512], F32, st) for i in range(2)]
    kro2 = [kb.sb(f"mp_kro2{i}", [32, 512], BF16, st) for i in range(3)]
    kro = [kb.sb(f"mp_kro{i}", [32, 512], BF16, st) for i in range(2)]
    krf = [kb.sb(f"mp_krf{i}", [32, 512], F32, st) for i in range(2)]
    vo = [kb.sb(f"mp_vo{i}", [128, 512], BF16, st) for i in range(3)]
    qi = 0
    for ti, (p0, n, rope, tl0) in enumerate(tiles):
        b = ti % 2
        CQ, CKV, KR, T96, T32 = cq[b], ckv[b], kr[b], t96[b], t32[b]
        kb.dma("sp", CQ[:, :, :n], pinT[3600:4368, :].rearrange("(k p) t -> p k t", p=128)[:, :, p0:p0 + n], writes=[CQ])
        kb.dma("sp", CKV[:, :, :n], pinT[4368:4624, :].rearrange("(k p) t -> p k t", p=128)[:, :, p0:p0 + n], writes=[CKV])
        kb.dma("sp", KR[:, :n], pinT[4624:4656, p0:p0 + n], writes=[KR])
        if rope:
            kb.dma("sp", T32[:, :, :n], cs32.rearrange("c d t -> d c t")[:, :, tl0:tl0 + n], writes=[T32])
        rmsnorm_tile(kb, C, CQ, n, PV(lambda k: qg[:, k:k + 1], [qg]), None, 0, cqn, SQ, RS, tmp, nk=6, dim=768)
        rmsnorm_tile(kb, C, CKV, n, PV(lambda k: kvg[:, k:k + 1], [kvg]), None, 0, ckvn, SQ, RS, tmp, nk=2, dim=256)
        KRO = kro[b]
        if rope:
            KRF = krf[b]
            ps = next_ps(C)
            kb.op("pe", lambda: nc.tensor.matmul(ps[:32, :n], lhsT=r32[:, :], rhs=KR[:, :n], start=True, stop=True), reads=[KR, r32], writes=[ps])
            kb.op("dve", lambda: nc.vector.tensor_tensor(out=KRF[:, :n], in0=ps[:32, :n], in1=T32[:, 1, :n], op=ALU.mult), reads=[ps, T32], writes=[KRF])
            KRG = krg2[b]
            kb.op("pool", lambda: nc.gpsimd.tensor_tensor(out=KRG[:, :n], in0=T32[:, 0, :n], in1=KR[:, :n], op=ALU.mult), reads=[T32, KR], writes=[KRG])
            kb.op("pool", lambda: nc.gpsimd.tensor_tensor(out=KRO[:, :n], in0=KRG[:, :n], in1=KRF[:, :n], op=ALU.add), reads=[KRG, KRF], writes=[KRO])
        else:
            kb.op("pool", lambda: nc.gpsimd.tensor_copy(out=KRO[:, :n], in_=KR[:, :n]), reads=[KR], writes=[KRO])
        for h in range(8):
            kb.dma("pool", kTd[h, 64:96, p0:p0 + n], KRO[:, :n], reads=[KRO])
        for h in range(8):
            ps = next_ps(C)
            for k in range(6):
                kb.op("pe", lambda: nc.tensor.matmul(ps[:64, :n], lhsT=wuq[:, k, h * 96:h * 96 + 64], rhs=cqn[:, k, :n], start=(k == 0), stop=(k == 5)),
                      reads=[cqn, wuq], writes=[ps])
            QO = ko[qi2[0] % 6]
            qi2[0] += 1
            kb.op("act", lambda: nc.scalar.copy(out=QO[:, :n], in_=ps[:64, :n]), reads=[ps], writes=[QO])
            kb.dma("pool", qTd[h, 0:64, p0:p0 + n], QO[:, :n], reads=[QO])
            ps = next_ps(C)
            for k in range(6):
                kb.op("pe", lambda: nc.tensor.matmul(ps[:32, :n], lhsT=wuq[:, k, h * 96 + 64:h * 96 + 96], rhs=cqn[:, k, :n], start=(k == 0), stop=(k == 5)),
                      reads=[cqn, wuq], writes=[ps])
            QR = qr_[qi % 3]
            kb.op("act", lambda: nc.scalar.copy(out=QR[:, :n], in_=ps[:32, :n]), reads=[ps], writes=[QR])
            if rope:
                QF, QG, QO2 = krf2[qi % 2], krg2[qi % 2], kro2[qi % 3]
                ps2 = next_ps(C)
                kb.op("pe", lambda: nc.tensor.matmul(ps2[:32, :n], lhsT=r32[:, :], rhs=QR[:, :n], start=True, stop=True), reads=[QR, r32], writes=[ps2])
                kb.op("dve", lambda: nc.vector.tensor_tensor(out=QF[:, :n], in0=ps2[:32, :n], in1=T32[:, 1, :n], op=ALU.mult), reads=[ps2, T32], writes=[QF])
                kb.op("pool", lambda: nc.gpsimd.tensor_tensor(out=QG[:, :n], in0=T32[:, 0, :n], in1=QR[:, :n], op=ALU.mult), reads=[T32, QR], writes=[QG])
                kb.op("pool", lambda: nc.gpsimd.tensor_tensor(out=QO2[:, :n], in0=QG[:, :n], in1=QF[:, :n], op=ALU.add), reads=[QG, QF], writes=[QO2])
                kb.dma("pool", qTd[h, 64:96, p0:p0 + n], QO2[:, :n], reads=[QO2])
            else:
                kb.dma("pool", qTd[h, 64:96, p0:p0 + n], QR[:, :n], reads=[QR])
            ps = next_ps(C)
            for k in range(2):
                kb.op("pe", lambda: nc.tensor.matmul(ps[:64, :n], lhsT=wukv[:, k, h * 128:h * 128 + 64], rhs=ckvn[:, k, :n], start=(k == 0), stop=(k == 1)),
                      reads=[ckvn, wukv], writes=[ps])
            KO = ko[qi2[0] % 6]
            qi2[0] += 1
            kb.op("act", lambda: nc.scalar.copy(out=KO[:, :n], in_=ps[:64, :n]), reads=[ps], writes=[KO])
            kb.dma("pool", kTd[h, 0:64, p0:p0 + n], KO[:, :n], reads=[KO])
            qi += 1
        for s in range(n // 128):
            ps = next_ps(C)
            for k in range(2):
                kb.op("pe", lambda: nc.tensor.matmul(ps[:, :].rearrange("p (h c) -> p h c", c=64), lhsT=ckvn[:, k, s * 128:(s + 1) * 128],
                                                     rhs=wukv[:, k, :].rearrange("p (h c) -> p h c", c=128)[:, :, 64:128], start=(k == 0), stop=(k == 1)),
                      reads=[ckvn, wukv], writes=[ps])
            VO = vo[s % 3]
            kb.op("dve", lambda: nc.vector.tensor_copy(out=VO[:, :], in_=ps[:, :]), reads=[ps], writes=[VO])
            kb.dma("pool", vd.rearrange("h t c -> t h c")[p0 + s * 128:p0 + (s + 1) * 128, :, :], VO[:, :].rearrange("p (h c) -> p h c", c=64), reads=[VO])


def stage_mla_attn(kb, C, st, jobs, qTd, kTd, vd, attnT, T):
    nc = kb.nc
    scale = 96.0 ** -0.5
    NKT = T // 128
    LOOK = 3
    kT = [kb.sb(f"at_k{i}", [96, T], BF16, st) for i in range(2)]
    V = [kb.sb(f"at_v{i}", [128, NKT, 65], BF16, st) for i in range(2)]
    Q = [kb.sb(f"at_q{i}", [96, 512], BF16, st) for i in range(2)]
    P = [kb.sb(f"at_p{i}", [128, 512], BF16, st) for i in range(6)]
    rrow = [kb.sb(f"at_rr{i}", [65, 512], F32, st) for i in range(2)]
    rbc = [kb.sb(f"at_rb{i}", [64, 512], F32, st) for i in range(2)]
    O = [kb.sb(f"at_o{i}", [64, 512], BF16, st) for i in range(2)]
    ones65 = kb.sb("at_ones", [65, 64], F32, st)
    kb.op("dve", lambda: nc.vector.memset(ones65[:], 1.0), writes=[ones65])
    for i in range(2):
        kb.op("pool", lambda: nc.gpsimd.memset(V[i][:, :, 64:65], 1.0), writes=[V[i]])
    sps = C.psum[0:4]
    aps = C.psum[4:8]
    si = 0
    pi_ = 0
    qi = 0
    for h in range(8):
        K_, V_ = kT[h % 2], V[h % 2]
        kb.dma("sp", K_[:, :], kTd[h], writes=[K_])
        kb.dma("sp", V_[:, :, 0:64], vd[h].rearrange("(j p) c -> p j c", p=128), writes=[V_])
        for (q0, nq, ktiles) in jobs:
            for o in range(0, nq, 512):
                n = min(512, nq - o)
                Qt = Q[qi % 2]
                po, pb = aps[(qi % 2) * 2], aps[(qi % 2) * 2 + 1]
                kb.dma("sp", Qt[:, :n], qTd[h, :, q0 + o:q0 + o + n], writes=[Qt])
                nk = len(ktiles)
                pend = []
                for ji in range(nk + LOOK):
                    if ji < nk:
                        j = ktiles[ji]
                        ps = sps[si % 4]
                        si += 1
                        Pt = P[pi_ % 6]
                        pi_ += 1
                        kb.op("pe", lambda: nc.tensor.matmul(ps[:, :n], lhsT=K_[:, j * 128:(j + 1) * 128], rhs=Qt[:, :n], start=True, stop=True),
                              reads=[K_, Qt], writes=[ps])
                        kb.op("act", lambda: nc.scalar.activation(out=Pt[:, :n], in_=ps[:, :n], func=AF.Exp, scale=scale), reads=[ps], writes=[Pt])
                        pend.append((j, Pt))
                    if ji >= LOOK:
                        jj = ji - LOOK
                        j, Pt = pend[jj]
                        kb.op("pe", lambda: nc.tensor.matmul(po[:65, :n], lhsT=V_[:, j, :], rhs=Pt[:, :n], start=(jj == 0), stop=(jj == nk - 1)),
                              reads=[V_, Pt], writes=[po])
                RR, RB, O_ = rrow[qi % 2], rbc[qi % 2], O[qi % 2]
                kb.op("dve", lambda: nc.vector.reciprocal(out=RR[64:65, :n], in_=po[64:65, :n]), reads=[po], writes=[RR])
                kb.op("pe", lambda: nc.tensor.matmul(pb[:64, :n], lhsT=ones65[64:65, :], rhs=RR[64:65, :n], start=True, stop=True), reads=[ones65, RR], writes=[pb])
                kb.op("act", lambda: nc.scalar.copy(out=RB[:, :n], in_=pb[:64, :n]), reads=[pb], writes=[RB])
                kb.op("dve", lambda: nc.vector.tensor_tensor(out=O_[:, :n], in0=po[:64, :n], in1=RB[:, :n], op=ALU.mult), reads=[po, RB], writes=[O_])
                kb.dma("pool", attnT[h * 64:(h + 1) * 64, q0 + o:q0 + o + n], O_[:, :n], reads=[O_])
                qi += 1


def hy_sin(kb, nc, out, ps, n, fr, tmpa, tmpb):
    kb.op("act", lambda: nc.scalar.activation(out=tmpa[:64, :n], in_=ps[:64, :n], func=AF.Sin, scale=fr[:, 0:1], bias=fr[:, 1:2]), reads=[ps, fr], writes=[tmpa])
    kb.op("act", lambda: nc.scalar.activation(out=tmpb[:64, :n], in_=ps[:64, :n], func=AF.Sin, scale=fr[:, 2:3], bias=fr[:, 3:4]), reads=[ps, fr], writes=[tmpb])
    kb.op("dve", lambda: nc.vector.tensor_tensor(out=tmpb[:64, :n], in0=tmpb[:64, :n], in1=tmpb[:64, :n], op=ALU.mult), reads=[tmpb], writes=[tmpb])
    kb.op("dve", lambda: nc.vector.tensor_scalar(out=tmpb[:64, :n], in0=tmpb[:64, :n], scalar1=-2.0, scalar2=1.0, op0=ALU.mult, op1=ALU.add), reads=[tmpb], writes=[tmpb])
    kb.op("dve", lambda: nc.vector.scalar_tensor_tensor(out=out[:64, :n], in0=tmpa[:64, :n], scalar=2.0, in1=tmpb[:64, :n], op0=ALU.mult, op1=ALU.mult), reads=[tmpa, tmpb], writes=[out])


def stage_hy_filter(kb, C, st, L, zT, tn, w1d, b1d, w2d, b2d, w3d, frd, decd, hTd):
    nc = kb.nc
    w1 = kb.sb("hf_w1", [33, 64], F32, st)
    w2 = kb.sb("hf_w2", [64, 64], F32, st)
    w3 = kb.sb("hf_w3", [64, 1024], F32, st)
    v = kb.sb("hf_v", [64, 3], F32, st)
    fr1 = kb.sb("hf_fr1", [64, 4], F32, st)
    fr2 = kb.sb("hf_fr2", [64, 4], F32, st)
    dec = kb.sb("hf_dec", [128, 8], F32, st)
    kb.dma("sp", w1[:], w1d, writes=[w1])
    kb.dma("sp", w2[:], w2d, writes=[w2])
    kb.dma("sp", w3[:], w3d, writes=[w3])
    kb.dma("sp", v[:, 0:1], b1d.rearrange("(p o) -> p o", o=1), writes=[v])
    kb.dma("sp", v[:, 1:2], b2d.rearrange("(p o) -> p o", o=1), writes=[v])
    kb.dma("sp", v[:, 2:3], frd.rearrange("(p o) -> p o", o=1), writes=[v])
    kb.dma("sp", dec[:], decd, writes=[dec])
    for fr, bi in ((fr1, 0), (fr2, 1)):
        kb.op("dve", lambda: nc.vector.tensor_scalar(out=fr[:, 0:1], in0=v[:, 2:3], scalar1=0.5, scalar2=None, op0=ALU.mult), reads=[v], writes=[fr])
        kb.op("dve", lambda: nc.vector.scalar_tensor_tensor(out=fr[:, 1:2], in0=v[:, 2:3], scalar=0.5, in1=v[:, bi:bi + 1], op0=ALU.mult, op1=ALU.mult), reads=[v], writes=[fr])
        kb.op("dve", lambda: nc.vector.tensor_scalar(out=fr[:, 2:4], in0=fr[:, 0:2], scalar1=0.5, scalar2=None, op0=ALU.mult), reads=[fr], writes=[fr])
    kb.op("act", lambda: nc.scalar.activation(out=dec[:], in_=dec[:], func=AF.Abs), reads=[dec], writes=[dec])
    kb.op("dve", lambda: nc.vector.tensor_scalar(out=dec[:], in0=dec[:], scalar1=-1.0, scalar2=None, op0=ALU.mult), reads=[dec], writes=[dec])
    z = [kb.sb(f"hf_z{i}", [33, 512], F32, st) for i in range(2)]
    tb = [kb.sb(f"hf_tb{i}", [128, 512], F32, st) for i in range(2)]
    ta = kb.sb("hf_ta", [64, 512], F32, st)
    tc_ = kb.sb("hf_tc", [64, 512], F32, st)
    h1 = kb.sb("hf_h1", [64, 512], F32, st)
    h2 = kb.sb("hf_h2", [64, 512], F32, st)
    win = [kb.sb(f"hf_win{i}", [128, 512], F32, st) for i in range(2)]
    ho = [kb.sb(f"hf_ho{i}", [128, 512], BF16, st) for i in range(3)]
    oi = 0
    for ti, o in enumerate(range(0, L, 512)):
        n = min(512, L - o)
        Z, TB = z[ti % 2], tb[ti % 2]
        kb.dma("sp", Z[:, :n], zT[:, o:o + n], writes=[Z])
        kb.dma("sp", TB[:, :n], tn[:, o:o + n], writes=[TB])
        ps = next_ps(C)
        kb.op("pe", lambda: nc.tensor.matmul(ps[:64, :n], lhsT=w1[:, :], rhs=Z[:, :n], start=True, stop=True), reads=[w1, Z], writes=[ps])
        hy_sin(kb, nc, h1, ps, n, fr1, ta, tc_)
        ps = next_ps(C)
        kb.op("pe", lambda: nc.tensor.matmul(ps[:64, :n], lhsT=w2[:, :], rhs=h1[:, :n], start=True, stop=True), reads=[w2, h1], writes=[ps])
        hy_sin(kb, nc, h2, ps, n, fr2, ta, tc_)
        for cch in range(8):
            ps = next_ps(C)
            kb.op("pe", lambda: nc.tensor.matmul(ps[:, :n], lhsT=w3[:, cch * 128:(cch + 1) * 128], rhs=h2[:, :n], start=True, stop=True), reads=[w3, h2], writes=[ps])
            W = win[cch % 2]
            kb.op("act", lambda: nc.scalar.activation(out=W[:, :n], in_=TB[:, :n], func=AF.Exp, scale=dec[:, cch:cch + 1]), reads=[TB, dec], writes=[W])
            HO = ho[oi % 3]
            oi += 1
            kb.op("dve", lambda: nc.vector.scalar_tensor_tensor(out=HO[:, :n], in0=W[:, :n], scalar=HY_SHIFT, in1=ps[:, :n], op0=ALU.add, op1=ALU.mult), reads=[W, ps], writes=[HO])
            if cch >= 4 and o == 0:
                kb.op("dve", lambda: nc.vector.memset(HO[:, 0:1], 0.0), reads=[HO], writes=[HO], waw=True)
            kb.dma("pool", hTd[cch * 128:(cch + 1) * 128, o:o + n], HO[:, :n], reads=[HO])


HY_SHIFT = 0.05


def stage_hy_conv3(kb, C, st, segs, pinT, cwd, cbd, uvT, x0T):
    nc = kb.nc
    cw = kb.sb("hc_w", [128, 3, 12], F32, st)
    cb = kb.sb("hc_b", [128, 12], F32, st)
    kb.dma("sp", cw[:], cwd, writes=[cw])
    kb.dma("sp", cb[:], cbd, writes=[cb])
    P = [kb.sb(f"hc_p{i}", [128, 12, 514], BF16, st) for i in range(2)]
    U = [kb.sb(f"hc_u{i}", [128, 512], F32, st) for i in range(6)]
    O = [kb.sb(f"hc_o{i}", [128, 512], BF16, st) for i in range(4)]
    ti = 0
    ui = 0
    oi = 0
    for (p0, Ls, d0) in segs:
        for o in range(0, Ls, 512):
            n = min(512, Ls - o)
            Pt = P[ti % 2]
            ti += 1
            lo = 1 if o == 0 else 0
            hi = 1 if o + n == Ls else 0
            if lo:
                kb.op("pool", lambda: nc.gpsimd.memset(Pt[:, :, 0:1], 0.0), writes=[Pt])
            if hi:
                kb.op("pool", lambda: nc.gpsimd.memset(Pt[:, :, n + 1:n + 2], 0.0), writes=[Pt])
            kb.dma("sp", Pt[:, :, lo:n + 2 - hi], pinT[0:1536, :].rearrange("(k p) t -> p k t", p=128)[:, :, p0 + o - 1 + lo:p0 + o + n + 1 - hi], writes=[Pt])
            us = []
            for k in range(12):
                Ut = U[ui % 6]
                ui += 1
                kb.op("act", lambda: nc.scalar.activation(out=Ut[:, :n], in_=Pt[:, k, 1:n + 1], func=AF.Identity, scale=cw[:, 1, k:k + 1], bias=cb[:, k:k + 1]), reads=[Pt, cw, cb], writes=[Ut])
                kb.op("dve", lambda: nc.vector.scalar_tensor_tensor(out=Ut[:, :n], in0=Pt[:, k, 0:n], scalar=cw[:, 0, k:k + 1], in1=Ut[:, :n], op0=ALU.mult, op1=ALU.add), reads=[Pt, cw, Ut], writes=[Ut])
                eng = "dve" if k < 4 else "pool"
                e_ = nc.vector if k < 4 else nc.gpsimd
                if k < 4:
                    Ot = O[oi % 4]
                    oi += 1
                    kb.op("dve", lambda: nc.vector.scalar_tensor_tensor(out=Ot[:, :n], in0=Pt[:, k, 2:n + 2], scalar=cw[:, 2, k:k + 1], in1=Ut[:, :n], op0=ALU.mult, op1=ALU.add), reads=[Pt, cw, Ut], writes=[Ot])
                    kb.dma("pool", x0T[k * 128:(k + 1) * 128, d0 + o:d0 + o + n], Ot[:, :n], reads=[Ot])
                else:
                    kb.op("dve", lambda: nc.vector.scalar_tensor_tensor(out=Ut[:, :n], in0=Pt[:, k, 2:n + 2], scalar=cw[:, 2, k:k + 1], in1=Ut[:, :n], op0=ALU.mult, op1=ALU.add), reads=[Pt, cw, Ut], writes=[Ut])
                    us.append(Ut)
                if k >= 8:
                    Ot = O[oi % 4]
                    oi += 1
                    X1 = us[k - 8]
                    kb.op("pool", lambda: nc.gpsimd.tensor_tensor(out=Ot[:, :n], in0=X1[:, :n], in1=Ut[:, :n], op=ALU.mult), reads=[X1, Ut], writes=[Ot])
                    kb.dma("pool", uvT[(k - 8) * 128:(k - 7) * 128, d0 + o:d0 + o + n], Ot[:, :n], reads=[Ot])


def load_fft_consts(kb, C, fad, fbd, twd):
    C.FA = kb.sb("FA", [128, 3, 256], BF16)
    C.FB = kb.sb("FB", [128, 4, 128], BF16)
    C.TW = kb.sb("TW", [128, 3, 128], F32)
    kb.dma("pool", C.FA[:], fad.rearrange("j p n -> p j n"), writes=[C.FA])
    kb.dma("pool", C.FB[:], fbd.rearrange("j p n -> p j n"), writes=[C.FB])
    kb.dma("sp", C.TW[:], twd.rearrange("j p n -> p j n"), writes=[C.TW])


def _twiddle(kb, C, nc, ps, Yr, Yi, c, ti_idx, tmps):
    psv = ps[:, :].rearrange("p (c r k) -> p c r k", c=2, r=2)
    Tr = C.TW[:, 0, :].unsqueeze(1).to_broadcast([128, 2, 128])
    Ti = C.TW[:, ti_idx, :].unsqueeze(1).to_broadcast([128, 2, 128])
    t1, t2, t3, t4 = tmps
    kb.op("dve", lambda: nc.vector.tensor_tensor(out=t1[:, :, :], in0=psv[:, :, 0, :], in1=Tr, op=ALU.mult), reads=[ps, C.TW], writes=[t1])
    kb.op("dve", lambda: nc.vector.tensor_tensor(out=t2[:, :, :], in0=psv[:, :, 1, :], in1=Ti, op=ALU.mult), reads=[ps, C.TW], writes=[t2])
    kb.op("pool", lambda: nc.gpsimd.tensor_tensor(out=Yr[:, c:c + 2, :], in0=t1[:, :, :], in1=t2[:, :, :], op=ALU.subtract), reads=[t1, t2], writes=[Yr])
    kb.op("dve", lambda: nc.vector.tensor_tensor(out=t3[:, :, :], in0=psv[:, :, 0, :], in1=Ti, op=ALU.mult), reads=[ps, C.TW], writes=[t3])
    kb.op("dve", lambda: nc.vector.tensor_tensor(out=t4[:, :, :], in0=psv[:, :, 1, :], in1=Tr, op=ALU.mult), reads=[ps, C.TW], writes=[t4])
    kb.op("pool", lambda: nc.gpsimd.tensor_tensor(out=Yi[:, c:c + 2, :], in0=t3[:, :, :], in1=t4[:, :, :], op=ALU.add), reads=[t3, t4], writes=[Yi])


def stage_hy_fft(kb, C, st, uv, hf, hb, yout, nb):
    nc = kb.nc
    GC = 32
    xin = [kb.sb(f"ff_x{i}", [64, GC, 128], BF16, st) for i in range(3)]
    Yr = [kb.sb(f"ff_yr{i}", [128, GC, 128], BF16, st) for i in range(3)]
    Yi = [kb.sb(f"ff_yi{i}", [128, GC, 128], BF16, st) for i in range(3)]
    Kr = kb.sb("ff_kr", [128, GC, 128], F32, st)
    Ki = kb.sb("ff_ki", [128, GC, 128], F32, st)
    Zr = kb.sb("ff_zr", [128, GC, 128], BF16, st)
    Zi = kb.sb("ff_zi", [128, GC, 128], BF16, st)
    tmpsA = [[kb.sb(f"ff_t{j}_{i}", [128, 2, 128], F32, st) for i in range(4)] for j in range(2)]
    tq = [kb.sb(f"ff_q{i}", [128, 512], F32, st) for i in range(4)]
    yo = [kb.sb(f"ff_yo{i}", [64, GC, 128], BF16, st) for i in range(2)]
    Cm, Sm, Sn, Cn = (C.FB[:, j, :] for j in range(4))
    pi_ = 0
    for g in range(512 // GC):
        c0 = g * GC
        for s, src in enumerate((uv, hf, hb)):
            kb.dma("sp", xin[s][:nb, :, :], src[c0:c0 + GC, :].rearrange("c (b p) -> b c p", p=128), writes=[xin[s]])
        for s in range(3):
            for c in range(0, GC, 2):
                ps = next_ps(C)
                for cc in range(2):
                    kb.op("pe", lambda: nc.tensor.matmul(ps[:, cc * 256:(cc + 1) * 256], lhsT=xin[s][:nb, c + cc, :], rhs=C.FA[:nb, 0, :], start=True, stop=True),
                          reads=[xin[s], C.FA], writes=[ps])
                _twiddle(kb, C, nc, ps, Yr[s], Yi[s], c, 1, tmpsA[pi_ % 2])
                pi_ += 1

        def q(Y, c):
            return Y[:, c:c + 4, :].rearrange("p c k -> p (c k)")
        for c in range(0, GC, 4):
            pr = next_ps(C)
            terms = [(Cm, Yr[1]), (Sm, Yi[1]), (Cm, Yr[2]), (Sm, Yi[2])]
            for i, (F_, Y_) in enumerate(terms):
                kb.op("pe", lambda: nc.tensor.matmul(pr[:, :], lhsT=F_, rhs=q(Y_, c), start=(i == 0), stop=(i == 3)), reads=[C.FB, Y_], writes=[pr])
            kb.op("act", lambda: nc.scalar.copy(out=q(Kr, c), in_=pr[:, :]), reads=[pr], writes=[Kr])
            pim = next_ps(C)
            terms = [(Cm, Yi[1]), (Sn, Yr[1]), (Cn, Yi[2]), (Sm, Yr[2])]
            for i, (F_, Y_) in enumerate(terms):
                kb.op("pe", lambda: nc.tensor.matmul(pim[:, :], lhsT=F_, rhs=q(Y_, c), start=(i == 0), stop=(i == 3)), reads=[C.FB, Y_], writes=[pim])
            kb.op("act", lambda: nc.scalar.copy(out=q(Ki, c), in_=pim[:, :]), reads=[pim], writes=[Ki])
        for c in range(0, GC, 4):
            pr = next_ps(C)
            for i, (F_, Y_) in enumerate([(Cm, Yr[0]), (Sm, Yi[0])]):
                kb.op("pe", lambda: nc.tensor.matmul(pr[:, :], lhsT=F_, rhs=q(Y_, c), start=(i == 0), stop=(i == 1)), reads=[C.FB, Y_], writes=[pr])
            pim = next_ps(C)
            for i, (F_, Y_) in enumerate([(Cm, Yi[0]), (Sn, Yr[0])]):
                kb.op("pe", lambda: nc.tensor.matmul(pim[:, :], lhsT=F_, rhs=q(Y_, c), start=(i == 0), stop=(i == 1)), reads=[C.FB, Y_], writes=[pim])
            kb.op("dve", lambda: nc.vector.tensor_tensor(out=tq[0][:, :], in0=pr[:, :], in1=q(Kr, c), op=ALU.mult), reads=[pr, Kr], writes=[tq[0]])
            kb.op("dve", lambda: nc.vector.tensor_tensor(out=tq[1][:, :], in0=pim[:, :], in1=q(Ki, c), op=ALU.mult), reads=[pim, Ki], writes=[tq[1]])
            kb.op("pool", lambda: nc.gpsimd.tensor_tensor(out=q(Zr, c), in0=tq[0][:, :], in1=tq[1][:, :], op=ALU.subtract), reads=[tq[0], tq[1]], writes=[Zr])
            kb.op("dve", lambda: nc.vector.tensor_tensor(out=tq[2][:, :], in0=pr[:, :], in1=q(Ki, c), op=ALU.mult), reads=[pr, Ki], writes=[tq[2]])
            kb.op("dve", lambda: nc.vector.tensor_tensor(out=tq[3][:, :], in0=pim[:, :], in1=q(Kr, c), op=ALU.mult), reads=[pim, Kr], writes=[tq[3]])
            kb.op("pool", lambda: nc.gpsimd.tensor_tensor(out=q(Zi, c), in0=tq[2][:, :], in1=tq[3][:, :], op=ALU.add), reads=[tq[2], tq[3]], writes=[Zi])
        for c in range(0, GC, 2):
            ps = next_ps(C)
            for cc in range(2):
                kb.op("pe", lambda: nc.tensor.matmul(ps[:, cc * 256:(cc + 1) * 256], lhsT=Zr[:, c + cc, :], rhs=C.FA[:, 1, :], start=True, stop=False), reads=[Zr, C.FA], writes=[ps])
                kb.op("pe", lambda: nc.tensor.matmul(ps[:, cc * 256:(cc + 1) * 256], lhsT=Zi[:, c + cc, :], rhs=C.FA[:, 2, :], start=False, stop=True), reads=[Zi, C.FA], writes=[ps])
            _twiddle(kb, C, nc, ps, Yr[0], Yi[0], c, 2, tmpsA[pi_ % 2])
            pi_ += 1
        YO = yo[g % 2]
        for c in range(0, GC, 4):
            ps = next_ps(C)
            kb.op("pe", lambda: nc.tensor.matmul(ps[:nb, :], lhsT=C.FB[:, 0, 0:nb], rhs=q(Yr[0], c), start=True, stop=False), reads=[C.FB, Yr[0]], writes=[ps])
            kb.op("pe", lambda: nc.tensor.matmul(ps[:nb, :], lhsT=C.FB[:, 2, 0:nb], rhs=q(Yi[0], c), start=False, stop=True), reads=[C.FB, Yi[0]], writes=[ps])
            kb.op("act", lambda: nc.scalar.activation(out=YO[:nb, c:c + 4, :].rearrange("p c k -> p (c k)"), in_=ps[:nb, :], func=AF.Copy, scale=1.0 / 16384.0), reads=[ps], writes=[YO])
        kb.dma("pool", yout[c0:c0 + GC, :].rearrange("c (b p) -> b c p", p=128), YO[:nb, :, :], reads=[YO])


def stage_hy_gate(kb, C, st, tiles, yconv, uvT, x0T, hbd, yT0):
    nc = kb.nc
    hb = kb.sb("hg_b", [128, 4], F32, st)
    kb.dma("sp", hb[:], hbd, writes=[hb])
    A = [kb.sb(f"hg_a{i}", [128, 3, 4, 512], BF16, st) for i in range(2)]
    T_ = [kb.sb(f"hg_t{i}", [128, 512], F32, st) for i in range(2)]
    O = [kb.sb(f"hg_o{i}", [128, 4, 512], BF16, st) for i in range(2)]
    for ti, (d0, n) in enumerate(tiles):
        At, Ot = A[ti % 2], O[ti % 2]
        for j, src in enumerate((yconv, uvT, x0T)):
            kb.dma("sp", At[:, j, :, :n], src.rearrange("(k p) t -> p k t", p=128)[:, :, d0:d0 + n], writes=[At])
        for k in range(4):
            Tt = T_[k % 2]
            kb.op("dve", lambda: nc.vector.scalar_tensor_tensor(out=Tt[:, :n], in0=At[:, 1, k, :n], scalar=hb[:, k:k + 1], in1=At[:, 0, k, :n], op0=ALU.mult, op1=ALU.add),
                  reads=[At, hb], writes=[Tt])
            kb.op("pool", lambda: nc.gpsimd.tensor_tensor(out=Ot[:, k, :n], in0=Tt[:, :n], in1=At[:, 2, k, :n], op=ALU.mult), reads=[Tt, At], writes=[Ot])
        kb.dma("pool", yT0.rearrange("(k p) t -> p k t", p=128)[:, :, d0:d0 + n], Ot[:, :, :n], reads=[Ot])


def stage_gdn_prep(kb, C, st, segs, pinT, cwd, alogd, dtbd, qkvT, gbT):
    nc = kb.nc
    cw = kb.sb("gp_w", [128, 3, 12], F32, st)
    kb.dma("sp", cw[:], cwd, writes=[cw])
    av = kb.sb("gp_av", [8, 2], F32, st)
    kb.dma("sp", av[:, 0:1], alogd.rearrange("(p o) -> p o", o=1), writes=[av])
    kb.dma("sp", av[:, 1:2], dtbd.rearrange("(p o) -> p o", o=1), writes=[av])
    kb.op("act", lambda: nc.scalar.activation(out=av[:, 0:1], in_=av[:, 0:1], func=AF.Exp), reads=[av], writes=[av])
    kb.op("dve", lambda: nc.vector.tensor_scalar(out=av[:, 0:1], in0=av[:, 0:1], scalar1=-1.0, scalar2=None, op0=ALU.mult), reads=[av], writes=[av])
    P = [kb.sb(f"gp_p{i}", [128, 12, 514], BF16, st) for i in range(2)]
    U = [kb.sb(f"gp_u{i}", [128, 512], F32, st) for i in range(4)]
    SQ = [kb.sb(f"gp_sq{i}", [128, 512], BF16, st) for i in range(2)]
    RS = [kb.sb(f"gp_rs{i}", [128, 512], F32, st) for i in range(2)]
    O = [kb.sb(f"gp_o{i}", [128, 512], F32, st) for i in range(4)]
    A8 = [kb.sb(f"gp_a8{i}", [8, 512], BF16, st) for i in range(2)]
    B8 = [kb.sb(f"gp_b8{i}", [8, 512], BF16, st) for i in range(2)]
    G8 = [kb.sb(f"gp_g8{i}", [8, 512], F32, st) for i in range(2)]
    E8 = [kb.sb(f"gp_e8{i}", [8, 512], F32, st) for i in range(2)]
    GT = [kb.sb(f"gp_gt{i}", [128, 16], F32, st) for i in range(3)]
    ti = ui = oi = gi = 0
    for (p0, Ls) in segs:
        for o in range(0, Ls, 512):
            n = min(512, Ls - o)
            Pt = P[ti % 2]
            lo = 1 if o == 0 else 0
            hi = 1 if o + n == Ls else 0
            if lo:
                kb.op("pool", lambda: nc.gpsimd.memset(Pt[:, :, 0:1], 0.0), writes=[Pt])
            if hi:
                kb.op("pool", lambda: nc.gpsimd.memset(Pt[:, :, n + 1:n + 2], 0.0), writes=[Pt])
            kb.dma("sp", Pt[:, :, lo:n + 2 - hi], pinT[1536:3072, :].rearrange("(k p) t -> p k t", p=128)[:, :, p0 + o - 1 + lo:p0 + o + n + 1 - hi], writes=[Pt])
            for k in range(12):
                Ut = U[ui % 4]
                ui += 1
                Ot = O[oi % 4]
                oi += 1
                kb.op("act", lambda: nc.scalar.activation(out=Ut[:, :n], in_=Pt[:, k, 1:n + 1], func=AF.Identity, scale=cw[:, 1, k:k + 1]), reads=[Pt, cw], writes=[Ut])
                kb.op("dve", lambda: nc.vector.scalar_tensor_tensor(out=Ut[:, :n], in0=Pt[:, k, 0:n], scalar=cw[:, 0, k:k + 1], in1=Ut[:, :n], op0=ALU.mult, op1=ALU.add), reads=[Pt, cw, Ut], writes=[Ut])
                kb.op("dve", lambda: nc.vector.scalar_tensor_tensor(out=Ut[:, :n], in0=Pt[:, k, 2:n + 2], scalar=cw[:, 2, k:k + 1], in1=Ut[:, :n], op0=ALU.mult, op1=ALU.add), reads=[Pt, cw, Ut], writes=[Ut])
                if k >= 8:
                    kb.op("act", lambda: nc.scalar.activation(out=Ot[:, :n], in_=Ut[:, :n], func=AF.Silu), reads=[Ut], writes=[Ot])
                else:
                    S_, R_ = SQ[k % 2], RS[k % 2]
                    kb.op("act", lambda: nc.scalar.activation(out=Ut[:, :n], in_=Ut[:, :n], func=AF.Silu), reads=[Ut], writes=[Ut])
                    kb.op("pool", lambda: nc.gpsimd.tensor_tensor(out=S_[:, :n], in0=Ut[:, :n], in1=Ut[:, :n], op=ALU.mult), reads=[Ut], writes=[S_])
                    ps = next_ps(C)
                    kb.op("pe", lambda: nc.tensor.matmul(ps[:, :n], lhsT=C.ones_bf[:], rhs=S_[:, :n], start=True, stop=True), reads=[S_, C.ones_bf], writes=[ps])
                    kb.op("act", lambda: nc.scalar.activation(out=R_[:, :n], in_=ps[:, :n], func=AF.Sqrt, bias=C.eps_t[:, 0:1]), reads=[ps, C.eps_t], writes=[R_])
                    kb.op("dve", lambda: nc.vector.reciprocal(out=R_[:, :n], in_=R_[:, :n]), reads=[R_], writes=[R_])
                    sc_ = (128.0 ** -0.5) if k < 4 else 1.0
                    kb.op("dve", lambda: nc.vector.scalar_tensor_tensor(out=Ot[:, :n], in0=Ut[:, :n], scalar=sc_, in1=R_[:, :n], op0=ALU.mult, op1=ALU.mult), reads=[Ut, R_], writes=[Ot])
                kb.dma("pool", qkvT[k * 128:(k + 1) * 128, p0 + o:p0 + o + n], Ot[:, :n], reads=[Ot])
            a8, b8, g8, e8 = A8[ti % 2], B8[ti % 2], G8[ti % 2], E8[ti % 2]
            kb.dma("sp", a8[:, :n], pinT[3584:3592, p0 + o:p0 + o + n], writes=[a8])
            kb.dma("sp", b8[:, :n], pinT[3592:3600, p0 + o:p0 + o + n], writes=[b8])
            kb.op("act", lambda: nc.scalar.activation(out=g8[:, :n], in_=a8[:, :n], func=AF.Exp, bias=av[:, 1:2]), reads=[a8, av], writes=[g8])
            kb.op("act", lambda: nc.scalar.activation(out=g8[:, :n], in_=g8[:, :n], func=AF.Ln, bias=C.one_t[:8, 0:1]), reads=[g8, C.one_t], writes=[g8])
            kb.op("dve", lambda: nc.vector.tensor_scalar(out=g8[:, :n], in0=g8[:, :n], scalar1=av[:, 0:1], scalar2=None, op0=ALU.mult), reads=[g8, av], writes=[g8])
            kb.op("act", lambda: nc.scalar.activation(out=e8[:, :n], in_=b8[:, :n], func=AF.Sigmoid), reads=[b8], writes=[e8])
            for s in range(n // 128):
                ps = next_ps(C)
                kb.op("pe", lambda: nc.tensor.transpose(ps[:, 0:8], g8[:, s * 128:(s + 1) * 128], C.ident_f[:8, :8]), reads=[g8, C.ident_f], writes=[ps])
                kb.op("pe", lambda: nc.tensor.transpose(ps[:, 8:16], e8[:, s * 128:(s + 1) * 128], C.ident_f[:8, :8]), reads=[e8, C.ident_f], writes=[ps])
                G_ = GT[gi % 3]
                gi += 1
                kb.op("dve", lambda: nc.vector.tensor_copy(out=G_[:, :], in_=ps[:, 0:16]), reads=[ps], writes=[G_])
                kb.dma("pool", gbT[p0 + o + s * 128:p0 + o + (s + 1) * 128, :], G_[:, :], reads=[G_])
            ti += 1


def load_gdn_consts(kb, C, trifd, tribd, ms2d, mi1d):
    C.triF = kb.sb("triF", [64, 64], F32)
    C.triB = kb.sb("triB", [64, 64], F32)
    C.mS2 = kb.sb("mS2", [64, 8, 64], F32)
    C.mI1 = kb.sb("mI1", [64, 8, 64], F32)
    C.ones_f = kb.sb("ones_f", [64, 128], F32)
    C.identI = kb.sb("identI", [64, 8, 64], F32)
    kb.dma("sp", C.triF[:], trifd, writes=[C.triF])
    kb.dma("sp", C.triB[:], tribd, writes=[C.triB])
    kb.dma("sp", C.mS2[:], ms2d, writes=[C.mS2])
    kb.dma("sp", C.mI1[:], mi1d, writes=[C.mI1])
    kb.op("dve", lambda: kb.nc.vector.memset(C.ones_f[:], 1.0), writes=[C.ones_f])
    for u in range(8):
        kb.op("dve", lambda: kb.nc.vector.tensor_copy(out=C.identI[:, u, :], in_=C.ident_f[:64, :64]), reads=[C.ident_f], writes=[C.identI])


def gdn_chain(kb, C, st, tag, d, order, qkvT, gbT, outd, banks):
    nc = kb.nc
    V_ = nc.vector
    NU = 4
    bi = [0]

    def nps():
        p = banks[bi[0] % len(banks)]
        bi[0] += 1
        return p

    def sb(nm, shape, dt):
        return kb.sb(f"g{tag}_{nm}", shape, dt, st)
    X = [sb(f"x{i}", [128, 12, 64], F32) for i in range(2)]
    GB = [sb(f"gb{i}", [64, 2, NU], F32) for i in range(2)]
    QT = sb("qt", [128, NU, 64], BF16)
    KT = sb("kt", [128, NU, 64], BF16)
    Gbc = sb("gbc", [64, NU, 128], F32)
    gcs = sb("gc", [64, NU], F32)
    sm = sb("sm", [128, 6, NU], F32)
    Dm = sb("dm", [64, NU, 64], F32)
    D2 = sb("d2", [64, NU, 64], F32)
    E1 = sb("e1", [64, NU, 64], F32)
    E2 = sb("e2", [64, NU, 64], F32)
    EG = sb("eg", [128, NU, 64], F32)
    Mm = sb("m", [64, NU, 64], F32)
    Nn = sb("n", [64, NU, 64], F32)
    AT = sb("at", [64, NU, 64], BF16)
    PN = sb("pn", [64, NU, 64], F32)
    PM = sb("pm", [64, NU, 64], F32)
    XN = [sb(f"xn{i}", [64, NU, 64], F32) for i in range(2)]
    XM = [sb(f"xm{i}", [64, NU, 64], F32) for i in range(2)]
    TT = sb("tt", [64, NU, 64], BF16)
    Kbg = sb("kbg", [64, NU, 128], BF16)
    Kd = sb("kd", [64, NU, 128], BF16)
    Vb = sb("vb", [64, NU, 128], BF16)
    Uu = sb("u", [64, NU, 128], F32)
    WT = sb("wt", [128, NU, 64], BF16)
    QD = sb("qd", [128, NU, 64], BF16)
    Vn = sb("vn", [64, NU, 128], BF16)
    Ot = [sb(f"o{i}", [64, NU, 128], F32) for i in range(2)]
    S = sb("s", [128, NU, 128], F32)
    Sb = sb("sb", [128, NU, 128], BF16)
    kb.op("dve", lambda: V_.memset(S[:], 0.0), writes=[S])
    kb.op("pool", lambda: nc.gpsimd.memset(Sb[:], 0.0), writes=[Sb])
    tri = C.triF if d == 0 else C.triB
    us = slice(d * 4, d * 4 + 4)
    W6 = NU * 64
    W12 = NU * 128

    def bc(ap2, shape):
        return ap2.unsqueeze(2).to_broadcast(shape)

    def v3(ps, p=64):
        return ps[:p, :W6].rearrange("p (u k) -> p u k", k=64)

    def f2(t):
        return t.rearrange("p u k -> p (u k)")
    for s, c in enumerate(order):
        Xd = X[s % 2]
        G = GB[s % 2]
        kb.dma("sp", Xd[:], qkvT.rearrange("(k p) t -> p k t", p=128)[:, :, c * 64:(c + 1) * 64], writes=[Xd])
        kb.dma("sp", G[:, :, :], gbT[c * 64:(c + 1) * 64, :].rearrange("t (a d h) -> t a d h", a=2, d=2)[:, :, d, :], writes=[G])
        yield
        kb.op("act", lambda: nc.scalar.copy(out=QT[:], in_=Xd[:, 0:4, :]), reads=[Xd], writes=[QT])
        kb.op("pool", lambda: nc.gpsimd.tensor_copy(out=KT[:], in_=Xd[:, 4:8, :]), reads=[Xd], writes=[KT])
        psK, psV = nps(), nps()
        for h in range(NU):
            kb.op("pe", lambda: nc.tensor.transpose(psK[:64, h * 128:(h + 1) * 128], Xd[:, 4 + h, :], C.ident_f[:]), reads=[Xd, C.ident_f], writes=[psK])
            kb.op("pe", lambda: nc.tensor.transpose(psV[:64, h * 128:(h + 1) * 128], Xd[:, 8 + h, :], C.ident_f[:]), reads=[Xd, C.ident_f], writes=[psV])
        kb.op("dve", lambda: V_.tensor_copy(out=Gbc[:], in_=bc(G[:, 0, :], [64, NU, 128])), reads=[G], writes=[Gbc])
        psg = nps()
        kb.op("pe", lambda: nc.tensor.matmul(psg[:64, 0:NU], lhsT=tri[:], rhs=G[:, 0, :], start=True, stop=True), reads=[tri, G], writes=[psg])
        kb.op("pe", lambda: nc.tensor.matmul(psg[:, 8:8 + NU], lhsT=C.ones_f[:], rhs=G[:, 0, :], start=True, stop=True), reads=[C.ones_f, G], writes=[psg])
        yield
        psr = nps()
        for u in range(NU):
            kb.op("pe", lambda: nc.tensor.matmul(psr[:, u * 64:(u + 1) * 64], lhsT=Gbc[:, u, :], rhs=tri[:], start=True, stop=True), reads=[Gbc, tri], writes=[psr])
        kb.op("dve", lambda: V_.tensor_copy(out=gcs[:], in_=psg[:64, 0:NU]), reads=[psg], writes=[gcs])
        yield
        kb.op("dve", lambda: V_.tensor_tensor(out=Dm[:], in0=v3(psr), in1=bc(gcs[:, :], [64, NU, 64]), op=ALU.subtract), reads=[psr, gcs], writes=[Dm])
        kb.op("act", lambda: nc.scalar.activation(out=f2(EG[:]), in_=psr[:, :W6], func=AF.Exp), reads=[psr], writes=[EG])
        kb.op("act", lambda: nc.scalar.activation(out=sm[:64, 0, :], in_=gcs[:, :], func=AF.Exp), reads=[gcs], writes=[sm])
        kb.op("dve", lambda: V_.tensor_tensor(out=sm[:64, 4, :], in0=psg[:64, 8:8 + NU], in1=gcs[:, :], op=ALU.subtract), reads=[psg, gcs], writes=[sm])
        yield
        kb.op("pool", lambda: nc.gpsimd.tensor_scalar(out=D2[:], in0=Dm[:], scalar1=-1.0, scalar2=0.0, op0=ALU.mult, op1=ALU.min), reads=[Dm], writes=[D2])
        kb.op("dve", lambda: V_.tensor_scalar(out=Dm[:], in0=Dm[:], scalar1=0.0, scalar2=None, op0=ALU.min), reads=[Dm], writes=[Dm])
        kb.op("act", lambda: nc.scalar.activation(out=sm[:64, 1, :], in_=sm[:64, 4, :], func=AF.Exp), reads=[sm], writes=[sm])
        kb.op("act", lambda: nc.scalar.activation(out=sm[:, 2, :], in_=psg[:, 8:8 + NU], func=AF.Exp), reads=[psg], writes=[sm])
        kb.op("dve", lambda: V_.tensor_tensor(out=sm[:64, 3, :], in0=sm[:64, 0, :], in1=G[:, 1, :], op=ALU.mult), reads=[sm, G], writes=[sm])
        yield
        kb.op("act", lambda: nc.scalar.activation(out=E1[:], in_=Dm[:], func=AF.Exp), reads=[Dm], writes=[E1])
        kb.op("act", lambda: nc.scalar.activation(out=E2[:], in_=D2[:], func=AF.Exp), reads=[D2], writes=[E2])
        pk4 = psK[:64, :W12].rearrange("p (u k) -> p u k", k=128)
        pv4 = psV[:64, :W12].rearrange("p (u k) -> p u k", k=128)
        kb.op("dve", lambda: V_.tensor_tensor(out=Kbg[:], in0=pk4, in1=bc(sm[:64, 3, :], [64, NU, 128]), op=ALU.mult), reads=[psK, sm], writes=[Kbg])
        kb.op("dve", lambda: V_.tensor_tensor(out=Kd[:], in0=pk4, in1=bc(sm[:64, 1, :], [64, NU, 128]), op=ALU.mult), reads=[psK, sm], writes=[Kd])
        kb.op("dve", lambda: V_.tensor_tensor(out=Vb[:], in0=pv4, in1=bc(G[:, 1, :], [64, NU, 128]), op=ALU.mult), reads=[psV, G], writes=[Vb])
        kb.op("pool", lambda: nc.gpsimd.tensor_tensor(out=QD[:], in0=QT[:], in1=EG[:], op=ALU.mult), reads=[QT, EG], writes=[QD])
        pkk, pqk = nps(), nps()
        for u in range(NU):
            kb.op("pe", lambda: nc.tensor.matmul(pkk[:64, u * 64:(u + 1) * 64], lhsT=KT[:, u, :], rhs=KT[:, u, :], start=True, stop=True), reads=[KT], writes=[pkk])
            kb.op("pe", lambda: nc.tensor.matmul(pqk[:64, u * 64:(u + 1) * 64], lhsT=KT[:, u, :], rhs=QT[:, u, :], start=True, stop=True), reads=[KT, QT], writes=[pqk])
        yield
        kb.op("pool", lambda: nc.gpsimd.tensor_tensor(out=E1[:], in0=E1[:], in1=C.mI1[:, us, :], op=ALU.mult), reads=[E1, C.mI1], writes=[E1])
        kb.op("pool", lambda: nc.gpsimd.tensor_tensor(out=E2[:], in0=E2[:], in1=C.mS2[:, us, :], op=ALU.mult), reads=[E2, C.mS2], writes=[E2])
        yield
        kb.op("dve", lambda: V_.tensor_tensor(out=Mm[:], in0=v3(pkk), in1=E2[:], op=ALU.mult), reads=[pkk, E2], writes=[Mm])
        kb.op("dve", lambda: V_.tensor_tensor(out=Mm[:], in0=Mm[:], in1=bc(G[:, 1, :], [64, NU, 64]), op=ALU.mult), reads=[Mm, G], writes=[Mm])
        kb.op("dve", lambda: V_.tensor_tensor(out=AT[:], in0=v3(pqk), in1=E1[:], op=ALU.mult), reads=[pqk, E1], writes=[AT])
        yield
        pn = nps()
        for u in range(NU):
            kb.op("pe", lambda: nc.tensor.transpose(pn[:64, u * 64:(u + 1) * 64], Mm[:, u, :], C.ident_f[:64, :64]), reads=[Mm, C.ident_f], writes=[pn])
        kb.op("pool", lambda: nc.gpsimd.tensor_tensor(out=PM[:], in0=C.identI[:, 0:NU, :], in1=Mm[:], op=ALU.subtract), reads=[C.identI, Mm], writes=[PM])
        yield
        kb.op("act", lambda: nc.scalar.copy(out=Nn[:], in_=v3(pn)), reads=[pn], writes=[Nn])
        kb.op("dve", lambda: V_.scalar_tensor_tensor(out=PN[:], in0=v3(pn), scalar=-1.0, in1=C.identI[:, 0:NU, :], op0=ALU.mult, op1=ALU.add), reads=[C.identI, pn], writes=[PN])
        yield
        pa, pb = nps(), nps()
        for u in range(NU):
            kb.op("pe", lambda: nc.tensor.matmul(pa[:64, u * 64:(u + 1) * 64], lhsT=Mm[:, u, :], rhs=Nn[:, u, :], start=True, stop=True), reads=[Mm, Nn], writes=[pa])
            kb.op("pe", lambda: nc.tensor.matmul(pb[:64, u * 64:(u + 1) * 64], lhsT=Nn[:, u, :], rhs=Mm[:, u, :], start=True, stop=True), reads=[Mm, Nn], writes=[pb])
        yield
        xn, xm = XN[0], XM[0]
        kb.op("act", lambda: nc.scalar.copy(out=xn[:], in_=v3(pa)), reads=[pa], writes=[xn])
        kb.op("dve", lambda: V_.tensor_copy(out=xm[:], in_=v3(pb)), reads=[pb], writes=[xm])
        yield
        for lv in range(5):
            last = lv == 4
            pa = nps()
            for u in range(NU):
                kb.op("pe", lambda: nc.tensor.matmul(pa[:64, u * 64:(u + 1) * 64], lhsT=PM[:, u, :], rhs=xn[:, u, :], start=True, stop=True), reads=[PM, xn], writes=[pa])
            if not last:
                pb = nps()
                for u in range(NU):
                    kb.op("pe", lambda: nc.tensor.matmul(pb[:64, u * 64:(u + 1) * 64], lhsT=xn[:, u, :], rhs=PM[:, u, :], start=True, stop=True), reads=[PM, xn], writes=[pb])
                pc, pd = nps(), nps()
                for u in range(NU):
                    kb.op("pe", lambda: nc.tensor.matmul(pc[:64, u * 64:(u + 1) * 64], lhsT=xm[:, u, :], rhs=xn[:, u, :], start=True, stop=True), reads=[xm, xn], writes=[pc])
                    kb.op("pe", lambda: nc.tensor.matmul(pd[:64, u * 64:(u + 1) * 64], lhsT=xn[:, u, :], rhs=xm[:, u, :], start=True, stop=True), reads=[xm, xn], writes=[pd])
                yield
                xn2, xm2 = XN[(lv + 1) % 2], XM[(lv + 1) % 2]
                kb.op("act", lambda: nc.scalar.copy(out=xn2[:], in_=v3(pc)), reads=[pc], writes=[xn2])
                kb.op("act", lambda: nc.scalar.copy(out=xm2[:], in_=v3(pd)), reads=[pd], writes=[xm2])
                kb.op("dve", lambda: V_.tensor_tensor(out=PM[:], in0=v3(pb), in1=PM[:], op=ALU.add), reads=[PM, pb], writes=[PM])
                kb.op("dve", lambda: V_.tensor_tensor(out=PN[:], in0=v3(pa), in1=PN[:], op=ALU.add), reads=[PN, pa], writes=[PN])
                xn, xm = xn2, xm2
                yield
            else:
                yield
                kb.op("dve", lambda: V_.tensor_tensor(out=TT[:], in0=v3(pa), in1=PN[:], op=ALU.add), reads=[PN, pa], writes=[TT])
                yield
        pu, pw = nps(), nps()
        for u in range(NU):
            kb.op("pe", lambda: nc.tensor.matmul(pu[:64, u * 128:(u + 1) * 128], lhsT=TT[:, u, :], rhs=Vb[:, u, :], start=True, stop=True), reads=[TT, Vb], writes=[pu])
            kb.op("pe", lambda: nc.tensor.matmul(pw[:, u * 64:(u + 1) * 64], lhsT=Kbg[:, u, :], rhs=TT[:, u, :], start=True, stop=True), reads=[TT, Kbg], writes=[pw])
        yield
        kb.op("act", lambda: nc.scalar.copy(out=f2(Uu[:]), in_=pu[:64, :W12]), reads=[pu], writes=[Uu])
        kb.op("act", lambda: nc.scalar.copy(out=f2(WT[:]), in_=pw[:, :W6]), reads=[pw], writes=[WT])
        yield
        pws = nps()
        for u in range(NU):
            kb.op("pe", lambda: nc.tensor.matmul(pws[:64, u * 128:(u + 1) * 128], lhsT=WT[:, u, :], rhs=Sb[:, u, :], start=True, stop=True), reads=[WT, Sb], writes=[pws])
        yield
        kb.op("dve", lambda: V_.scalar_tensor_tensor(out=f2(Vn[:]), in0=pws[:64, :W12], scalar=-1.0, in1=f2(Uu[:]), op0=ALU.mult, op1=ALU.add), reads=[Uu, pws], writes=[Vn])
        yield
        po, pss = nps(), nps()
        for u in range(NU):
            kb.op("pe", lambda: nc.tensor.matmul(po[:64, u * 128:(u + 1) * 128], lhsT=QD[:, u, :], rhs=Sb[:, u, :], start=True, stop=False), reads=[QD, Sb], writes=[po])
            kb.op("pe", lambda: nc.tensor.matmul(po[:64, u * 128:(u + 1) * 128], lhsT=AT[:, u, :], rhs=Vn[:, u, :], start=False, stop=True), reads=[AT, Vn], writes=[po])
        for u in range(NU):
            kb.op("pe", lambda: nc.tensor.matmul(pss[:, u * 128:(u + 1) * 128], lhsT=Kd[:, u, :], rhs=Vn[:, u, :], start=True, stop=True), reads=[Kd, Vn], writes=[pss])
        kb.op("dve", lambda: V_.tensor_tensor(out=S[:], in0=S[:], in1=bc(sm[:, 2, :], [128, NU, 128]), op=ALU.mult), reads=[S, sm], writes=[S])
        yield
        O_ = Ot[s % 2]
        kb.op("act", lambda: nc.scalar.copy(out=f2(O_[:]), in_=po[:64, :W12]), reads=[po], writes=[O_])
        kb.dma("pool", outd[c * 64:(c + 1) * 64, :], f2(O_[:]), reads=[O_])
        kb.op("dve", lambda: V_.tensor_tensor(out=f2(S[:]), in0=pss[:, :W12], in1=f2(S[:]), op=ALU.add), reads=[S, pss], writes=[S])
        yield
        kb.op("act", lambda: nc.scalar.copy(out=Sb[:], in_=S[:]), reads=[S], writes=[Sb])
        yield


def stage_gdn_scan(kb, C, st, fo, bo, qkvT, gbT, ofd, obd):
    gens = [gdn_chain(kb, C, st, "f", 0, fo, qkvT, gbT, ofd, C.psum[0:4]),
            gdn_chain(kb, C, st, "b", 1, bo, qkvT, gbT, obd, C.psum[4:8])]
    alive = list(gens)
    while alive:
        for g in list(alive):
            try:
                next(g)
            except StopIteration:
                alive.remove(g)


def stage_gdn_out(kb, C, st, tiles, ofd, obd, pinT, gnd, yT1):
    nc = kb.nc
    gbc = kb.sb("go_g", [128, 128], F32, st)
    kb.dma("sp", gbc[:], gnd, writes=[gbc])
    Z = [kb.sb(f"go_z{i}", [128, 4, 512], BF16, st) for i in range(2)]
    ZS = [kb.sb(f"go_zs{i}", [128, 4, 512], F32, st) for i in range(2)]
    OF = [kb.sb(f"go_of{i}", [128, 512], F32, st) for i in range(2)]
    OB = [kb.sb(f"go_ob{i}", [128, 512], F32, st) for i in range(2)]
    junk = kb.sb("go_junk", [128, 4, 128], F32, st)
    ss = [kb.sb(f"go_ss{i}", [128, 4], F32, st) for i in range(2)]
    ON = [kb.sb(f"go_on{i}", [128, 512], F32, st) for i in range(2)]
    Y = [kb.sb(f"go_y{i}", [128, 4, 512], BF16, st) for i in range(2)]
    si = 0
    for ti, (p0, n) in enumerate(tiles):
        Zt, ZSt, Yt = Z[ti % 2], ZS[ti % 2], Y[ti % 2]
        kb.dma("sp", Zt[:, :, :n], pinT[3072:3584, :].rearrange("(k p) t -> p k t", p=128)[:, :, p0:p0 + n], writes=[Zt])
        kb.op("act", lambda: nc.scalar.activation(out=ZSt[:, :, :n], in_=Zt[:, :, :n], func=AF.Silu), reads=[Zt], writes=[ZSt])
        pts = [next_ps(C) for _ in range(4)]
        for s in range(n // 128):
            of_, ob_, ss_, on_ = OF[si % 2], OB[si % 2], ss[si % 2], ON[si % 2]
            si += 1
            r0 = p0 + s * 128
            kb.dma("sp", of_[:], ofd[r0:r0 + 128, :], writes=[of_])
            kb.dma("sp", ob_[:], obd[r0:r0 + 128, :], writes=[ob_])
            kb.op("dve", lambda: nc.vector.tensor_tensor(out=of_[:], in0=of_[:], in1=ob_[:], op=ALU.add), reads=[of_, ob_], writes=[of_])
            for h in range(4):
                kb.op("act", lambda: nc.scalar.activation(out=junk[:, h, :], in_=of_[:, h * 128:(h + 1) * 128], func=AF.Square, accum_out=ss_[:, h:h + 1]), reads=[of_], writes=[junk, ss_], waw=(h == 0))
            kb.op("act", lambda: nc.scalar.activation(out=ss_[:], in_=ss_[:], func=AF.Sqrt, scale=1.0 / 128.0, bias=C.eps_t[:, 0:1]), reads=[ss_, C.eps_t], writes=[ss_])
            kb.op("dve", lambda: nc.vector.reciprocal(out=ss_[:], in_=ss_[:]), reads=[ss_], writes=[ss_])
            for h in range(4):
                kb.op("dve", lambda: nc.vector.scalar_tensor_tensor(out=on_[:, h * 128:(h + 1) * 128], in0=of_[:, h * 128:(h + 1) * 128], scalar=ss_[:, h:h + 1], in1=gbc[:],
                                                                   op0=ALU.mult, op1=ALU.mult), reads=[of_, ss_, gbc], writes=[on_])
            for h in range(4):
                kb.op("pe", lambda: nc.tensor.transpose(pts[h][:, s * 128:(s + 1) * 128], on_[:, h * 128:(h + 1) * 128], C.ident_f[:]), reads=[on_, C.ident_f], writes=[pts[h]])
        for h in range(4):
            kb.op("dve", lambda: nc.vector.tensor_tensor(out=Yt[:, h, :n], in0=pts[h][:, :n], in1=ZSt[:, h, :n], op=ALU.mult), reads=[pts[h], ZSt], writes=[Yt])
        kb.dma("pool", yT1.rearrange("(k p) t -> p k t", p=128)[:, :, p0:p0 + n], Yt[:, :, :n], reads=[Yt])

def rope_consts(S=8192, GW=64):
    nf=8
    inv=(10000.0**(-np.arange(nf,dtype=np.float32)/nf)).astype(np.float32)
    t=np.arange(S); row=(t//GW).astype(np.float32); col=(t%GW).astype(np.float32)
    c32=np.zeros((32,S),np.float32); s32=np.zeros((32,S),np.float32)
    for d in range(32):
        pos=row if d<16 else col
        ang=(pos*inv[d%8]).astype(np.float32)
        c32[d]=np.cos(ang); s32[d]=np.sin(ang)
    Rm=np.zeros((32,32),np.float32)
    for m in range(32):
        if m%16<8: Rm[m,m+8]=-1.0
        else: Rm[m,m-8]=1.0
    r32T=np.ascontiguousarray(Rm.T)
    c96=np.ones((96,S),np.float32); s96=np.zeros((96,S),np.float32)
    c96[64:]=c32; s96[64:]=s32
    R96=np.zeros((96,96),np.float32); R96[64:,64:]=Rm
    return np.stack([c96,s96]),np.stack([c32,s32]),np.ascontiguousarray(R96.T),r32T

def fft_consts():
    import ml_dtypes
    p=np.arange(128)
    ang=2*np.pi*np.outer(p,p)/128.0
    Cm=np.cos(ang); Sm=np.sin(ang)
    FA=np.stack([np.concatenate([Cm,-Sm],1),np.concatenate([Cm,Sm],1),np.concatenate([-Sm,Cm],1)]).astype(np.float32)
    FB=np.stack([Cm,Sm,-Sm,-Cm]).astype(np.float32)
    a2=2*np.pi*np.outer(p,p)/16384.0
    TW=np.stack([np.cos(a2),-np.sin(a2),np.sin(a2)]).astype(np.float32)
    return FA,FB,TW

def hyena_pos(L):
    t=np.linspace(0.0,1.0,L,dtype=np.float32)[:,None]
    w=((2.0*np.pi/L)*np.arange(L,dtype=np.float32))[:,None].astype(np.float32)
    f=np.linspace(1e-4,15,16,dtype=np.float32)[None,:]
    z=np.concatenate([t,np.cos(f*w),-np.sin(f*w)],-1).astype(np.float32)
    return np.ascontiguousarray(z.T), np.ascontiguousarray(t[:,0])

def gdn_consts():
    i=np.arange(64)
    triF=(i[:,None]<=i[None,:]).astype(np.float32)
    triB=(i[:,None]>=i[None,:]).astype(np.float32)
    mS2=np.zeros((64,8,64),np.float32); mI1=np.zeros((64,8,64),np.float32)
    for u in range(8):
        if u<4:
            mS2[:,u,:]=(i[None,:]<i[:,None]); mI1[:,u,:]=(i[None,:]>=i[:,None])
        else:
            mS2[:,u,:]=(i[None,:]>i[:,None]); mI1[:,u,:]=(i[None,:]<=i[:,None])
    return triF,triB,mS2,mI1


CTXL = 256


def build_program(S=8192, final=True, debug=False, nlayers=2):
    T = CTXL + S
    kb = KB()
    C = Ctx()
    nc = kb.nc
    setup_consts(kb, C)
    EI = "ExternalInput"
    d = {}

    def inp(name, shape, dt=F32):
        d[name] = kb.dram(name, shape, dt, EI)
        return d[name]
    xT = inp("xT", [1024, S])
    cxT = inp("cxT", [1024, CTXL])
    c2 = inp("c2", [128, 8, 2])
    inp("w_ada", [2, 1024, 6144]); inp("b_ada_l", [2, 128, 48]); inp("g1_l", [2, 128, 8]); inp("g2_l", [2, 128, 8])
    inp("w_in", [2, 1024, NIN])
    inp("hy_conv_w", [2, 128, 3, 12]); inp("hy_conv_b", [2, 128, 12]); inp("hy_f_w1", [2, 33, 64]); inp("hy_f_b1", [2, 64]); inp("hy_f_w2", [2, 64, 64])
    inp("hy_f_b2", [2, 64]); inp("hy_f_w3", [2, 64, 1024]); inp("hy_f_freq", [2, 64]); inp("hy_decay", [2, 128, 8]); inp("hy_bias", [2, 128, 4])
    inp("hy_out", [2, 512, 1024]); inp("gdn_conv_w", [2, 128, 3, 12]); inp("gdn_a_log", [2, 8]); inp("gdn_dt_bias", [2, 8]); inp("gdn_norm_g", [2, 128, 128])
    inp("gdn_out", [2, 512, 1024]); inp("qg_l", [2, 128, 6]); inp("mla_w_uq", [2, 768, 768]); inp("kvg_l", [2, 128, 2]); inp("mla_w_ukv", [2, 256, 1024])
    inp("mla_out", [2, 512, 1024]); inp("w_out", [2, 1024, 1024]); inp("moe_w1", [2, 16, 1024, 512]); inp("moe_w3", [2, 16, 1024, 512]); inp("moe_w2", [2, 16, 512, 1024])
    inp("router_w", [1024, 16]); inp("router_b", [128, 16]); inp("fg_l", [128, 8])
    inp("r32", [32, 32]); inp("cs32", [2, 32, S]); inp("fad", [3, 128, 256]); inp("fbd", [4, 128, 128]); inp("twd", [3, 128, 128])
    inp("zT_lat", [33, S]); inp("tn_lat", [128, S]); inp("zT_ctx", [33, CTXL]); inp("tn_ctx", [128, CTXL])
    inp("trif", [64, 64]); inp("trib", [64, 64]); inp("ms2", [64, 8, 64]); inp("mi1", [64, 8, 64])
    outT = kb.dram("outT", [1024, S], F32, "ExternalOutput")
    dk = "ExternalOutput" if debug else "Internal"
    pinT = kb.dram("pinT", [NIN, T], BF16, dk)
    XA = kb.dram("XA", [1024, T], F32, dk)
    XB = kb.dram("XB", [1024, T], F32, dk)
    hT_lat = kb.dram("hT_lat", [1024, S], BF16)
    hT_ctx = kb.dram("hT_ctx", [1024, CTXL], BF16)
    uvT = kb.dram("uvT", [512, T], BF16)
    x0T = kb.dram("x0T", [512, T], BF16)
    yconv = kb.dram("yconv", [512, T], BF16)
    yT = kb.dram("yT", [3, 512, T], BF16, dk)
    qkvT = kb.dram("qkvT", [1536, T], F32)
    gbT = kb.dram("gbT", [T, 16], F32)
    ofd = kb.dram("ofd", [T, 512], F32)
    obd = kb.dram("obd", [T, 512], F32)
    qTd = kb.dram("qTd", [8, 96, T], BF16)
    kTd = kb.dram("kTd", [8, 96, T], BF16)
    vd = kb.dram("vd", [8, T, 64], BF16)
    load_fft_consts(kb, C, d["fad"], d["fbd"], d["twd"])
    load_gdn_consts(kb, C, d["trif"], d["trib"], d["ms2"], d["mi1"])
    modv = kb.sb("modv", [128, 48, 2], F32)
    AB = kb.sb("AB", [128, 6, 2, 8], F32)
    rw_sb = kb.sb("rw_sb", [128, 8, 16], F32)
    rb_bc = kb.sb("rb_bc", [128, 16], F32)
    r32 = kb.sb("r32_s", [32, 32], BF16)
    qg = kb.sb("qg_s", [128, 6], F32)
    kvg = kb.sb("kvg_s", [128, 2], F32)
    fg = kb.sb("fg_s", [128, 8], F32)
    kb.dma("sp", rw_sb[:], d["router_w"].rearrange("(k p) e -> p k e", p=128), writes=[rw_sb])
    kb.dma("sp", rb_bc[:], d["router_b"], writes=[rb_bc])
    kb.dma("pool", r32[:], d["r32"], writes=[r32])
    kb.dma("sp", fg[:], d["fg_l"], writes=[fg])
    lat_tiles = [(o, min(512, S - o)) for o in range(0, S, 512)]
    NC_ = T // 64
    fo = list(range(NC_))
    bo = [3, 2, 1, 0] + list(range(NC_ - 1, 3, -1))

    def stage():
        kb.barrier()
        return ExitStack()
    for l in range(nlayers):
        first = l == 0
        upd = first
        xsrc, cxsrc = (xT, cxT) if first else (XB[:, CTXL:], XB[:, 0:CTXL])
        st = stage()
        stage_mod(kb, C, st, c2, d["w_ada"][l], d["b_ada_l"][l], d["g1_l"][l], d["g2_l"][l], modv, AB)
        kb.dma("sp", qg[:], d["qg_l"][l], writes=[qg])
        kb.dma("sp", kvg[:], d["kvg_l"][l], writes=[kvg])
        st.close()
        st = stage()
        w_sb = kb.sb("w_sb", [128, 8, NIN], BF16, st)
        for k in range(8):
            kb.dma("pool", w_sb[:, k, :], d["w_in"][l][k * 128:(k + 1) * 128, :], writes=[w_sb])
        tiles = [(cxsrc, 0, CTXL, 0, 1)] + [(xsrc, o, n, CTXL + o, 0) for (o, n) in lat_tiles]
        stage_in(kb, C, st, tiles, AB, w_sb, pinT)
        st.close()
        st = stage()
        stage_hy_filter(kb, C, st, S, d["zT_lat"], d["tn_lat"], d["hy_f_w1"][l], d["hy_f_b1"][l], d["hy_f_w2"][l], d["hy_f_b2"][l], d["hy_f_w3"][l],
                        d["hy_f_freq"][l], d["hy_decay"][l], hT_lat)
        st.close()
        if upd:
            st = stage()
            stage_hy_filter(kb, C, st, CTXL, d["zT_ctx"], d["tn_ctx"], d["hy_f_w1"][l], d["hy_f_b1"][l], d["hy_f_w2"][l], d["hy_f_b2"][l], d["hy_f_w3"][l],
                            d["hy_f_freq"][l], d["hy_decay"][l], hT_ctx)
            st.close()
        st = stage()
        segs = ([(0, CTXL, 0)] if upd else []) + [(CTXL, S, CTXL)]
        stage_hy_conv3(kb, C, st, segs, pinT, d["hy_conv_w"][l], d["hy_conv_b"][l], uvT, x0T)
        st.close()
        st = stage()
        stage_hy_fft(kb, C, st, uvT[:, CTXL:], hT_lat[0:512, :], hT_lat[512:1024, :], yconv[:, CTXL:], S // 128)
        st.close()
        if upd:
            st = stage()
            stage_hy_fft(kb, C, st, uvT[:, 0:CTXL], hT_ctx[0:512, :], hT_ctx[512:1024, :], yconv[:, 0:CTXL], CTXL // 128)
            st.close()
        st = stage()
        gt = ([(0, CTXL)] if upd else []) + [(CTXL + o, n) for (o, n) in lat_tiles]
        stage_hy_gate(kb, C, st, gt, yconv, uvT, x0T, d["hy_bias"][l], yT[0])
        st.close()
        st = stage()
        stage_gdn_prep(kb, C, st, [(0, CTXL), (CTXL, S)], pinT, d["gdn_conv_w"][l], d["gdn_a_log"][l], d["gdn_dt_bias"][l], qkvT, gbT)
        st.close()
        st = stage()
        stage_gdn_scan(kb, C, st, fo, bo, qkvT, gbT, ofd, obd)
        st.close()
        st = stage()
        stage_gdn_out(kb, C, st, gt, ofd, obd, pinT, d["gdn_norm_g"][l], yT[1])
        st.close()
        st = stage()
        wuq = kb.sb("wuq_s", [128, 6, 768], BF16, st)
        wukv = kb.sb("wukv_s", [128, 2, 1024], BF16, st)
        kb.dma("pool", wuq[:], d["mla_w_uq"][l].rearrange("(k p) n -> p k n", p=128), writes=[wuq])
        kb.dma("pool", wukv[:], d["mla_w_ukv"][l].rearrange("(k p) n -> p k n", p=128), writes=[wukv])
        mt = [(0, CTXL, False, 0)] + [(CTXL + o, n, True, o) for (o, n) in lat_tiles]
        stage_mla_prep(kb, C, st, mt, pinT, qg, kvg, wuq, wukv, None, r32, None, d["cs32"], qTd, kTd, vd)
        st.close()
        st = stage()
        jobs = [(CTXL, S, list(range(T // 128)))] + ([(0, CTXL, [0, 1])] if upd else [])
        stage_mla_attn(kb, C, st, jobs, qTd, kTd, vd, yT[2], T)
        st.close()
        st = stage()
        w3s = kb.sb("w3s", [128, 3, 4, 1024], BF16, st)
        wos = kb.sb("wos", [128, 8, 1024], BF16, st)
        for j, nm in enumerate(("hy_out", "gdn_out", "mla_out")):
            kb.dma("pool", w3s[:, j], d[nm][l].rearrange("(k p) n -> p k n", p=128), writes=[w3s])
        kb.dma("pool", wos[:], d["w_out"][l].rearrange("(k p) n -> p k n", p=128), writes=[wos])
        mtiles = ([(cxsrc, 0, CTXL, 0, 1, XA[:, 0:CTXL])] if upd else []) + [(xsrc, o, n, CTXL + o, 0, XA[:, CTXL:]) for (o, n) in lat_tiles]
        stage_merge(kb, C, st, mtiles, AB, yT, pinT, w3s, wos, None)
        st.close()
        st = stage()
        supers = ([(XA[:, 0:CTXL], 0, CTXL, 1, XB[:, 0:CTXL])] if upd else []) + [(XA[:, CTXL:], o, min(1024, S - o), 0, XB[:, CTXL:]) for o in range(0, S, 1024)]
        stage_moe(kb, C, st, supers, AB, rw_sb, rb_bc, d["moe_w1"][l], d["moe_w3"][l], d["moe_w2"][l])
        st.close()
    st = stage()
    X = [kb.sb(f"fn_x{i}", [128, 8, 512], F32, st) for i in range(2)]
    SQ = kb.sb("fn_sq", [128, 8, 512], BF16, st)
    RS = kb.sb("fn_rs", [128, 512], F32, st)
    tmp = [kb.sb(f"fn_t{i}", [128, 512], F32, st) for i in range(2)]
    H = [kb.sb(f"fn_h{i}", [128, 8, 512], F32, st) for i in range(2)]
    src = XB[:, CTXL:].rearrange("(k p) t -> p k t", p=128)
    dst = outT.rearrange("(k p) t -> p k t", p=128)
    for ti, (o, n) in enumerate(lat_tiles):
        Xt, Ht = X[ti % 2], H[ti % 2]
        kb.dma("sp", Xt[:, :, :n], src[:, :, o:o + n], writes=[Xt])
        rmsnorm_tile(kb, C, Xt, n, PV(lambda k: fg[:, k:k + 1], [fg]), None, 0, Ht, SQ, RS, tmp)
        kb.dma("pool", dst[:, :, o:o + n], Ht[:, :, :n], reads=[Ht])
    kb.finish()
    st.close()
    return kb


def _lay(v, k):
    return np.ascontiguousarray(np.asarray(v, np.float32).reshape(k, 128).T)


def make_inputs(inputs, b, S=8192):
    f = lambda a: np.ascontiguousarray(np.asarray(a, np.float32))
    x = np.asarray(inputs["x"][b], np.float32)
    im = {}
    im["xT"] = np.ascontiguousarray(x.T)
    im["cxT"] = np.ascontiguousarray(np.asarray(inputs["ctx"][b], np.float32).T)
    im["c2"] = np.ascontiguousarray(np.stack([_lay(inputs["c"][b], 8), _lay(inputs["c_ctx"], 8)], -1))
    return im


def shared_inputs(inputs, S=8192):
    f = lambda a: np.ascontiguousarray(np.asarray(a, np.float32))
    sh = {}
    for nm in ("w_ada", "w_in", "hy_f_w1", "hy_f_b1", "hy_f_w2", "hy_f_b2", "hy_f_w3", "hy_f_freq", "hy_out",
               "gdn_out", "mla_w_uq", "mla_w_ukv", "mla_out", "w_out", "moe_w1", "moe_w3", "moe_w2", "router_w"):
        sh[nm] = f(inputs[nm])
    cwl = lambda w: np.ascontiguousarray(f(w).reshape(2, 3, 12, 128).transpose(0, 3, 1, 2))
    vl = lambda v, k: np.ascontiguousarray(f(v).reshape(2, k, 128).transpose(0, 2, 1))
    sh["hy_conv_w"] = cwl(inputs["hy_conv_w"]); sh["gdn_conv_w"] = cwl(inputs["gdn_conv_w"])
    sh["hy_conv_b"] = vl(inputs["hy_conv_b"], 12); sh["hy_decay"] = vl(inputs["hy_decay"], 8); sh["hy_bias"] = vl(inputs["hy_bias"], 4)
    sh["gdn_norm_g"] = np.ascontiguousarray(np.broadcast_to(f(inputs["gdn_norm_g"])[:, None, :], (2, 128, 128)))
    sh["router_b"] = np.ascontiguousarray(np.broadcast_to(f(inputs["router_b"])[None, :], (128, 16)))
    sh["b_ada_l"] = np.stack([np.ascontiguousarray(f(inputs["b_ada"])[l].reshape(48, 128).T) for l in range(2)])
    sh["g1_l"] = np.stack([_lay(inputs["norm1_g"][l], 8) for l in range(2)])
    sh["g2_l"] = np.stack([_lay(inputs["norm2_g"][l], 8) for l in range(2)])
    sh["qg_l"] = np.stack([_lay(inputs["mla_q_norm_g"][l], 6) for l in range(2)])
    sh["kvg_l"] = np.stack([_lay(inputs["mla_kv_norm_g"][l], 2) for l in range(2)])
    sh["fg_l"] = _lay(inputs["final_norm_g"], 8)
    sh["gdn_a_log"] = f(inputs["gdn_a_log"]).reshape(2, 8)
    sh["gdn_dt_bias"] = f(inputs["gdn_dt_bias"]).reshape(2, 8)
    cs96, cs32, r96T, r32T = rope_consts(S)
    sh["r32"] = r32T
    sh["cs32"] = cs32
    FA, FB, TW = fft_consts()
    sh["fad"], sh["fbd"], sh["twd"] = FA, FB, TW
    sh["zT_lat"], tl_ = hyena_pos(S)
    sh["zT_ctx"], tc_ = hyena_pos(CTXL)
    sh["tn_lat"] = np.ascontiguousarray(np.broadcast_to(tl_[None, :], (128, S)))
    sh["tn_ctx"] = np.ascontiguousarray(np.broadcast_to(tc_[None, :], (128, CTXL)))
    sh["trif"], sh["trib"], sh["ms2"], sh["mi1"] = gdn_consts()
    sh["ident_f_d"] = np.eye(128, dtype=np.float32)
    return sh


def kernel(**inputs):
    x = np.asarray(inputs["x"])
    B, S, D_ = x.shape
    kb = build_program(S)
    sh = shared_inputs(inputs, S)
    in_maps = []
    for b in range(B):
        im = dict(sh)
        im.update(make_inputs(inputs, b, S))
        in_maps.append(im)
    res = run_bass_kernel_spmd(kb.nc, in_maps, core_ids=list(range(B)))
    out = np.stack([np.ascontiguousarray(np.asarray(r["outT"], np.float32).T) for r in res.results], 0)
    return out.astype(np.float32)
```

```python
import numpy as np
from contextlib import ExitStack
import concourse.bass as bass
import concourse.mybir as mybir
from concourse.bass_utils import run_bass_kernel_spmd

F32 = mybir.dt.float32
BF16 = mybir.dt.bfloat16
AF = mybir.ActivationFunctionType
ALU = mybir.AluOpType
AX = mybir.AxisListType


class Buf:
    __slots__ = ("t", "w", "r", "pr", "excl")

    def __init__(self, t=None, excl=False):
        self.t = t
        self.w = []
        self.r = []
        self.pr = []
        self.excl = excl

    def __getitem__(self, k):
        return self.t[k]


class KB:
    SEM_EPOCH = 20000
    NDMA = 10

    def __init__(self):
        self.nc = bass.Bass("TRN2", target_bir_lowering=False)
        nc = self.nc
        self.es = ExitStack()
        self.eng = {"pe": nc.tensor, "act": nc.scalar, "dve": nc.vector, "pool": nc.gpsimd, "sp": nc.sync}
        self.csem = {}
        self.seen = {e: {} for e in self.eng}
        self.dpool = {}
        self.nsem = 0
        self.last_tok = {}
        self.out_toks = []
        self.ninst = 0

    def _newsem(self, name):
        self.nsem += 1
        return self.es.enter_context(self.nc.semaphore(f"{name}_{self.nsem}"))

    def sb(self, name, shape, dt, stack=None):
        self.nsem += 1
        name = f"{name}_u{self.nsem}"
        t = (stack or self.es).enter_context(self.nc.sbuf_tensor(name, list(shape), dt))
        return Buf(t)

    def ps(self, name, shape=(128, 512), dt=F32, stack=None):
        t = (stack or self.es).enter_context(self.nc.psum_tensor(name, list(shape), dt))
        return Buf(t, excl=True)

    def dram(self, name, shape, dt, kind="Internal"):
        return self.nc.dram_tensor(name, list(shape), dt, kind=kind).ap()

    def _wait(self, e, toks):
        en = self.eng[e]
        best = {}
        for (s, v) in toks:
            if best.get(s, (None, 0))[1] < v:
                best[s] = (s, v)
        for s, v in best.values():
            if self.seen[e].get(s.num, 0) < v:
                en.wait_ge(s, v)
                self.seen[e][s.num] = v
                self.ninst += 1

    def _deps(self, e, reads, writes, waw):
        toks = []
        for b in reads:
            toks.extend(b.w)
            if getattr(b, "excl", False):
                toks.extend(b.r)
        for b in writes:
            toks.extend(b.r)
            toks.extend(b.pr)
            if waw or b.r:
                toks.extend(b.w)
        return toks

    def _commit(self, tok, reads, writes):
        for b in writes:
            if b.r:
                b.pr = list(b.r) + list(b.w)
                b.w = [tok]
                b.r = []
            else:
                b.w = [t for t in b.w if t[0] is not tok[0]] + [tok]
        for b in reads:
            if b not in writes:
                b.r = [t for t in b.r if t[0] is not tok[0]] + [tok]

    def op(self, e, fn, reads=(), writes=(), waw=False):
        toks = self._deps(e, reads, writes, waw)
        if e == "pe":
            mysem = self.csem.get("pe")
            if mysem is not None:
                toks = [t for t in toks if t[0] is not mysem[0]]
        self._wait(e, toks)
        s = self.csem.get(e)
        if s is None or s[1] >= self.SEM_EPOCH:
            s = [self._newsem("c" + e), 0]
            self.csem[e] = s
        ins = fn()
        s[1] += 1
        ins.then_inc(s[0], 1)
        tok = (s[0], s[1])
        self.last_tok[e] = tok
        self._commit(tok, reads, writes)
        self.ninst += 1
        return tok

    def dma(self, q, out, in_, reads=(), writes=(), waw=False, **kw):
        toks = self._deps(q, reads, writes, waw)
        pool = self.dpool.setdefault(q, {"sems": [], "uses": [], "i": 0})
        if len(pool["sems"]) < self.NDMA:
            pool["sems"].append(self._newsem("d" + q))
            pool["uses"].append(0)
            k = len(pool["sems"]) - 1
        else:
            k = pool["i"] % self.NDMA
        pool["i"] += 1
        s = pool["sems"][k]
        u = pool["uses"][k]
        if u > 0:
            toks.append((s, 16 * u))
        self._wait(q, toks)
        ins = self.eng[q].dma_start(out=out, in_=in_, **kw)
        ins.then_inc(s, 16)
        pool["uses"][k] = u + 1
        tok = (s, 16 * (u + 1))
        self._commit(tok, reads, writes)
        self.ninst += 1
        return tok

    def all_tokens(self):
        toks = list(self.last_tok.values())
        for q, pool in self.dpool.items():
            for s, u in zip(pool["sems"], pool["uses"]):
                if u > 0:
                    toks.append((s, 16 * u))
        return toks

    def barrier(self):
        toks = self.all_tokens()
        for e in self.eng:
            self._wait(e, toks)

    def finish(self):
        self.barrier()


D = 1024
NIN = 7728
IN_SEGS = [("hy", 0, 1536), ("qkv", 1536, 1536), ("z", 3072, 512), ("ab", 3584, 16), ("cq", 3600, 768),
           ("ckv", 4368, 256), ("kr", 4624, 32), ("gate", 4656, 3072)]


def mchunks():
    out = []
    for name, c0, w in IN_SEGS:
        o = 0
        while o < w:
            m = min(128, w - o)
            out.append((name, c0 + o, m))
            o += m
    return out


class Ctx:
    pass


def setup_consts(kb, C):
    C.ones_bf = kb.sb("ones_bf", [128, 128], BF16)
    kb.op("dve", lambda: kb.nc.vector.memset(C.ones_bf[:], 1.0), writes=[C.ones_bf])
    C.eps_t = kb.sb("eps_t", [128, 1], F32)
    kb.op("dve", lambda: kb.nc.vector.memset(C.eps_t[:], 1e-6), writes=[C.eps_t])
    C.one_t = kb.sb("one_t", [128, 1], F32)
    kb.op("dve", lambda: kb.nc.vector.memset(C.one_t[:], 1.0), writes=[C.one_t])
    C.psum = [kb.ps(f"ps{i}") for i in range(8)]
    C.ident_f = kb.sb("ident_f", [128, 128], F32)
    C.ident_d = kb.dram("ident_f_d", [128, 128], F32, "ExternalInput")
    kb.dma("sp", C.ident_f[:], C.ident_d, writes=[C.ident_f])
    C.psi = 0


def next_ps(C):
    p = C.psum[C.psi % 8]
    C.psi += 1
    return p


def load_w_bf16(kb, dst, dst_ap, src_ap, q="pool"):
    return kb.dma(q, dst_ap, src_ap, writes=[dst])


def stage_in(kb, C, st, tiles, AB, w_sb, pinT):
    nc = kb.nc
    NB = 2
    xt = [kb.sb(f"in_x{i}", [128, 8, 512], F32, st) for i in range(1)]
    sq = [kb.sb(f"in_sq{i}", [128, 8, 512], BF16, st) for i in range(1)]
    rs = [kb.sb(f"in_rs{i}", [128, 512], F32, st) for i in range(NB)]
    tmp = [kb.sb(f"in_tmp{i}", [128, 512], F32, st) for i in range(4)]
    hT = [kb.sb(f"in_h{i}", [128, 8, 512], BF16, st) for i in range(NB)]
    ob = [kb.sb(f"in_o{i}", [128, 512], BF16, st) for i in range(6)]
    mcs = mchunks()
    oi = 0
    for ti, (src, t0, n, d0, which) in enumerate(tiles):
        b = ti % NB
        X, SQ, RS, H = xt[0], sq[0], rs[b], hT[b]
        kb.dma("sp", X[:, :, :n], src.rearrange("(k p) t -> p k t", p=128)[:, :, t0:t0 + n], writes=[X])
        rmsnorm_tile(kb, C, X, n, PV(lambda k: AB[:, 0, which, k:k + 1], [AB]), PV(lambda k: AB[:, 1, which, k:k + 1], [AB]), which, H, SQ, RS, tmp)
        for mi, (name, c0, m) in enumerate(mcs):
            ps = next_ps(C)
            for k in range(8):
                kb.op("pe", lambda: nc.tensor.matmul(ps[:m, :n], lhsT=w_sb[:, k, c0:c0 + m], rhs=H[:, k, :n], start=(k == 0), stop=(k == 7)),
                      reads=[H, w_sb], writes=[ps])
            O = ob[oi % 6]
            oi += 1
            if name == "gate":
                kb.op("act", lambda: nc.scalar.activation(out=O[:m, :n], in_=ps[:m, :n], func=AF.Sigmoid), reads=[ps], writes=[O])
            elif mi % 2 == 0:
                kb.op("dve", lambda: nc.vector.tensor_copy(out=O[:m, :n], in_=ps[:m, :n]), reads=[ps], writes=[O])
            else:
                kb.op("act", lambda: nc.scalar.copy(out=O[:m, :n], in_=ps[:m, :n]), reads=[ps], writes=[O])
            kb.dma("pool", pinT[c0:c0 + m, d0:d0 + n], O[:m, :n], reads=[O])


def stage_mod(kb, C, st, c2, wada, bada, g1, g2, modv, AB):
    nc = kb.nc
    sc = kb.sb("mod_sc", [128, 8, 2], F32, st)
    ba = kb.sb("mod_ba", [128, 48], F32, st)
    gg = kb.sb("mod_g", [128, 2, 8], F32, st)
    wa = [kb.sb(f"mod_wa{i}", [128, 8, 1536], F32, st) for i in range(2)]
    kb.dma("sp", sc[:], c2, writes=[sc])
    kb.dma("sp", ba[:], bada, writes=[ba])
    kb.dma("sp", gg[:, 0, :], g1, writes=[gg])
    kb.dma("sp", gg[:, 1, :], g2, writes=[gg])
    kb.op("act", lambda: nc.scalar.activation(out=sc[:], in_=sc[:], func=AF.Silu), reads=[sc], writes=[sc])
    for cg in range(4):
        W = wa[cg % 2]
        kb.dma("sp", W[:], wada.rearrange("(k p) n -> p k n", p=128)[:, :, cg * 1536:(cg + 1) * 1536], writes=[W])
        for m in range(12):
            j = cg * 12 + m
            ps = next_ps(C)
            for k in range(8):
                kb.op("pe", lambda: nc.tensor.matmul(ps[:, 0:2], lhsT=W[:, k, m * 128:(m + 1) * 128], rhs=sc[:, k, :], start=(k == 0), stop=(k == 7)),
                      reads=[W, sc], writes=[ps])
            kb.op("dve", lambda: nc.vector.tensor_scalar(out=modv[:, j, :], in0=ps[:, 0:2], scalar1=ba[:, j:j + 1], scalar2=None, op0=ALU.add),
                  reads=[ps, ba], writes=[modv])
    for which in range(2):
        for half, gi in ((0, 0), (1, 1)):
            o = half * 24
            kb.op("dve", lambda: nc.vector.scalar_tensor_tensor(out=AB[:, half * 3 + 0, which, :], in0=modv[:, o + 8:o + 16, which], scalar=1.0, in1=gg[:, gi, :],
                                                               op0=ALU.add, op1=ALU.mult), reads=[modv, gg], writes=[AB])
            kb.op("dve", lambda: nc.vector.tensor_copy(out=AB[:, half * 3 + 1, which, :], in_=modv[:, o:o + 8, which]), reads=[modv], writes=[AB])
            kb.op("dve", lambda: nc.vector.tensor_copy(out=AB[:, half * 3 + 2, which, :], in_=modv[:, o + 16:o + 24, which]), reads=[modv], writes=[AB])


def rmsnorm_tile(kb, C, X, n, Avec, Bvec, which, H, SQ, RS, tmp, Hf=None, nk=8, dim=D):
    nc = kb.nc
    kb.op("act", lambda: nc.scalar.activation(out=SQ[:, :nk, :n], in_=X[:, :nk, :n], func=AF.Square), reads=[X], writes=[SQ])
    pss = next_ps(C)
    for k in range(nk):
        kb.op("pe", lambda: nc.tensor.matmul(pss[:, :n], lhsT=C.ones_bf[:], rhs=SQ[:, k, :n], start=(k == 0), stop=(k == nk - 1)),
              reads=[SQ, C.ones_bf], writes=[pss])
    kb.op("act", lambda: nc.scalar.activation(out=RS[:, :n], in_=pss[:, :n], func=AF.Sqrt, scale=1.0 / dim, bias=C.eps_t[:, 0:1]), reads=[pss, C.eps_t], writes=[RS])
    kb.op("dve", lambda: nc.vector.reciprocal(out=RS[:, :n], in_=RS[:, :n]), reads=[RS], writes=[RS])
    for k in range(nk):
        T = tmp[k % len(tmp)]
        kb.op("dve", lambda: nc.vector.scalar_tensor_tensor(out=T[:, :n], in0=X[:, k, :n], scalar=Avec(k), in1=RS[:, :n],
                                                           op0=ALU.mult, op1=ALU.mult), reads=[X, RS] + Avec.bufs, writes=[T])
        if Bvec is not None:
            kb.op("act", lambda: nc.scalar.activation(out=H[:, k, :n], in_=T[:, :n], func=AF.Identity, bias=Bvec(k)),
                  reads=[T] + Bvec.bufs, writes=[H])
            if Hf is not None:
                kb.op("act", lambda: nc.scalar.activation(out=Hf[:, k, :n], in_=T[:, :n], func=AF.Identity, bias=Bvec(k)),
                      reads=[T] + Bvec.bufs, writes=[Hf])
        else:
            kb.op("act", lambda: nc.scalar.copy(out=H[:, k, :n], in_=T[:, :n]), reads=[T], writes=[H])


class PV:
    def __init__(self, fn, bufs):
        self.fn = fn
        self.bufs = bufs

    def __call__(self, k):
        return self.fn(k)


def stage_merge(kb, C, st, tiles, AB, yT, pinT, w3, wout, xoutT):
    nc = kb.nc
    NB = 2
    xt = [kb.sb(f"mg_x{i}", [128, 8, 512], F32, st) for i in range(NB)]
    yy = [kb.sb(f"mg_y{i}", [128, 3, 4, 512], BF16, st) for i in range(NB)]
    gt = [kb.sb(f"mg_g{i}", [128, 24, 512], BF16, st) for i in range(1)]
    mg = [kb.sb(f"mg_m{i}", [128, 8, 512], BF16, st) for i in range(NB)]
    tt = [kb.sb(f"mg_t{i}", [128, 3, 512], F32, st) for i in range(2)]
    xo = [kb.sb(f"mg_xo{i}", [128, 8, 512], F32, st) for i in range(1)]
    for ti, (src, t0, n, p0, which, dst) in enumerate(tiles):
        b = ti % NB
        X, Y, G, M, XO = xt[b], yy[b], gt[0], mg[b], xo[0]
        kb.dma("sp", X[:, :, :n], src.rearrange("(k p) t -> p k t", p=128)[:, :, t0:t0 + n], writes=[X])
        for j in range(3):
            kb.dma("sp", Y[:, j, :, :n], yT[j].rearrange("(k p) t -> p k t", p=128)[:, :, p0:p0 + n], writes=[Y])
        kb.dma("sp", G[:, :, :n], pinT[4656:7728, :].rearrange("(k p) t -> p k t", p=128)[:, :, p0:p0 + n], writes=[G])
        for m in range(8):
            TT = tt[m % 2]
            pss = []
            for j in range(3):
                ps = next_ps(C)
                pss.append(ps)
                for k in range(4):
                    kb.op("pe", lambda: nc.tensor.matmul(ps[:, :n], lhsT=w3[:, j, k, m * 128:(m + 1) * 128], rhs=Y[:, j, k, :n], start=(k == 0), stop=(k == 3)),
                          reads=[Y, w3], writes=[ps])
            for j in range(3):
                kb.op("dve", lambda: nc.vector.tensor_tensor(out=TT[:, j, :n], in0=pss[j][:, :n], in1=G[:, j * 8 + m, :n], op=ALU.mult),
                      reads=[pss[j], G], writes=[TT])
            kb.op("pool", lambda: nc.gpsimd.tensor_tensor(out=TT[:, 0, :n], in0=TT[:, 0, :n], in1=TT[:, 1, :n], op=ALU.add), reads=[TT], writes=[TT])
            kb.op("pool", lambda: nc.gpsimd.tensor_tensor(out=M[:, m, :n], in0=TT[:, 0, :n], in1=TT[:, 2, :n], op=ALU.add), reads=[TT], writes=[M])
        for m in range(8):
            ps = next_ps(C)
            for k in range(8):
                kb.op("pe", lambda: nc.tensor.matmul(ps[:, :n], lhsT=wout[:, k, m * 128:(m + 1) * 128], rhs=M[:, k, :n], start=(k == 0), stop=(k == 7)),
                      reads=[M, wout], writes=[ps])
            kb.op("dve", lambda: nc.vector.scalar_tensor_tensor(out=XO[:, m, :n], in0=ps[:, :n], scalar=AB[:, 2, which, m:m + 1], in1=X[:, m, :n],
                                                               op0=ALU.mult, op1=ALU.add), reads=[ps, AB, X], writes=[XO])
        kb.dma("pool", dst.rearrange("(k p) t -> p k t", p=128)[:, :, t0:t0 + n], XO[:, :, :n], reads=[XO])


def stage_moe(kb, C, st, supers, AB, rw_sb, rb_bc, w1d, w3d, w2d):
    nc = kb.nc
    X = kb.sb("moe_x", [128, 8, 512], F32, st)
    SQ = kb.sb("moe_sq", [128, 8, 512], BF16, st)
    RS = kb.sb("moe_rs", [128, 512], F32, st)
    tmp = [kb.sb(f"moe_tmp{i}", [128, 512], F32, st) for i in range(2)]
    Hf = kb.sb("moe_hf", [128, 8, 512], F32, st)
    Hb = kb.sb("moe_hb", [128, 8, 1024], BF16, st)
    acc = kb.sb("moe_acc", [128, 8, 1024], F32, st)
    gate = kb.sb("moe_gate", [128, 8, 16], F32, st)
    rt = [kb.sb(f"moe_rt{i}", [128, 64], F32, st) for i in range(2)]
    w1 = [kb.sb(f"moe_w1_{i}", [128, 8, 512], BF16, st) for i in range(2)]
    w3 = [kb.sb(f"moe_w3_{i}", [128, 8, 512], BF16, st) for i in range(2)]
    w2 = [kb.sb(f"moe_w2_{i}", [128, 4, 1024], BF16, st) for i in range(2)]
    he = [kb.sb(f"moe_he{i}", [128, 4, 512], BF16, st) for i in range(2)]
    sl = [kb.sb(f"moe_sl{i}", [128, 512], F32, st) for i in range(2)]
    xo = [kb.sb(f"moe_xo{i}", [128, 8, 512], F32, st) for i in range(1)]
    wi = 0
    for (src, t0, n, which, dst) in supers:
        tl = [(o, min(512, n - o)) for o in range(0, n, 512)]
        srcv = src.rearrange("(k p) t -> p k t", p=128)
        dstv = dst.rearrange("(k p) t -> p k t", p=128)
        for (o, tn) in tl:
            kb.dma("sp", X[:, :, :tn], srcv[:, :, t0 + o:t0 + o + tn], writes=[X])
            Hview = Buf(None)
            rmsnorm_tile(kb, C, X, tn, PV(lambda k: AB[:, 3, which, k:k + 1], [AB]), PV(lambda k: AB[:, 4, which, k:k + 1], [AB]), which,
                         _Off(Hb, o), SQ, RS, tmp, Hf=Hf)
            for s in range(tn // 128):
                sg = (o // 128) + s
                R = rt[sg % 2]
                ps = next_ps(C)
                for k in range(8):
                    kb.op("pe", lambda: nc.tensor.matmul(ps[:, 0:16], lhsT=Hf[:, k, s * 128:(s + 1) * 128], rhs=rw_sb[:, k, :], start=(k == 0), stop=(k == 7)),
                          reads=[Hf, rw_sb], writes=[ps])
                sc = R[:, 0:16]
                sel = R[:, 16:32]
                kb.op("act", lambda: nc.scalar.activation(out=sc, in_=ps[:, 0:16], func=AF.Sigmoid), reads=[ps], writes=[R])
                kb.op("dve", lambda: nc.vector.tensor_tensor(out=sel, in0=sc, in1=rb_bc[:, :], op=ALU.add), reads=[R, rb_bc], writes=[R])
                sel3 = R[:, 16:32].rearrange("p (g j) -> p g j", j=4)
                P3 = R[:, 32:56].rearrange("p (g j) -> p g j", j=6)
                pi = 0
                for a in range(4):
                    for b2 in range(a + 1, 4):
                        kb.op("dve", lambda: nc.vector.tensor_tensor(out=P3[:, :, pi], in0=sel3[:, :, a], in1=sel3[:, :, b2], op=ALU.add), reads=[R], writes=[R])
                        pi += 1
                kb.op("dve", lambda: nc.vector.tensor_reduce(out=R[:, 56:60], in_=P3, axis=AX.X, op=ALU.max), reads=[R], writes=[R])
                kb.op("dve", lambda: nc.vector.tensor_reduce(out=R[:, 60:61], in_=R[:, 56:60], axis=AX.X, op=ALU.max), reads=[R], writes=[R])
                kb.op("dve", lambda: nc.vector.tensor_scalar(out=R[:, 56:60], in0=R[:, 56:60], scalar1=R[:, 60:61], scalar2=None, op0=ALU.is_ge), reads=[R], writes=[R])
                kb.op("dve", lambda: nc.vector.scalar_tensor_tensor(out=sel3, in0=sel3, scalar=2.0, in1=R[:, 56:60].unsqueeze(2).to_broadcast([128, 4, 4]),
                                                                   op0=ALU.add, op1=ALU.mult), reads=[R], writes=[R])
                M1 = R[:, 32:48]
                M2 = R[:, 48:64]
                kb.op("dve", lambda: nc.vector.tensor_reduce(out=R[:, 61:62], in_=sel, axis=AX.X, op=ALU.max), reads=[R], writes=[R])
                G = gate[:, sg, :]
                kb.op("dve", lambda: nc.vector.tensor_scalar(out=G, in0=sel, scalar1=R[:, 61:62], scalar2=None, op0=ALU.is_ge), reads=[R], writes=[gate])
                kb.op("dve", lambda: nc.vector.scalar_tensor_tensor(out=M1, in0=G, scalar=-10.0, in1=sel, op0=ALU.mult, op1=ALU.add), reads=[R, gate], writes=[R])
                kb.op("dve", lambda: nc.vector.tensor_reduce(out=R[:, 61:62], in_=M1, axis=AX.X, op=ALU.max), reads=[R], writes=[R])
                kb.op("dve", lambda: nc.vector.scalar_tensor_tensor(out=G, in0=M1, scalar=R[:, 61:62], in1=G, op0=ALU.is_ge, op1=ALU.add), reads=[R, gate], writes=[gate])
                kb.op("dve", lambda: nc.vector.tensor_tensor(out=G, in0=G, in1=sc, op=ALU.mult), reads=[R, gate], writes=[gate])
                kb.op("dve", lambda: nc.vector.tensor_reduce(out=R[:, 62:63], in_=G, axis=AX.X, op=ALU.add), reads=[gate], writes=[R])
                kb.op("dve", lambda: nc.vector.reciprocal(out=R[:, 62:63], in_=R[:, 62:63]), reads=[R], writes=[R])
                kb.op("dve", lambda: nc.vector.tensor_scalar(out=G, in0=G, scalar1=R[:, 62:63], scalar2=None, op0=ALU.mult), reads=[R, gate], writes=[gate])
        for e in range(16):
            W1, W3, W2 = w1[wi % 2], w3[wi % 2], w2[wi % 2]
            wi += 1
            kb.dma("pool", W1[:], w1d[e].rearrange("(k p) f -> p k f", p=128), writes=[W1])
            kb.dma("pool", W3[:], w3d[e].rearrange("(k p) f -> p k f", p=128), writes=[W3])
            kb.dma("pool", W2[:], w2d[e].rearrange("(k p) f -> p k f", p=128), writes=[W2])
            for ti, (o, tn) in enumerate(tl):
                HE = he[ti % 2]
                for m in range(4):
                    p1 = next_ps(C)
                    p3 = next_ps(C)
                    for k in range(8):
                        kb.op("pe", lambda: nc.tensor.matmul(p1[:, :tn], lhsT=W1[:, k, m * 128:(m + 1) * 128], rhs=Hb[:, k, o:o + tn], start=(k == 0), stop=(k == 7)),
                              reads=[Hb, W1], writes=[p1])
                    for k in range(8):
                        kb.op("pe", lambda: nc.tensor.matmul(p3[:, :tn], lhsT=W3[:, k, m * 128:(m + 1) * 128], rhs=Hb[:, k, o:o + tn], start=(k == 0), stop=(k == 7)),
                              reads=[Hb, W3], writes=[p3])
                    S = sl[m % 2]
                    kb.op("act", lambda: nc.scalar.activation(out=S[:, :tn], in_=p1[:, :tn], func=AF.Silu), reads=[p1], writes=[S])
                    kb.op("dve", lambda: nc.vector.tensor_tensor(out=HE[:, m, :tn], in0=p3[:, :tn], in1=S[:, :tn], op=ALU.mult), reads=[p3, S], writes=[HE])
                for s in range(tn // 128):
                    sg = (o // 128) + s
                    for hf in range(2):
                        po = next_ps(C)
                        for m in range(4):
                            kb.op("pe", lambda: nc.tensor.matmul(po[:, :], lhsT=HE[:, m, s * 128:(s + 1) * 128], rhs=W2[:, m, hf * 512:(hf + 1) * 512], start=(m == 0), stop=(m == 3)),
                                  reads=[HE, W2], writes=[po])
                        A = acc[:, sg, hf * 512:(hf + 1) * 512]
                        if e == 0:
                            kb.op("dve", lambda: nc.vector.tensor_scalar(out=A, in0=po[:, :], scalar1=gate[:, sg, e:e + 1], scalar2=None, op0=ALU.mult),
                                  reads=[po, gate], writes=[acc])
                        else:
                            kb.op("dve", lambda: nc.vector.scalar_tensor_tensor(out=A, in0=po[:, :], scalar=gate[:, sg, e:e + 1], in1=A, op0=ALU.mult, op1=ALU.add),
                                  reads=[po, gate, acc], writes=[acc])
        XO = xo[0]
        for (o, tn) in tl:
            kb.dma("sp", X[:, :, :tn], srcv[:, :, t0 + o:t0 + o + tn], writes=[X])
            for m in range(8):
                pt = next_ps(C)
                for s in range(tn // 128):
                    sg = (o // 128) + s
                    kb.op("pe", lambda: nc.tensor.transpose(pt[:, s * 128:(s + 1) * 128], acc[:, sg, m * 128:(m + 1) * 128], C.ident_f[:]),
                          reads=[acc, C.ident_f], writes=[pt])
                kb.op("dve", lambda: nc.vector.scalar_tensor_tensor(out=XO[:, m, :tn], in0=pt[:, :tn], scalar=AB[:, 5, which, m:m + 1], in1=X[:, m, :tn],
                                                                   op0=ALU.mult, op1=ALU.add), reads=[pt, AB, X], writes=[XO])
            kb.dma("pool", dstv[:, :, t0 + o:t0 + o + tn], XO[:, :, :tn], reads=[XO])


class _Off:
    def __init__(self, b, off):
        self.b = b
        self.off = off

    def __getitem__(self, key):
        p, k, sl_ = key
        return self.b.t[p, k, self.off + (sl_.start or 0):self.off + sl_.stop]

    @property
    def w(self):
        return self.b.w

    @w.setter
    def w(self, v):
        self.b.w = v

    @property
    def pr(self):
        return self.b.pr

    @pr.setter
    def pr(self, v):
        self.b.pr = v

    @property
    def r(self):
        return self.b.r

    @r.setter
    def r(self, v):
        self.b.r = v


def stage_mla_prep(kb, C, st, tiles, pinT, qg, kvg, wuq, wukv, r96, r32, cs96, cs32, qTd, kTd, vd):
    nc = kb.nc
    cq = [kb.sb(f"mp_cq{i}", [128, 6, 512], BF16, st) for i in range(2)]
    ckv = [kb.sb(f"mp_ckv{i}", [128, 2, 512], BF16, st) for i in range(2)]
    kr = [kb.sb(f"mp_kr{i}", [32, 512], BF16, st) for i in range(2)]
    SQ = kb.sb("mp_sq", [128, 6, 512], BF16, st)
    RS = kb.sb("mp_rs", [128, 512], F32, st)
    tmp = [kb.sb(f"mp_tmp{i}", [128, 512], F32, st) for i in range(2)]
    cqn = kb.sb("mp_cqn", [128, 6, 512], BF16, st)
    ckvn = kb.sb("mp_ckvn", [128, 2, 512], BF16, st)
    t96 = [kb.sb(f"mp_t96{i}", [96, 2, 512], F32, st) for i in range(2)]
    t32 = [kb.sb(f"mp_t32{i}", [32, 2, 512], F32, st) for i in range(2)]
    qb = [kb.sb(f"mp_qb{i}", [96, 512], BF16, st) for i in range(2)]
    qf = [kb.sb(f"mp_qf{i}", [96, 512], F32, st) for i in range(2)]
    qo = [kb.sb(f"mp_qo{i}", [96, 512], BF16, st) for i in range(3)]
    ko = [kb.sb(f"mp_ko{i}", [64, 512], BF16, st) for i in range(6)]
    qi2 = [0]
    qr_ = [kb.sb(f"mp_qr{i}", [32, 512], BF16, st) for i in range(3)]
    krf2 = [kb.sb(f"mp_krf2{i}", [32, 512], F32, st) for i in range(2)]
    krg2 = [kb.sb(f"mp_krg2{i}", [32, 512], F32, st) for i in range(2)]
    kro2 = [kb.sb(f"mp_kro2{i}", [32, 512], BF16, st) for i in range(3)]
    kro = [kb.sb(f"mp_kro{i}", [32, 512], BF16, st) for i in range(2)]
    krf = [kb.sb(f"mp_krf{i}", [32, 512], F32, st) for i in range(2)]
    vo = [kb.sb(f"mp_vo{i}", [128, 512], BF16, st) for i in range(3)]
    qi = 0
    for ti, (p0, n, rope, tl0) in enumerate(tiles):
        b = ti % 2
        CQ, CKV, KR, T96, T32 = cq[b], ckv[b], kr[b], t96[b], t32[b]
        kb.dma("sp", CQ[:, :, :n], pinT[3600:4368, :].rearrange("(k p) t -> p k t", p=128)[:, :, p0:p0 + n], writes=[CQ])
        kb.dma("sp", CKV[:, :, :n], pinT[4368:4624, :].rearrange("(k p) t -> p k t", p=128)[:, :, p0:p0 + n], writes=[CKV])
        kb.dma("sp", KR[:, :n], pinT[4624:4656, p0:p0 + n], writes=[KR])
        if rope:
            kb.dma("sp", T32[:, :, :n], cs32.rearrange("c d t -> d c t")[:, :, tl0:tl0 + n], writes=[T32])
        rmsnorm_tile(kb, C, CQ, n, PV(lambda k: qg[:, k:k + 1], [qg]), None, 0, cqn, SQ, RS, tmp, nk=6, dim=768)
        rmsnorm_tile(kb, C, CKV, n, PV(lambda k: kvg[:, k:k + 1], [kvg]), None, 0, ckvn, SQ, RS, tmp, nk=2, dim=256)
        KRO = kro[b]
        if rope:
            KRF = krf[b]
            ps = next_ps(C)
            kb.op("pe", lambda: nc.tensor.matmul(ps[:32, :n], lhsT=r32[:, :], rhs=KR[:, :n], start=True, stop=True), reads=[KR, r32], writes=[ps])
            kb.op("dve", lambda: nc.vector.tensor_tensor(out=KRF[:, :n], in0=ps[:32, :n], in1=T32[:, 1, :n], op=ALU.mult), reads=[ps, T32], writes=[KRF])
            KRG = krg2[b]
            kb.op("pool", lambda: nc.gpsimd.tensor_tensor(out=KRG[:, :n], in0=T32[:, 0, :n], in1=KR[:, :n], op=ALU.mult), reads=[T32, KR], writes=[KRG])
            kb.op("pool", lambda: nc.gpsimd.tensor_tensor(out=KRO[:, :n], in0=KRG[:, :n], in1=KRF[:, :n], op=ALU.add), reads=[KRG, KRF], writes=[KRO])
        else:
            kb.op("pool", lambda: nc.gpsimd.tensor_copy(out=KRO[:, :n], in_=KR[:, :n]), reads=[KR], writes=[KRO])
        for h in range(8):
            kb.dma("pool", kTd[h, 64:96, p0:p0 + n], KRO[:, :n], reads=[KRO])
        for h in range(8):
            ps = next_ps(C)
            for k in range(6):
                kb.op("pe", lambda: nc.tensor.matmul(ps[:64, :n], lhsT=wuq[:, k, h * 96:h * 96 + 64], rhs=cqn[:, k, :n], start=(k == 0), stop=(k == 5)),
                      reads=[cqn, wuq], writes=[ps])
            QO = ko[qi2[0] % 6]
            qi2[0] += 1
            kb.op("act", lambda: nc.scalar.copy(out=QO[:, :n], in_=ps[:64, :n]), reads=[ps], writes=[QO])
            kb.dma("pool", qTd[h, 0:64, p0:p0 + n], QO[:, :n], reads=[QO])
            ps = next_ps(C)
            for k in range(6):
                kb.op("pe", lambda: nc.tensor.matmul(ps[:32, :n], lhsT=wuq[:, k, h * 96 + 64:h * 96 + 96], rhs=cqn[:, k, :n], start=(k == 0), stop=(k == 5)),
                      reads=[cqn, wuq], writes=[ps])
            QR = qr_[qi % 3]
            kb.op("act", lambda: nc.scalar.copy(out=QR[:, :n], in_=ps[:32, :n]), reads=[ps], writes=[QR])
            if rope:
                QF, QG, QO2 = krf2[qi % 2], krg2[qi % 2], kro2[qi % 3]
                ps2 = next_ps(C)
                kb.op("pe", lambda: nc.tensor.matmul(ps2[:32, :n], lhsT=r32[:, :], rhs=QR[:, :n], start=True, stop=True), reads=[QR, r32], writes=[ps2])
                kb.op("dve", lambda: nc.vector.tensor_tensor(out=QF[:, :n], in0=ps2[:32, :n], in1=T32[:, 1, :n], op=ALU.mult), reads=[ps2, T32], writes=[QF])
                kb.op("pool", lambda: nc.gpsimd.tensor_tensor(out=QG[:, :n], in0=T32[:, 0, :n], in1=QR[:, :n], op=ALU.mult), reads=[T32, QR], writes=[QG])
                kb.op("pool", lambda: nc.gpsimd.tensor_tensor(out=QO2[:, :n], in0=QG[:, :n], in1=QF[:, :n], op=ALU.add), reads=[QG, QF], writes=[QO2])
                kb.dma("pool", qTd[h, 64:96, p0:p0 + n], QO2[:, :n], reads=[QO2])
            else:
                kb.dma("pool", qTd[h, 64:96, p0:p0 + n], QR[:, :n], reads=[QR])
            ps = next_ps(C)
            for k in range(2):
                kb.op("pe", lambda: nc.tensor.matmul(ps[:64, :n], lhsT=wukv[:, k, h * 128:h * 128 + 64], rhs=ckvn[:, k, :n], start=(k == 0), stop=(k == 1)),
                      reads=[ckvn, wukv], writes=[ps])
            KO = ko[qi2[0] % 6]
            qi2[0] += 1
            kb.op("act", lambda: nc.scalar.copy(out=KO[:, :n], in_=ps[:64, :n]), reads=[ps], writes=[KO])
            kb.dma("pool", kTd[h, 0:64, p0:p0 + n], KO[:, :n], reads=[KO])
            qi += 1
        for s in range(n // 128):
            ps = next_ps(C)
            for k in range(2):
                kb.op("pe", lambda: nc.tensor.matmul(ps[:, :].rearrange("p (h c) -> p h c", c=64), lhsT=ckvn[:, k, s * 128:(s + 1) * 128],
                                                     rhs=wukv[:, k, :].rearrange("p (h c) -> p h c", c=128)[:, :, 64:128], start=(k == 0), stop=(k == 1)),
                      reads=[ckvn, wukv], writes=[ps])
            VO = vo[s % 3]
            kb.op("dve", lambda: nc.vector.tensor_copy(out=VO[:, :], in_=ps[:, :]), reads=[ps], writes=[VO])
            kb.dma("pool", vd.rearrange("h t c -> t h c")[p0 + s * 128:p0 + (s + 1) * 128, :, :], VO[:, :].rearrange("p (h c) -> p h c", c=64), reads=[VO])


def stage_mla_attn(kb, C, st, jobs, qTd, kTd, vd, attnT, T):
    nc = kb.nc
    scale = 96.0 ** -0.5
    NKT = T // 128
    LOOK = 3
    kT = [kb.sb(f"at_k{i}", [96, T], BF16, st) for i in range(2)]
    V = [kb.sb(f"at_v{i}", [128, NKT, 65], BF16, st) for i in range(2)]
    Q = [kb.sb(f"at_q{i}", [96, 512], BF16, st) for i in range(2)]
    P = [kb.sb(f"at_p{i}", [128, 512], BF16, st) for i in range(6)]
    rrow = [kb.sb(f"at_rr{i}", [65, 512], F32, st) for i in range(2)]
    rbc = [kb.sb(f"at_rb{i}", [64, 512], F32, st) for i in range(2)]
    O = [kb.sb(f"at_o{i}", [64, 512], BF16, st) for i in range(2)]
    ones65 = kb.sb("at_ones", [65, 64], F32, st)
    kb.op("dve", lambda: nc.vector.memset(ones65[:], 1.0), writes=[ones65])
    for i in range(2):
        kb.op("pool", lambda: nc.gpsimd.memset(V[i][:, :, 64:65], 1.0), writes=[V[i]])
    sps = C.psum[0:4]
    aps = C.psum[4:8]
    si = 0
    pi_ = 0
    qi = 0
    for h in range(8):
        K_, V_ = kT[h % 2], V[h % 2]
        kb.dma("sp", K_[:, :], kTd[h], writes=[K_])
        kb.dma("sp", V_[:, :, 0:64], vd[h].rearrange("(j p) c -> p j c", p=128), writes=[V_])
        for (q0, nq, ktiles) in jobs:
            for o in range(0, nq, 512):
                n = min(512, nq - o)
                Qt = Q[qi % 2]
                po, pb = aps[(qi % 2) * 2], aps[(qi % 2) * 2 + 1]
                kb.dma("sp", Qt[:, :n], qTd[h, :, q0 + o:q0 + o + n], writes=[Qt])
                nk = len(ktiles)
                pend = []
                for ji in range(nk + LOOK):
                    if ji < nk:
                        j = ktiles[ji]
                        ps = sps[si % 4]
                        si += 1
                        Pt = P[pi_ % 6]
                        pi_ += 1
                        kb.op("pe", lambda: nc.tensor.matmul(ps[:, :n], lhsT=K_[:, j * 128:(j + 1) * 128], rhs=Qt[:, :n], start=True, stop=True),
                              reads=[K_, Qt], writes=[ps])
                        kb.op("act", lambda: nc.scalar.activation(out=Pt[:, :n], in_=ps[:, :n], func=AF.Exp, scale=scale), reads=[ps], writes=[Pt])
                        pend.append((j, Pt))
                    if ji >= LOOK:
                        jj = ji - LOOK
                        j, Pt = pend[jj]
                        kb.op("pe", lambda: nc.tensor.matmul(po[:65, :n], lhsT=V_[:, j, :], rhs=Pt[:, :n], start=(jj == 0), stop=(jj == nk - 1)),
                              reads=[V_, Pt], writes=[po])
                RR, RB, O_ = rrow[qi % 2], rbc[qi % 2], O[qi % 2]
                kb.op("dve", lambda: nc.vector.reciprocal(out=RR[64:65, :n], in_=po[64:65, :n]), reads=[po], writes=[RR])
                kb.op("pe", lambda: nc.tensor.matmul(pb[:64, :n], lhsT=ones65[64:65, :], rhs=RR[64:65, :n], start=True, stop=True), reads=[ones65, RR], writes=[pb])
                kb.op("act", lambda: nc.scalar.copy(out=RB[:, :n], in_=pb[:64, :n]), reads=[pb], writes=[RB])
                kb.op("dve", lambda: nc.vector.tensor_tensor(out=O_[:, :n], in0=po[:64, :n], in1=RB[:, :n], op=ALU.mult), reads=[po, RB], writes=[O_])
                kb.dma("pool", attnT[h * 64:(h + 1) * 64, q0 + o:q0 + o + n], O_[:, :n], reads=[O_])
                qi += 1


def hy_sin(kb, nc, out, ps, n, fr, tmpa, tmpb):
    kb.op("act", lambda: nc.scalar.activation(out=tmpa[:64, :n], in_=ps[:64, :n], func=AF.Sin, scale=fr[:, 0:1], bias=fr[:, 1:2]), reads=[ps, fr], writes=[tmpa])
    kb.op("act", lambda: nc.scalar.activation(out=tmpb[:64, :n], in_=ps[:64, :n], func=AF.Sin, scale=fr[:, 2:3], bias=fr[:, 3:4]), reads=[ps, fr], writes=[tmpb])
    kb.op("dve", lambda: nc.vector.tensor_tensor(out=tmpb[:64, :n], in0=tmpb[:64, :n], in1=tmpb[:64, :n], op=ALU.mult), reads=[tmpb], writes=[tmpb])
    kb.op("dve", lambda: nc.vector.tensor_scalar(out=tmpb[:64, :n], in0=tmpb[:64, :n], scalar1=-2.0, scalar2=1.0, op0=ALU.mult, op1=ALU.add), reads=[tmpb], writes=[tmpb])
    kb.op("dve", lambda: nc.vector.scalar_tensor_tensor(out=out[:64, :n], in0=tmpa[:64, :n], scalar=2.0, in1=tmpb[:64, :n], op0=ALU.mult, op1=ALU.mult), reads=[tmpa, tmpb], writes=[out])


def stage_hy_filter(kb, C, st, L, zT, tn, w1d, b1d, w2d, b2d, w3d, frd, decd, hTd):
    nc = kb.nc
    w1 = kb.sb("hf_w1", [33, 64], F32, st)
    w2 = kb.sb("hf_w2", [64, 64], F32, st)
    w3 = kb.sb("hf_w3", [64, 1024], F32, st)
    v = kb.sb("hf_v", [64, 3], F32, st)
    fr1 = kb.sb("hf_fr1", [64, 4], F32, st)
    fr2 = kb.sb("hf_fr2", [64, 4], F32, st)
    dec = kb.sb("hf_dec", [128, 8], F32, st)
    kb.dma("sp", w1[:], w1d, writes=[w1])
    kb.dma("sp", w2[:], w2d, writes=[w2])
    kb.dma("sp", w3[:], w3d, writes=[w3])
    kb.dma("sp", v[:, 0:1], b1d.rearrange("(p o) -> p o", o=1), writes=[v])
    kb.dma("sp", v[:, 1:2], b2d.rearrange("(p o) -> p o", o=1), writes=[v])
    kb.dma("sp", v[:, 2:3], frd.rearrange("(p o) -> p o", o=1), writes=[v])
    kb.dma("sp", dec[:], decd, writes=[dec])
    for fr, bi in ((fr1, 0), (fr2, 1)):
        kb.op("dve", lambda: nc.vector.tensor_scalar(out=fr[:, 0:1], in0=v[:, 2:3], scalar1=0.5, scalar2=None, op0=ALU.mult), reads=[v], writes=[fr])
        kb.op("dve", lambda: nc.vector.scalar_tensor_tensor(out=fr[:, 1:2], in0=v[:, 2:3], scalar=0.5, in1=v[:, bi:bi + 1], op0=ALU.mult, op1=ALU.mult), reads=[v], writes=[fr])
        kb.op("dve", lambda: nc.vector.tensor_scalar(out=fr[:, 2:4], in0=fr[:, 0:2], scalar1=0.5, scalar2=None, op0=ALU.mult), reads=[fr], writes=[fr])
    kb.op("act", lambda: nc.scalar.activation(out=dec[:], in_=dec[:], func=AF.Abs), reads=[dec], writes=[dec])
    kb.op("dve", lambda: nc.vector.tensor_scalar(out=dec[:], in0=dec[:], scalar1=-1.0, scalar2=None, op0=ALU.mult), reads=[dec], writes=[dec])
    z = [kb.sb(f"hf_z{i}", [33, 512], F32, st) for i in range(2)]
    tb = [kb.sb(f"hf_tb{i}", [128, 512], F32, st) for i in range(2)]
    ta = kb.sb("hf_ta", [64, 512], F32, st)
    tc_ = kb.sb("hf_tc", [64, 512], F32, st)
    h1 = kb.sb("hf_h1", [64, 512], F32, st)
    h2 = kb.sb("hf_h2", [64, 512], F32, st)
    win = [kb.sb(f"hf_win{i}", [128, 512], F32, st) for i in range(2)]
    ho = [kb.sb(f"hf_ho{i}", [128, 512], BF16, st) for i in range(3)]
    oi = 0
    for ti, o in enumerate(range(0, L, 512)):
        n = min(512, L - o)
        Z, TB = z[ti % 2], tb[ti % 2]
        kb.dma("sp", Z[:, :n], zT[:, o:o + n], writes=[Z])
        kb.dma("sp", TB[:, :n], tn[:, o:o + n], writes=[TB])
        ps = next_ps(C)
        kb.op("pe", lambda: nc.tensor.matmul(ps[:64, :n], lhsT=w1[:, :], rhs=Z[:, :n], start=True, stop=True), reads=[w1, Z], writes=[ps])
        hy_sin(kb, nc, h1, ps, n, fr1, ta, tc_)
        ps = next_ps(C)
        kb.op("pe", lambda: nc.tensor.matmul(ps[:64, :n], lhsT=w2[:, :], rhs=h1[:, :n], start=True, stop=True), reads=[w2, h1], writes=[ps])
        hy_sin(kb, nc, h2, ps, n, fr2, ta, tc_)
        for cch in range(8):
            ps = next_ps(C)
            kb.op("pe", lambda: nc.tensor.matmul(ps[:, :n], lhsT=w3[:, cch * 128:(cch + 1) * 128], rhs=h2[:, :n], start=True, stop=True), reads=[w3, h2], writes=[ps])
            W = win[cch % 2]
            kb.op("act", lambda: nc.scalar.activation(out=W[:, :n], in_=TB[:, :n], func=AF.Exp, scale=dec[:, cch:cch + 1]), reads=[TB, dec], writes=[W])
            HO = ho[oi % 3]
            oi += 1
            kb.op("dve", lambda: nc.vector.scalar_tensor_tensor(out=HO[:, :n], in0=W[:, :n], scalar=HY_SHIFT, in1=ps[:, :n], op0=ALU.add, op1=ALU.mult), reads=[W, ps], writes=[HO])
            if cch >= 4 and o == 0:
                kb.op("dve", lambda: nc.vector.memset(HO[:, 0:1], 0.0), reads=[HO], writes=[HO], waw=True)
            kb.dma("pool", hTd[cch * 128:(cch + 1) * 128, o:o + n], HO[:, :n], reads=[HO])


HY_SHIFT = 0.05


def stage_hy_conv3(kb, C, st, segs, pinT, cwd, cbd, uvT, x0T):
    nc = kb.nc
    cw = kb.sb("hc_w", [128, 3, 12], F32, st)
    cb = kb.sb("hc_b", [128, 12], F32, st)
    kb.dma("sp", cw[:], cwd, writes=[cw])
    kb.dma("sp", cb[:], cbd, writes=[cb])
    P = [kb.sb(f"hc_p{i}", [128, 12, 514], BF16, st) for i in range(2)]
    U = [kb.sb(f"hc_u{i}", [128, 512], F32, st) for i in range(6)]
    O = [kb.sb(f"hc_o{i}", [128, 512], BF16, st) for i in range(4)]
    ti = 0
    ui = 0
    oi = 0
    for (p0, Ls, d0) in segs:
        for o in range(0, Ls, 512):
            n = min(512, Ls - o)
            Pt = P[ti % 2]
            ti += 1
            lo = 1 if o == 0 else 0
            hi = 1 if o + n == Ls else 0
            if lo:
                kb.op("pool", lambda: nc.gpsimd.memset(Pt[:, :, 0:1], 0.0), writes=[Pt])
            if hi:
                kb.op("pool", lambda: nc.gpsimd.memset(Pt[:, :, n + 1:n + 2], 0.0), writes=[Pt])
            kb.dma("sp", Pt[:, :, lo:n + 2 - hi], pinT[0:1536, :].rearrange("(k p) t -> p k t", p=128)[:, :, p0 + o - 1 + lo:p0 + o + n + 1 - hi], writes=[Pt])
            us = []
            for k in range(12):
                Ut = U[ui % 6]
                ui += 1
                kb.op("act", lambda: nc.scalar.activation(out=Ut[:, :n], in_=Pt[:, k, 1:n + 1], func=AF.Identity, scale=cw[:, 1, k:k + 1], bias=cb[:, k:k + 1]), reads=[Pt, cw, cb], writes=[Ut])
                kb.op("dve", lambda: nc.vector.scalar_tensor_tensor(out=Ut[:, :n], in0=Pt[:, k, 0:n], scalar=cw[:, 0, k:k + 1], in1=Ut[:, :n], op0=ALU.mult, op1=ALU.add), reads=[Pt, cw, Ut], writes=[Ut])
                eng = "dve" if k < 4 else "pool"
                e_ = nc.vector if k < 4 else nc.gpsimd
                if k < 4:
                    Ot = O[oi % 4]
                    oi += 1
                    kb.op("dve", lambda: nc.vector.scalar_tensor_tensor(out=Ot[:, :n], in0=Pt[:, k, 2:n + 2], scalar=cw[:, 2, k:k + 1], in1=Ut[:, :n], op0=ALU.mult, op1=ALU.add), reads=[Pt, cw, Ut], writes=[Ot])
                    kb.dma("pool", x0T[k * 128:(k + 1) * 128, d0 + o:d0 + o + n], Ot[:, :n], reads=[Ot])
                else:
                    kb.op("dve", lambda: nc.vector.scalar_tensor_tensor(out=Ut[:, :n], in0=Pt[:, k, 2:n + 2], scalar=cw[:, 2, k:k + 1], in1=Ut[:, :n], op0=ALU.mult, op1=ALU.add), reads=[Pt, cw, Ut], writes=[Ut])
                    us.append(Ut)
                if k >= 8:
                    Ot = O[oi % 4]
                    oi += 1
                    X1 = us[k - 8]
                    kb.op("pool", lambda: nc.gpsimd.tensor_tensor(out=Ot[:, :n], in0=X1[:, :n], in1=Ut[:, :n], op=ALU.mult), reads=[X1, Ut], writes=[Ot])
                    kb.dma("pool", uvT[(k - 8) * 128:(k - 7) * 128, d0 + o:d0 + o + n], Ot[:, :n], reads=[Ot])


def load_fft_consts(kb, C, fad, fbd, twd):
    C.FA = kb.sb("FA", [128, 3, 256], BF16)
    C.FB = kb.sb("FB", [128, 4, 128], BF16)
    C.TW = kb.sb("TW", [128, 3, 128], F32)
    kb.dma("pool", C.FA[:], fad.rearrange("j p n -> p j n"), writes=[C.FA])
    kb.dma("pool", C.FB[:], fbd.rearrange("j p n -> p j n"), writes=[C.FB])
    kb.dma("sp", C.TW[:], twd.rearrange("j p n -> p j n"), writes=[C.TW])


def _twiddle(kb, C, nc, ps, Yr, Yi, c, ti_idx, tmps):
    psv = ps[:, :].rearrange("p (c r k) -> p c r k", c=2, r=2)
    Tr = C.TW[:, 0, :].unsqueeze(1).to_broadcast([128, 2, 128])
    Ti = C.TW[:, ti_idx, :].unsqueeze(1).to_broadcast([128, 2, 128])
    t1, t2, t3, t4 = tmps
    kb.op("dve", lambda: nc.vector.tensor_tensor(out=t1[:, :, :], in0=psv[:, :, 0, :], in1=Tr, op=ALU.mult), reads=[ps, C.TW], writes=[t1])
    kb.op("dve", lambda: nc.vector.tensor_tensor(out=t2[:, :, :], in0=psv[:, :, 1, :], in1=Ti, op=ALU.mult), reads=[ps, C.TW], writes=[t2])
    kb.op("pool", lambda: nc.gpsimd.tensor_tensor(out=Yr[:, c:c + 2, :], in0=t1[:, :, :], in1=t2[:, :, :], op=ALU.subtract), reads=[t1, t2], writes=[Yr])
    kb.op("dve", lambda: nc.vector.tensor_tensor(out=t3[:, :, :], in0=psv[:, :, 0, :], in1=Ti, op=ALU.mult), reads=[ps, C.TW], writes=[t3])
    kb.op("dve", lambda: nc.vector.tensor_tensor(out=t4[:, :, :], in0=psv[:, :, 1, :], in1=Tr, op=ALU.mult), reads=[ps, C.TW], writes=[t4])
    kb.op("pool", lambda: nc.gpsimd.tensor_tensor(out=Yi[:, c:c + 2, :], in0=t3[:, :, :], in1=t4[:, :, :], op=ALU.add), reads=[t3, t4], writes=[Yi])


def stage_hy_fft(kb, C, st, uv, hf, hb, yout, nb):
    nc = kb.nc
    GC = 32
    xin = [kb.sb(f"ff_x{i}", [64, GC, 128], BF16, st) for i in range(3)]
    Yr = [kb.sb(f"ff_yr{i}", [128, GC, 128], BF16, st) for i in range(3)]
    Yi = [kb.sb(f"ff_yi{i}", [128, GC, 128], BF16, st) for i in range(3)]
    Kr = kb.sb("ff_kr", [128, GC, 128], F32, st)
    Ki = kb.sb("ff_ki", [128, GC, 128], F32, st)
    Zr = kb.sb("ff_zr", [128, GC, 128], BF16, st)
    Zi = kb.sb("ff_zi", [128, GC, 128], BF16, st)
    tmpsA = [[kb.sb(f"ff_t{j}_{i}", [128, 2, 128], F32, st) for i in range(4)] for j in range(2)]
    tq = [kb.sb(f"ff_q{i}", [128, 512], F32, st) for i in range(4)]
    yo = [kb.sb(f"ff_yo{i}", [64, GC, 128], BF16, st) for i in range(2)]
    Cm, Sm, Sn, Cn = (C.FB[:, j, :] for j in range(4))
    pi_ = 0
    for g in range(512 // GC):
        c0 = g * GC
        for s, src in enumerate((uv, hf, hb)):
            kb.dma("sp", xin[s][:nb, :, :], src[c0:c0 + GC, :].rearrange("c (b p) -> b c p", p=128), writes=[xin[s]])
        for s in range(3):
            for c in range(0, GC, 2):
                ps = next_ps(C)
                for cc in range(2):
                    kb.op("pe", lambda: nc.tensor.matmul(ps[:, cc * 256:(cc + 1) * 256], lhsT=xin[s][:nb, c + cc, :], rhs=C.FA[:nb, 0, :], start=True, stop=True),
                          reads=[xin[s], C.FA], writes=[ps])
                _twiddle(kb, C, nc, ps, Yr[s], Yi[s], c, 1, tmpsA[pi_ % 2])
                pi_ += 1

        def q(Y, c):
            return Y[:, c:c + 4, :].rearrange("p c k -> p (c k)")
        for c in range(0, GC, 4):
            pr = next_ps(C)
            terms = [(Cm, Yr[1]), (Sm, Yi[1]), (Cm, Yr[2]), (Sm, Yi[2])]
            for i, (F_, Y_) in enumerate(terms):
                kb.op("pe", lambda: nc.tensor.matmul(pr[:, :], lhsT=F_, rhs=q(Y_, c), start=(i == 0), stop=(i == 3)), reads=[C.FB, Y_], writes=[pr])
            kb.op("act", lambda: nc.scalar.copy(out=q(Kr, c), in_=pr[:, :]), reads=[pr], writes=[Kr])
            pim = next_ps(C)
            terms = [(Cm, Yi[1]), (Sn, Yr[1]), (Cn, Yi[2]), (Sm, Yr[2])]
            for i, (F_, Y_) in enumerate(terms):
                kb.op("pe", lambda: nc.tensor.matmul(pim[:, :], lhsT=F_, rhs=q(Y_, c), start=(i == 0), stop=(i == 3)), reads=[C.FB, Y_], writes=[pim])
            kb.op("act", lambda: nc.scalar.copy(out=q(Ki, c), in_=pim[:, :]), reads=[pim], writes=[Ki])
        for c in range(0, GC, 4):
            pr = next_ps(C)
            for i, (F_, Y_) in enumerate([(Cm, Yr[0]), (Sm, Yi[0])]):
                kb.op("pe", lambda: nc.tensor.matmul(pr[:, :], lhsT=F_, rhs=q(Y_, c), start=(i == 0), stop=(i == 1)), reads=[C.FB, Y_], writes=[pr])
            pim = next_ps(C)
            for i, (F_, Y_) in enumerate([(Cm, Yi[0]), (Sn, Yr[0])]):
                kb.op("pe", lambda: nc.tensor.matmul(pim[:, :], lhsT=F_, rhs=q(Y_, c), start=(i == 0), stop=(i == 1)), reads=[C.FB, Y_], writes=[pim])
            kb.op("dve", lambda: nc.vector.tensor_tensor(out=tq[0][:, :], in0=pr[:, :], in1=q(Kr, c), op=ALU.mult), reads=[pr, Kr], writes=[tq[0]])
            kb.op("dve", lambda: nc.vector.tensor_tensor(out=tq[1][:, :], in0=pim[:, :], in1=q(Ki, c), op=ALU.mult), reads=[pim, Ki], writes=[tq[1]])
            kb.op("pool", lambda: nc.gpsimd.tensor_tensor(out=q(Zr, c), in0=tq[0][:, :], in1=tq[1][:, :], op=ALU.subtract), reads=[tq[0], tq[1]], writes=[Zr])
            kb.op("dve", lambda: nc.vector.tensor_tensor(out=tq[2][:, :], in0=pr[:, :], in1=q(Ki, c), op=ALU.mult), reads=[pr, Ki], writes=[tq[2]])
            kb.op("dve", lambda: nc.vector.tensor_tensor(out=tq[3][:, :], in0=pim[:, :], in1=q(Kr, c), op=ALU.mult), reads=[pim, Kr], writes=[tq[3]])
            kb.op("pool", lambda: nc.gpsimd.tensor_tensor(out=q(Zi, c), in0=tq[2][:, :], in1=tq[3][:, :], op=ALU.add), reads=[tq[2], tq[3]], writes=[Zi])
        for c in range(0, GC, 2):
            ps = next_ps(C)
            for cc in range(2):
                kb.op("pe", lambda: nc.tensor.matmul(ps[:, cc * 256:(cc + 1) * 256], lhsT=Zr[:, c + cc, :], rhs=C.FA[:, 1, :], start=True, stop=False), reads=[Zr, C.FA], writes=[ps])
                kb.op("pe", lambda: nc.tensor.matmul(ps[:, cc * 256:(cc + 1) * 256], lhsT=Zi[:, c + cc, :], rhs=C.FA[:, 2, :], start=False, stop=True), reads=[Zi, C.FA], writes=[ps])
            _twiddle(kb, C, nc, ps, Yr[0], Yi[0], c, 2, tmpsA[pi_ % 2])
            pi_ += 1
        YO = yo[g % 2]
        for c in range(0, GC, 4):
            ps = next_ps(C)
            kb.op("pe", lambda: nc.tensor.matmul(ps[:nb, :], lhsT=C.FB[:, 0, 0:nb], rhs=q(Yr[0], c), start=True, stop=False), reads=[C.FB, Yr[0]], writes=[ps])
            kb.op("pe", lambda: nc.tensor.matmul(ps[:nb, :], lhsT=C.FB[:, 2, 0:nb], rhs=q(Yi[0], c), start=False, stop=True), reads=[C.FB, Yi[0]], writes=[ps])
            kb.op("act", lambda: nc.scalar.activation(out=YO[:nb, c:c + 4, :].rearrange("p c k -> p (c k)"), in_=ps[:nb, :], func=AF.Copy, scale=1.0 / 16384.0), reads=[ps], writes=[YO])
        kb.dma("pool", yout[c0:c0 + GC, :].rearrange("c (b p) -> b c p", p=128), YO[:nb, :, :], reads=[YO])


def stage_hy_gate(kb, C, st, tiles, yconv, uvT, x0T, hbd, yT0):
    nc = kb.nc
    hb = kb.sb("hg_b", [128, 4], F32, st)
    kb.dma("sp", hb[:], hbd, writes=[hb])
    A = [kb.sb(f"hg_a{i}", [128, 3, 4, 512], BF16, st) for i in range(2)]
    T_ = [kb.sb(f"hg_t{i}", [128, 512], F32, st) for i in range(2)]
    O = [kb.sb(f"hg_o{i}", [128, 4, 512], BF16, st) for i in range(2)]
    for ti, (d0, n) in enumerate(tiles):
        At, Ot = A[ti % 2], O[ti % 2]
        for j, src in enumerate((yconv, uvT, x0T)):
            kb.dma("sp", At[:, j, :, :n], src.rearrange("(k p) t -> p k t", p=128)[:, :, d0:d0 + n], writes=[At])
        for k in range(4):
            Tt = T_[k % 2]
            kb.op("dve", lambda: nc.vector.scalar_tensor_tensor(out=Tt[:, :n], in0=At[:, 1, k, :n], scalar=hb[:, k:k + 1], in1=At[:, 0, k, :n], op0=ALU.mult, op1=ALU.add),
                  reads=[At, hb], writes=[Tt])
            kb.op("pool", lambda: nc.gpsimd.tensor_tensor(out=Ot[:, k, :n], in0=Tt[:, :n], in1=At[:, 2, k, :n], op=ALU.mult), reads=[Tt, At], writes=[Ot])
        kb.dma("pool", yT0.rearrange("(k p) t -> p k t", p=128)[:, :, d0:d0 + n], Ot[:, :, :n], reads=[Ot])


def stage_gdn_prep(kb, C, st, segs, pinT, cwd, alogd, dtbd, qkvT, gbT):
    nc = kb.nc
    cw = kb.sb("gp_w", [128, 3, 12], F32, st)
    kb.dma("sp", cw[:], cwd, writes=[cw])
    av = kb.sb("gp_av", [8, 2], F32, st)
    kb.dma("sp", av[:, 0:1], alogd.rearrange("(p o) -> p o", o=1), writes=[av])
    kb.dma("sp", av[:, 1:2], dtbd.rearrange("(p o) -> p o", o=1), writes=[av])
    kb.op("act", lambda: nc.scalar.activation(out=av[:, 0:1], in_=av[:, 0:1], func=AF.Exp), reads=[av], writes=[av])
    kb.op("dve", lambda: nc.vector.tensor_scalar(out=av[:, 0:1], in0=av[:, 0:1], scalar1=-1.0, scalar2=None, op0=ALU.mult), reads=[av], writes=[av])
    P = [kb.sb(f"gp_p{i}", [128, 12, 514], BF16, st) for i in range(2)]
    U = [kb.sb(f"gp_u{i}", [128, 512], F32, st) for i in range(4)]
    SQ = [kb.sb(f"gp_sq{i}", [128, 512], BF16, st) for i in range(2)]
    RS = [kb.sb(f"gp_rs{i}", [128, 512], F32, st) for i in range(2)]
    O = [kb.sb(f"gp_o{i}", [128, 512], F32, st) for i in range(4)]
    A8 = [kb.sb(f"gp_a8{i}", [8, 512], BF16, st) for i in range(2)]
    B8 = [kb.sb(f"gp_b8{i}", [8, 512], BF16, st) for i in range(2)]
    G8 = [kb.sb(f"gp_g8{i}", [8, 512], F32, st) for i in range(2)]
    E8 = [kb.sb(f"gp_e8{i}", [8, 512], F32, st) for i in range(2)]
    GT = [kb.sb(f"gp_gt{i}", [128, 16], F32, st) for i in range(3)]
    ti = ui = oi = gi = 0
    for (p0, Ls) in segs:
        for o in range(0, Ls, 512):
            n = min(512, Ls - o)
            Pt = P[ti % 2]
            lo = 1 if o == 0 else 0
            hi = 1 if o + n == Ls else 0
            if lo:
                kb.op("pool", lambda: nc.gpsimd.memset(Pt[:, :, 0:1], 0.0), writes=[Pt])
            if hi:
                kb.op("pool", lambda: nc.gpsimd.memset(Pt[:, :, n + 1:n + 2], 0.0), writes=[Pt])
            kb.dma("sp", Pt[:, :, lo:n + 2 - hi], pinT[1536:3072, :].rearrange("(k p) t -> p k t", p=128)[:, :, p0 + o - 1 + lo:p0 + o + n + 1 - hi], writes=[Pt])
            for k in range(12):
                Ut = U[ui % 4]
                ui += 1
                Ot = O[oi % 4]
                oi += 1
                kb.op("act", lambda: nc.scalar.activation(out=Ut[:, :n], in_=Pt[:, k, 1:n + 1], func=AF.Identity, scale=cw[:, 1, k:k + 1]), reads=[Pt, cw], writes=[Ut])
                kb.op("dve", lambda: nc.vector.scalar_tensor_tensor(out=Ut[:, :n], in0=Pt[:, k, 0:n], scalar=cw[:, 0, k:k + 1], in1=Ut[:, :n], op0=ALU.mult, op1=ALU.add), reads=[Pt, cw, Ut], writes=[Ut])
                kb.op("dve", lambda: nc.vector.scalar_tensor_tensor(out=Ut[:, :n], in0=Pt[:, k, 2:n + 2], scalar=cw[:, 2, k:k + 1], in1=Ut[:, :n], op0=ALU.mult, op1=ALU.add), reads=[Pt, cw, Ut], writes=[Ut])
                if k >= 8:
                    kb.op("act", lambda: nc.scalar.activation(out=Ot[:, :n], in_=Ut[:, :n], func=AF.Silu), reads=[Ut], writes=[Ot])
                else:
                    S_, R_ = SQ[k % 2], RS[k % 2]
                    kb.op("act", lambda: nc.scalar.activation(out=Ut[:, :n], in_=Ut[:, :n], func=AF.Silu), reads=[Ut], writes=[Ut])
                    kb.op("pool", lambda: nc.gpsimd.tensor_tensor(out=S_[:, :n], in0=Ut[:, :n], in1=Ut[:, :n], op=ALU.mult), reads=[Ut], writes=[S_])
                    ps = next_ps(C)
                    kb.op("pe", lambda: nc.tensor.matmul(ps[:, :n], lhsT=C.ones_bf[:], rhs=S_[:, :n], start=True, stop=True), reads=[S_, C.ones_bf], writes=[ps])
                    kb.op("act", lambda: nc.scalar.activation(out=R_[:, :n], in_=ps[:, :n], func=AF.Sqrt, bias=C.eps_t[:, 0:1]), reads=[ps, C.eps_t], writes=[R_])
                    kb.op("dve", lambda: nc.vector.reciprocal(out=R_[:, :n], in_=R_[:, :n]), reads=[R_], writes=[R_])
                    sc_ = (128.0 ** -0.5) if k < 4 else 1.0
                    kb.op("dve", lambda: nc.vector.scalar_tensor_tensor(out=Ot[:, :n], in0=Ut[:, :n], scalar=sc_, in1=R_[:, :n], op0=ALU.mult, op1=ALU.mult), reads=[Ut, R_], writes=[Ot])
                kb.dma("pool", qkvT[k * 128:(k + 1) * 128, p0 + o:p0 + o + n], Ot[:, :n], reads=[Ot])
            a8, b8, g8, e8 = A8[ti % 2], B8[ti % 2], G8[ti % 2], E8[ti % 2]
            kb.dma("sp", a8[:, :n], pinT[3584:3592, p0 + o:p0 + o + n], writes=[a8])
            kb.dma("sp", b8[:, :n], pinT[3592:3600, p0 + o:p0 + o + n], writes=[b8])
            kb.op("act", lambda: nc.scalar.activation(out=g8[:, :n], in_=a8[:, :n], func=AF.Exp, bias=av[:, 1:2]), reads=[a8, av], writes=[g8])
            kb.op("act", lambda: nc.scalar.activation(out=g8[:, :n], in_=g8[:, :n], func=AF.Ln, bias=C.one_t[:8, 0:1]), reads=[g8, C.one_t], writes=[g8])
            kb.op("dve", lambda: nc.vector.tensor_scalar(out=g8[:, :n], in0=g8[:, :n], scalar1=av[:, 0:1], scalar2=None, op0=ALU.mult), reads=[g8, av], writes=[g8])
            kb.op("act", lambda: nc.scalar.activation(out=e8[:, :n], in_=b8[:, :n], func=AF.Sigmoid), reads=[b8], writes=[e8])
            for s in range(n // 128):
                ps = next_ps(C)
                kb.op("pe", lambda: nc.tensor.transpose(ps[:, 0:8], g8[:, s * 128:(s + 1) * 128], C.ident_f[:8, :8]), reads=[g8, C.ident_f], writes=[ps])
                kb.op("pe", lambda: nc.tensor.transpose(ps[:, 8:16], e8[:, s * 128:(s + 1) * 128], C.ident_f[:8, :8]), reads=[e8, C.ident_f], writes=[ps])
                G_ = GT[gi % 3]
                gi += 1
                kb.op("dve", lambda: nc.vector.tensor_copy(out=G_[:, :], in_=ps[:, 0:16]), reads=[ps], writes=[G_])
                kb.dma("pool", gbT[p0 + o + s * 128:p0 + o + (s + 1) * 128, :], G_[:, :], reads=[G_])
            ti += 1


def load_gdn_consts(kb, C, trifd, tribd, ms2d, mi1d):
    C.triF = kb.sb("triF", [64, 64], F32)
    C.triB = kb.sb("triB", [64, 64], F32)
    C.mS2 = kb.sb("mS2", [64, 8, 64], F32)
    C.mI1 = kb.sb("mI1", [64, 8, 64], F32)
    C.ones_f = kb.sb("ones_f", [64, 128], F32)
    C.identI = kb.sb("identI", [64, 8, 64], F32)
    kb.dma("sp", C.triF[:], trifd, writes=[C.triF])
    kb.dma("sp", C.triB[:], tribd, writes=[C.triB])
    kb.dma("sp", C.mS2[:], ms2d, writes=[C.mS2])
    kb.dma("sp", C.mI1[:], mi1d, writes=[C.mI1])
    kb.op("dve", lambda: kb.nc.vector.memset(C.ones_f[:], 1.0), writes=[C.ones_f])
    for u in range(8):
        kb.op("dve", lambda: kb.nc.vector.tensor_copy(out=C.identI[:, u, :], in_=C.ident_f[:64, :64]), reads=[C.ident_f], writes=[C.identI])


def gdn_chain(kb, C, st, tag, d, order, qkvT, gbT, outd, banks):
    nc = kb.nc
    V_ = nc.vector
    NU = 4
    bi = [0]

    def nps():
        p = banks[bi[0] % len(banks)]
        bi[0] += 1
        return p

    def sb(nm, shape, dt):
        return kb.sb(f"g{tag}_{nm}", shape, dt, st)
    X = [sb(f"x{i}", [128, 12, 64], F32) for i in range(2)]
    GB = [sb(f"gb{i}", [64, 2, NU], F32) for i in range(2)]
    QT = sb("qt", [128, NU, 64], BF16)
    KT = sb("kt", [128, NU, 64], BF16)
    Gbc = sb("gbc", [64, NU, 128], F32)
    gcs = sb("gc", [64, NU], F32)
    sm = sb("sm", [128, 6, NU], F32)
    Dm = sb("dm", [64, NU, 64], F32)
    D2 = sb("d2", [64, NU, 64], F32)
    E1 = sb("e1", [64, NU, 64], F32)
    E2 = sb("e2", [64, NU, 64], F32)
    EG = sb("eg", [128, NU, 64], F32)
    Mm = sb("m", [64, NU, 64], F32)
    Nn = sb("n", [64, NU, 64], BF16)
    Mb = sb("mb", [64, NU, 64], BF16)
    AT = sb("at", [64, NU, 64], BF16)
    PN = sb("pn", [64, NU, 64], F32)
    PM = sb("pm", [64, NU, 64], BF16)
    XN = [sb(f"xn{i}", [64, NU, 64], BF16) for i in range(2)]
    XM = [sb(f"xm{i}", [64, NU, 64], BF16) for i in range(2)]
    TT = sb("tt", [64, NU, 64], BF16)
    Kbg = sb("kbg", [64, NU, 128], BF16)
    Kd = sb("kd", [64, NU, 128], BF16)
    Vb = sb("vb", [64, NU, 128], BF16)
    Uu = sb("u", [64, NU, 128], F32)
    WT = sb("wt", [128, NU, 64], BF16)
    QD = sb("qd", [128, NU, 64], BF16)
    Vn = sb("vn", [64, NU, 128], BF16)
    Ot = [sb(f"o{i}", [64, NU, 128], F32) for i in range(2)]
    S = sb("s", [128, NU, 128], F32)
    Sb = sb("sb", [128, NU, 128], BF16)
    kb.op("dve", lambda: V_.memset(S[:], 0.0), writes=[S])
    kb.op("pool", lambda: nc.gpsimd.memset(Sb[:], 0.0), writes=[Sb])
    tri = C.triF if d == 0 else C.triB
    us = slice(d * 4, d * 4 + 4)
    W6 = NU * 64
    W12 = NU * 128

    def bc(ap2, shape):
        return ap2.unsqueeze(2).to_broadcast(shape)

    def v3(ps, p=64):
        return ps[:p, :W6].rearrange("p (u k) -> p u k", k=64)

    def f2(t):
        return t.rearrange("p u k -> p (u k)")
    for s, c in enumerate(order):
        Xd = X[s % 2]
        G = GB[s % 2]
        kb.dma("sp", Xd[:], qkvT.rearrange("(k p) t -> p k t", p=128)[:, :, c * 64:(c + 1) * 64], writes=[Xd])
        kb.dma("sp", G[:, :, :], gbT[c * 64:(c + 1) * 64, :].rearrange("t (a d h) -> t a d h", a=2, d=2)[:, :, d, :], writes=[G])
        yield
        kb.op("act", lambda: nc.scalar.copy(out=QT[:], in_=Xd[:, 0:4, :]), reads=[Xd], writes=[QT])
        kb.op("pool", lambda: nc.gpsimd.tensor_copy(out=KT[:], in_=Xd[:, 4:8, :]), reads=[Xd], writes=[KT])
        psK, psV = nps(), nps()
        for h in range(NU):
            kb.op("pe", lambda: nc.tensor.transpose(psK[:64, h * 128:(h + 1) * 128], Xd[:, 4 + h, :], C.ident_f[:]), reads=[Xd, C.ident_f], writes=[psK])
            kb.op("pe", lambda: nc.tensor.transpose(psV[:64, h * 128:(h + 1) * 128], Xd[:, 8 + h, :], C.ident_f[:]), reads=[Xd, C.ident_f], writes=[psV])
        kb.op("dve", lambda: V_.tensor_copy(out=Gbc[:], in_=bc(G[:, 0, :], [64, NU, 128])), reads=[G], writes=[Gbc])
        psg = nps()
        kb.op("pe", lambda: nc.tensor.matmul(psg[:64, 0:NU], lhsT=tri[:], rhs=G[:, 0, :], start=True, stop=True), reads=[tri, G], writes=[psg])
        kb.op("pe", lambda: nc.tensor.matmul(psg[:, 8:8 + NU], lhsT=C.ones_f[:], rhs=G[:, 0, :], start=True, stop=True), reads=[C.ones_f, G], writes=[psg])
        yield
        psr = nps()
        for u in range(NU):
            kb.op("pe", lambda: nc.tensor.matmul(psr[:, u * 64:(u + 1) * 64], lhsT=Gbc[:, u, :], rhs=tri[:], start=True, stop=True), reads=[Gbc, tri], writes=[psr])
        kb.op("dve", lambda: V_.tensor_copy(out=gcs[:], in_=psg[:64, 0:NU]), reads=[psg], writes=[gcs])
        yield
        kb.op("dve", lambda: V_.tensor_tensor(out=Dm[:], in0=v3(psr), in1=bc(gcs[:, :], [64, NU, 64]), op=ALU.subtract), reads=[psr, gcs], writes=[Dm])
        kb.op("act", lambda: nc.scalar.activation(out=f2(EG[:]), in_=psr[:, :W6], func=AF.Exp), reads=[psr], writes=[EG])
        kb.op("act", lambda: nc.scalar.activation(out=sm[:64, 0, :], in_=gcs[:, :], func=AF.Exp), reads=[gcs], writes=[sm])
        kb.op("dve", lambda: V_.tensor_tensor(out=sm[:64, 4, :], in0=psg[:64, 8:8 + NU], in1=gcs[:, :], op=ALU.subtract), reads=[psg, gcs], writes=[sm])
        yield
        kb.op("pool", lambda: nc.gpsimd.tensor_scalar(out=D2[:], in0=Dm[:], scalar1=-1.0, scalar2=0.0, op0=ALU.mult, op1=ALU.min), reads=[Dm], writes=[D2])
        kb.op("dve", lambda: V_.tensor_scalar(out=Dm[:], in0=Dm[:], scalar1=0.0, scalar2=None, op0=ALU.min), reads=[Dm], writes=[Dm])
        kb.op("act", lambda: nc.scalar.activation(out=sm[:64, 1, :], in_=sm[:64, 4, :], func=AF.Exp), reads=[sm], writes=[sm])
        kb.op("act", lambda: nc.scalar.activation(out=sm[:, 2, :], in_=psg[:, 8:8 + NU], func=AF.Exp), reads=[psg], writes=[sm])
        kb.op("dve", lambda: V_.tensor_tensor(out=sm[:64, 3, :], in0=sm[:64, 0, :], in1=G[:, 1, :], op=ALU.mult), reads=[sm, G], writes=[sm])
        yield
        kb.op("act", lambda: nc.scalar.activation(out=E1[:], in_=Dm[:], func=AF.Exp), reads=[Dm], writes=[E1])
        kb.op("act", lambda: nc.scalar.activation(out=E2[:], in_=D2[:], func=AF.Exp), reads=[D2], writes=[E2])
        pk4 = psK[:64, :W12].rearrange("p (u k) -> p u k", k=128)
        pv4 = psV[:64, :W12].rearrange("p (u k) -> p u k", k=128)
        kb.op("dve", lambda: V_.tensor_tensor(out=Kbg[:], in0=pk4, in1=bc(sm[:64, 3, :], [64, NU, 128]), op=ALU.mult), reads=[psK, sm], writes=[Kbg])
        kb.op("dve", lambda: V_.tensor_tensor(out=Kd[:], in0=pk4, in1=bc(sm[:64, 1, :], [64, NU, 128]), op=ALU.mult), reads=[psK, sm], writes=[Kd])
        kb.op("dve", lambda: V_.tensor_tensor(out=Vb[:], in0=pv4, in1=bc(G[:, 1, :], [64, NU, 128]), op=ALU.mult), reads=[psV, G], writes=[Vb])
        kb.op("pool", lambda: nc.gpsimd.tensor_tensor(out=QD[:], in0=QT[:], in1=EG[:], op=ALU.mult), reads=[QT, EG], writes=[QD])
        pkk, pqk = nps(), nps()
        for u in range(NU):
            kb.op("pe", lambda: nc.tensor.matmul(pkk[:64, u * 64:(u + 1) * 64], lhsT=KT[:, u, :], rhs=KT[:, u, :], start=True, stop=True), reads=[KT], writes=[pkk])
            kb.op("pe", lambda: nc.tensor.matmul(pqk[:64, u * 64:(u + 1) * 64], lhsT=KT[:, u, :], rhs=QT[:, u, :], start=True, stop=True), reads=[KT, QT], writes=[pqk])
        yield
        kb.op("pool", lambda: nc.gpsimd.tensor_tensor(out=E1[:], in0=E1[:], in1=C.mI1[:, us, :], op=ALU.mult), reads=[E1, C.mI1], writes=[E1])
        kb.op("pool", lambda: nc.gpsimd.tensor_tensor(out=E2[:], in0=E2[:], in1=C.mS2[:, us, :], op=ALU.mult), reads=[E2, C.mS2], writes=[E2])
        yield
        kb.op("dve", lambda: V_.tensor_tensor(out=Mm[:], in0=v3(pkk), in1=E2[:], op=ALU.mult), reads=[pkk, E2], writes=[Mm])
        kb.op("dve", lambda: V_.tensor_tensor(out=Mm[:], in0=Mm[:], in1=bc(G[:, 1, :], [64, NU, 64]), op=ALU.mult), reads=[Mm, G], writes=[Mm])
        kb.op("dve", lambda: V_.tensor_tensor(out=AT[:], in0=v3(pqk), in1=E1[:], op=ALU.mult), reads=[pqk, E1], writes=[AT])
        yield
        pn = nps()
        for u in range(NU):
            kb.op("pe", lambda: nc.tensor.transpose(pn[:64, u * 64:(u + 1) * 64], Mm[:, u, :], C.ident_f[:64, :64]), reads=[Mm, C.ident_f], writes=[pn])
        kb.op("pool", lambda: nc.gpsimd.tensor_tensor(out=PM[:], in0=C.identI[:, 0:NU, :], in1=Mm[:], op=ALU.subtract), reads=[C.identI, Mm], writes=[PM])
        kb.op("act", lambda: nc.scalar.copy(out=Mb[:], in_=Mm[:]), reads=[Mm], writes=[Mb])
        yield
        kb.op("act", lambda: nc.scalar.copy(out=Nn[:], in_=v3(pn)), reads=[pn], writes=[Nn])
        kb.op("dve", lambda: V_.scalar_tensor_tensor(out=PN[:], in0=v3(pn), scalar=-1.0, in1=C.identI[:, 0:NU, :], op0=ALU.mult, op1=ALU.add), reads=[C.identI, pn], writes=[PN])
        yield
        pa, pb = nps(), nps()
        for u in range(NU):
            kb.op("pe", lambda: nc.tensor.matmul(pa[:64, u * 64:(u + 1) * 64], lhsT=Mb[:, u, :], rhs=Nn[:, u, :], start=True, stop=True), reads=[Mb, Nn], writes=[pa])
            kb.op("pe", lambda: nc.tensor.matmul(pb[:64, u * 64:(u + 1) * 64], lhsT=Nn[:, u, :], rhs=Mb[:, u, :], start=True, stop=True), reads=[Mb, Nn], writes=[pb])
        yield
        xn, xm = XN[0], XM[0]
        kb.op("act", lambda: nc.scalar.copy(out=xn[:], in_=v3(pa)), reads=[pa], writes=[xn])
        kb.op("dve", lambda: V_.tensor_copy(out=xm[:], in_=v3(pb)), reads=[pb], writes=[xm])
        yield
        for lv in range(5):
            last = lv == 4
            pa = nps()
            for u in range(NU):
                kb.op("pe", lambda: nc.tensor.matmul(pa[:64, u * 64:(u + 1) * 64], lhsT=PM[:, u, :], rhs=xn[:, u, :], start=True, stop=True), reads=[PM, xn], writes=[pa])
            if not last:
                pb = nps()
                for u in range(NU):
                    kb.op("pe", lambda: nc.tensor.matmul(pb[:64, u * 64:(u + 1) * 64], lhsT=xn[:, u, :], rhs=PM[:, u, :], start=True, stop=True), reads=[PM, xn], writes=[pb])
                pc, pd = nps(), nps()
                for u in range(NU):
                    kb.op("pe", lambda: nc.tensor.matmul(pc[:64, u * 64:(u + 1) * 64], lhsT=xm[:, u, :], rhs=xn[:, u, :], start=True, stop=True), reads=[xm, xn], writes=[pc])
                    kb.op("pe", lambda: nc.tensor.matmul(pd[:64, u * 64:(u + 1) * 64], lhsT=xn[:, u, :], rhs=xm[:, u, :], start=True, stop=True), reads=[xm, xn], writes=[pd])
                yield
                xn2, xm2 = XN[(lv + 1) % 2], XM[(lv + 1) % 2]
                kb.op("act", lambda: nc.scalar.copy(out=xn2[:], in_=v3(pc)), reads=[pc], writes=[xn2])
                kb.op("act", lambda: nc.scalar.copy(out=xm2[:], in_=v3(pd)), reads=[pd], writes=[xm2])
                kb.op("dve", lambda: V_.tensor_tensor(out=PM[:], in0=v3(pb), in1=PM[:], op=ALU.add), reads=[PM, pb], writes=[PM])
                kb.op("dve", lambda: V_.tensor_tensor(out=PN[:], in0=v3(pa), in1=PN[:], op=ALU.add), reads=[PN, pa], writes=[PN])
                xn, xm = xn2, xm2
                yield
            else:
                yield
                kb.op("dve", lambda: V_.tensor_tensor(out=TT[:], in0=v3(pa), in1=PN[:], op=ALU.add), reads=[PN, pa], writes=[TT])
                yield
        pu, pw = nps(), nps()
        for u in range(NU):
            kb.op("pe", lambda: nc.tensor.matmul(pu[:64, u * 128:(u + 1) * 128], lhsT=TT[:, u, :], rhs=Vb[:, u, :], start=True, stop=True), reads=[TT, Vb], writes=[pu])
            kb.op("pe", lambda: nc.tensor.matmul(pw[:, u * 64:(u + 1) * 64], lhsT=Kbg[:, u, :], rhs=TT[:, u, :], start=True, stop=True), reads=[TT, Kbg], writes=[pw])
        yield
        kb.op("act", lambda: nc.scalar.copy(out=f2(Uu[:]), in_=pu[:64, :W12]), reads=[pu], writes=[Uu])
        kb.op("act", lambda: nc.scalar.copy(out=f2(WT[:]), in_=pw[:, :W6]), reads=[pw], writes=[WT])
        yield
        pws = nps()
        for u in range(NU):
            kb.op("pe", lambda: nc.tensor.matmul(pws[:64, u * 128:(u + 1) * 128], lhsT=WT[:, u, :], rhs=Sb[:, u, :], start=True, stop=True), reads=[WT, Sb], writes=[pws])
        yield
        kb.op("dve", lambda: V_.scalar_tensor_tensor(out=f2(Vn[:]), in0=pws[:64, :W12], scalar=-1.0, in1=f2(Uu[:]), op0=ALU.mult, op1=ALU.add), reads=[Uu, pws], writes=[Vn])
        yield
        po, pss = nps(), nps()
        for u in range(NU):
            kb.op("pe", lambda: nc.tensor.matmul(po[:64, u * 128:(u + 1) * 128], lhsT=QD[:, u, :], rhs=Sb[:, u, :], start=True, stop=False), reads=[QD, Sb], writes=[po])
            kb.op("pe", lambda: nc.tensor.matmul(po[:64, u * 128:(u + 1) * 128], lhsT=AT[:, u, :], rhs=Vn[:, u, :], start=False, stop=True), reads=[AT, Vn], writes=[po])
        for u in range(NU):
            kb.op("pe", lambda: nc.tensor.matmul(pss[:, u * 128:(u + 1) * 128], lhsT=Kd[:, u, :], rhs=Vn[:, u, :], start=True, stop=True), reads=[Kd, Vn], writes=[pss])
        kb.op("dve", lambda: V_.tensor_tensor(out=S[:], in0=S[:], in1=bc(sm[:, 2, :], [128, NU, 128]), op=ALU.mult), reads=[S, sm], writes=[S])
        yield
        O_ = Ot[s % 2]
        kb.op("act", lambda: nc.scalar.copy(out=f2(O_[:]), in_=po[:64, :W12]), reads=[po], writes=[O_])
        kb.dma("pool", outd[c * 64:(c + 1) * 64, :], f2(O_[:]), reads=[O_])
        kb.op("dve", lambda: V_.tensor_tensor(out=f2(S[:]), in0=pss[:, :W12], in1=f2(S[:]), op=ALU.add), reads=[S, pss], writes=[S])
        yield
        kb.op("act", lambda: nc.scalar.copy(out=Sb[:], in_=S[:]), reads=[S], writes=[Sb])
        yield


def stage_gdn_scan(kb, C, st, fo, bo, qkvT, gbT, ofd, obd):
    gens = [gdn_chain(kb, C, st, "f", 0, fo, qkvT, gbT, ofd, C.psum[0:4]),
            gdn_chain(kb, C, st, "b", 1, bo, qkvT, gbT, obd, C.psum[4:8])]
    alive = list(gens)
    while alive:
        for g in list(alive):
            try:
                next(g)
            except StopIteration:
                alive.remove(g)


def stage_gdn_out(kb, C, st, tiles, ofd, obd, pinT, gnd, yT1):
    nc = kb.nc
    gbc = kb.sb("go_g", [128, 128], F32, st)
    kb.dma("sp", gbc[:], gnd, writes=[gbc])
    Z = [kb.sb(f"go_z{i}", [128, 4, 512], BF16, st) for i in range(2)]
    ZS = [kb.sb(f"go_zs{i}", [128, 4, 512], F32, st) for i in range(2)]
    OF = [kb.sb(f"go_of{i}", [128, 512], F32, st) for i in range(2)]
    OB = [kb.sb(f"go_ob{i}", [128, 512], F32, st) for i in range(2)]
    junk = kb.sb("go_junk", [128, 4, 128], F32, st)
    ss = [kb.sb(f"go_ss{i}", [128, 4], F32, st) for i in range(2)]
    ON = [kb.sb(f"go_on{i}", [128, 512], F32, st) for i in range(2)]
    Y = [kb.sb(f"go_y{i}", [128, 4, 512], BF16, st) for i in range(2)]
    si = 0
    for ti, (p0, n) in enumerate(tiles):
        Zt, ZSt, Yt = Z[ti % 2], ZS[ti % 2], Y[ti % 2]
        kb.dma("sp", Zt[:, :, :n], pinT[3072:3584, :].rearrange("(k p) t -> p k t", p=128)[:, :, p0:p0 + n], writes=[Zt])
        kb.op("act", lambda: nc.scalar.activation(out=ZSt[:, :, :n], in_=Zt[:, :, :n], func=AF.Silu), reads=[Zt], writes=[ZSt])
        pts = [next_ps(C) for _ in range(4)]
        for s in range(n // 128):
            of_, ob_, ss_, on_ = OF[si % 2], OB[si % 2], ss[si % 2], ON[si % 2]
            si += 1
            r0 = p0 + s * 128
            kb.dma("sp", of_[:], ofd[r0:r0 + 128, :], writes=[of_])
            kb.dma("sp", ob_[:], obd[r0:r0 + 128, :], writes=[ob_])
            kb.op("dve", lambda: nc.vector.tensor_tensor(out=of_[:], in0=of_[:], in1=ob_[:], op=ALU.add), reads=[of_, ob_], writes=[of_])
            for h in range(4):
                kb.op("act", lambda: nc.scalar.activation(out=junk[:, h, :], in_=of_[:, h * 128:(h + 1) * 128], func=AF.Square, accum_out=ss_[:, h:h + 1]), reads=[of_], writes=[junk, ss_], waw=(h == 0))
            kb.op("act", lambda: nc.scalar.activation(out=ss_[:], in_=ss_[:], func=AF.Sqrt, scale=1.0 / 128.0, bias=C.eps_t[:, 0:1]), reads=[ss_, C.eps_t], writes=[ss_])
            kb.op("dve", lambda: nc.vector.reciprocal(out=ss_[:], in_=ss_[:]), reads=[ss_], writes=[ss_])
            for h in range(4):
                kb.op("dve", lambda: nc.vector.scalar_tensor_tensor(out=on_[:, h * 128:(h + 1) * 128], in0=of_[:, h * 128:(h + 1) * 128], scalar=ss_[:, h:h + 1], in1=gbc[:],
                                                                   op0=ALU.mult, op1=ALU.mult), reads=[of_, ss_, gbc], writes=[on_])
            for h in range(4):
                kb.op("pe", lambda: nc.tensor.transpose(pts[h][:, s * 128:(s + 1) * 128], on_[:, h * 128:(h + 1) * 128], C.ident_f[:]), reads=[on_, C.ident_f], writes=[pts[h]])
        for h in range(4):
            kb.op("dve", lambda: nc.vector.tensor_tensor(out=Yt[:, h, :n], in0=pts[h][:, :n], in1=ZSt[:, h, :n], op=ALU.mult), reads=[pts[h], ZSt], writes=[Yt])
        kb.dma("pool", yT1.rearrange("(k p) t -> p k t", p=128)[:, :, p0:p0 + n], Yt[:, :, :n], reads=[Yt])

def rope_consts(S=8192, GW=64):
    nf=8
    inv=(10000.0**(-np.arange(nf,dtype=np.float32)/nf)).astype(np.float32)
    t=np.arange(S); row=(t//GW).astype(np.float32); col=(t%GW).astype(np.float32)
    c32=np.zeros((32,S),np.float32); s32=np.zeros((32,S),np.float32)
    for d in range(32):
        pos=row if d<16 else col
        ang=(pos*inv[d%8]).astype(np.float32)
        c32[d]=np.cos(ang); s32[d]=np.sin(ang)
    Rm=np.zeros((32,32),np.float32)
    for m in range(32):
        if m%16<8: Rm[m,m+8]=-1.0
        else: Rm[m,m-8]=1.0
    r32T=np.ascontiguousarray(Rm.T)
    c96=np.ones((96,S),np.float32); s96=np.zeros((96,S),np.float32)
    c96[64:]=c32; s96[64:]=s32
    R96=np.zeros((96,96),np.float32); R96[64:,64:]=Rm
    return np.stack([c96,s96]),np.stack([c32,s32]),np.ascontiguousarray(R96.T),r32T

def fft_consts():
    import ml_dtypes
    p=np.arange(128)
    ang=2*np.pi*np.outer(p,p)/128.0
    Cm=np.cos(ang); Sm=np.sin(ang)
    FA=np.stack([np.concatenate([Cm,-Sm],1),np.concatenate([Cm,Sm],1),np.concatenate([-Sm,Cm],1)]).astype(np.float32)
    FB=np.stack([Cm,Sm,-Sm,-Cm]).astype(np.float32)
    a2=2*np.pi*np.outer(p,p)/16384.0
    TW=np.stack([np.cos(a2),-np.sin(a2),np.sin(a2)]).astype(np.float32)
    return FA,FB,TW

def hyena_pos(L):
    t=np.linspace(0.0,1.0,L,dtype=np.float32)[:,None]
    w=((2.0*np.pi/L)*np.arange(L,dtype=np.float32))[:,None].astype(np.float32)
    f=np.linspace(1e-4,15,16,dtype=np.float32)[None,:]
    z=np.concatenate([t,np.cos(f*w),-np.sin(f*w)],-1).astype(np.float32)
    return np.ascontiguousarray(z.T), np.ascontiguousarray(t[:,0])

def gdn_consts():
    i=np.arange(64)
    triF=(i[:,None]<=i[None,:]).astype(np.float32)
    triB=(i[:,None]>=i[None,:]).astype(np.float32)
    mS2=np.zeros((64,8,64),np.float32); mI1=np.zeros((64,8,64),np.float32)
    for u in range(8):
        if u<4:
            mS2[:,u,:]=(i[None,:]<i[:,None]); mI1[:,u,:]=(i[None,:]>=i[:,None])
        else:
            mS2[:,u,:]=(i[None,:]>i[:,None]); mI1[:,u,:]=(i[None,:]<=i[:,None])
    return triF,triB,mS2,mI1


CTXL = 256


def build_program(S=8192, final=True, debug=False, nlayers=2):
    T = CTXL + S
    kb = KB()
    C = Ctx()
    nc = kb.nc
    setup_consts(kb, C)
    EI = "ExternalInput"
    d = {}

    def inp(name, shape, dt=F32):
        d[name] = kb.dram(name, shape, dt, EI)
        return d[name]
    xT = inp("xT", [1024, S])
    cxT = inp("cxT", [1024, CTXL])
    c2 = inp("c2", [128, 8, 2])
    inp("w_ada", [2, 1024, 6144]); inp("b_ada_l", [2, 128, 48]); inp("g1_l", [2, 128, 8]); inp("g2_l", [2, 128, 8])
    inp("w_in", [2, 1024, NIN])
    inp("hy_conv_w", [2, 128, 3, 12]); inp("hy_conv_b", [2, 128, 12]); inp("hy_f_w1", [2, 33, 64]); inp("hy_f_b1", [2, 64]); inp("hy_f_w2", [2, 64, 64])
    inp("hy_f_b2", [2, 64]); inp("hy_f_w3", [2, 64, 1024]); inp("hy_f_freq", [2, 64]); inp("hy_decay", [2, 128, 8]); inp("hy_bias", [2, 128, 4])
    inp("hy_out", [2, 512, 1024]); inp("gdn_conv_w", [2, 128, 3, 12]); inp("gdn_a_log", [2, 8]); inp("gdn_dt_bias", [2, 8]); inp("gdn_norm_g", [2, 128, 128])
    inp("gdn_out", [2, 512, 1024]); inp("qg_l", [2, 128, 6]); inp("mla_w_uq", [2, 768, 768]); inp("kvg_l", [2, 128, 2]); inp("mla_w_ukv", [2, 256, 1024])
    inp("mla_out", [2, 512, 1024]); inp("w_out", [2, 1024, 1024]); inp("moe_w1", [2, 16, 1024, 512]); inp("moe_w3", [2, 16, 1024, 512]); inp("moe_w2", [2, 16, 512, 1024])
    inp("router_w", [1024, 16]); inp("router_b", [128, 16]); inp("fg_l", [128, 8])
    inp("r32", [32, 32]); inp("cs32", [2, 32, S]); inp("fad", [3, 128, 256]); inp("fbd", [4, 128, 128]); inp("twd", [3, 128, 128])
    inp("zT_lat", [33, S]); inp("tn_lat", [128, S]); inp("zT_ctx", [33, CTXL]); inp("tn_ctx", [128, CTXL])
    inp("trif", [64, 64]); inp("trib", [64, 64]); inp("ms2", [64, 8, 64]); inp("mi1", [64, 8, 64])
    outT = kb.dram("outT", [1024, S], F32, "ExternalOutput")
    dk = "ExternalOutput" if debug else "Internal"
    pinT = kb.dram("pinT", [NIN, T], BF16, dk)
    XA = kb.dram("XA", [1024, T], F32, dk)
    XB = kb.dram("XB", [1024, T], F32, dk)
    hT_lat = kb.dram("hT_lat", [1024, S], BF16)
    hT_ctx = kb.dram("hT_ctx", [1024, CTXL], BF16)
    uvT = kb.dram("uvT", [512, T], BF16)
    x0T = kb.dram("x0T", [512, T], BF16)
    yconv = kb.dram("yconv", [512, T], BF16)
    yT = kb.dram("yT", [3, 512, T], BF16, dk)
    qkvT = kb.dram("qkvT", [1536, T], F32)
    gbT = kb.dram("gbT", [T, 16], F32)
    ofd = kb.dram("ofd", [T, 512], F32)
    obd = kb.dram("obd", [T, 512], F32)
    qTd = kb.dram("qTd", [8, 96, T], BF16)
    kTd = kb.dram("kTd", [8, 96, T], BF16)
    vd = kb.dram("vd", [8, T, 64], BF16)
    load_fft_consts(kb, C, d["fad"], d["fbd"], d["twd"])
    load_gdn_consts(kb, C, d["trif"], d["trib"], d["ms2"], d["mi1"])
    modv = kb.sb("modv", [128, 48, 2], F32)
    AB = kb.sb("AB", [128, 6, 2, 8], F32)
    rw_sb = kb.sb("rw_sb", [128, 8, 16], F32)
    rb_bc = kb.sb("rb_bc", [128, 16], F32)
    r32 = kb.sb("r32_s", [32, 32], BF16)
    qg = kb.sb("qg_s", [128, 6], F32)
    kvg = kb.sb("kvg_s", [128, 2], F32)
    fg = kb.sb("fg_s", [128, 8], F32)
    kb.dma("sp", rw_sb[:], d["router_w"].rearrange("(k p) e -> p k e", p=128), writes=[rw_sb])
    kb.dma("sp", rb_bc[:], d["router_b"], writes=[rb_bc])
    kb.dma("pool", r32[:], d["r32"], writes=[r32])
    kb.dma("sp", fg[:], d["fg_l"], writes=[fg])
    lat_tiles = [(o, min(512, S - o)) for o in range(0, S, 512)]
    NC_ = T // 64
    fo = list(range(NC_))
    bo = [3, 2, 1, 0] + list(range(NC_ - 1, 3, -1))

    def stage():
        kb.barrier()
        return ExitStack()
    for l in range(nlayers):
        first = l == 0
        upd = first
        xsrc, cxsrc = (xT, cxT) if first else (XB[:, CTXL:], XB[:, 0:CTXL])
        st = stage()
        stage_mod(kb, C, st, c2, d["w_ada"][l], d["b_ada_l"][l], d["g1_l"][l], d["g2_l"][l], modv, AB)
        kb.dma("sp", qg[:], d["qg_l"][l], writes=[qg])
        kb.dma("sp", kvg[:], d["kvg_l"][l], writes=[kvg])
        st.close()
        st = stage()
        w_sb = kb.sb("w_sb", [128, 8, NIN], BF16, st)
        for k in range(8):
            kb.dma("pool", w_sb[:, k, :], d["w_in"][l][k * 128:(k + 1) * 128, :], writes=[w_sb])
        tiles = [(cxsrc, 0, CTXL, 0, 1)] + [(xsrc, o, n, CTXL + o, 0) for (o, n) in lat_tiles]
        stage_in(kb, C, st, tiles, AB, w_sb, pinT)
        st.close()
        st = stage()
        stage_hy_filter(kb, C, st, S, d["zT_lat"], d["tn_lat"], d["hy_f_w1"][l], d["hy_f_b1"][l], d["hy_f_w2"][l], d["hy_f_b2"][l], d["hy_f_w3"][l],
                        d["hy_f_freq"][l], d["hy_decay"][l], hT_lat)
        st.close()
        if upd:
            st = stage()
            stage_hy_filter(kb, C, st, CTXL, d["zT_ctx"], d["tn_ctx"], d["hy_f_w1"][l], d["hy_f_b1"][l], d["hy_f_w2"][l], d["hy_f_b2"][l], d["hy_f_w3"][l],
                            d["hy_f_freq"][l], d["hy_decay"][l], hT_ctx)
            st.close()
        st = stage()
        segs = ([(0, CTXL, 0)] if upd else []) + [(CTXL, S, CTXL)]
        stage_hy_conv3(kb, C, st, segs, pinT, d["hy_conv_w"][l], d["hy_conv_b"][l], uvT, x0T)
        st.close()
        st = stage()
        stage_hy_fft(kb, C, st, uvT[:, CTXL:], hT_lat[0:512, :], hT_lat[512:1024, :], yconv[:, CTXL:], S // 128)
        st.close()
        if upd:
            st = stage()
            stage_hy_fft(kb, C, st, uvT[:, 0:CTXL], hT_ctx[0:512, :], hT_ctx[512:1024, :], yconv[:, 0:CTXL], CTXL // 128)
            st.close()
        st = stage()
        gt = ([(0, CTXL)] if upd else []) + [(CTXL + o, n) for (o, n) in lat_tiles]
        stage_hy_gate(kb, C, st, gt, yconv, uvT, x0T, d["hy_bias"][l], yT[0])
        st.close()
        st = stage()
        stage_gdn_prep(kb, C, st, [(0, CTXL), (CTXL, S)], pinT, d["gdn_conv_w"][l], d["gdn_a_log"][l], d["gdn_dt_bias"][l], qkvT, gbT)
        st.close()
        st = stage()
        stage_gdn_scan(kb, C, st, fo, bo, qkvT, gbT, ofd, obd)
        st.close()
        st = stage()
        stage_gdn_out(kb, C, st, gt, ofd, obd, pinT, d["gdn_norm_g"][l], yT[1])
        st.close()
        st = stage()
        wuq = kb.sb("wuq_s", [128, 6, 768], BF16, st)
        wukv = kb.sb("wukv_s", [128, 2, 1024], BF16, st)
        kb.dma("pool", wuq[:], d["mla_w_uq"][l].rearrange("(k p) n -> p k n", p=128), writes=[wuq])
        kb.dma("pool", wukv[:], d["mla_w_ukv"][l].rearrange("(k p) n -> p k n", p=128), writes=[wukv])
        mt = [(0, CTXL, False, 0)] + [(CTXL + o, n, True, o) for (o, n) in lat_tiles]
        stage_mla_prep(kb, C, st, mt, pinT, qg, kvg, wuq, wukv, None, r32, None, d["cs32"], qTd, kTd, vd)
        st.close()
        st = stage()
        jobs = [(CTXL, S, list(range(T // 128)))] + ([(0, CTXL, [0, 1])] if upd else [])
        stage_mla_attn(kb, C, st, jobs, qTd, kTd, vd, yT[2], T)
        st.close()
        st = stage()
        w3s = kb.sb("w3s", [128, 3, 4, 1024], BF16, st)
        wos = kb.sb("wos", [128, 8, 1024], BF16, st)
        for j, nm in enumerate(("hy_out", "gdn_out", "mla_out")):
            kb.dma("pool", w3s[:, j], d[nm][l].rearrange("(k p) n -> p k n", p=128), writes=[w3s])
        kb.dma("pool", wos[:], d["w_out"][l].rearrange("(k p) n -> p k n", p=128), writes=[wos])
        mtiles = ([(cxsrc, 0, CTXL, 0, 1, XA[:, 0:CTXL])] if upd else []) + [(xsrc, o, n, CTXL + o, 0, XA[:, CTXL:]) for (o, n) in lat_tiles]
        stage_merge(kb, C, st, mtiles, AB, yT, pinT, w3s, wos, None)
        st.close()
        st = stage()
        supers = ([(XA[:, 0:CTXL], 0, CTXL, 1, XB[:, 0:CTXL])] if upd else []) + [(XA[:, CTXL:], o, min(1024, S - o), 0, XB[:, CTXL:]) for o in range(0, S, 1024)]
        stage_moe(kb, C, st, supers, AB, rw_sb, rb_bc, d["moe_w1"][l], d["moe_w3"][l], d["moe_w2"][l])
        st.close()
    st = stage()
    X = [kb.sb(f"fn_x{i}", [128, 8, 512], F32, st) for i in range(2)]
    SQ = kb.sb("fn_sq", [128, 8, 512], BF16, st)
    RS = kb.sb("fn_rs", [128, 512], F32, st)
    tmp = [kb.sb(f"fn_t{i}", [128, 512], F32, st) for i in range(2)]
    H = [kb.sb(f"fn_h{i}", [128, 8, 512], F32, st) for i in range(2)]
    src = XB[:, CTXL:].rearrange("(k p) t -> p k t", p=128)
    dst = outT.rearrange("(k p) t -> p k t", p=128)
    for ti, (o, n) in enumerate(lat_tiles):
        Xt, Ht = X[ti % 2], H[ti % 2]
        kb.dma("sp", Xt[:, :, :n], src[:, :, o:o + n], writes=[Xt])
        rmsnorm_tile(kb, C, Xt, n, PV(lambda k: fg[:, k:k + 1], [fg]), None, 0, Ht, SQ, RS, tmp)
        kb.dma("pool", dst[:, :, o:o + n], Ht[:, :, :n], reads=[Ht])
    kb.finish()
    st.close()
    return kb


def _lay(v, k):
    return np.ascontiguousarray(np.asarray(v, np.float32).reshape(k, 128).T)


def make_inputs(inputs, b, S=8192):
    f = lambda a: np.ascontiguousarray(np.asarray(a, np.float32))
    x = np.asarray(inputs["x"][b], np.float32)
    im = {}
    im["xT"] = np.ascontiguousarray(x.T)
    im["cxT"] = np.ascontiguousarray(np.asarray(inputs["ctx"][b], np.float32).T)
    im["c2"] = np.ascontiguousarray(np.stack([_lay(inputs["c"][b], 8), _lay(inputs["c_ctx"], 8)], -1))
    return im


def shared_inputs(inputs, S=8192):
    f = lambda a: np.ascontiguousarray(np.asarray(a, np.float32))
    sh = {}
    for nm in ("w_ada", "w_in", "hy_f_w1", "hy_f_b1", "hy_f_w2", "hy_f_b2", "hy_f_w3", "hy_f_freq", "hy_out",
               "gdn_out", "mla_w_uq", "mla_w_ukv", "mla_out", "w_out", "moe_w1", "moe_w3", "moe_w2", "router_w"):
        sh[nm] = f(inputs[nm])
    cwl = lambda w: np.ascontiguousarray(f(w).reshape(2, 3, 12, 128).transpose(0, 3, 1, 2))
    vl = lambda v, k: np.ascontiguousarray(f(v).reshape(2, k, 128).transpose(0, 2, 1))
    sh["hy_conv_w"] = cwl(inputs["hy_conv_w"]); sh["gdn_conv_w"] = cwl(inputs["gdn_conv_w"])
    sh["hy_conv_b"] = vl(inputs["hy_conv_b"], 12); sh["hy_decay"] = vl(inputs["hy_decay"], 8); sh["hy_bias"] = vl(inputs["hy_bias"], 4)
    sh["gdn_norm_g"] = np.ascontiguousarray(np.broadcast_to(f(inputs["gdn_norm_g"])[:, None, :], (2, 128, 128)))
    sh["router_b"] = np.ascontiguousarray(np.broadcast_to(f(inputs["router_b"])[None, :], (128, 16)))
    sh["b_ada_l"] = np.stack([np.ascontiguousarray(f(inputs["b_ada"])[l].reshape(48, 128).T) for l in range(2)])
    sh["g1_l"] = np.stack([_lay(inputs["norm1_g"][l], 8) for l in range(2)])
    sh["g2_l"] = np.stack([_lay(inputs["norm2_g"][l], 8) for l in range(2)])
    sh["qg_l"] = np.stack([_lay(inputs["mla_q_norm_g"][l], 6) for l in range(2)])
    sh["kvg_l"] = np.stack([_lay(inputs["mla_kv_norm_g"][l], 2) for l in range(2)])
    sh["fg_l"] = _lay(inputs["final_norm_g"], 8)
    sh["gdn_a_log"] = f(inputs["gdn_a_log"]).reshape(2, 8)
    sh["gdn_dt_bias"] = f(inputs["gdn_dt_bias"]).reshape(2, 8)
    cs96, cs32, r96T, r32T = rope_consts(S)
    sh["r32"] = r32T
    sh["cs32"] = cs32
    FA, FB, TW = fft_consts()
    sh["fad"], sh["fbd"], sh["twd"] = FA, FB, TW
    sh["zT_lat"], tl_ = hyena_pos(S)
    sh["zT_ctx"], tc_ = hyena_pos(CTXL)
    sh["tn_lat"] = np.ascontiguousarray(np.broadcast_to(tl_[None, :], (128, S)))
    sh["tn_ctx"] = np.ascontiguousarray(np.broadcast_to(tc_[None, :], (128, CTXL)))
    sh["trif"], sh["trib"], sh["ms2"], sh["mi1"] = gdn_consts()
    sh["ident_f_d"] = np.eye(128, dtype=np.float32)
    return sh


def kernel(**inputs):
    x = np.asarray(inputs["x"])
    B, S, D_ = x.shape
    kb = build_program(S)
    sh = shared_inputs(inputs, S)
    in_maps = []
    for b in range(B):
        im = dict(sh)
        im.update(make_inputs(inputs, b, S))
        in_maps.append(im)
    res = run_bass_kernel_spmd(kb.nc, in_maps, core_ids=list(range(B)))
    out = np.stack([np.ascontiguousarray(np.asarray(r["outT"], np.float32).T) for r in res.results], 0)
    return out.astype(np.float32)
```
